# Optimizing a Trainium2 kernel written in Bass

```python
import jax, jax.numpy as jnp
from jax import lax
import numpy as np

D_MODEL = 1024
BATCH = 8
SEQ = 2048
DEPTH = 2

CONV_CH = 256
CONV_WIDTH = 31
NSA_HEADS = 4
NSA_HEAD_DIM = 64
CMP_BLOCK = 32
CMP_STRIDE = 16
SEL_BLOCK = 64
SEL_TOPN = 16
WINDOW = 512
MLA_HEADS = 4
MLA_Q_RANK = 256
MLA_KV_RANK = 128
MLA_NOPE = 64
MLA_ROPE = 32
MLA_V = 64
SB_HEADS = 4
SB_HEAD_DIM = 64
N_BRANCH = 4
BRANCH_W = 256
ROPE_THETA = 10000.0
Q_BLOCK = 128
LN_EPS = 1e-5
RMS_EPS = 1e-6
D_FF = 2816
N_EXPERTS = 8
TOP_K = 2
D_FF_EXPERT = 3584
MOE_BLOCK = 512
P_DIM = 256
N_DENSE = (DEPTH + 1) // 2
N_MOE = DEPTH // 2
DEEPNORM_ALPHA = (2 * DEPTH) ** 0.25
DEEPNORM_BETA = (8 * DEPTH) ** -0.25
IN_SIZES = (2 * CONV_CH, NSA_HEADS * NSA_HEAD_DIM, 6 * NSA_HEAD_DIM, 3 * NSA_HEADS,
            MLA_Q_RANK, MLA_KV_RANK, MLA_ROPE, 3 * SB_HEADS * SB_HEAD_DIM, N_BRANCH * D_MODEL)
D_IN = sum(IN_SIZES)

kernel_name = 'hybrid_conv_nsa_mla_stickbreak_moe_deepnorm'

F32 = jnp.float32


def _split_points():
    pts, acc = [], 0
    for s in IN_SIZES[:-1]:
        acc += s
        pts.append(acc)
    return pts


def layer_norm(x, g, b):
    xf = x.astype(F32)
    mu = jnp.mean(xf, axis=-1, keepdims=True)
    var = jnp.mean(jnp.square(xf - mu), axis=-1, keepdims=True)
    return ((xf - mu) * lax.rsqrt(var + LN_EPS) * g.astype(F32) + b.astype(F32)).astype(x.dtype)


def rms_norm(x, g):
    xf = x.astype(F32)
    return (xf * lax.rsqrt(jnp.mean(xf * xf, axis=-1, keepdims=True) + RMS_EPS) * g.astype(F32)).astype(x.dtype)


def rope(x, pos):
    half = x.shape[-1] // 2
    inv = ROPE_THETA ** (-jnp.arange(half, dtype=F32) / half)
    ang = pos.astype(F32)[..., None] * inv
    cos = jnp.cos(ang)[:, :, None, :]
    sin = jnp.sin(ang)[:, :, None, :]
    x1 = x[..., :half].astype(F32)
    x2 = x[..., half:].astype(F32)
    return jnp.concatenate([x1 * cos - x2 * sin, x2 * cos + x1 * sin], axis=-1).astype(x.dtype)


def _to_blocks(a, nb):
    return jnp.moveaxis(a.reshape((a.shape[0], nb, Q_BLOCK) + a.shape[2:]), 1, 0)


def _from_blocks(a):
    a = jnp.moveaxis(a, 0, 1)
    return a.reshape((a.shape[0], -1) + a.shape[3:])


def conformer_conv(u, w_dw, b_dw, ln_g, ln_b):
    a, g = jnp.split(u, 2, axis=-1)
    h = a * jax.nn.sigmoid(g)
    h = lax.conv_general_dilated(h, w_dw[:, None, :], (1,), [(CONV_WIDTH - 1, 0)],
                                 dimension_numbers=('NWC', 'WIO', 'NWC'),
                                 feature_group_count=CONV_CH) + b_dw
    h = layer_norm(h, ln_g, ln_b)
    return jax.nn.silu(h)


def nsa_attention(q, kv, gate_logits, pos, cmp_pe, cmp_w1, cmp_w2):
    B, S, H, Dh = q.shape
    scale = Dh ** -0.5
    t = jnp.arange(S)
    n_cmp = (S - CMP_BLOCK) // CMP_STRIDE + 1
    blk_idx = jnp.arange(n_cmp)[:, None] * CMP_STRIDE + jnp.arange(CMP_BLOCK)[None, :]
    blocks = kv[:, :, 0:2][:, blk_idx] + cmp_pe
    flat = jnp.transpose(blocks, (0, 1, 3, 2, 4)).reshape(B, n_cmp, 2, CMP_BLOCK * Dh)
    hid = jax.nn.gelu(jnp.einsum('bjcf,cfe->bjce', flat, cmp_w1))
    comp = jnp.einsum('bjce,ceo->bjco', hid, cmp_w2)
    k_c, v_c = comp[:, :, 0], comp[:, :, 1]
    cmp_end = jnp.arange(n_cmp) * CMP_STRIDE + CMP_BLOCK - 1
    valid = cmp_end[None, :] <= t[:, None]
    s = jnp.einsum('bthd,bjd->bhtj', q, k_c).astype(F32) * scale
    s = jnp.where(valid, s, -jnp.inf)
    m = jnp.max(s, axis=-1, keepdims=True)
    m = jnp.where(jnp.isfinite(m), m, 0.0)
    e = jnp.exp(s - m)
    den = jnp.sum(e, axis=-1, keepdims=True)
    p_cmp = e / jnp.where(den > 0, den, 1.0)
    o_cmp = jnp.einsum('bhtj,bjd->bthd', p_cmp.astype(v_c.dtype), v_c)
    n_sel = S // SEL_BLOCK
    topn = min(SEL_TOPN, n_sel)
    sel_start = jnp.arange(n_sel) * SEL_BLOCK
    cmp_start = jnp.arange(n_cmp) * CMP_STRIDE
    overlap = ((cmp_start[:, None] < sel_start[None, :] + SEL_BLOCK) &
               (cmp_start[:, None] + CMP_BLOCK > sel_start[None, :])).astype(F32)
    imp = jnp.einsum('bhtj,jn->btn', p_cmp, overlap)
    cur = t // SEL_BLOCK
    n_ids = jnp.arange(n_sel)
    forced = (n_ids[None, :] == 0) | (n_ids[None, :] == cur[:, None]) | (n_ids[None, :] == cur[:, None] - 1)
    imp = jnp.where(forced, jnp.inf, imp)
    imp = jnp.where(n_ids[None, :] > cur[:, None], -jnp.inf, imp)
    top_val, sel_idx = lax.top_k(imp, topn)
    sel_ok = top_val > -jnp.inf
    q_r = rope(q, pos)
    k_s = rope(kv[:, :, 2:3], pos)[:, :, 0]
    k_w = rope(kv[:, :, 4:5], pos)[:, :, 0]
    k_sb = k_s.reshape(B, n_sel, SEL_BLOCK, Dh)
    v_sb = kv[:, :, 3].reshape(B, n_sel, SEL_BLOCK, Dh)
    pad = jnp.zeros((B, WINDOW, Dh), k_w.dtype)
    k_wp = jnp.concatenate([pad, k_w], axis=1)
    v_wp = jnp.concatenate([pad, kv[:, :, 5]], axis=1)
    b_ids = jnp.arange(B)[:, None, None]
    nb = S // Q_BLOCK

    def block(args):
        qb, idx, ok, i = args
        qpos = i * Q_BLOCK + jnp.arange(Q_BLOCK)
        kg = k_sb[b_ids, idx]
        vg = v_sb[b_ids, idx]
        kpos = idx[..., None] * SEL_BLOCK + jnp.arange(SEL_BLOCK)
        msk = ok[..., None] & (kpos <= qpos[None, :, None, None])
        ss = jnp.einsum('bqhd,bqnld->bhqnl', qb, kg).astype(F32) * scale
        ss = jnp.where(msk[:, None], ss, -jnp.inf).reshape(B, H, Q_BLOCK, topn * SEL_BLOCK)
        ps = jax.nn.softmax(ss, axis=-1).astype(vg.dtype).reshape(B, H, Q_BLOCK, topn, SEL_BLOCK)
        o_s = jnp.einsum('bhqnl,bqnld->bqhd', ps, vg)
        start = i * Q_BLOCK
        kw = lax.dynamic_slice_in_dim(k_wp, start, WINDOW + Q_BLOCK, axis=1)
        vw = lax.dynamic_slice_in_dim(v_wp, start, WINDOW + Q_BLOCK, axis=1)
        wpos = start - WINDOW + jnp.arange(WINDOW + Q_BLOCK)
        mw = ((wpos[None, :] <= qpos[:, None]) & (wpos[None, :] > qpos[:, None] - WINDOW) &
              (wpos[None, :] >= 0))
        sw = jnp.einsum('bqhd,bkd->bhqk', qb, kw).astype(F32) * scale
        pw = jax.nn.softmax(jnp.where(mw, sw, -jnp.inf), axis=-1).astype(vw.dtype)
        o_w = jnp.einsum('bhqk,bkd->bqhd', pw, vw)
        return o_s, o_w

    o_s, o_w = lax.map(block, (_to_blocks(q_r, nb), _to_blocks(sel_idx, nb),
                               _to_blocks(sel_ok, nb), jnp.arange(nb)))
    o_s, o_w = _from_blocks(o_s), _from_blocks(o_w)
    g = jax.nn.sigmoid(gate_logits.reshape(B, S, H, 3))
    o = g[..., 0:1] * o_cmp + g[..., 1:2] * o_s + g[..., 2:3] * o_w
    return o.reshape(B, S, H * Dh)


def causal_softmax_attention(q, k, v, scale):
    B, S, H, _ = q.shape
    nb = S // Q_BLOCK
    kpos = jnp.arange(S)

    def block(args):
        qb, i = args
        qpos = i * Q_BLOCK + jnp.arange(Q_BLOCK)
        s = jnp.einsum('bqhd,bkhd->bhqk', qb, k).astype(F32) * scale
        s = jnp.where(kpos[None, :] <= qpos[:, None], s, -jnp.inf)
        p = jax.nn.softmax(s, axis=-1).astype(v.dtype)
        return jnp.einsum('bhqk,bkhd->bqhd', p, v)

    return _from_blocks(lax.map(block, (_to_blocks(q, nb), jnp.arange(nb))))


def mla_attention(q_lat, kv_lat, k_rope, pos, q_norm, kv_norm, w_uq, w_ukv):
    B, S, _ = q_lat.shape
    q = (rms_norm(q_lat, q_norm) @ w_uq).reshape(B, S, MLA_HEADS, MLA_NOPE + MLA_ROPE)
    q = jnp.concatenate([q[..., :MLA_NOPE], rope(q[..., MLA_NOPE:], pos)], axis=-1)
    kv = (rms_norm(kv_lat, kv_norm) @ w_ukv).reshape(B, S, MLA_HEADS, MLA_NOPE + MLA_V)
    k_r = jnp.broadcast_to(rope(k_rope[:, :, None, :], pos), (B, S, MLA_HEADS, MLA_ROPE))
    k = jnp.concatenate([kv[..., :MLA_NOPE], k_r], axis=-1)
    v = kv[..., MLA_NOPE:]
    o = causal_softmax_attention(q, k, v, (MLA_NOPE + MLA_ROPE) ** -0.5)
    return o.reshape(B, S, MLA_HEADS * MLA_V)


def stick_breaking_attention(qkv):
    B, S, _ = qkv.shape
    q, k, v = jnp.split(qkv.reshape(B, S, 3, SB_HEADS, SB_HEAD_DIM), 3, axis=2)
    q, k, v = q[:, :, 0], k[:, :, 0], v[:, :, 0]
    scale = SB_HEAD_DIM ** -0.5
    nb = S // Q_BLOCK
    kpos = jnp.arange(S)

    def block(args):
        qb, i = args
        qpos = i * Q_BLOCK + jnp.arange(Q_BLOCK)
        mask = kpos[None, :] < qpos[:, None]
        z = jnp.einsum('bqhd,bkhd->bhqk', qb, k).astype(F32) * scale
        log_beta = jax.nn.log_sigmoid(z)
        log_keep = jnp.where(mask, jax.nn.log_sigmoid(-z), 0.0)
        later = lax.cumsum(log_keep, axis=3, reverse=True) - log_keep
        a = jnp.where(mask, jnp.exp(log_beta + later), 0.0)
        return jnp.einsum('bhqk,bkhd->bqhd', a.astype(v.dtype), v)

    o = _from_blocks(lax.map(block, (_to_blocks(q, nb), jnp.arange(nb))))
    return o.reshape(B, S, SB_HEADS * SB_HEAD_DIM)


def swiglu(x, w_in, w_out):
    a, u = jnp.split(x @ w_in, 2, axis=-1)
    return (jax.nn.silu(a) * u) @ w_out


def moe_swiglu(x, w_router, w_in, w_out):
    B, S, D = x.shape
    xt = x.reshape(-1, D)
    N = xt.shape[0]
    logits = (xt @ w_router).astype(F32)
    top_val, top_idx = lax.top_k(logits, TOP_K)
    gate = jax.nn.softmax(top_val, axis=-1)
    flat_e = top_idx.reshape(-1)
    flat_tok = jnp.repeat(jnp.arange(N, dtype=jnp.int32), TOP_K)
    flat_w = gate.reshape(-1)
    order = jnp.argsort(flat_e)
    e_sorted = flat_e[order]
    counts = jnp.bincount(flat_e, length=N_EXPERTS)
    padded = ((counts + MOE_BLOCK - 1) // MOE_BLOCK) * MOE_BLOCK
    start = jnp.cumsum(counts) - counts
    start_pad = jnp.cumsum(padded) - padded
    dest = start_pad[e_sorted] + (jnp.arange(N * TOP_K) - start[e_sorted])
    n_rows = ((N * TOP_K + MOE_BLOCK - 1) // MOE_BLOCK) * MOE_BLOCK + N_EXPERTS * MOE_BLOCK
    row_tok = jnp.full((n_rows,), N, jnp.int32).at[dest].set(flat_tok[order])
    row_w = jnp.zeros((n_rows,), F32).at[dest].set(flat_w[order])
    n_blk = n_rows // MOE_BLOCK
    blk_expert = jnp.searchsorted(jnp.cumsum(padded), jnp.arange(n_blk) * MOE_BLOCK, side='right')
    blk_expert = jnp.minimum(blk_expert, N_EXPERTS - 1)
    x_pad = jnp.concatenate([xt, jnp.zeros((1, D), xt.dtype)], axis=0)
    xs = x_pad[row_tok].reshape(n_blk, MOE_BLOCK, D)

    def expert_block(args):
        xb, e = args
        a, u = jnp.split(xb @ w_in[e], 2, axis=-1)
        return (jax.nn.silu(a) * u) @ w_out[e]

    ys = lax.map(expert_block, (xs, blk_expert)).reshape(n_rows, D)
    ys = ys * row_w[:, None].astype(ys.dtype)
    out = jax.ops.segment_sum(ys, row_tok, num_segments=N + 1)[:N]
    return out.reshape(B, S, D)


def setup_inputs(seed: int = 0) -> dict:
    key = jax.random.key(seed)
    ks = iter(jax.random.split(key, 40))
    nrm = lambda shape, s: jax.random.normal(next(ks), shape, F32) * s
    gain = lambda shape: 1.0 + 0.01 * jax.random.normal(next(ks), shape, F32)
    x = jax.random.normal(next(ks), (BATCH, SEQ, D_MODEL), F32)
    p = jax.random.normal(next(ks), (DEPTH, BATCH, SEQ, P_DIM), F32)
    offset = jax.random.randint(next(ks), (BATCH, 1), 0, 4096, dtype=jnp.int32)
    positions = offset + jnp.arange(SEQ, dtype=jnp.int32)[None, :]
    return {
        'x': x, 'p': p, 'positions': positions,
        'w_in': nrm((DEPTH, D_MODEL, D_IN), D_MODEL ** -0.5),
        'conv_w': nrm((DEPTH, CONV_WIDTH, CONV_CH), CONV_WIDTH ** -0.5),
        'conv_b': nrm((DEPTH, CONV_CH), 0.01),
        'conv_ln_g': gain((DEPTH, CONV_CH)),
        'conv_ln_b': nrm((DEPTH, CONV_CH), 0.01),
        'nsa_cmp_pe': nrm((DEPTH, CMP_BLOCK, 2, NSA_HEAD_DIM), 0.02),
        'nsa_cmp_w1': nrm((DEPTH, 2, CMP_BLOCK * NSA_HEAD_DIM, NSA_HEAD_DIM), (CMP_BLOCK * NSA_HEAD_DIM) ** -0.5),
        'nsa_cmp_w2': nrm((DEPTH, 2, NSA_HEAD_DIM, NSA_HEAD_DIM), NSA_HEAD_DIM ** -0.5),
        'mla_q_norm': gain((DEPTH, MLA_Q_RANK)),
        'mla_kv_norm': gain((DEPTH, MLA_KV_RANK)),
        'mla_w_uq': nrm((DEPTH, MLA_Q_RANK, MLA_HEADS * (MLA_NOPE + MLA_ROPE)), MLA_Q_RANK ** -0.5),
        'mla_w_ukv': nrm((DEPTH, MLA_KV_RANK, MLA_HEADS * (MLA_NOPE + MLA_V)), MLA_KV_RANK ** -0.5),
        'w_branch': nrm((DEPTH, N_BRANCH, BRANCH_W, D_MODEL), BRANCH_W ** -0.5),
        'w_out': nrm((DEPTH, D_MODEL, D_MODEL), D_MODEL ** -0.5 * DEEPNORM_BETA),
        'ln1_g': gain((DEPTH, D_MODEL)),
        'ln1_b': nrm((DEPTH, D_MODEL), 0.01),
        'ffn_w_in': nrm((N_DENSE, D_MODEL, 2 * D_FF), D_MODEL ** -0.5),
        'ffn_w_out': nrm((N_DENSE, D_FF, D_MODEL), D_FF ** -0.5 * DEEPNORM_BETA),
        'moe_router': nrm((N_MOE, D_MODEL, N_EXPERTS), D_MODEL ** -0.5),
        'moe_w_in': nrm((N_MOE, N_EXPERTS, D_MODEL, 2 * D_FF_EXPERT), D_MODEL ** -0.5),
        'moe_w_out': nrm((N_MOE, N_EXPERTS, D_FF_EXPERT, D_MODEL), D_FF_EXPERT ** -0.5 * DEEPNORM_BETA),
        'ple_w_gate': nrm((DEPTH, D_MODEL, D_MODEL), D_MODEL ** -0.5),
        'ple_w_proj': nrm((DEPTH, P_DIM, D_MODEL), P_DIM ** -0.5 * DEEPNORM_BETA),
        'ln2_g': gain((DEPTH, D_MODEL)),
        'ln2_b': nrm((DEPTH, D_MODEL), 0.01),
    }


def reference(x, p, positions, w_in, conv_w, conv_b, conv_ln_g, conv_ln_b, nsa_cmp_pe, nsa_cmp_w1,
              nsa_cmp_w2, mla_q_norm, mla_kv_norm, mla_w_uq, mla_w_ukv, w_branch, w_out, ln1_g, ln1_b,
              ffn_w_in, ffn_w_out, moe_router, moe_w_in, moe_w_out, ple_w_gate, ple_w_proj, ln2_g, ln2_b):
    B, S, _ = x.shape
    pts = _split_points()
    for i in range(DEPTH):
        u = x @ w_in[i]
        c_glu, nq, nkv, ng, mq, mkv, mkr, sbqkv, bg = jnp.split(u, pts, axis=-1)
        y_a = conformer_conv(c_glu, conv_w[i], conv_b[i], conv_ln_g[i], conv_ln_b[i])
        y_b = nsa_attention(nq.reshape(B, S, NSA_HEADS, NSA_HEAD_DIM),
                            nkv.reshape(B, S, 6, NSA_HEAD_DIM), ng, positions,
                            nsa_cmp_pe[i], nsa_cmp_w1[i], nsa_cmp_w2[i])
        y_c = mla_attention(mq, mkv, mkr, positions, mla_q_norm[i], mla_kv_norm[i],
                            mla_w_uq[i], mla_w_ukv[i])
        y_d = stick_breaking_attention(sbqkv)
        branches = jnp.stack([y_a, y_b, y_c, y_d], axis=2)
        proj = jnp.einsum('bsnc,ncd->bsnd', branches, w_branch[i])
        gates = jax.nn.sigmoid(bg.reshape(B, S, N_BRANCH, D_MODEL))
        mixed = jnp.einsum('bsnd,bsnd->bsd', gates, proj) @ w_out[i]
        x = layer_norm(DEEPNORM_ALPHA * x + mixed, ln1_g[i], ln1_b[i])
        if i % 2 == 0:
            f = swiglu(x, ffn_w_in[i // 2], ffn_w_out[i // 2])
        else:
            f = moe_swiglu(x, moe_router[i // 2], moe_w_in[i // 2], moe_w_out[i // 2])
        ple = jax.nn.sigmoid(x @ ple_w_gate[i]) * (p[i] @ ple_w_proj[i])
        x = layer_norm(DEEPNORM_ALPHA * x + f + ple, ln2_g[i], ln2_b[i])
    return x
```

```python
import math
import contextlib
import numpy as np
import concourse.bass as bass
import concourse.mybir as mybir
from concourse.bass_utils import run_bass_kernel_spmd

F32 = mybir.dt.float32
BF16 = mybir.dt.bfloat16
I32 = mybir.dt.int32
AF = mybir.ActivationFunctionType
ALU = mybir.AluOpType
AX = mybir.AxisListType

T = 2048
D = 1024
NB = 16
ALPHA = 4.0 ** 0.25
LN_EPS = 1e-5
RMS_EPS = 1e-6
THETA = 10000.0
D_FF = 2816
D_FFE = 3584
NE = 8

ENGS = ("pe", "act", "dve", "pool", "sp")
N_DMA_SEMS = 6


class Op:
    __slots__ = ("eng", "fn", "deps", "is_dma", "signal", "sig_val", "dma_sem", "dma_val",
                 "dma_prev", "idx")

    def __init__(self, eng, fn, is_dma):
        self.eng = eng
        self.fn = fn
        self.deps = []
        self.is_dma = is_dma
        self.signal = False
        self.sig_val = 0
        self.dma_sem = None
        self.dma_val = 0
        self.dma_prev = 0
        self.idx = 0


class Sched:
    def __init__(self):
        self.ops = {e: [] for e in ENGS}
        self.last_w = {}
        self.readers = {}
        self.all_ops = []

    def op(self, eng, fn, reads=(), writes=(), dma=False, acc=False):
        o = Op(eng, fn, dma)
        deps = []
        for k in reads:
            w = self.last_w.get(k)
            if w is not None:
                deps.append(w)
            if isinstance(k, tuple) and k[0] == "pb":
                for r in self.readers.get(k, ()):
                    if r.eng != eng:
                        deps.append(r)
        for k in writes:
            w = self.last_w.get(k)
            if w is not None and not (acc and w.eng == eng and not w.is_dma):
                deps.append(w)
            for r in self.readers.get(k, ()):
                deps.append(r)
        seen = set()
        for d in deps:
            if id(d) not in seen and d is not o:
                seen.add(id(d))
                o.deps.append(d)
        for k in reads:
            lst = self.readers.setdefault(k, [])
            if not dma:
                for i, r in enumerate(lst):
                    if r.eng == eng and not r.is_dma:
                        lst[i] = o
                        break
                else:
                    lst.append(o)
            else:
                lst.append(o)
        for k in writes:
            self.last_w[k] = o
            self.readers[k] = []
        o.idx = len(self.ops[eng])
        self.ops[eng].append(o)
        self.all_ops.append(o)
        return o

    def barrier(self):
        lasts = []
        for e in ENGS:
            ops = self.ops[e]
            nd = 0
            got_real = False
            for o in reversed(ops):
                if o.fn is None:
                    continue
                if o.is_dma:
                    if nd < N_DMA_SEMS:
                        lasts.append(o)
                        nd += 1
                elif not got_real:
                    lasts.append(o)
                    got_real = True
                if got_real and nd >= N_DMA_SEMS:
                    break
        for e in ENGS:
            o = Op(e, None, False)
            o.deps = [l for l in lasts]
            o.idx = len(self.ops[e])
            self.ops[e].append(o)
            self.all_ops.append(o)
        self.last_w = {}
        self.readers = {}

    def emit(self, nc, final_wait_ops=()):
        for fo in final_wait_ops:
            if not fo.is_dma:
                fo.signal = True
        for o in self.all_ops:
            for d in o.deps:
                if not d.is_dma:
                    d.signal = True
        cnt = {e: 0 for e in ENGS}
        for e in ENGS:
            for o in self.ops[e]:
                if o.signal and not o.is_dma:
                    cnt[e] += 1
                    o.sig_val = cnt[e]
        dma_count = {}
        for e in ENGS:
            k = 0
            for o in self.ops[e]:
                if o.is_dma:
                    j = k % N_DMA_SEMS
                    k += 1
                    key = (e, j)
                    prev = dma_count.get(key, 0)
                    o.dma_sem = key
                    o.dma_prev = prev
                    o.dma_val = prev + 16
                    dma_count[key] = prev + 16
        with contextlib.ExitStack() as st:
            sems = {e: st.enter_context(nc.semaphore("s_" + e)) for e in ENGS if cnt[e] > 0}
            dsems = {key: st.enter_context(nc.semaphore("d_%s%d" % key)) for key in dma_count}
            block = st.enter_context(nc.Block())
            regs = {"pe": block.tensor, "act": block.scalar, "dve": block.vector,
                    "pool": block.gpsimd, "sp": block.sync}

            def make(e):
                def body(eng):
                    known = {}
                    for o in self.ops[e]:
                        waits = {}
                        for d in o.deps:
                            if d.is_dma:
                                s, v = dsems[d.dma_sem], d.dma_val
                            else:
                                s, v = sems[d.eng], d.sig_val
                            kk = id(s)
                            if known.get(kk, 0) >= v:
                                continue
                            if kk not in waits or waits[kk][1] < v:
                                waits[kk] = (s, v)
                        if o.is_dma and o.dma_prev > 0:
                            s = dsems[o.dma_sem]
                            kk = id(s)
                            if known.get(kk, 0) < o.dma_prev:
                                if kk not in waits or waits[kk][1] < o.dma_prev:
                                    waits[kk] = (s, o.dma_prev)
                        for kk, (s, v) in waits.items():
                            eng.wait_ge(s, v)
                            known[kk] = v
                        if o.fn is None:
                            continue
                        ins = o.fn(eng)
                        if o.is_dma:
                            ins.then_inc(dsems[o.dma_sem], 16)
                        elif o.signal:
                            ins.then_inc(sems[e], 1)
                    if e == "sp":
                        for fo in final_wait_ops:
                            if fo.is_dma:
                                eng.wait_ge(dsems[fo.dma_sem], fo.dma_val)
                            else:
                                eng.wait_ge(sems[fo.eng], fo.sig_val)
                return body

            for e in ENGS:
                if self.ops[e] or e == "sp":
                    regs[e](make(e))


OFF_CONV, OFF_NQ, OFF_NK, OFF_MISC, OFF_KR, OFF_SB, OFF_TOK, OFF_GATE = 0, 512, 1024, 1536, 2048, 2304, 2816, 3328
NCOLS_R = 7424


def _win_index():
    sw64 = lambda b: list(range(b + 32, b + 64)) + list(range(b, b + 32))
    sw32 = lambda b: list(range(b + 16, b + 32)) + list(range(b, b + 16))
    idx = []
    idx += list(range(0, 512))
    idx += list(range(512, 768))
    for h in range(4):
        idx += sw64(512 + 64 * h)
    ks, kw = 896, 1024
    idx += list(range(ks, ks + 64)) * 2 + sw64(ks) * 2 + list(range(kw, kw + 64)) * 2 + sw64(kw) * 2
    idx += list(range(768, 896)) + list(range(1164, 1420)) + list(range(1420, 1548))
    idx += [-1] * 64 + list(range(1548, 1580)) + [-1] * 32
    idx += [-1] * 64 + sw32(1548) + [-1] * 32
    idx += list(range(1580, 1580 + 512))
    idx += list(range(960, 1024)) + list(range(1088, 1152)) + list(range(1152, 1164)) + [-1] * 116
    idx += list(range(1580 + 512, 1580 + 768))
    idx += list(range(2348, 6444))
    assert len(idx) == NCOLS_R
    return np.array(idx)


def _host_consts():
    c = {}
    c["ident"] = np.eye(128, dtype=np.float32)
    p = np.arange(128)[:, None]
    f = np.arange(128)[None, :]
    cm = np.zeros((128, 4, 128), np.float32)
    cm[:, 0] = (p <= f)
    cm[:, 1] = (p < f)
    cm[:, 2] = (p > f)
    cm[:, 3] = 1.0
    c["cmask"] = cm
    j = np.arange(128)[:, None]
    t = np.arange(T)[None, :]
    c["cmpvalid"] = ((16 * j + 31 <= t) & (j < 127)).astype(np.float32)
    n = np.arange(32)[:, None]
    c["eexp"] = ((t // 64) == n).astype(np.float32)
    jj = np.arange(127)
    nn = np.arange(32)
    ov = ((jj[:, None] * 16 < nn[None, :] * 64 + 64) & (jj[:, None] * 16 + 32 > nn[None, :] * 64)).astype(np.float32)
    ovl = np.zeros((128, 33), np.float32)
    ovl[:127, :32] = ov
    ovl[:127, 32] = 1.0
    c["ovl"] = ovl
    cur = (np.arange(T) // 64)[:, None]
    nid = np.arange(32)[None, :]
    forced = (nid == 0) | (nid == cur) | (nid == cur - 1)
    future = nid > cur
    keep = (~forced & ~future).astype(np.float32)
    add = np.where(future, -1e30, np.where(forced, 100.0, 0.0)).astype(np.float32)
    ka = np.zeros((128, 2, 16, 32), np.float32)
    ka[:, 0] = keep.reshape(16, 128, 32).transpose(1, 0, 2)
    ka[:, 1] = add.reshape(16, 128, 32).transpose(1, 0, 2)
    c["keepadd"] = ka
    rc = np.zeros((128, 4), np.float32)
    pp = np.arange(128)
    rc[:, 0] = THETA ** (-(pp % 32).astype(np.float64) / 32.0)
    rc[:, 1] = np.where((pp % 64) < 32, -1.0, 1.0)
    m = (pp >= 64) & (pp < 96)
    rc[m, 2] = THETA ** (-((pp[m] - 64) % 16).astype(np.float64) / 16.0)
    rc[m, 3] = np.where((pp[m] - 64) < 16, -1.0, 1.0)
    c["ropec"] = rc
    ind = np.zeros((128, 16, 16), np.float32)
    for kb in range(16):
        ind[:, kb, kb] = 1.0
    c["indt"] = ind
    sg = np.zeros((16, 16, 128), np.float32)
    for kb in range(16):
        sg[kb + 1:, kb, :] = 1.0
    c["selgt"] = sg
    return c


CONST_SHAPES = {"ident": [128, 128], "cmask": [128, 4, 128], "cmpvalid": [128, T], "eexp": [32, T],
                "ovl": [128, 33], "keepadd": [128, 2, 16, 32], "ropec": [128, 4], "indt": [128, 16, 16],
                "selgt": [16, 16, 128]}


class MK:
    NW = 3

    def __init__(self, layers=(0, 1), debug=False, stop_after=None):
        self.layers = layers
        self.debug = debug
        self.stop_after = stop_after
        self.nc = bass.Bass("TRN2", target_bir_lowering=False)
        self.S = Sched()
        self.st = contextlib.ExitStack()
        self.st.enter_context(self.nc.allow_low_precision(reason="bf16 matmul operands / fp32 accumulation by design"))
        self.wi = 0
        self.stg_i = 0
        self.si = 0
        self.ai = 0
        self.finals = []
        self.dbg_outs = {}

    def din(self, name, shape, dt=F32):
        return self.nc.dram_tensor(name, list(shape), dt, kind="ExternalInput").ap()

    def dout(self, name, shape, dt=F32):
        return self.nc.dram_tensor(name, list(shape), dt, kind="ExternalOutput").ap()

    def sb(self, name, shape, dt):
        return self.st.enter_context(self.nc.sbuf_tensor(name, list(shape), dt))

    def arena_reset(self):
        self.S.barrier()
        self.aoff = 0

    def carve(self, shape, dt, parts=128):
        n = 1
        for s in shape:
            n *= s
        nbytes = n * (4 if dt in (F32, I32) else 2)
        nbytes = (nbytes + 63) // 64 * 64
        off = self.aoff
        self.aoff += nbytes
        assert self.aoff <= self.ARENA_BYTES, (self.aoff, self.ARENA_BYTES)
        v = self.arena[0:parts, off // 2:(off + n * (4 if dt in (F32, I32) else 2)) // 2]
        if dt != BF16:
            v = v.bitcast(dt)
        if len(shape) == 2:
            v = v.rearrange("p (a b) -> p a b", a=shape[0])
        elif len(shape) == 3:
            v = v.rearrange("p (a b c) -> p a b c", a=shape[0], b=shape[1])
        return v

    def short(self):
        b = self.si % 4
        self.si += 1
        return b

    def accb(self):
        b = 4 + self.ai % 4
        self.ai += 1
        return b

    def mm(self, out, lhsT, rhs, start, stop, r, w):
        self.S.op("pe", lambda e: e.matmul(out, lhsT=lhsT, rhs=rhs, start=start, stop=stop),
                  reads=r, writes=w, acc=True)

    def tr(self, out, in_, r, w, parts=128):
        idt = self.ident[0:parts, 0:parts]
        self.S.op("pe", lambda e: e.transpose(out, in_, idt), reads=list(r) + ["ident"], writes=w, acc=True)

    def A(self, out, in_, func, r, w, bias=None, scale=None, accum=None):
        kw = {}
        if bias is not None:
            kw["bias"] = bias
        if scale is not None:
            kw["scale"] = scale
        if accum is not None:
            kw["accum_out"] = accum
        self.S.op("act", lambda e: e.activation(out=out, in_=in_, func=func, **kw), reads=r, writes=w)

    def TT(self, eng, out, in0, in1, op, r, w):
        self.S.op(eng, lambda e: e.tensor_tensor(out=out, in0=in0, in1=in1, op=op), reads=r, writes=w)

    def TS(self, eng, out, in0, s1, s2, op0, op1, r, w):
        if op1 is None:
            self.S.op(eng, lambda e: e.tensor_scalar(out=out, in0=in0, scalar1=s1, scalar2=None, op0=op0),
                      reads=r, writes=w)
        else:
            self.S.op(eng, lambda e: e.tensor_scalar(out=out, in0=in0, scalar1=s1, scalar2=s2, op0=op0, op1=op1),
                      reads=r, writes=w)

    def STT(self, eng, out, in0, scalar, in1, op0, op1, r, w):
        self.S.op(eng, lambda e: e.scalar_tensor_tensor(out=out, in0=in0, scalar=scalar, in1=in1, op0=op0, op1=op1),
                  reads=r, writes=w)

    def CP(self, eng, out, in_, r, w):
        if eng == "act":
            self.S.op("act", lambda e: e.activation(out=out, in_=in_, func=AF.Copy), reads=r, writes=w)
        else:
            self.S.op(eng, lambda e: e.tensor_copy(out=out, in_=in_), reads=r, writes=w)

    def MS(self, eng, out, val, w):
        self.S.op(eng, lambda e: e.memset(out, val), writes=w)

    def dma(self, out, in_, r, w, eng="sp"):
        return self.S.op(eng, lambda e: e.dma_start(out=out, in_=in_), reads=r, writes=w, dma=True)

    CAST_ENGS = ("pool", "act", "pool", "dve")

    def cast_load(self, dst, src, parts, free_shape, dkey):
        n = 1
        for x_ in free_shape:
            n *= x_
        assert n <= 2048, n
        si_ = self.stg_i % 2
        ce = self.CAST_ENGS[self.stg_i % 4]
        self.stg_i += 1
        stg = self.stage[si_][0:parts, 0:n]
        if len(free_shape) == 2:
            stg = stg.rearrange("p (a b) -> p a b", a=free_shape[0])
        elif len(free_shape) == 3:
            stg = stg.rearrange("p (a b c) -> p a b c", a=free_shape[0], b=free_shape[1])
        sk = ("stg", si_)
        self.dma(stg, src, [], [sk])
        self.CP(ce, dst, stg, [sk], [dkey])

    def dma_w1(self, w1t, src, c):
        si_ = self.stg_i % 2
        ce = self.CAST_ENGS[self.stg_i % 4]
        self.stg_i += 1
        ps_ = slice(c * 64, (c + 1) * 64)
        stg = self.stage[si_][ps_, 0:2048].rearrange("p (l e) -> p l e", l=32)
        sk = ("stg", si_)
        self.dma(stg, src.rearrange("(l d) e -> d l e", d=64), [], [sk])
        self.CP(ce, w1t[ps_, :, :], stg, [sk], ["w1t"])

    def wload(self, src, kc, n, rows=128):
        slot = self.wi % self.NW
        self.wi += 1
        assert kc * n <= 4096
        v = self.wring[slot][0:rows, 0:kc * n].rearrange("p (c n) -> p c n", c=kc)
        srcv = src.rearrange("(c p) n -> p c n", p=rows)
        key = ("w", slot)
        step = max(1, 2048 // n)
        for k0 in range(0, kc, step):
            k1 = min(kc, k0 + step)
            self.cast_load(v[:, k0:k1, :], srcv[:, k0:k1, :], rows, [k1 - k0, n], key)
        return v, key

    def proj_fm(self, wv, wkey, c0, M, tt, src=None, srckey=None, kc=8):
        b = self.short()
        src = self.xT if src is None else src
        srckey = ("xT", tt) if srckey is None else srckey
        for k in range(kc):
            self.mm(self.pb[b][0:M, :], wv[:, k, c0:c0 + M], src[:, k, tt * 512:(tt + 1) * 512],
                    k == 0, k == kc - 1, [wkey, srckey], [("pb", b)])
        return b

    def build(self):
        nc, S = self.nc, self.S
        L = self.layers
        x_in = self.din("x", [T, D])
        p_in = self.din("p", [2, T, 256])
        pos_in = self.din("pos", [1, T], I32)
        win = self.din("win", [2, D, NCOLS_R])
        convp = self.din("convp", [2, 128, 2, 34])
        pe_r = self.din("pe_r", [2, 128, 32])
        w1 = self.din("w1", [2, 2, 2048, 64])
        w2k = self.din("w2k", [2, 64, 128])
        w2v = self.din("w2v", [2, 64, 64])
        qn = self.din("qn", [2, 128, 2])
        kvn = self.din("kvn", [2, 128, 1])
        uq = self.din("uq", [2, 256, 768])
        ukv = self.din("ukv", [2, 128, 512])
        wbr = self.din("wbr", [2, 4, 256, D])
        wout = self.din("wout", [2, D, D])
        ln1g = self.din("ln1g", [2, D])
        ln1b = self.din("ln1b", [2, D])
        ln2g = self.din("ln2g", [2, D])
        ln2b = self.din("ln2b", [2, D])
        lite = self.stop_after is not None and self.stop_after[0] in ("conv", "nsa", "mla", "sb", "mix")
        need_ffn = (0 in L) and not lite
        need_moe = (1 in L) and not (lite and self.stop_after[1] == 0) and self.stop_after != ("ffn", 0)
        ffn_in = self.din("ffn_in", [D, 2 * D_FF]) if need_ffn else None
        ffn_out = self.din("ffn_out", [D_FF, D]) if need_ffn else None
        router = self.din("router", [D, NE])
        moe_in = self.din("moe_in", [NE, D, 2 * D_FFE]) if need_moe else None
        moe_out = self.din("moe_out", [NE, D_FFE, D]) if need_moe else None
        pleg = self.din("pleg", [2, D, D])
        plep = self.din("plep", [2, 256, D])
        cst = {k: self.din("c_" + k, v) for k, v in CONST_SHAPES.items()}
        y_out = self.dout("y", [T, D])
        xa = self.dout("xa", [T, D])
        xb = self.dout("xb", [T, D])
        tabs_d = self.dout("tabs_d", [2, 128, 2 * T], BF16)
        self.dram = dict(locals())

        self.xT = self.sb("xT", [128, 8, T], BF16)
        self.wring = [self.sb("wr%d" % i, [128, 4096], BF16) for i in range(self.NW)]
        self.stage = [self.sb("stg%d" % i, [128, 2048], F32) for i in range(2)]
        self.ident = self.sb("ident", [128, 128], F32)
        self.cmask = self.sb("cmask", [128, 4, 128], BF16)
        self.onesf = self.sb("onesf", [128, 128], F32)
        self.gates = self.sb("gates", [128, NB, NE], F32)
        self.pb = [self.st.enter_context(nc.psum_tensor("pb%d" % i, [128, 512], F32)) for i in range(8)]
        self.ARENA_BYTES = 133 * 1024
        self.arena = self.sb("arena", [128, self.ARENA_BYTES // 2], BF16)
        self.aoff = 0

        self.dma(self.ident[:], cst["ident"], [], ["ident"])
        self.cast_load(self.cmask[:], cst["cmask"], 128, [4, 128], "cmask")
        self.MS("dve", self.onesf[:], 1.0, ["onesf"])

        self.setup_rope(pos_in, cst)
        self.load_xT(x_in)

        res_in = x_in
        outs = [(xa, xb), (xa, y_out)]
        for li in L:
            mid, fin = outs[li]
            if li == 1:
                res_in = xb
            if self.mixer_phase(li, res_in, mid):
                break
            if self.stop_after == ("mix", li):
                break
            self.ffn_phase(li, mid, fin)
            if self.stop_after == ("ffn", li):
                break
        S.emit(nc, final_wait_ops=self.finals)
        self.st.close()
        return nc

    def setup_rope(self, pos_in, cst):
        self.arena_reset()
        self.tabN = self.carve([2, T], BF16)
        self.tabM = self.carve([2, T], BF16)
        pi = self.carve([T], I32)
        pf = self.carve([T], F32)
        ang = self.carve([T], F32)
        kf = self.carve([T], F32)
        ki = self.carve([T], I32)
        rc = self.carve([4], F32)
        self.dma(pi, pos_in[0:1, :].to_broadcast([128, T]), [], ["pi"])
        self.dma(rc, cst["ropec"], [], ["rc"])
        self.CP("dve", pf, pi, ["pi"], ["pf"])
        for tab, ic, sc in ((self.tabN, 0, 1), (self.tabM, 2, 3)):
            for which in range(2):
                shift = math.pi / 2 if which == 0 else 0.0
                self.TS("dve", ang, pf, rc[:, ic:ic + 1], shift, ALU.mult, ALU.add, ["pf", "rc"], ["ang"])
                self.TS("dve", kf, ang, 1.0 / (2 * math.pi), None, ALU.mult, None, ["ang"], ["kf"])
                self.CP("dve", ki, kf, ["kf"], ["ki"])
                self.CP("dve", kf, ki, ["ki"], ["kf"])
                self.STT("dve", ang, kf, -2 * math.pi, ang, ALU.mult, ALU.add, ["kf", "ang"], ["ang"])
                self.TS("dve", kf, ang, math.pi, -2 * math.pi, ALU.is_gt, ALU.mult, ["ang"], ["kf"])
                self.TT("dve", ang, ang, kf, ALU.add, ["ang", "kf"], ["ang"])
                self.TS("dve", kf, ang, -math.pi, 2 * math.pi, ALU.is_lt, ALU.mult, ["ang"], ["kf"])
                self.TT("dve", ang, ang, kf, ALU.add, ["ang", "kf"], ["ang"])
                self.A(ang, ang, AF.Sin, ["ang"], ["ang"])
                if which == 0:
                    self.CP("dve", tab[:, 0, :], ang, ["ang"], ["tab"])
                else:
                    self.TS("dve", tab[:, 1, :], ang, rc[:, sc:sc + 1], None, ALU.mult, None, ["ang", "rc"], ["tab"])
        td = self.dram["tabs_d"]
        self.dma(td[0], self.tabN.rearrange("p a t -> p (a t)"), ["tab"], ["tabs_d"])
        self.dma(td[1], self.tabM.rearrange("p a t -> p (a t)"), ["tab"], ["tabs_d"])

    def x_to_xT(self, xblk, xkey, tb, rt=None):
        tt = tb // 4
        for half in range(2):
            b = self.short()
            for c in range(4):
                cc = half * 4 + c
                self.tr(self.pb[b][:, c * 128:(c + 1) * 128], xblk[:, cc * 128:(cc + 1) * 128], [xkey], [("pb", b)])
            dst = self.xT[:, half * 4:half * 4 + 4, tb * 128:(tb + 1) * 128]
            src = self.pb[b][:, :].rearrange("p (c t) -> p c t", c=4)
            self.CP("act" if half == 0 else "dve", dst, src, [("pb", b)], [("xT", tt)])
            if rt is not None:
                rt(half, b)

    def load_xT(self, x_in):
        self.arena_reset()
        xbs = [self.carve([D], F32) for _ in range(2)]
        for tb in range(NB):
            xb_ = xbs[tb % 2]
            key = ("xblk", tb % 2)
            self.dma(xb_, x_in[tb * 128:(tb + 1) * 128, :], [], [key])
            self.x_to_xT(xb_, key, tb)

    def mixer_phase(self, li, res_in, res_out):
        d = self.dram
        self.arena_reset()
        self.yT = [self.carve([2, T], BF16) for _ in range(4)]
        self.mixer_base = self.aoff
        for n, (nm, fn) in enumerate((("conv", self.conv_branch), ("nsa", self.nsa_branch), ("mla", self.mla_branch),
                                     ("sb", self.sb_branch))):
            only = getattr(self, "only", None)
            if only is None or nm in only:
                fn(li)
                self.dbg("yT%d_%d" % (n, li), self.yT[n], [128, 2, T], ["yT%d" % n])
            self.aoff = self.mixer_base
            self.S.barrier()
            if self.stop_after == (nm, li):
                return True
        self.merge_ln1(li, res_in, res_out)

    def dbg(self, name, ap, shape, keys, dt=BF16):
        if not self.debug:
            return
        o = self.dout("dbg_" + name, shape, dt)
        self.finals.append(self.dma(o, ap, keys, []))

    def conv_branch(self, li):
        d = self.dram
        win = d["win"]
        cp = self.carve([2, 34], F32)
        self.dma(cp, d["convp"][li], [], ["cp"])
        hp = self.carve([2, 30 + T], F32)
        acc = self.carve([2, T], F32)
        sig = [self.carve([512], F32) for _ in range(2)]
        self.MS("pool", hp[:, :, 0:30], 0.0, ["hp"])
        wv, wk = self.wload(win[li][:, OFF_CONV:OFF_CONV + 512], 8, 512)
        for tt in range(4):
            for c in range(2):
                ba = self.proj_fm(wv, wk, c * 128, 128, tt)
                bg = self.proj_fm(wv, wk, 256 + c * 128, 128, tt)
                sg = sig[c]
                self.A(sg, self.pb[bg][:, :], AF.Sigmoid, [("pb", bg)], [("sig", c)])
                self.TT("dve", hp[:, c, 30 + tt * 512:30 + (tt + 1) * 512], self.pb[ba][:, :], sg, ALU.mult,
                        [("pb", ba), ("sig", c)], ["hp"])
        for c in range(2):
            eng = "dve"
            self.TS(eng, acc[:, c, :], hp[:, c, 0:T], cp[:, c, 0:1], cp[:, c, 31:32], ALU.mult, ALU.add,
                    ["hp", "cp"], [("acc", c)])
            for w in range(1, 31):
                self.STT(eng, acc[:, c, :], hp[:, c, w:w + T], cp[:, c, w:w + 1], acc[:, c, :], ALU.mult, ALU.add,
                         ["hp", "cp", ("acc", c)], [("acc", c)])
        sq = [self.carve([512], F32) for _ in range(2)]
        m2 = self.carve([512], F32)
        rstd = self.carve([512], F32)
        dd = [self.carve([512], F32) for _ in range(2)]
        for tt in range(4):
            sl = slice(tt * 512, (tt + 1) * 512)
            bm = self.short()
            for c in range(2):
                self.mm(self.pb[bm][:, :], self.onesf[:, :], acc[:, c, sl], c == 0, c == 1,
                        ["onesf", ("acc", c)], [("pb", bm)])
            bq = self.short()
            for c in range(2):
                self.A(sq[c], acc[:, c, sl], AF.Square, [("acc", c)], [("sq", c)])
            for c in range(2):
                self.mm(self.pb[bq][:, :], self.onesf[:, :], sq[c], c == 0, c == 1,
                        ["onesf", ("sq", c)], [("pb", bq)])
            self.A(m2, self.pb[bm][:, :], AF.Square, [("pb", bm)], ["m2"], scale=1.0 / 256)
            self.STT("dve", rstd, self.pb[bq][:, :], 1.0 / 256, m2, ALU.mult, ALU.subtract, [("pb", bq), "m2"], ["rstd"])
            self.A(rstd, rstd, AF.Sqrt, ["rstd"], ["rstd"], bias=LN_EPS)
            self.S.op("dve", lambda e, r=rstd: e.reciprocal(out=r, in_=r), reads=["rstd"], writes=["rstd"])
            for c in range(2):
                self.STT("dve", dd[c], self.pb[bm][:, :], -1.0 / 256, acc[:, c, sl], ALU.mult, ALU.add,
                         [("pb", bm), ("acc", c)], [("dd", c)])
                self.TT("dve", dd[c], dd[c], rstd, ALU.mult, [("dd", c), "rstd"], [("dd", c)])
                self.A(self.yT[0][:, c, sl], dd[c], AF.Silu, [("dd", c), "cp"], ["yT0"],
                       scale=cp[:, c, 32:33], bias=cp[:, c, 33:34])

    def y_to_yT(self, ytile, ykey, n, qb):
        b = self.short()
        for c in range(2):
            self.tr(self.pb[b][:, c * 128:(c + 1) * 128], ytile[:, c * 128:(c + 1) * 128], [ykey], [("pb", b)])
        self.CP("act", self.yT[n][:, :, qb * 128:(qb + 1) * 128],
                self.pb[b][:, 0:256].rearrange("p (c t) -> p c t", c=2), [("pb", b)], ["yT%d" % n])

    def softmax_attn_block(self, qb, kbs, kT_fn, qT_ap, v_fn, scale, extra_mask=None, nm=""):
        ob = self.accb()
        n = len(kbs)
        for g0 in range(0, n, 4):
            grp = kbs[g0:g0 + 4]
            sbk = self.short()
            for i, (kb, mt) in enumerate(grp):
                kT, kkeys = kT_fn(kb)
                self.mm(self.pb[sbk][:, i * 128:(i + 1) * 128], kT, qT_ap[0], True, True,
                        list(kkeys) + list(qT_ap[1]), [("pb", sbk)])
            ei = self.ei % 3
            self.ei += 1
            E = self.Ebuf[ei]
            ek = ("E", ei)
            w = len(grp) * 128
            self.A(E[:, 0:w], self.pb[sbk][:, 0:w], AF.Exp, [("pb", sbk)], [ek], scale=scale)
            if extra_mask is not None:
                mtile, mkey = extra_mask
                self.TT("pool", E[:, 0:w], E[:, 0:w], mtile[:, g0 * 128:g0 * 128 + w], ALU.mult, [ek, mkey], [ek])
            else:
                for i, (kb, mt) in enumerate(grp):
                    if mt is not None:
                        mi = {"le": 0, "lt": 1, "gt": 2}[mt]
                        self.TT("pool", E[:, i * 128:(i + 1) * 128], E[:, i * 128:(i + 1) * 128],
                                self.cmask[:, mi, :], ALU.mult, [ek, "cmask"], [ek])
            for i, (kb, mt) in enumerate(grp):
                v, vkeys = v_fn(kb)
                gi = g0 + i
                self.mm(self.pb[ob][:, 0:65], E[:, i * 128:(i + 1) * 128], v, gi == 0, gi == n - 1,
                        [ek] + list(vkeys), [("pb", ob)])
        return ob

    def nsa_branch(self, li):
        d = self.dram
        win = d["win"]
        cst = {k: d["cst"][k] for k in d["cst"]}
        self.tabN = self.carve([2, T], BF16)
        self.dma(self.tabN.rearrange("p a t -> p (a t)"), d["tabs_d"][0], [], ["tab"])
        QT = self.carve([2, T], BF16)
        QR = self.carve([2, T], BF16)
        KS = self.carve([T], BF16)
        KW = self.carve([T], BF16)
        KCV = self.carve([T], BF16)
        vS = self.carve([NB, 65], BF16)
        vW = self.carve([NB, 65], BF16)
        sg = self.carve([NB, 12], F32)
        cmpvalid = self.carve([T], BF16)
        eexp = self.carve([T], BF16, parts=32)
        keepadd = self.carve([2, NB, 32], F32)
        VC = self.carve([97], BF16)
        w1t = self.carve([32, 64], BF16)
        pet = self.carve([32], BF16)
        w2kt = self.carve([128], BF16, parts=64)
        w2vt = self.carve([64], BF16, parts=64)
        hid = [self.carve([127], BF16, parts=64) for _ in range(2)]
        hb = self.carve([2], F32, parts=64)
        kcT = self.carve([127], BF16)
        t1 = [self.carve([512], F32) for _ in range(2)]
        t2 = [self.carve([512], F32) for _ in range(2)]
        self.Ebuf = [self.carve([512], BF16) for _ in range(3)]
        self.ei = 0
        ytile = [self.carve([256], F32) for _ in range(2)]
        selmask = [self.carve([NB * 128], BF16) for _ in range(2)]
        small = [self.carve([64], F32) for _ in range(2)]
        imp = [self.carve([32], F32) for _ in range(2)]
        scr = [self.carve([32], F32) for _ in range(2)]
        selT = [self.carve([128], BF16, parts=32) for _ in range(2)]
        self.cast_load(cmpvalid, cst["cmpvalid"], 128, [T], "cmpvalid")
        self.cast_load(eexp, cst["eexp"], 32, [T], "eexp")
        self.dma(keepadd, cst["keepadd"], [], ["keepadd"])
        self.cast_load(VC[:, 64:97], cst["ovl"], 128, [33], "VCc")
        for c in range(2):
            self.dma_w1(w1t, d["w1"][li, c], c)
        self.cast_load(pet, d["pe_r"][li], 128, [32], "pet")
        self.cast_load(w2kt, d["w2k"][li], 64, [128], "w2kt")
        self.cast_load(w2vt, d["w2v"][li], 64, [64], "w2vt")
        self.MS("dve", vS[:, :, 64:65], 1.0, ["vS"])
        self.MS("dve", vW[:, :, 64:65], 1.0, ["vW"])
        wv, wk = self.wload(win[li][:, OFF_NQ:OFF_NQ + 512], 8, 512)
        for tt in range(4):
            sl = slice(tt * 512, (tt + 1) * 512)
            for c in range(2):
                b0 = self.proj_fm(wv, wk, c * 128, 128, tt)
                b1 = self.proj_fm(wv, wk, 256 + c * 128, 128, tt)
                self.CP("act", QT[:, c, sl], self.pb[b0][:, :], [("pb", b0)], ["QT"])
                self.TT("dve", t1[c], self.pb[b0][:, :], self.tabN[:, 0, sl], ALU.mult, [("pb", b0), "tab"], [("t1", c)])
                self.TT("dve", t2[c], self.pb[b1][:, :], self.tabN[:, 1, sl], ALU.mult, [("pb", b1), "tab"], [("t2", c)])
                self.TT("pool", QR[:, c, sl], t1[c], t2[c], ALU.add, [("t1", c), ("t2", c)], ["QR"])
        wv, wk = self.wload(win[li][:, OFF_NK:OFF_NK + 512], 8, 512)
        for tt in range(4):
            sl = slice(tt * 512, (tt + 1) * 512)
            for c, dst, dk in ((0, KS, "KS"), (1, KW, "KW")):
                b0 = self.proj_fm(wv, wk, c * 256, 128, tt)
                b1 = self.proj_fm(wv, wk, c * 256 + 128, 128, tt)
                self.TT("dve", t1[c], self.pb[b0][:, :], self.tabN[:, 0, sl], ALU.mult, [("pb", b0), "tab"], [("t1", c)])
                self.TT("dve", t2[c], self.pb[b1][:, :], self.tabN[:, 1, sl], ALU.mult, [("pb", b1), "tab"], [("t2", c)])
                self.TT("pool", dst[:, sl], t1[c], t2[c], ALU.add, [("t1", c), ("t2", c)], [dk])
        wvm, wkm = self.wload(win[li][:, OFF_MISC:OFF_MISC + 128], 8, 128)
        for tt in range(4):
            sl = slice(tt * 512, (tt + 1) * 512)
            b0 = self.proj_fm(wvm, wkm, 0, 128, tt)
            self.CP("act", KCV[:, sl], self.pb[b0][:, :], [("pb", b0)], ["KCV"])
        wvt, wkt = self.wload(win[li][:, OFF_TOK:OFF_TOK + 140], 8, 140)
        for tb in range(NB):
            b = self.short()
            for k in range(8):
                self.mm(self.pb[b][:, 0:140], self.xT[:, k, tb * 128:(tb + 1) * 128], wvt[:, k, :], k == 0, k == 7,
                        [wkt, ("xT", tb // 4)], [("pb", b)])
            self.CP("act", vS[:, tb, 0:64], self.pb[b][:, 0:64], [("pb", b)], ["vS"])
            self.CP("dve", vW[:, tb, 0:64], self.pb[b][:, 64:128], [("pb", b)], ["vW"])
            self.A(sg[:, tb, :], self.pb[b][:, 128:140], AF.Sigmoid, [("pb", b)], ["sg"])
        for c in range(2):
            ps_ = slice(c * 64, (c + 1) * 64)
            b = self.short()
            for l in range(32):
                self.mm(self.pb[b][0:64, 0:127], w1t[ps_, l, :], KCV[ps_, l:l + 16 * 126 + 1:16], l == 0, l == 31,
                        ["w1t", "KCV"], [("pb", b)])
            b2 = self.short()
            for l in range(32):
                self.mm(self.pb[b2][0:64, 0:1], w1t[ps_, l, :], pet[ps_, l:l + 1], l == 0, l == 31,
                        ["w1t", "pet"], [("pb", b2)])
            self.CP("dve", hb[:, c:c + 1], self.pb[b2][0:64, 0:1], [("pb", b2)], ["hb"])
            self.A(hid[c], self.pb[b][0:64, 0:127], AF.Gelu_apprx_tanh, [("pb", b), "hb"], [("hid", c)],
                   bias=hb[:, c:c + 1])
        b = self.short()
        self.mm(self.pb[b][:, 0:127], w2kt[:, :], hid[0], True, True, ["w2kt", ("hid", 0)], [("pb", b)])
        self.CP("act", kcT, self.pb[b][:, 0:127], [("pb", b)], ["kcT"])
        b = self.short()
        self.mm(self.pb[b][0:127, 0:64], hid[1], w2vt[:, :], True, True, ["w2vt", ("hid", 1)], [("pb", b)])
        self.CP("act", VC[0:127, 0:64], self.pb[b][0:127, 0:64], [("pb", b)], ["VCv"])
        for qb in range(NB):
            qs = slice(qb * 128, (qb + 1) * 128)
            yt = ytile[qb % 2]
            yk = ("yt", qb % 2)
            sm = small[qb % 2]
            smk = ("small", qb % 2)
            im = imp[qb % 2]
            imk = ("imp", qb % 2)
            sbk2 = [self.short(), self.short()]
            for h in range(4):
                hp_ = slice((h % 2) * 64, (h % 2) * 64 + 64)
                bb_ = sbk2[h % 2]
                self.mm(self.pb[bb_][0:127, (h // 2) * 128:(h // 2 + 1) * 128], kcT[hp_, :], QT[hp_, h // 2, qs], True, True,
                        ["kcT", "QT"], [("pb", bb_)])
            ei = self.ei % 3
            self.ei += 1
            E = self.Ebuf[ei]
            ek = ("E", ei)
            for h in range(4):
                bb_ = sbk2[h % 2]
                self.A(E[0:127, h * 128:(h + 1) * 128], self.pb[bb_][0:127, (h // 2) * 128:(h // 2 + 1) * 128], AF.Exp,
                       [("pb", bb_)], [ek], scale=0.125)
            for h in range(4):
                self.TT("pool", E[0:127, h * 128:(h + 1) * 128], E[0:127, h * 128:(h + 1) * 128], cmpvalid[0:127, qs],
                        ALU.mult, [ek, "cmpvalid"], [ek])
            ob = self.accb()
            for h in range(4):
                self.mm(self.pb[ob][:, h * 97:(h + 1) * 97], E[0:127, h * 128:(h + 1) * 128], VC[0:127, :], True, True,
                        [ek, "VCv", "VCc"], [("pb", ob)])
            P = self.pb[ob]
            for h in range(4):
                self.TS("dve", sm[:, h:h + 1], P[:, 97 * h + 96:97 * h + 97], 1e-30, None, ALU.max, None, [("pb", ob)], [smk])
            self.S.op("dve", lambda e, s_=sm: e.reciprocal(out=s_[:, 0:4], in_=s_[:, 0:4]), reads=[smk], writes=[smk])
            for h in range(4):
                if h == 0:
                    self.TS("dve", im, P[:, 64:96], sm[:, 0:1], None, ALU.mult, None, [("pb", ob), smk], [imk])
                else:
                    self.STT("dve", im, P[:, 97 * h + 64:97 * h + 96], sm[:, h:h + 1], im, ALU.mult, ALU.add,
                             [("pb", ob), smk, imk], [imk])
            for h in range(4):
                self.TT("dve", sm[:, 4 + h:5 + h], sm[:, h:h + 1], sg[:, qb, 3 * h:3 * h + 1], ALU.mult, [smk, "sg"], [smk])
                self.A(yt[:, h * 64:(h + 1) * 64], P[:, 97 * h:97 * h + 64], AF.Copy, [("pb", ob), smk], [yk],
                       scale=sm[:, 4 + h:5 + h])
            mk = None
            if qb >= 8:
                sc_ = scr[qb % 2]
                sck = ("scr", qb % 2)
                self.TT("dve", im, im, keepadd[:, 0, qb, :], ALU.mult, [imk, "keepadd"], [imk])
                self.TT("dve", im, im, keepadd[:, 1, qb, :], ALU.add, [imk, "keepadd"], [imk])
                self.S.op("dve", lambda e, s_=sm, i_=im: e.max(out=s_[:, 8:16], in_=i_), reads=[imk, smk], writes=[smk])
                self.S.op("dve", lambda e, s_=sm, i_=im, c_=sc_: e.match_replace(out=c_, in_to_replace=s_[:, 8:16],
                                                                                in_values=i_, imm_value=-1e30),
                          reads=[imk, smk], writes=[sck])
                self.S.op("dve", lambda e, s_=sm, c_=sc_: e.max(out=s_[:, 16:24], in_=c_), reads=[sck, smk], writes=[smk])
                self.TS("dve", sc_, im, sm[:, 23:24], None, ALU.is_ge, None, [imk, smk], [sck])
                b = self.short()
                self.tr(self.pb[b][0:32, 0:128], sc_, [sck], [("pb", b)])
                sT = selT[qb % 2]
                stk = ("selT", qb % 2)
                self.CP("act", sT, self.pb[b][0:32, 0:128], [("pb", b)], [stk])
                smt = selmask[qb % 2]
                mk = ("selmask", qb % 2)
                for g0 in range(0, qb + 1, 4):
                    n = min(4, qb + 1 - g0)
                    b = self.short()
                    for i in range(n):
                        kb = g0 + i
                        self.mm(self.pb[b][:, i * 128:(i + 1) * 128], eexp[:, kb * 128:(kb + 1) * 128], sT, True, True,
                                ["eexp", stk], [("pb", b)])
                    self.CP("act", smt[:, g0 * 128:(g0 + n) * 128], self.pb[b][:, 0:n * 128], [("pb", b)], [mk])
                self.TT("pool", smt[:, qb * 128:(qb + 1) * 128], smt[:, qb * 128:(qb + 1) * 128], self.cmask[:, 0, :],
                        ALU.mult, [mk, "cmask"], [mk])
            for h in range(4):
                hp_ = slice((h % 2) * 64, (h % 2) * 64 + 64)
                qT_ap = (QR[hp_, h // 2, qs], ["QR"])
                for br, KT, kkey, V, vkey, gcol in ((1, KS, "KS", vS, "vS", 3 * h + 1), (2, KW, "KW", vW, "vW", 3 * h + 2)):
                    if br == 1:
                        kbs = [(kb, "le" if kb == qb else None) for kb in range(qb + 1)]
                        em = (selmask[qb % 2], mk) if qb >= 8 else None
                    else:
                        kbs = [(kb, "le" if kb == qb else ("gt" if kb == qb - 4 else None))
                               for kb in range(max(0, qb - 4), qb + 1)]
                        em = None
                    ob = self.softmax_attn_block(
                        qb, kbs, lambda kb, KT=KT, kkey=kkey: (KT[hp_, kb * 128:(kb + 1) * 128], [kkey]), qT_ap,
                        lambda kb, V=V, vkey=vkey: (V[:, kb, :], [vkey]), 0.125, extra_mask=em)
                    fk = ("fac", qb % 2)
                    fac = sm[:, 24 + 2 * h + (br - 1):25 + 2 * h + (br - 1)]
                    self.S.op("dve", lambda e, f_=fac, o_=self.pb[ob][:, 64:65]: e.reciprocal(out=f_, in_=o_),
                              reads=[("pb", ob), smk], writes=[smk])
                    self.TT("dve", fac, fac, sg[:, qb, gcol:gcol + 1], ALU.mult, [smk, "sg"], [smk])
                    self.STT("dve", yt[:, h * 64:(h + 1) * 64], self.pb[ob][:, 0:64], fac, yt[:, h * 64:(h + 1) * 64],
                             ALU.mult, ALU.add, [("pb", ob), smk, yk], [yk])
            self.y_to_yT(yt, yk, 1, qb)

    def mla_branch(self, li):
        d = self.dram
        win = d["win"]
        self.tabM = self.carve([2, T], BF16)
        self.dma(self.tabM.rearrange("p a t -> p (a t)"), d["tabs_d"][1], [], ["tab"])
        qg = self.carve([2, T], BF16)
        kvg = self.carve([T], BF16)
        CS = self.carve([2, T], BF16)
        rkv = self.carve([T], BF16)
        rkt = self.carve([NB], F32)
        Vm = self.carve([NB, 4, 65], BF16)
        QH = [self.carve([T], BF16) for _ in range(2)]
        KH = [self.carve([T], BF16) for _ in range(2)]
        KR = self.carve([T], BF16)
        nrm = self.carve([4], F32)
        sq = [self.carve([512], F32) for _ in range(2)]
        t1 = [self.carve([512], F32) for _ in range(2)]
        t2 = [self.carve([512], F32) for _ in range(2)]
        rs = self.carve([512], F32)
        self.Ebuf = [self.carve([512], BF16) for _ in range(3)]
        self.ei = 0
        self.ymla = self.carve([NB, 256], F32)
        small = [self.carve([8], F32) for _ in range(2)]
        self.dma(nrm[:, 0:2], d["qn"][li], [], ["nrm"])
        self.dma(nrm[:, 2:3], d["kvn"][li], [], ["nrm"])
        self.MS("dve", Vm[:, :, :, 64:65], 1.0, ["Vm"])
        wvm, wkm = self.wload(win[li][:, OFF_MISC + 128:OFF_MISC + 512], 8, 384)
        for tt in range(4):
            sl = slice(tt * 512, (tt + 1) * 512)
            bq = []
            for c in range(2):
                b0 = self.proj_fm(wvm, wkm, c * 128, 128, tt)
                bq.append(b0)
                self.A(qg[:, c, sl], self.pb[b0][:, :], AF.Copy, [("pb", b0), "nrm"], ["qg"], scale=nrm[:, c:c + 1])
                self.A(sq[c], self.pb[b0][:, :], AF.Square, [("pb", b0)], [("sq", c)])
            bs = self.short()
            for c in range(2):
                self.mm(self.pb[bs][:, :], self.onesf[:, :], sq[c], c == 0, c == 1, ["onesf", ("sq", c)], [("pb", bs)])
            self.A(rs, self.pb[bs][:, :], AF.Sqrt, [("pb", bs)], ["rs"], scale=1.0 / 256, bias=RMS_EPS)
            self.S.op("dve", lambda e, r=rs: e.reciprocal(out=r, in_=r), reads=["rs"], writes=["rs"])
            for w in range(2):
                self.TT("dve", CS[:, w, sl], self.tabM[:, w, sl], rs, ALU.mult, ["tab", "rs"], ["CS"])
            b0 = self.proj_fm(wvm, wkm, 256, 128, tt)
            self.A(kvg[:, sl], self.pb[b0][:, :], AF.Copy, [("pb", b0), "nrm"], ["kvg"], scale=nrm[:, 2:3])
            self.A(sq[0], self.pb[b0][:, :], AF.Square, [("pb", b0)], [("sq", 0)])
            bs = self.short()
            self.mm(self.pb[bs][:, :], self.onesf[:, :], sq[0], True, True, ["onesf", ("sq", 0)], [("pb", bs)])
            self.A(rs, self.pb[bs][:, :], AF.Sqrt, [("pb", bs)], ["rs"], scale=1.0 / 128, bias=RMS_EPS)
            self.S.op("dve", lambda e, r=rs, o=rkv[:, sl]: e.reciprocal(out=o, in_=r), reads=["rs"], writes=["rkv"])
            bt = self.short()
            for i in range(4):
                self.mm(self.pb[bt][:, i:i + 1], sq[0][:, i * 128:(i + 1) * 128], self.onesf[:, 0:1], True, True,
                        [("sq", 0), "onesf"], [("pb", bt)])
            self.A(rkt[:, tt * 4:tt * 4 + 4], self.pb[bt][:, 0:4], AF.Sqrt, [("pb", bt)], ["rkt"], scale=1.0 / 128,
                   bias=RMS_EPS)
        self.S.op("dve", lambda e: e.reciprocal(out=rkt, in_=rkt), reads=["rkt"], writes=["rkt"])
        wvr, wkr = self.wload(win[li][:, OFF_KR:OFF_KR + 256], 8, 256)
        r9 = slice(64, 96)
        for tt in range(4):
            sl = slice(tt * 512, (tt + 1) * 512)
            b0 = self.proj_fm(wvr, wkr, 0, 96, tt)
            b1 = self.proj_fm(wvr, wkr, 128, 96, tt)
            self.TT("dve", t1[0][r9, :], self.pb[b0][r9, :], self.tabM[r9, 0, sl], ALU.mult, [("pb", b0), "tab"], [("t1", 0)])
            self.TT("dve", t2[0][r9, :], self.pb[b1][r9, :], self.tabM[r9, 1, sl], ALU.mult, [("pb", b1), "tab"], [("t2", 0)])
            self.TT("pool", KR[r9, sl], t1[0][r9, :], t2[0][r9, :], ALU.add, [("t1", 0), ("t2", 0)], ["KR"])
        wvu, wku = self.wload(d["ukv"][li], 1, 512)
        for tb in range(NB):
            b = self.short()
            self.mm(self.pb[b][:, 0:256], kvg[:, tb * 128:(tb + 1) * 128], wvu[:, 0, 256:512], True, True,
                    ["kvg", wku], [("pb", b)])
            self.A(Vm[:, tb, :, 0:64], self.pb[b][:, 0:256].rearrange("p (h c) -> p h c", h=4), AF.Copy,
                   [("pb", b), "rkt"], ["Vm"], scale=rkt[:, tb:tb + 1])
        wvq, wkq = self.wload(d["uq"][li], 2, 768)
        scale = 96.0 ** -0.5
        for h in range(4):
            Q = QH[h % 2]
            K = KH[h % 2]
            qk = ("QH", h % 2)
            kk = ("KH", h % 2)
            for tt in range(4):
                sl = slice(tt * 512, (tt + 1) * 512)
                ba = self.proj_fm(wvq, wkq, (2 * h) * 96, 96, tt, src=qg, srckey="qg", kc=2)
                bb = self.proj_fm(wvq, wkq, (2 * h + 1) * 96, 96, tt, src=qg, srckey="qg", kc=2)
                c = tt % 2
                self.TT("dve", t1[c][0:96, :], self.pb[ba][0:96, :], CS[0:96, 0, sl], ALU.mult, [("pb", ba), "CS"], [("t1", c)])
                self.TT("dve", t2[c][0:96, :], self.pb[bb][0:96, :], CS[0:96, 1, sl], ALU.mult, [("pb", bb), "CS"], [("t2", c)])
                self.TT("pool", Q[0:96, sl], t1[c][0:96, :], t2[c][0:96, :], ALU.add, [("t1", c), ("t2", c)], [qk])
                bk = self.short()
                self.mm(self.pb[bk][0:64, :], wvu[:, 0, h * 64:(h + 1) * 64], kvg[:, sl], True, True, [wku, "kvg"], [("pb", bk)])
                self.TT("dve", K[0:64, sl], self.pb[bk][0:64, :], rkv[0:64, sl], ALU.mult, [("pb", bk), "rkv"], [kk])
            self.CP("pool", K[r9, :], KR[r9, :], ["KR"], [kk])
            for qb in range(NB):
                qs = slice(qb * 128, (qb + 1) * 128)
                sm = small[qb % 2]
                smk = ("small", qb % 2)
                kbs = [(kb, "le" if kb == qb else None) for kb in range(qb + 1)]
                ob = self.softmax_attn_block(
                    qb, kbs, lambda kb: (K[0:96, kb * 128:(kb + 1) * 128], [kk]), (Q[0:96, qs], [qk]),
                    lambda kb: (Vm[:, kb, h, :], ["Vm"]), scale)
                self.S.op("dve", lambda e, s_=sm, o_=self.pb[ob][:, 64:65]: e.reciprocal(out=s_[:, 0:1], in_=o_),
                          reads=[("pb", ob)], writes=[smk])
                self.A(self.ymla[:, qb, h * 64:(h + 1) * 64], self.pb[ob][:, 0:64], AF.Copy, [("pb", ob), smk],
                       [("ymla", qb)], scale=sm[:, 0:1])
        for qb in range(NB):
            self.y_to_yT(self.ymla[:, qb, :], ("ymla", qb), 2, qb)

    def sb_branch(self, li):
        d = self.dram
        win = d["win"]
        cst = d["cst"]
        QT = self.carve([2, T], BF16)
        KT = self.carve([2, T], BF16)
        V = self.carve([NB, 256], BF16)
        indt = self.carve([16, 16], BF16)
        selgt = self.carve([16, 128], BF16, parts=16)
        sp = [self.carve([NB * 128], F32) for _ in range(2)]
        lk = [self.carve([NB * 128], BF16) for _ in range(2)]
        ex = [self.carve([512], F32) for _ in range(2)]
        ar = [self.carve([512], F32) for _ in range(2)]
        aa = [self.carve([512], BF16) for _ in range(3)]
        ts_ = [self.carve([128], BF16, parts=16) for _ in range(2)]
        ytile = [self.carve([256], F32) for _ in range(2)]
        self.cast_load(indt, cst["indt"], 128, [16, 16], "indt")
        self.cast_load(selgt, cst["selgt"], 16, [16, 128], "selgt")
        wv, wk = self.wload(win[li][:, OFF_SB:OFF_SB + 512], 8, 512)
        for tt in range(4):
            sl = slice(tt * 512, (tt + 1) * 512)
            for c in range(2):
                b0 = self.proj_fm(wv, wk, c * 128, 128, tt)
                self.CP("act", QT[:, c, sl], self.pb[b0][:, :], [("pb", b0)], ["QT"])
                b1 = self.proj_fm(wv, wk, 256 + c * 128, 128, tt)
                self.CP("dve", KT[:, c, sl], self.pb[b1][:, :], [("pb", b1)], ["KT"])
        wvt, wkt = self.wload(win[li][:, OFF_TOK + 256:OFF_TOK + 512], 8, 256)
        for tb in range(NB):
            b = self.short()
            for k in range(8):
                self.mm(self.pb[b][:, 0:256], self.xT[:, k, tb * 128:(tb + 1) * 128], wvt[:, k, :], k == 0, k == 7,
                        [wkt, ("xT", tb // 4)], [("pb", b)])
            self.CP("act", V[:, tb, :], self.pb[b][:, 0:256], [("pb", b)], ["V"])
        it = 0
        ai_ = 0
        for qb in range(NB):
            qs = slice(qb * 128, (qb + 1) * 128)
            yt = ytile[qb % 2]
            yk = ("yt", qb % 2)
            for h in range(4):
                hp_ = slice((h % 2) * 64, (h % 2) * 64 + 64)
                spt, lkt, tst = sp[it % 2], lk[it % 2], ts_[it % 2]
                spk, lkk, tsk = ("sp", it % 2), ("lk", it % 2), ("ts", it % 2)
                it += 1
                nk = qb + 1
                for g0 in range(0, nk, 4):
                    n = min(4, nk - g0)
                    w = n * 128
                    sbk = self.short()
                    for i in range(n):
                        kb = g0 + i
                        self.mm(self.pb[sbk][:, i * 128:(i + 1) * 128], KT[hp_, h // 2, kb * 128:(kb + 1) * 128],
                                QT[hp_, h // 2, qs], True, True, ["KT", "QT"], [("pb", sbk)])
                    e_ = ex[(g0 // 4) % 2]
                    exk = ("ex", (g0 // 4) % 2)
                    self.A(e_[:, 0:w], self.pb[sbk][:, 0:w], AF.Exp, [("pb", sbk)], [exk], scale=-0.125)
                    self.A(spt[:, g0 * 128:g0 * 128 + w], e_[:, 0:w], AF.Ln, [exk], [spk], bias=1.0)
                    self.STT("dve", lkt[:, g0 * 128:g0 * 128 + w], self.pb[sbk][:, 0:w], -0.125, spt[:, g0 * 128:g0 * 128 + w],
                             ALU.mult, ALU.subtract, [("pb", sbk), spk], [lkk])
                self.TT("pool", lkt[:, qb * 128:(qb + 1) * 128], lkt[:, qb * 128:(qb + 1) * 128], self.cmask[:, 1, :],
                        ALU.mult, [lkk, "cmask"], [lkk])
                bts = self.short()
                for kb in range(nk):
                    self.mm(self.pb[bts][0:16, 0:128], indt[:, kb, :], lkt[:, kb * 128:(kb + 1) * 128], kb == 0, kb == nk - 1,
                            ["indt", lkk], [("pb", bts)])
                self.CP("act", tst, self.pb[bts][0:16, 0:128], [("pb", bts)], [tsk])
                ob = self.accb()
                for g0 in range(0, nk, 4):
                    n = min(4, nk - g0)
                    w = n * 128
                    lb = self.short()
                    for i in range(n):
                        kb = g0 + i
                        self.mm(self.pb[lb][:, i * 128:(i + 1) * 128], self.cmask[:, 2, :], lkt[:, kb * 128:(kb + 1) * 128],
                                True, False, ["cmask", lkk], [("pb", lb)])
                        self.mm(self.pb[lb][:, i * 128:(i + 1) * 128], selgt[:, kb, :], tst, False, True,
                                ["selgt", tsk], [("pb", lb)])
                    a_ = ar[(g0 // 4) % 2]
                    ark = ("ar", (g0 // 4) % 2)
                    self.TT("dve", a_[:, 0:w], self.pb[lb][:, 0:w], spt[:, g0 * 128:g0 * 128 + w], ALU.subtract,
                            [("pb", lb), spk], [ark])
                    at = aa[ai_ % 3]
                    ak = ("aa", ai_ % 3)
                    ai_ += 1
                    self.A(at[:, 0:w], a_[:, 0:w], AF.Exp, [ark], [ak])
                    if g0 + n == nk:
                        i = n - 1
                        self.TT("pool", at[:, i * 128:(i + 1) * 128], at[:, i * 128:(i + 1) * 128], self.cmask[:, 1, :],
                                ALU.mult, [ak, "cmask"], [ak])
                    for i in range(n):
                        kb = g0 + i
                        self.mm(self.pb[ob][:, 0:64], at[:, i * 128:(i + 1) * 128], V[:, kb, h * 64:(h + 1) * 64],
                                kb == 0, kb == nk - 1, [ak, "V"], [("pb", ob)])
                self.CP("act", yt[:, h * 64:(h + 1) * 64], self.pb[ob][:, 0:64], [("pb", ob)], [yk])
            self.y_to_yT(yt, yk, 3, qb)

    def layer_norm_block(self, h, hk, gb, tb, res_out, li, route):
        st = self.lnst[tb % 2]
        sk = ("lnst", tb % 2)
        junk = self.lnjunk
        self.A(junk, h, AF.Copy, [hk], ["lnjunk", sk], accum=st[:, 0:1])
        self.A(junk, h, AF.Square, [hk], ["lnjunk", sk], accum=st[:, 1:2])
        self.TS("dve", st[:, 2:3], st[:, 0:1], 1.0 / D, None, ALU.mult, None, [sk], [sk])
        self.TT("dve", st[:, 3:4], st[:, 2:3], st[:, 2:3], ALU.mult, [sk], [sk])
        self.STT("dve", st[:, 4:5], st[:, 1:2], 1.0 / D, st[:, 3:4], ALU.mult, ALU.subtract, [sk], [sk])
        self.A(st[:, 4:5], st[:, 4:5], AF.Sqrt, [sk], [sk], bias=LN_EPS)
        self.S.op("dve", lambda e, s_=st: e.reciprocal(out=s_[:, 5:6], in_=s_[:, 4:5]), reads=[sk], writes=[sk])
        self.STT("dve", st[:, 6:7], st[:, 2:3], -1.0, st[:, 5:6], ALU.mult, ALU.mult, [sk], [sk])
        self.A(h, h, AF.Identity, [hk, sk], [hk], scale=st[:, 5:6], bias=st[:, 6:7])
        self.TT("dve", h, h, gb[:, 0, :], ALU.mult, [hk, "lngb"], [hk])
        self.TT("pool", h, h, gb[:, 1, :], ALU.add, [hk, "lngb"], [hk])
        o = self.dma(res_out[tb * 128:(tb + 1) * 128, :], h, [hk], [("res", id(res_out), tb)])
        self.x_to_xT(h, hk, tb, rt=route)
        return o

    def merge_ln1(self, li, res_in, res_out):
        d = self.dram
        win = d["win"]
        HT = 1024
        mp = self.carve([8, HT], BF16)
        accm = self.carve([8, HT], F32)
        sgt = [self.carve([512], BF16) for _ in range(2)]
        prod = [self.carve([512], F32) for _ in range(2)]
        gb = self.carve([2, D], F32)
        hbuf = [self.carve([D], F32) for _ in range(2)]
        xin = [self.carve([D], F32) for _ in range(2)]
        self.lnst = [self.carve([8], F32) for _ in range(2)]
        self.lnjunk = self.carve([D], BF16)
        self.dma(gb[:, 0, :], d["ln1g"][li:li + 1, :].to_broadcast([128, D]), [], ["lngb"])
        self.dma(gb[:, 1, :], d["ln1b"][li:li + 1, :].to_broadcast([128, D]), [], ["lngb"])
        route = None
        if li == 1:
            route = self.make_router(li)
        for th in range(2):
            for n in range(4):
                wvb, wkb = self.wload(d["wbr"][li, n], 2, D)
                for q4 in range(2):
                    c0 = OFF_GATE + n * D + q4 * 512
                    wvg, wkg = self.wload(win[li][:, c0:c0 + 512], 8, 512)
                    for cc in range(4):
                        dc = q4 * 4 + cc
                        for t2 in range(2):
                            tt = th * 2 + t2
                            sl = slice(t2 * 512, (t2 + 1) * 512)
                            bg = self.proj_fm(wvg, wkg, cc * 128, 128, tt)
                            bp = self.proj_fm(wvb, wkb, dc * 128, 128, tt, src=self.yT[n], srckey="yT%d" % n, kc=2)
                            s_ = sgt[t2]
                            sk = ("sgt", t2)
                            self.A(s_, self.pb[bg][:, :], AF.Sigmoid, [("pb", bg)], [sk])
                            ak = ("accm", dc, t2)
                            if n == 0:
                                self.TT("dve", accm[:, dc, sl], self.pb[bp][:, :], s_, ALU.mult, [("pb", bp), sk], [ak])
                            else:
                                p_ = prod[t2]
                                pk = ("prod", t2)
                                self.TT("dve", p_, self.pb[bp][:, :], s_, ALU.mult, [("pb", bp), sk], [pk])
                                if n < 3:
                                    self.TT("pool", accm[:, dc, sl], accm[:, dc, sl], p_, ALU.add, [ak, pk], [ak])
                                else:
                                    self.TT("pool", mp[:, dc, sl], accm[:, dc, sl], p_, ALU.add, [ak, pk], [("mp", dc, t2)])
            wo = [self.wload(d["wout"][li][:, hh * 512:(hh + 1) * 512], 8, 512) for hh in range(2)]
            for j in range(8):
                tb = th * 8 + j
                xi = xin[tb % 2]
                xk = ("xin", tb % 2)
                self.dma(xi, res_in[tb * 128:(tb + 1) * 128, :], [("res", id(res_in), tb)], [xk])
                h = hbuf[tb % 2]
                hk = ("hbuf", tb % 2)
                for hh in range(2):
                    ob = self.accb()
                    for k in range(8):
                        self.mm(self.pb[ob][:, :], mp[:, k, j * 128:(j + 1) * 128], wo[hh][0][:, k, :], k == 0, k == 7,
                                [("mp", k, j // 4), wo[hh][1]], [("pb", ob)])
                    self.STT("dve", h[:, hh * 512:(hh + 1) * 512], xi[:, hh * 512:(hh + 1) * 512], ALPHA, self.pb[ob][:, :],
                             ALU.mult, ALU.add, [xk, ("pb", ob)], [hk])
                rt = (lambda half, b, tb=tb: route(tb, half, b)) if route else None
                o = self.layer_norm_block(h, hk, gb, tb, res_out, li, rt)
                if self.stop_after == ("mix", li):
                    self.finals.append(o)

    def layer_norm_block(self, h, hk, gb, tb, res_out, li, route):
        st = self.lnst[tb % 2]
        sk = ("lnst", tb % 2)
        junk = self.lnjunk
        self.MS("dve", st[:, 0:2], 0.0, [sk])
        self.A(junk, h, AF.Copy, [hk, sk], ["lnjunk", sk], accum=st[:, 0:1])
        self.A(junk, h, AF.Square, [hk, sk], ["lnjunk", sk], accum=st[:, 1:2])
        self.TS("dve", st[:, 2:3], st[:, 0:1], 1.0 / D, None, ALU.mult, None, [sk], [sk])
        self.TT("dve", st[:, 3:4], st[:, 2:3], st[:, 2:3], ALU.mult, [sk], [sk])
        self.STT("dve", st[:, 4:5], st[:, 1:2], 1.0 / D, st[:, 3:4], ALU.mult, ALU.subtract, [sk], [sk])
        self.A(st[:, 4:5], st[:, 4:5], AF.Sqrt, [sk], [sk], bias=LN_EPS)
        self.S.op("dve", lambda e, s_=st: e.reciprocal(out=s_[:, 5:6], in_=s_[:, 4:5]), reads=[sk], writes=[sk])
        self.STT("dve", st[:, 6:7], st[:, 2:3], -1.0, st[:, 5:6], ALU.mult, ALU.mult, [sk], [sk])
        self.A(h, h, AF.Identity, [hk, sk], [hk], scale=st[:, 5:6], bias=st[:, 6:7])
        self.TT("dve", h, h, gb[:, 0, :], ALU.mult, [hk, "lngb"], [hk])
        self.TT("pool", h, h, gb[:, 1, :], ALU.add, [hk, "lngb"], [hk])
        o = self.dma(res_out[tb * 128:(tb + 1) * 128, :], h, [hk], [("res", id(res_out), tb)])
        self.x_to_xT(h, hk, tb, rt=route)
        return o

    def make_router(self, li):
        d = self.dram
        rw = self.carve([8, NE], F32)
        self.dma(rw, d["router"].rearrange("(c p) e -> p c e", p=128), [], ["rw"])
        xf = [self.carve([512], F32) for _ in range(2)]
        lg = [self.carve([32], F32) for _ in range(2)]
        state = {}

        def route(tb, half, b):
            x_ = xf[half]
            xk = ("xf", half)
            self.CP("dve", x_, self.pb[b][:, :], [("pb", b)], [xk])
            if half == 0:
                state["bank"] = self.accb()
            rb = state["bank"]
            for c in range(4):
                k = half * 4 + c
                self.mm(self.pb[rb][:, 0:NE], x_[:, c * 128:(c + 1) * 128], rw[:, k, :], k == 0, k == 7,
                        [xk, "rw"], [("pb", rb)])
            if half == 1:
                l_ = lg[tb % 2]
                lk = ("lg", tb % 2)
                self.CP("dve", l_[:, 0:8], self.pb[rb][:, 0:NE], [("pb", rb)], [lk])
                self.S.op("dve", lambda e, l_=l_: e.max(out=l_[:, 8:16], in_=l_[:, 0:8]), reads=[lk], writes=[lk])
                self.TT("dve", l_[:, 16:17], l_[:, 9:10], l_[:, 8:9], ALU.subtract, [lk], [lk])
                self.A(l_[:, 16:17], l_[:, 16:17], AF.Exp, [lk], [lk])
                self.TS("dve", l_[:, 16:17], l_[:, 16:17], 1.0, None, ALU.add, None, [lk], [lk])
                self.S.op("dve", lambda e, l_=l_: e.reciprocal(out=l_[:, 17:18], in_=l_[:, 16:17]), reads=[lk], writes=[lk])
                self.TS("dve", l_[:, 18:19], l_[:, 8:9], -1.0, None, ALU.mult, None, [lk], [lk])
                self.A(l_[:, 24:32], l_[:, 0:8], AF.Exp, [lk], [lk], bias=l_[:, 18:19])
                self.TS("dve", l_[:, 0:8], l_[:, 0:8], l_[:, 9:10], l_[:, 17:18], ALU.is_ge, ALU.mult, [lk], [lk])
                self.TT("dve", self.gates[:, tb, :], l_[:, 0:8], l_[:, 24:32], ALU.mult, [lk], ["gates"])
        return route

    def ffn_phase(self, li, res_in, res_out):
        d = self.dram
        self.arena_reset()
        G = 1024
        moe = (li == 1)
        dff = D_FFE if moe else D_FF
        nfc = dff // 128
        hT = self.carve([nfc, G], BF16)
        facc = self.carve([8, D], F32)
        gb = self.carve([2, D], F32)
        sa = [self.carve([512], BF16) for _ in range(2)]
        pblk = [self.carve([256], F32) for _ in range(2)]
        pT = [self.carve([2, 128], BF16) for _ in range(2)]
        ple = self.carve([D], F32)
        hbuf = [self.carve([D], F32) for _ in range(2)]
        xin = self.carve([D], F32)
        self.lnst = [self.carve([8], F32) for _ in range(2)]
        self.lnjunk = self.carve([D], BF16)
        self.dma(gb[:, 0, :], d["ln2g"][li:li + 1, :].to_broadcast([128, D]), [], ["lngb"])
        self.dma(gb[:, 1, :], d["ln2b"][li:li + 1, :].to_broadcast([128, D]), [], ["lngb"])
        for g in range(T // G):
            experts = list(range(NE)) if moe else [None]
            for e in experts:
                w_in = d["moe_in"][e] if moe else d["ffn_in"]
                w_out = d["moe_out"][e] if moe else d["ffn_out"]
                for f0 in range(0, nfc, 4):
                    nf = min(4, nfc - f0)
                    wa, wak = self.wload(w_in[:, f0 * 128:(f0 + nf) * 128], 8, nf * 128)
                    wu, wuk = self.wload(w_in[:, dff + f0 * 128:dff + (f0 + nf) * 128], 8, nf * 128)
                    for fi in range(nf):
                        fc = f0 + fi
                        for t2 in range(G // 512):
                            tt = g * (G // 512) + t2
                            ba = self.proj_fm(wa, wak, fi * 128, 128, tt)
                            bu = self.proj_fm(wu, wuk, fi * 128, 128, tt)
                            s_ = sa[t2 % 2]
                            sk = ("sa", t2 % 2)
                            self.A(s_, self.pb[ba][:, :], AF.Silu, [("pb", ba)], [sk])
                            self.TT("dve", hT[:, fc, t2 * 512:(t2 + 1) * 512], self.pb[bu][:, :], s_, ALU.mult,
                                    [("pb", bu), sk], [("hT", fc)])
                for ps_ in range(2):
                    for f0 in range(0, nfc, 4):
                        nf = min(4, nfc - f0)
                        wo, wok = self.wload(w_out[f0 * 128:(f0 + nf) * 128, :], nf, D)
                        for fi in range(nf):
                            fc = f0 + fi
                            for j4 in range(4):
                                j = ps_ * 4 + j4
                                for hh in range(2):
                                    b = j4 * 2 + hh
                                    self.mm(self.pb[b][:, :], hT[:, fc, j * 128:(j + 1) * 128],
                                            wo[:, fi, hh * 512:(hh + 1) * 512], fc == 0, fc == nfc - 1,
                                            [("hT", fc), wok], [("pb", b)])
                    for j4 in range(4):
                        j = ps_ * 4 + j4
                        tb = g * 8 + j
                        for hh in range(2):
                            b = j4 * 2 + hh
                            dst = facc[:, j, hh * 512:(hh + 1) * 512]
                            fk = ("facc", j, hh)
                            if not moe:
                                self.CP("act" if hh == 0 else "dve", dst, self.pb[b][:, :], [("pb", b)], [fk])
                            elif e == 0:
                                self.TS("dve", dst, self.pb[b][:, :], self.gates[:, tb, e:e + 1], None, ALU.mult, None,
                                        [("pb", b), "gates"], [fk])
                            else:
                                self.STT("dve", dst, self.pb[b][:, :], self.gates[:, tb, e:e + 1], dst, ALU.mult, ALU.add,
                                         [("pb", b), "gates", fk], [fk])
            wg = [self.wload(d["pleg"][li][:, hh * 512:(hh + 1) * 512], 8, 512) for hh in range(2)]
            wp, wpk = self.wload(d["plep"][li], 2, D)
            for j in range(8):
                tb = g * 8 + j
                pb_ = pblk[j % 2]
                pk = ("pblk", j % 2)
                self.dma(pb_, d["p_in"][li, tb * 128:(tb + 1) * 128, :], [], [pk])
                b = self.short()
                for c in range(2):
                    self.tr(self.pb[b][:, c * 128:(c + 1) * 128], pb_[:, c * 128:(c + 1) * 128], [pk], [("pb", b)])
                pt = pT[j % 2]
                ptk = ("pT", j % 2)
                self.CP("act", pt, self.pb[b][:, 0:256].rearrange("p (c t) -> p c t", c=2), [("pb", b)], [ptk])
                for hh in range(2):
                    bg_ = self.short()
                    for k in range(8):
                        self.mm(self.pb[bg_][:, :], self.xT[:, k, tb * 128:(tb + 1) * 128], wg[hh][0][:, k, :], k == 0, k == 7,
                                [("xT", tb // 4), wg[hh][1]], [("pb", bg_)])
                    bp_ = self.short()
                    for c in range(2):
                        self.mm(self.pb[bp_][:, :], pt[:, c, :], wp[:, c, hh * 512:(hh + 1) * 512],
                                c == 0, c == 1, [ptk, wpk], [("pb", bp_)])
                    self.A(ple[:, hh * 512:(hh + 1) * 512], self.pb[bg_][:, :], AF.Sigmoid, [("pb", bg_)], ["ple"])
                    self.TT("dve", ple[:, hh * 512:(hh + 1) * 512], ple[:, hh * 512:(hh + 1) * 512], self.pb[bp_][:, :],
                            ALU.mult, ["ple", ("pb", bp_)], ["ple"])
                self.dma(xin, res_in[tb * 128:(tb + 1) * 128, :], [("res", id(res_in), tb)], ["xin"])
                h = hbuf[j % 2]
                hk = ("hbuf", j % 2)
                self.STT("dve", h, xin, ALPHA, ple, ALU.mult, ALU.add, ["xin", "ple"], [hk])
                self.TT("pool", h, h, facc[:, j, :], ALU.add, [hk, ("facc", j, 0), ("facc", j, 1)], [hk])
                o = self.layer_norm_block(h, hk, gb, tb, res_out, li, None)
                if li == self.layers[-1] or self.stop_after == ("ffn", li):
                    self.finals.append(o)


_IDX = _win_index()


def prepare_inputs(inputs):
    f = lambda a: np.ascontiguousarray(np.asarray(a))
    w_in = f(inputs["w_in"])
    win = np.zeros((2, D, NCOLS_R), np.float32)
    valid = _IDX >= 0
    win[:, :, valid] = w_in[:, :, _IDX[valid]]
    conv_w = f(inputs["conv_w"])
    convp = np.zeros((2, 128, 2, 34), np.float32)
    for c in range(2):
        convp[:, :, c, 0:31] = conv_w[:, :, c * 128:(c + 1) * 128].transpose(0, 2, 1)
        convp[:, :, c, 31] = f(inputs["conv_b"])[:, c * 128:(c + 1) * 128]
        convp[:, :, c, 32] = f(inputs["conv_ln_g"])[:, c * 128:(c + 1) * 128]
        convp[:, :, c, 33] = f(inputs["conv_ln_b"])[:, c * 128:(c + 1) * 128]
    pe = f(inputs["nsa_cmp_pe"])
    pe_r = np.ascontiguousarray(pe.transpose(0, 2, 3, 1).reshape(2, 128, 32))
    w2 = f(inputs["nsa_cmp_w2"])
    w2k = np.ascontiguousarray(np.concatenate([w2[:, 0], w2[:, 0]], axis=2))
    w2v = np.ascontiguousarray(w2[:, 1])
    qn = np.ascontiguousarray(f(inputs["mla_q_norm"]).reshape(2, 2, 128).transpose(0, 2, 1))
    kvn = np.ascontiguousarray(f(inputs["mla_kv_norm"]).reshape(2, 128, 1))
    wuq = f(inputs["mla_w_uq"])
    cols = []
    for h in range(4):
        b = h * 96
        cols += list(range(b, b + 96))
        cols += list(range(b, b + 64)) + list(range(b + 80, b + 96)) + list(range(b + 64, b + 80))
    uq = np.ascontiguousarray(wuq[:, :, cols])
    wukv = f(inputs["mla_w_ukv"])
    cols = []
    for h in range(4):
        cols += list(range(h * 128, h * 128 + 64))
    for h in range(4):
        cols += list(range(h * 128 + 64, h * 128 + 128))
    ukv = np.ascontiguousarray(wukv[:, :, cols])
    shared = {
        "win": win, "convp": convp, "pe_r": pe_r, "w1": f(inputs["nsa_cmp_w1"]), "w2k": w2k, "w2v": w2v,
        "qn": qn, "kvn": kvn, "uq": uq, "ukv": ukv, "wbr": f(inputs["w_branch"]), "wout": f(inputs["w_out"]),
        "ln1g": f(inputs["ln1_g"]), "ln1b": f(inputs["ln1_b"]), "ln2g": f(inputs["ln2_g"]), "ln2b": f(inputs["ln2_b"]),
        "ffn_in": f(inputs["ffn_w_in"])[0], "ffn_out": f(inputs["ffn_w_out"])[0], "router": f(inputs["moe_router"])[0],
        "moe_in": f(inputs["moe_w_in"])[0], "moe_out": f(inputs["moe_w_out"])[0],
        "pleg": f(inputs["ple_w_gate"]), "plep": f(inputs["ple_w_proj"]),
    }
    for k, v in _host_consts().items():
        shared["c_" + k] = v
    x = f(inputs["x"])
    p = f(inputs["p"])
    pos = f(inputs["positions"]).astype(np.int32)
    in_maps = []
    for b in range(8):
        m = dict(shared)
        m["x"] = x[b]
        m["p"] = np.ascontiguousarray(p[:, b])
        m["pos"] = pos[b:b + 1]
        in_maps.append(m)
    return in_maps


def kernel(**inputs):
    in_maps = prepare_inputs(inputs)
    nc = MK().build()
    res = run_bass_kernel_spmd(nc, in_maps, core_ids=list(range(8)))
    return np.stack([np.asarray(r["y"], dtype=np.float32) for r in res.results], axis=0)
```

```python
import math
import contextlib
import numpy as np
import concourse.bass as bass
import concourse.mybir as mybir
from concourse.bass_utils import run_bass_kernel_spmd

F32 = mybir.dt.float32
BF16 = mybir.dt.bfloat16
I32 = mybir.dt.int32
AF = mybir.ActivationFunctionType
ALU = mybir.AluOpType
AX = mybir.AxisListType

T = 2048
D = 1024
NB = 16
ALPHA = 4.0 ** 0.25
LN_EPS = 1e-5
RMS_EPS = 1e-6
THETA = 10000.0
D_FF = 2816
D_FFE = 3584
NE = 8

ENGS = ("pe", "act", "dve", "pool", "sp")
N_DMA_SEMS = 6


class Op:
    __slots__ = ("eng", "fn", "deps", "is_dma", "signal", "sig_val", "dma_sem", "dma_val",
                 "dma_prev", "idx")

    def __init__(self, eng, fn, is_dma):
        self.eng = eng
        self.fn = fn
        self.deps = []
        self.is_dma = is_dma
        self.signal = False
        self.sig_val = 0
        self.dma_sem = None
        self.dma_val = 0
        self.dma_prev = 0
        self.idx = 0


class Sched:
    def __init__(self):
        self.ops = {e: [] for e in ENGS}
        self.last_w = {}
        self.readers = {}
        self.all_ops = []

    def op(self, eng, fn, reads=(), writes=(), dma=False, acc=False):
        o = Op(eng, fn, dma)
        deps = []
        for k in reads:
            w = self.last_w.get(k)
            if w is not None:
                deps.append(w)
            if isinstance(k, tuple) and k[0] == "pb":
                for r in self.readers.get(k, ()):
                    if r.eng != eng:
                        deps.append(r)
        for k in writes:
            w = self.last_w.get(k)
            if w is not None and not (acc and w.eng == eng and not w.is_dma):
                deps.append(w)
            for r in self.readers.get(k, ()):
                deps.append(r)
        seen = set()
        for d in deps:
            if id(d) not in seen and d is not o:
                seen.add(id(d))
                o.deps.append(d)
        for k in reads:
            lst = self.readers.setdefault(k, [])
            if not dma:
                for i, r in enumerate(lst):
                    if r.eng == eng and not r.is_dma:
                        lst[i] = o
                        break
                else:
                    lst.append(o)
            else:
                lst.append(o)
        for k in writes:
            self.last_w[k] = o
            self.readers[k] = []
        o.idx = len(self.ops[eng])
        self.ops[eng].append(o)
        self.all_ops.append(o)
        return o

    def barrier(self):
        lasts = []
        for e in ENGS:
            ops = self.ops[e]
            nd = 0
            got_real = False
            for o in reversed(ops):
                if o.fn is None:
                    continue
                if o.is_dma:
                    if nd < N_DMA_SEMS:
                        lasts.append(o)
                        nd += 1
                elif not got_real:
                    lasts.append(o)
                    got_real = True
                if got_real and nd >= N_DMA_SEMS:
                    break
        for e in ENGS:
            o = Op(e, None, False)
            o.deps = [l for l in lasts]
            o.idx = len(self.ops[e])
            self.ops[e].append(o)
            self.all_ops.append(o)
        self.last_w = {}
        self.readers = {}

    def emit(self, nc, final_wait_ops=()):
        for fo in final_wait_ops:
            if not fo.is_dma:
                fo.signal = True
        for o in self.all_ops:
            for d in o.deps:
                if not d.is_dma:
                    d.signal = True
        cnt = {e: 0 for e in ENGS}
        for e in ENGS:
            for o in self.ops[e]:
                if o.signal and not o.is_dma:
                    cnt[e] += 1
                    o.sig_val = cnt[e]
        dma_count = {}
        for e in ENGS:
            k = 0
            for o in self.ops[e]:
                if o.is_dma:
                    j = k % N_DMA_SEMS
                    k += 1
                    key = (e, j)
                    prev = dma_count.get(key, 0)
                    o.dma_sem = key
                    o.dma_prev = prev
                    o.dma_val = prev + 16
                    dma_count[key] = prev + 16
        with contextlib.ExitStack() as st:
            sems = {e: st.enter_context(nc.semaphore("s_" + e)) for e in ENGS if cnt[e] > 0}
            dsems = {key: st.enter_context(nc.semaphore("d_%s%d" % key)) for key in dma_count}
            block = st.enter_context(nc.Block())
            regs = {"pe": block.tensor, "act": block.scalar, "dve": block.vector,
                    "pool": block.gpsimd, "sp": block.sync}

            def make(e):
                def body(eng):
                    known = {}
                    for o in self.ops[e]:
                        waits = {}
                        for d in o.deps:
                            if d.is_dma:
                                s, v = dsems[d.dma_sem], d.dma_val
                            else:
                                s, v = sems[d.eng], d.sig_val
                            kk = id(s)
                            if known.get(kk, 0) >= v:
                                continue
                            if kk not in waits or waits[kk][1] < v:
                                waits[kk] = (s, v)
                        if o.is_dma and o.dma_prev > 0:
                            s = dsems[o.dma_sem]
                            kk = id(s)
                            if known.get(kk, 0) < o.dma_prev:
                                if kk not in waits or waits[kk][1] < o.dma_prev:
                                    waits[kk] = (s, o.dma_prev)
                        for kk, (s, v) in waits.items():
                            eng.wait_ge(s, v)
                            known[kk] = v
                        if o.fn is None:
                            continue
                        ins = o.fn(eng)
                        if o.is_dma:
                            ins.then_inc(dsems[o.dma_sem], 16)
                        elif o.signal:
                            ins.then_inc(sems[e], 1)
                    if e == "sp":
                        for fo in final_wait_ops:
                            if fo.is_dma:
                                eng.wait_ge(dsems[fo.dma_sem], fo.dma_val)
                            else:
                                eng.wait_ge(sems[fo.eng], fo.sig_val)
                return body

            for e in ENGS:
                if self.ops[e] or e == "sp":
                    regs[e](make(e))


OFF_CONV, OFF_NQ, OFF_NK, OFF_MISC, OFF_KR, OFF_SB, OFF_TOK, OFF_GATE = 0, 512, 1024, 1536, 2048, 2304, 2816, 3328
NCOLS_R = 7424


def _win_index():
    sw64 = lambda b: list(range(b + 32, b + 64)) + list(range(b, b + 32))
    sw32 = lambda b: list(range(b + 16, b + 32)) + list(range(b, b + 16))
    idx = []
    idx += list(range(0, 512))
    idx += list(range(512, 768))
    for h in range(4):
        idx += sw64(512 + 64 * h)
    ks, kw = 896, 1024
    idx += list(range(ks, ks + 64)) * 2 + sw64(ks) * 2 + list(range(kw, kw + 64)) * 2 + sw64(kw) * 2
    idx += list(range(768, 896)) + list(range(1164, 1420)) + list(range(1420, 1548))
    idx += [-1] * 64 + list(range(1548, 1580)) + [-1] * 32
    idx += [-1] * 64 + sw32(1548) + [-1] * 32
    idx += list(range(1580, 1580 + 512))
    idx += list(range(960, 1024)) + list(range(1088, 1152)) + list(range(1152, 1164)) + [-1] * 116
    idx += list(range(1580 + 512, 1580 + 768))
    idx += list(range(2348, 6444))
    assert len(idx) == NCOLS_R
    return np.array(idx)


def _host_consts():
    c = {}
    c["ident"] = np.eye(128, dtype=np.float32)
    p = np.arange(128)[:, None]
    f = np.arange(128)[None, :]
    cm = np.zeros((128, 4, 128), np.float32)
    cm[:, 0] = (p <= f)
    cm[:, 1] = (p < f)
    cm[:, 2] = (p > f)
    cm[:, 3] = 1.0
    c["cmask"] = cm
    j = np.arange(128)[:, None]
    t = np.arange(T)[None, :]
    c["cmpvalid"] = ((16 * j + 31 <= t) & (j < 127)).astype(np.float32)
    n = np.arange(32)[:, None]
    c["eexp"] = ((t // 64) == n).astype(np.float32)
    jj = np.arange(127)
    nn = np.arange(32)
    ov = ((jj[:, None] * 16 < nn[None, :] * 64 + 64) & (jj[:, None] * 16 + 32 > nn[None, :] * 64)).astype(np.float32)
    ovl = np.zeros((128, 33), np.float32)
    ovl[:127, :32] = ov
    ovl[:127, 32] = 1.0
    c["ovl"] = ovl
    cur = (np.arange(T) // 64)[:, None]
    nid = np.arange(32)[None, :]
    forced = (nid == 0) | (nid == cur) | (nid == cur - 1)
    future = nid > cur
    keep = (~forced & ~future).astype(np.float32)
    add = np.where(future, -1e30, np.where(forced, 100.0, 0.0)).astype(np.float32)
    ka = np.zeros((128, 2, 16, 32), np.float32)
    ka[:, 0] = keep.reshape(16, 128, 32).transpose(1, 0, 2)
    ka[:, 1] = add.reshape(16, 128, 32).transpose(1, 0, 2)
    c["keepadd"] = ka
    rc = np.zeros((128, 4), np.float32)
    pp = np.arange(128)
    rc[:, 0] = THETA ** (-(pp % 32).astype(np.float64) / 32.0)
    rc[:, 1] = np.where((pp % 64) < 32, -1.0, 1.0)
    m = (pp >= 64) & (pp < 96)
    rc[m, 2] = THETA ** (-((pp[m] - 64) % 16).astype(np.float64) / 16.0)
    rc[m, 3] = np.where((pp[m] - 64) < 16, -1.0, 1.0)
    c["ropec"] = rc
    ind = np.zeros((128, 16, 16), np.float32)
    for kb in range(16):
        ind[:, kb, kb] = 1.0
    c["indt"] = ind
    sg = np.zeros((16, 16, 128), np.float32)
    for kb in range(16):
        sg[kb + 1:, kb, :] = 1.0
    c["selgt"] = sg
    return c


CONST_SHAPES = {"ident": [128, 128], "cmask": [128, 4, 128], "cmpvalid": [128, T], "eexp": [32, T],
                "ovl": [128, 33], "keepadd": [128, 2, 16, 32], "ropec": [128, 4], "indt": [128, 16, 16],
                "selgt": [16, 16, 128]}


class MK:
    NW = 3

    def __init__(self, layers=(0, 1), debug=False, stop_after=None):
        self.layers = layers
        self.debug = debug
        self.stop_after = stop_after
        self.nc = bass.Bass("TRN2", target_bir_lowering=False)
        self.S = Sched()
        self.st = contextlib.ExitStack()
        self.st.enter_context(self.nc.allow_low_precision(reason="bf16 matmul operands / fp32 accumulation by design"))
        self.wi = 0
        self.stg_i = 0
        self.si = 0
        self.ai = 0
        self.finals = []
        self.dbg_outs = {}

    def din(self, name, shape, dt=F32):
        return self.nc.dram_tensor(name, list(shape), dt, kind="ExternalInput").ap()

    def dout(self, name, shape, dt=F32):
        return self.nc.dram_tensor(name, list(shape), dt, kind="ExternalOutput").ap()

    def sb(self, name, shape, dt):
        return self.st.enter_context(self.nc.sbuf_tensor(name, list(shape), dt))

    def arena_reset(self):
        self.S.barrier()
        self.aoff = 0

    def carve(self, shape, dt, parts=128):
        n = 1
        for s in shape:
            n *= s
        nbytes = n * (4 if dt in (F32, I32) else 2)
        nbytes = (nbytes + 63) // 64 * 64
        off = self.aoff
        self.aoff += nbytes
        assert self.aoff <= self.ARENA_BYTES, (self.aoff, self.ARENA_BYTES)
        v = self.arena[0:parts, off // 2:(off + n * (4 if dt in (F32, I32) else 2)) // 2]
        if dt != BF16:
            v = v.bitcast(dt)
        if len(shape) == 2:
            v = v.rearrange("p (a b) -> p a b", a=shape[0])
        elif len(shape) == 3:
            v = v.rearrange("p (a b c) -> p a b c", a=shape[0], b=shape[1])
        return v

    def short(self):
        b = self.si % 4
        self.si += 1
        return b

    def accb(self):
        b = 4 + self.ai % 4
        self.ai += 1
        return b

    def mm(self, out, lhsT, rhs, start, stop, r, w):
        self.S.op("pe", lambda e: e.matmul(out, lhsT=lhsT, rhs=rhs, start=start, stop=stop),
                  reads=r, writes=w, acc=True)

    def tr(self, out, in_, r, w, parts=128):
        idt = self.ident[0:parts, 0:parts]
        self.S.op("pe", lambda e: e.transpose(out, in_, idt), reads=list(r) + ["ident"], writes=w, acc=True)

    def A(self, out, in_, func, r, w, bias=None, scale=None, accum=None):
        kw = {}
        if bias is not None:
            kw["bias"] = bias
        if scale is not None:
            kw["scale"] = scale
        if accum is not None:
            kw["accum_out"] = accum
        self.S.op("act", lambda e: e.activation(out=out, in_=in_, func=func, **kw), reads=r, writes=w)

    def TT(self, eng, out, in0, in1, op, r, w):
        self.S.op(eng, lambda e: e.tensor_tensor(out=out, in0=in0, in1=in1, op=op), reads=r, writes=w)

    def TS(self, eng, out, in0, s1, s2, op0, op1, r, w):
        if op1 is None:
            self.S.op(eng, lambda e: e.tensor_scalar(out=out, in0=in0, scalar1=s1, scalar2=None, op0=op0),
                      reads=r, writes=w)
        else:
            self.S.op(eng, lambda e: e.tensor_scalar(out=out, in0=in0, scalar1=s1, scalar2=s2, op0=op0, op1=op1),
                      reads=r, writes=w)

    def STT(self, eng, out, in0, scalar, in1, op0, op1, r, w):
        self.S.op(eng, lambda e: e.scalar_tensor_tensor(out=out, in0=in0, scalar=scalar, in1=in1, op0=op0, op1=op1),
                  reads=r, writes=w)

    def CP(self, eng, out, in_, r, w):
        if eng == "act":
            self.S.op("act", lambda e: e.activation(out=out, in_=in_, func=AF.Copy), reads=r, writes=w)
        else:
            self.S.op(eng, lambda e: e.tensor_copy(out=out, in_=in_), reads=r, writes=w)

    def MS(self, eng, out, val, w):
        self.S.op(eng, lambda e: e.memset(out, val), writes=w)

    def dma(self, out, in_, r, w, eng="sp"):
        return self.S.op(eng, lambda e: e.dma_start(out=out, in_=in_), reads=r, writes=w, dma=True)

    CAST_ENGS = ("dve", "act", "dve", "act")

    def cast_load(self, dst, src, parts, free_shape, dkey):
        n = 1
        for x_ in free_shape:
            n *= x_
        assert n <= 2048, n
        si_ = self.stg_i % len(self.stage)
        ce = self.CAST_ENGS[self.stg_i % 4]
        self.stg_i += 1
        stg = self.stage[si_][0:parts, 0:n]
        if len(free_shape) == 2:
            stg = stg.rearrange("p (a b) -> p a b", a=free_shape[0])
        elif len(free_shape) == 3:
            stg = stg.rearrange("p (a b c) -> p a b c", a=free_shape[0], b=free_shape[1])
        sk = ("stg", si_)
        self.dma(stg, src, [], [sk])
        self.CP(ce, dst, stg, [sk], [dkey])

    def dma_w1(self, w1t, src, c):
        si_ = self.stg_i % len(self.stage)
        ce = self.CAST_ENGS[self.stg_i % 4]
        self.stg_i += 1
        ps_ = slice(c * 64, (c + 1) * 64)
        stg = self.stage[si_][ps_, 0:2048].rearrange("p (l e) -> p l e", l=32)
        sk = ("stg", si_)
        self.dma(stg, src.rearrange("(l d) e -> d l e", d=64), [], [sk])
        self.CP(ce, w1t[ps_, :, :], stg, [sk], ["w1t"])

    def wload(self, src, kc, n, rows=128):
        slot = self.wi % self.NW
        self.wi += 1
        assert kc * n <= 4096
        v = self.wring[slot][0:rows, 0:kc * n].rearrange("p (c n) -> p c n", c=kc)
        srcv = src.rearrange("(c p) n -> p c n", p=rows)
        key = ("w", slot)
        step = max(1, 2048 // n)
        for k0 in range(0, kc, step):
            k1 = min(kc, k0 + step)
            self.cast_load(v[:, k0:k1, :], srcv[:, k0:k1, :], rows, [k1 - k0, n], key)
        return v, key

    def proj_fm(self, wv, wkey, c0, M, tt, src=None, srckey=None, kc=8):
        b = self.short()
        src = self.xT if src is None else src
        srckey = ("xT", tt) if srckey is None else srckey
        for k in range(kc):
            self.mm(self.pb[b][0:M, :], wv[:, k, c0:c0 + M], src[:, k, tt * 512:(tt + 1) * 512],
                    k == 0, k == kc - 1, [wkey, srckey], [("pb", b)])
        return b

    def build(self):
        nc, S = self.nc, self.S
        L = self.layers
        x_in = self.din("x", [T, D])
        p_in = self.din("p", [2, T, 256])
        pos_in = self.din("pos", [1, T], I32)
        win = self.din("win", [2, D, NCOLS_R])
        convp = self.din("convp", [2, 128, 2, 34])
        pe_r = self.din("pe_r", [2, 128, 32])
        w1 = self.din("w1", [2, 2, 2048, 64])
        w2k = self.din("w2k", [2, 64, 128])
        w2v = self.din("w2v", [2, 64, 64])
        qn = self.din("qn", [2, 128, 2])
        kvn = self.din("kvn", [2, 128, 1])
        uq = self.din("uq", [2, 256, 768])
        ukv = self.din("ukv", [2, 128, 512])
        wbr = self.din("wbr", [2, 4, 256, D])
        wout = self.din("wout", [2, D, D])
        ln1g = self.din("ln1g", [2, D])
        ln1b = self.din("ln1b", [2, D])
        ln2g = self.din("ln2g", [2, D])
        ln2b = self.din("ln2b", [2, D])
        lite = self.stop_after is not None and self.stop_after[0] in ("conv", "nsa", "mla", "sb", "mix")
        need_ffn = (0 in L) and not lite
        need_moe = (1 in L) and not (lite and self.stop_after[1] == 0) and self.stop_after != ("ffn", 0)
        ffn_in = self.din("ffn_in", [D, 2 * D_FF]) if need_ffn else None
        ffn_out = self.din("ffn_out", [D_FF, D]) if need_ffn else None
        router = self.din("router", [D, NE])
        moe_in = self.din("moe_in", [NE, D, 2 * D_FFE]) if need_moe else None
        moe_out = self.din("moe_out", [NE, D_FFE, D]) if need_moe else None
        pleg = self.din("pleg", [2, D, D])
        plep = self.din("plep", [2, 256, D])
        cst = {k: self.din("c_" + k, v) for k, v in CONST_SHAPES.items()}
        y_out = self.dout("y", [T, D])
        xa = self.dout("xa", [T, D])
        xb = self.dout("xb", [T, D])
        tabs_d = self.dout("tabs_d", [2, 128, 2 * T], BF16)
        self.dram = dict(locals())

        self.xT = self.sb("xT", [128, 8, T], BF16)
        self.wring = [self.sb("wr%d" % i, [128, 4096], BF16) for i in range(self.NW)]
        self.stage = [self.sb("stg%d" % i, [128, 2048], F32) for i in range(2)]
        self.ident = self.sb("ident", [128, 128], F32)
        self.cmask = self.sb("cmask", [128, 4, 128], BF16)
        self.onesf = self.sb("onesf", [128, 128], F32)
        self.gates = self.sb("gates", [128, NB, NE], F32)
        self.pb = [self.st.enter_context(nc.psum_tensor("pb%d" % i, [128, 512], F32)) for i in range(8)]
        self.ARENA_BYTES = 133 * 1024
        self.arena = self.sb("arena", [128, self.ARENA_BYTES // 2], BF16)
        self.aoff = 0

        self.dma(self.ident[:], cst["ident"], [], ["ident"])
        self.cast_load(self.cmask[:], cst["cmask"], 128, [4, 128], "cmask")
        self.MS("dve", self.onesf[:], 1.0, ["onesf"])

        self.setup_rope(pos_in, cst)
        self.load_xT(x_in)

        res_in = x_in
        outs = [(xa, xb), (xa, y_out)]
        for li in L:
            mid, fin = outs[li]
            if li == 1:
                res_in = xb
            if self.mixer_phase(li, res_in, mid):
                break
            if self.stop_after == ("mix", li):
                break
            self.ffn_phase(li, mid, fin)
            if self.stop_after == ("ffn", li):
                break
        S.emit(nc, final_wait_ops=self.finals)
        self.st.close()
        return nc

    def setup_rope(self, pos_in, cst):
        self.arena_reset()
        self.tabN = self.carve([2, T], BF16)
        self.tabM = self.carve([2, T], BF16)
        pi = self.carve([T], I32)
        pf = self.carve([T], F32)
        ang = self.carve([T], F32)
        kf = self.carve([T], F32)
        ki = self.carve([T], I32)
        rc = self.carve([4], F32)
        self.dma(pi, pos_in[0:1, :].to_broadcast([128, T]), [], ["pi"])
        self.dma(rc, cst["ropec"], [], ["rc"])
        self.CP("dve", pf, pi, ["pi"], ["pf"])
        for tab, ic, sc in ((self.tabN, 0, 1), (self.tabM, 2, 3)):
            for which in range(2):
                shift = math.pi / 2 if which == 0 else 0.0
                self.TS("dve", ang, pf, rc[:, ic:ic + 1], shift, ALU.mult, ALU.add, ["pf", "rc"], ["ang"])
                self.TS("dve", kf, ang, 1.0 / (2 * math.pi), None, ALU.mult, None, ["ang"], ["kf"])
                self.CP("dve", ki, kf, ["kf"], ["ki"])
                self.CP("dve", kf, ki, ["ki"], ["kf"])
                self.STT("dve", ang, kf, -2 * math.pi, ang, ALU.mult, ALU.add, ["kf", "ang"], ["ang"])
                self.TS("dve", kf, ang, math.pi, -2 * math.pi, ALU.is_gt, ALU.mult, ["ang"], ["kf"])
                self.TT("dve", ang, ang, kf, ALU.add, ["ang", "kf"], ["ang"])
                self.TS("dve", kf, ang, -math.pi, 2 * math.pi, ALU.is_lt, ALU.mult, ["ang"], ["kf"])
                self.TT("dve", ang, ang, kf, ALU.add, ["ang", "kf"], ["ang"])
                self.A(ang, ang, AF.Sin, ["ang"], ["ang"])
                if which == 0:
                    self.CP("dve", tab[:, 0, :], ang, ["ang"], ["tab"])
                else:
                    self.TS("dve", tab[:, 1, :], ang, rc[:, sc:sc + 1], None, ALU.mult, None, ["ang", "rc"], ["tab"])
        td = self.dram["tabs_d"]
        self.dma(td[0], self.tabN.rearrange("p a t -> p (a t)"), ["tab"], ["tabs_d"])
        self.dma(td[1], self.tabM.rearrange("p a t -> p (a t)"), ["tab"], ["tabs_d"])

    def x_to_xT(self, xblk, xkey, tb, rt=None):
        tt = tb // 4
        for half in range(2):
            b = self.short()
            for c in range(4):
                cc = half * 4 + c
                self.tr(self.pb[b][:, c * 128:(c + 1) * 128], xblk[:, cc * 128:(cc + 1) * 128], [xkey], [("pb", b)])
            dst = self.xT[:, half * 4:half * 4 + 4, tb * 128:(tb + 1) * 128]
            src = self.pb[b][:, :].rearrange("p (c t) -> p c t", c=4)
            self.CP("act" if half == 0 else "dve", dst, src, [("pb", b)], [("xT", tt)])
            if rt is not None:
                rt(half, b)

    def load_xT(self, x_in):
        self.arena_reset()
        xbs = [self.carve([D], F32) for _ in range(2)]
        for tb in range(NB):
            xb_ = xbs[tb % 2]
            key = ("xblk", tb % 2)
            self.dma(xb_, x_in[tb * 128:(tb + 1) * 128, :], [], [key])
            self.x_to_xT(xb_, key, tb)

    def mixer_phase(self, li, res_in, res_out):
        d = self.dram
        self.arena_reset()
        self.stage = self.stage[0:2]
        self.yT = [self.carve([2, T], BF16) for _ in range(4)]
        self.mixer_base = self.aoff
        for n, (nm, fn) in enumerate((("conv", self.conv_branch), ("nsa", self.nsa_branch), ("mla", self.mla_branch),
                                     ("sb", self.sb_branch))):
            only = getattr(self, "only", None)
            if only is None or nm in only:
                fn(li)
                self.dbg("yT%d_%d" % (n, li), self.yT[n], [128, 2, T], ["yT%d" % n])
            self.aoff = self.mixer_base
            self.S.barrier()
            if self.stop_after == (nm, li):
                return True
        self.merge_ln1(li, res_in, res_out)

    def dbg(self, name, ap, shape, keys, dt=BF16):
        if not self.debug:
            return
        o = self.dout("dbg_" + name, shape, dt)
        self.finals.append(self.dma(o, ap, keys, []))

    def conv_branch(self, li):
        d = self.dram
        win = d["win"]
        cp = self.carve([2, 34], F32)
        self.dma(cp, d["convp"][li], [], ["cp"])
        hp = self.carve([2, 30 + T], F32)
        acc = self.carve([2, T], F32)
        sig = [self.carve([512], F32) for _ in range(2)]
        self.MS("pool", hp[:, :, 0:30], 0.0, ["hp"])
        wv, wk = self.wload(win[li][:, OFF_CONV:OFF_CONV + 512], 8, 512)
        for tt in range(4):
            for c in range(2):
                ba = self.proj_fm(wv, wk, c * 128, 128, tt)
                bg = self.proj_fm(wv, wk, 256 + c * 128, 128, tt)
                sg = sig[c]
                self.A(sg, self.pb[bg][:, :], AF.Sigmoid, [("pb", bg)], [("sig", c)])
                self.TT("dve", hp[:, c, 30 + tt * 512:30 + (tt + 1) * 512], self.pb[ba][:, :], sg, ALU.mult,
                        [("pb", ba), ("sig", c)], ["hp"])
        for c in range(2):
            eng = "dve"
            self.TS(eng, acc[:, c, :], hp[:, c, 0:T], cp[:, c, 0:1], cp[:, c, 31:32], ALU.mult, ALU.add,
                    ["hp", "cp"], [("acc", c)])
            for w in range(1, 31):
                self.STT(eng, acc[:, c, :], hp[:, c, w:w + T], cp[:, c, w:w + 1], acc[:, c, :], ALU.mult, ALU.add,
                         ["hp", "cp", ("acc", c)], [("acc", c)])
        sq = [self.carve([512], F32) for _ in range(2)]
        m2 = self.carve([512], F32)
        rstd = self.carve([512], F32)
        dd = [self.carve([512], F32) for _ in range(2)]
        for tt in range(4):
            sl = slice(tt * 512, (tt + 1) * 512)
            bm = self.short()
            for c in range(2):
                self.mm(self.pb[bm][:, :], self.onesf[:, :], acc[:, c, sl], c == 0, c == 1,
                        ["onesf", ("acc", c)], [("pb", bm)])
            bq = self.short()
            for c in range(2):
                self.A(sq[c], acc[:, c, sl], AF.Square, [("acc", c)], [("sq", c)])
            for c in range(2):
                self.mm(self.pb[bq][:, :], self.onesf[:, :], sq[c], c == 0, c == 1,
                        ["onesf", ("sq", c)], [("pb", bq)])
            self.A(m2, self.pb[bm][:, :], AF.Square, [("pb", bm)], ["m2"], scale=1.0 / 256)
            self.STT("dve", rstd, self.pb[bq][:, :], 1.0 / 256, m2, ALU.mult, ALU.subtract, [("pb", bq), "m2"], ["rstd"])
            self.A(rstd, rstd, AF.Sqrt, ["rstd"], ["rstd"], bias=LN_EPS)
            self.S.op("dve", lambda e, r=rstd: e.reciprocal(out=r, in_=r), reads=["rstd"], writes=["rstd"])
            for c in range(2):
                self.STT("dve", dd[c], self.pb[bm][:, :], -1.0 / 256, acc[:, c, sl], ALU.mult, ALU.add,
                         [("pb", bm), ("acc", c)], [("dd", c)])
                self.TT("dve", dd[c], dd[c], rstd, ALU.mult, [("dd", c), "rstd"], [("dd", c)])
                self.A(self.yT[0][:, c, sl], dd[c], AF.Silu, [("dd", c), "cp"], ["yT0"],
                       scale=cp[:, c, 32:33], bias=cp[:, c, 33:34])

    def y_to_yT(self, ytile, ykey, n, qb):
        b = self.short()
        for c in range(2):
            self.tr(self.pb[b][:, c * 128:(c + 1) * 128], ytile[:, c * 128:(c + 1) * 128], [ykey], [("pb", b)])
        self.CP("act", self.yT[n][:, :, qb * 128:(qb + 1) * 128],
                self.pb[b][:, 0:256].rearrange("p (c t) -> p c t", c=2), [("pb", b)], ["yT%d" % n])

    def softmax_attn_block(self, qb, kbs, kT_fn, qT_ap, v_fn, scale, extra_mask=None, nm=""):
        ob = self.accb()
        n = len(kbs)
        for g0 in range(0, n, 4):
            grp = kbs[g0:g0 + 4]
            sbk = self.short()
            for i, (kb, mt) in enumerate(grp):
                kT, kkeys = kT_fn(kb)
                self.mm(self.pb[sbk][:, i * 128:(i + 1) * 128], kT, qT_ap[0], True, True,
                        list(kkeys) + list(qT_ap[1]), [("pb", sbk)])
            ei = self.ei % 3
            self.ei += 1
            E = self.Ebuf[ei]
            ek = ("E", ei)
            w = len(grp) * 128
            self.A(E[:, 0:w], self.pb[sbk][:, 0:w], AF.Exp, [("pb", sbk)], [ek], scale=scale)
            if extra_mask is not None:
                mtile, mkey = extra_mask
                self.TT("pool", E[:, 0:w], E[:, 0:w], mtile[:, g0 * 128:g0 * 128 + w], ALU.mult, [ek, mkey], [ek])
            else:
                for i, (kb, mt) in enumerate(grp):
                    if mt is not None:
                        mi = {"le": 0, "lt": 1, "gt": 2}[mt]
                        self.TT("pool", E[:, i * 128:(i + 1) * 128], E[:, i * 128:(i + 1) * 128],
                                self.cmask[:, mi, :], ALU.mult, [ek, "cmask"], [ek])
            for i, (kb, mt) in enumerate(grp):
                v, vkeys = v_fn(kb)
                gi = g0 + i
                self.mm(self.pb[ob][:, 0:65], E[:, i * 128:(i + 1) * 128], v, gi == 0, gi == n - 1,
                        [ek] + list(vkeys), [("pb", ob)])
        return ob

    def nsa_branch(self, li):
        d = self.dram
        win = d["win"]
        cst = {k: d["cst"][k] for k in d["cst"]}
        self.tabN = self.carve([2, T], BF16)
        self.dma(self.tabN.rearrange("p a t -> p (a t)"), d["tabs_d"][0], [], ["tab"])
        QT = self.carve([2, T], BF16)
        QR = self.carve([2, T], BF16)
        KS = self.carve([T], BF16)
        KW = self.carve([T], BF16)
        KCV = self.carve([T], BF16)
        vS = self.carve([NB, 65], BF16)
        vW = self.carve([NB, 65], BF16)
        sg = self.carve([NB, 12], F32)
        cmpvalid = self.carve([T], BF16)
        eexp = self.carve([T], BF16, parts=32)
        keepadd = self.carve([2, NB, 32], F32)
        VC = self.carve([97], BF16)
        w1t = self.carve([32, 64], BF16)
        pet = self.carve([32], BF16)
        w2kt = self.carve([128], BF16, parts=64)
        w2vt = self.carve([64], BF16, parts=64)
        hid = [self.carve([127], BF16, parts=64) for _ in range(2)]
        hb = self.carve([2], F32, parts=64)
        kcT = self.carve([127], BF16)
        t1 = [self.carve([512], F32) for _ in range(2)]
        t2 = [self.carve([512], F32) for _ in range(2)]
        self.Ebuf = [self.carve([512], BF16) for _ in range(3)]
        self.ei = 0
        ytile = [self.carve([256], F32) for _ in range(2)]
        selmask = [self.carve([NB * 128], BF16) for _ in range(2)]
        small = [self.carve([64], F32) for _ in range(2)]
        imp = [self.carve([32], F32) for _ in range(2)]
        scr = [self.carve([32], F32) for _ in range(2)]
        selT = [self.carve([128], BF16, parts=32) for _ in range(2)]
        self.cast_load(cmpvalid, cst["cmpvalid"], 128, [T], "cmpvalid")
        self.cast_load(eexp, cst["eexp"], 32, [T], "eexp")
        self.dma(keepadd, cst["keepadd"], [], ["keepadd"])
        self.cast_load(VC[:, 64:97], cst["ovl"], 128, [33], "VCc")
        for c in range(2):
            self.dma_w1(w1t, d["w1"][li, c], c)
        self.cast_load(pet, d["pe_r"][li], 128, [32], "pet")
        self.cast_load(w2kt, d["w2k"][li], 64, [128], "w2kt")
        self.cast_load(w2vt, d["w2v"][li], 64, [64], "w2vt")
        self.MS("dve", vS[:, :, 64:65], 1.0, ["vS"])
        self.MS("dve", vW[:, :, 64:65], 1.0, ["vW"])
        wv, wk = self.wload(win[li][:, OFF_NQ:OFF_NQ + 512], 8, 512)
        for tt in range(4):
            sl = slice(tt * 512, (tt + 1) * 512)
            for c in range(2):
                b0 = self.proj_fm(wv, wk, c * 128, 128, tt)
                b1 = self.proj_fm(wv, wk, 256 + c * 128, 128, tt)
                self.CP("act", QT[:, c, sl], self.pb[b0][:, :], [("pb", b0)], ["QT"])
                self.TT("dve", t1[c], self.pb[b0][:, :], self.tabN[:, 0, sl], ALU.mult, [("pb", b0), "tab"], [("t1", c)])
                self.TT("dve", t2[c], self.pb[b1][:, :], self.tabN[:, 1, sl], ALU.mult, [("pb", b1), "tab"], [("t2", c)])
                self.TT("pool", QR[:, c, sl], t1[c], t2[c], ALU.add, [("t1", c), ("t2", c)], ["QR"])
        wv, wk = self.wload(win[li][:, OFF_NK:OFF_NK + 512], 8, 512)
        for tt in range(4):
            sl = slice(tt * 512, (tt + 1) * 512)
            for c, dst, dk in ((0, KS, "KS"), (1, KW, "KW")):
                b0 = self.proj_fm(wv, wk, c * 256, 128, tt)
                b1 = self.proj_fm(wv, wk, c * 256 + 128, 128, tt)
                self.TT("dve", t1[c], self.pb[b0][:, :], self.tabN[:, 0, sl], ALU.mult, [("pb", b0), "tab"], [("t1", c)])
                self.TT("dve", t2[c], self.pb[b1][:, :], self.tabN[:, 1, sl], ALU.mult, [("pb", b1), "tab"], [("t2", c)])
                self.TT("pool", dst[:, sl], t1[c], t2[c], ALU.add, [("t1", c), ("t2", c)], [dk])
        wvm, wkm = self.wload(win[li][:, OFF_MISC:OFF_MISC + 128], 8, 128)
        for tt in range(4):
            sl = slice(tt * 512, (tt + 1) * 512)
            b0 = self.proj_fm(wvm, wkm, 0, 128, tt)
            self.CP("act", KCV[:, sl], self.pb[b0][:, :], [("pb", b0)], ["KCV"])
        wvt, wkt = self.wload(win[li][:, OFF_TOK:OFF_TOK + 140], 8, 140)
        for tb in range(NB):
            b = self.short()
            for k in range(8):
                self.mm(self.pb[b][:, 0:140], self.xT[:, k, tb * 128:(tb + 1) * 128], wvt[:, k, :], k == 0, k == 7,
                        [wkt, ("xT", tb // 4)], [("pb", b)])
            self.CP("act", vS[:, tb, 0:64], self.pb[b][:, 0:64], [("pb", b)], ["vS"])
            self.CP("dve", vW[:, tb, 0:64], self.pb[b][:, 64:128], [("pb", b)], ["vW"])
            self.A(sg[:, tb, :], self.pb[b][:, 128:140], AF.Sigmoid, [("pb", b)], ["sg"])
        for c in range(2):
            ps_ = slice(c * 64, (c + 1) * 64)
            b = self.short()
            for l in range(32):
                self.mm(self.pb[b][0:64, 0:127], w1t[ps_, l, :], KCV[ps_, l:l + 16 * 126 + 1:16], l == 0, l == 31,
                        ["w1t", "KCV"], [("pb", b)])
            b2 = self.short()
            for l in range(32):
                self.mm(self.pb[b2][0:64, 0:1], w1t[ps_, l, :], pet[ps_, l:l + 1], l == 0, l == 31,
                        ["w1t", "pet"], [("pb", b2)])
            self.CP("dve", hb[:, c:c + 1], self.pb[b2][0:64, 0:1], [("pb", b2)], ["hb"])
            self.A(hid[c], self.pb[b][0:64, 0:127], AF.Gelu_apprx_tanh, [("pb", b), "hb"], [("hid", c)],
                   bias=hb[:, c:c + 1])
        b = self.short()
        self.mm(self.pb[b][:, 0:127], w2kt[:, :], hid[0], True, True, ["w2kt", ("hid", 0)], [("pb", b)])
        self.CP("act", kcT, self.pb[b][:, 0:127], [("pb", b)], ["kcT"])
        b = self.short()
        self.mm(self.pb[b][0:127, 0:64], hid[1], w2vt[:, :], True, True, ["w2vt", ("hid", 1)], [("pb", b)])
        self.CP("act", VC[0:127, 0:64], self.pb[b][0:127, 0:64], [("pb", b)], ["VCv"])
        for qb in range(NB):
            qs = slice(qb * 128, (qb + 1) * 128)
            yt = ytile[qb % 2]
            yk = ("yt", qb % 2)
            sm = small[qb % 2]
            smk = ("small", qb % 2)
            im = imp[qb % 2]
            imk = ("imp", qb % 2)
            sbk2 = [self.short(), self.short()]
            for h in range(4):
                hp_ = slice((h % 2) * 64, (h % 2) * 64 + 64)
                bb_ = sbk2[h % 2]
                self.mm(self.pb[bb_][0:127, (h // 2) * 128:(h // 2 + 1) * 128], kcT[hp_, :], QT[hp_, h // 2, qs], True, True,
                        ["kcT", "QT"], [("pb", bb_)])
            ei = self.ei % 3
            self.ei += 1
            E = self.Ebuf[ei]
            ek = ("E", ei)
            for h in range(4):
                bb_ = sbk2[h % 2]
                self.A(E[0:127, h * 128:(h + 1) * 128], self.pb[bb_][0:127, (h // 2) * 128:(h // 2 + 1) * 128], AF.Exp,
                       [("pb", bb_)], [ek], scale=0.125)
            for h in range(4):
                self.TT("pool", E[0:127, h * 128:(h + 1) * 128], E[0:127, h * 128:(h + 1) * 128], cmpvalid[0:127, qs],
                        ALU.mult, [ek, "cmpvalid"], [ek])
            ob = self.accb()
            for h in range(4):
                self.mm(self.pb[ob][:, h * 97:(h + 1) * 97], E[0:127, h * 128:(h + 1) * 128], VC[0:127, :], True, True,
                        [ek, "VCv", "VCc"], [("pb", ob)])
            P = self.pb[ob]
            for h in range(4):
                self.TS("dve", sm[:, h:h + 1], P[:, 97 * h + 96:97 * h + 97], 1e-30, None, ALU.max, None, [("pb", ob)], [smk])
            self.S.op("dve", lambda e, s_=sm: e.reciprocal(out=s_[:, 0:4], in_=s_[:, 0:4]), reads=[smk], writes=[smk])
            for h in range(4):
                if h == 0:
                    self.TS("dve", im, P[:, 64:96], sm[:, 0:1], None, ALU.mult, None, [("pb", ob), smk], [imk])
                else:
                    self.STT("dve", im, P[:, 97 * h + 64:97 * h + 96], sm[:, h:h + 1], im, ALU.mult, ALU.add,
                             [("pb", ob), smk, imk], [imk])
            for h in range(4):
                self.TT("dve", sm[:, 4 + h:5 + h], sm[:, h:h + 1], sg[:, qb, 3 * h:3 * h + 1], ALU.mult, [smk, "sg"], [smk])
                self.A(yt[:, h * 64:(h + 1) * 64], P[:, 97 * h:97 * h + 64], AF.Copy, [("pb", ob), smk], [yk],
                       scale=sm[:, 4 + h:5 + h])
            mk = None
            if qb >= 8:
                sc_ = scr[qb % 2]
                sck = ("scr", qb % 2)
                self.TT("dve", im, im, keepadd[:, 0, qb, :], ALU.mult, [imk, "keepadd"], [imk])
                self.TT("dve", im, im, keepadd[:, 1, qb, :], ALU.add, [imk, "keepadd"], [imk])
                self.S.op("dve", lambda e, s_=sm, i_=im: e.max(out=s_[:, 8:16], in_=i_), reads=[imk, smk], writes=[smk])
                self.S.op("dve", lambda e, s_=sm, i_=im, c_=sc_: e.match_replace(out=c_, in_to_replace=s_[:, 8:16],
                                                                                in_values=i_, imm_value=-1e30),
                          reads=[imk, smk], writes=[sck])
                self.S.op("dve", lambda e, s_=sm, c_=sc_: e.max(out=s_[:, 16:24], in_=c_), reads=[sck, smk], writes=[smk])
                self.TS("dve", sc_, im, sm[:, 23:24], None, ALU.is_ge, None, [imk, smk], [sck])
                b = self.short()
                self.tr(self.pb[b][0:32, 0:128], sc_, [sck], [("pb", b)])
                sT = selT[qb % 2]
                stk = ("selT", qb % 2)
                self.CP("act", sT, self.pb[b][0:32, 0:128], [("pb", b)], [stk])
                smt = selmask[qb % 2]
                mk = ("selmask", qb % 2)
                for g0 in range(0, qb + 1, 4):
                    n = min(4, qb + 1 - g0)
                    b = self.short()
                    for i in range(n):
                        kb = g0 + i
                        self.mm(self.pb[b][:, i * 128:(i + 1) * 128], eexp[:, kb * 128:(kb + 1) * 128], sT, True, True,
                                ["eexp", stk], [("pb", b)])
                    self.CP("act", smt[:, g0 * 128:(g0 + n) * 128], self.pb[b][:, 0:n * 128], [("pb", b)], [mk])
                self.TT("pool", smt[:, qb * 128:(qb + 1) * 128], smt[:, qb * 128:(qb + 1) * 128], self.cmask[:, 0, :],
                        ALU.mult, [mk, "cmask"], [mk])
            for h in range(4):
                hp_ = slice((h % 2) * 64, (h % 2) * 64 + 64)
                qT_ap = (QR[hp_, h // 2, qs], ["QR"])
                for br, KT, kkey, V, vkey, gcol in ((1, KS, "KS", vS, "vS", 3 * h + 1), (2, KW, "KW", vW, "vW", 3 * h + 2)):
                    if br == 1:
                        kbs = [(kb, "le" if kb == qb else None) for kb in range(qb + 1)]
                        em = (selmask[qb % 2], mk) if qb >= 8 else None
                    else:
                        kbs = [(kb, "le" if kb == qb else ("gt" if kb == qb - 4 else None))
                               for kb in range(max(0, qb - 4), qb + 1)]
                        em = None
                    ob = self.softmax_attn_block(
                        qb, kbs, lambda kb, KT=KT, kkey=kkey: (KT[hp_, kb * 128:(kb + 1) * 128], [kkey]), qT_ap,
                        lambda kb, V=V, vkey=vkey: (V[:, kb, :], [vkey]), 0.125, extra_mask=em)
                    fk = ("fac", qb % 2)
                    fac = sm[:, 24 + 2 * h + (br - 1):25 + 2 * h + (br - 1)]
                    self.S.op("dve", lambda e, f_=fac, o_=self.pb[ob][:, 64:65]: e.reciprocal(out=f_, in_=o_),
                              reads=[("pb", ob), smk], writes=[smk])
                    self.TT("dve", fac, fac, sg[:, qb, gcol:gcol + 1], ALU.mult, [smk, "sg"], [smk])
                    self.STT("dve", yt[:, h * 64:(h + 1) * 64], self.pb[ob][:, 0:64], fac, yt[:, h * 64:(h + 1) * 64],
                             ALU.mult, ALU.add, [("pb", ob), smk, yk], [yk])
            self.y_to_yT(yt, yk, 1, qb)

    def mla_branch(self, li):
        d = self.dram
        win = d["win"]
        self.tabM = self.carve([2, T], BF16)
        self.dma(self.tabM.rearrange("p a t -> p (a t)"), d["tabs_d"][1], [], ["tab"])
        qg = self.carve([2, T], BF16)
        kvg = self.carve([T], BF16)
        CS = self.carve([2, T], BF16)
        rkv = self.carve([T], BF16)
        rkt = self.carve([NB], F32)
        Vm = self.carve([NB, 4, 65], BF16)
        QH = [self.carve([T], BF16) for _ in range(2)]
        KH = [self.carve([T], BF16) for _ in range(2)]
        KR = self.carve([T], BF16)
        nrm = self.carve([4], F32)
        sq = [self.carve([512], F32) for _ in range(2)]
        t1 = [self.carve([512], F32) for _ in range(2)]
        t2 = [self.carve([512], F32) for _ in range(2)]
        rs = self.carve([512], F32)
        self.Ebuf = [self.carve([512], BF16) for _ in range(3)]
        self.ei = 0
        self.ymla = self.carve([NB, 256], F32)
        small = [self.carve([8], F32) for _ in range(2)]
        self.dma(nrm[:, 0:2], d["qn"][li], [], ["nrm"])
        self.dma(nrm[:, 2:3], d["kvn"][li], [], ["nrm"])
        self.MS("dve", Vm[:, :, :, 64:65], 1.0, ["Vm"])
        wvm, wkm = self.wload(win[li][:, OFF_MISC + 128:OFF_MISC + 512], 8, 384)
        for tt in range(4):
            sl = slice(tt * 512, (tt + 1) * 512)
            bq = []
            for c in range(2):
                b0 = self.proj_fm(wvm, wkm, c * 128, 128, tt)
                bq.append(b0)
                self.A(qg[:, c, sl], self.pb[b0][:, :], AF.Copy, [("pb", b0), "nrm"], ["qg"], scale=nrm[:, c:c + 1])
                self.A(sq[c], self.pb[b0][:, :], AF.Square, [("pb", b0)], [("sq", c)])
            bs = self.short()
            for c in range(2):
                self.mm(self.pb[bs][:, :], self.onesf[:, :], sq[c], c == 0, c == 1, ["onesf", ("sq", c)], [("pb", bs)])
            self.A(rs, self.pb[bs][:, :], AF.Sqrt, [("pb", bs)], ["rs"], scale=1.0 / 256, bias=RMS_EPS)
            self.S.op("dve", lambda e, r=rs: e.reciprocal(out=r, in_=r), reads=["rs"], writes=["rs"])
            for w in range(2):
                self.TT("dve", CS[:, w, sl], self.tabM[:, w, sl], rs, ALU.mult, ["tab", "rs"], ["CS"])
            b0 = self.proj_fm(wvm, wkm, 256, 128, tt)
            self.A(kvg[:, sl], self.pb[b0][:, :], AF.Copy, [("pb", b0), "nrm"], ["kvg"], scale=nrm[:, 2:3])
            self.A(sq[0], self.pb[b0][:, :], AF.Square, [("pb", b0)], [("sq", 0)])
            bs = self.short()
            self.mm(self.pb[bs][:, :], self.onesf[:, :], sq[0], True, True, ["onesf", ("sq", 0)], [("pb", bs)])
            self.A(rs, self.pb[bs][:, :], AF.Sqrt, [("pb", bs)], ["rs"], scale=1.0 / 128, bias=RMS_EPS)
            self.S.op("dve", lambda e, r=rs, o=rkv[:, sl]: e.reciprocal(out=o, in_=r), reads=["rs"], writes=["rkv"])
            bt = self.short()
            for i in range(4):
                self.mm(self.pb[bt][:, i:i + 1], sq[0][:, i * 128:(i + 1) * 128], self.onesf[:, 0:1], True, True,
                        [("sq", 0), "onesf"], [("pb", bt)])
            self.A(rkt[:, tt * 4:tt * 4 + 4], self.pb[bt][:, 0:4], AF.Sqrt, [("pb", bt)], ["rkt"], scale=1.0 / 128,
                   bias=RMS_EPS)
        self.S.op("dve", lambda e: e.reciprocal(out=rkt, in_=rkt), reads=["rkt"], writes=["rkt"])
        wvr, wkr = self.wload(win[li][:, OFF_KR:OFF_KR + 256], 8, 256)
        r9 = slice(64, 96)
        for tt in range(4):
            sl = slice(tt * 512, (tt + 1) * 512)
            b0 = self.proj_fm(wvr, wkr, 0, 96, tt)
            b1 = self.proj_fm(wvr, wkr, 128, 96, tt)
            self.TT("dve", t1[0][r9, :], self.pb[b0][r9, :], self.tabM[r9, 0, sl], ALU.mult, [("pb", b0), "tab"], [("t1", 0)])
            self.TT("dve", t2[0][r9, :], self.pb[b1][r9, :], self.tabM[r9, 1, sl], ALU.mult, [("pb", b1), "tab"], [("t2", 0)])
            self.TT("pool", KR[r9, sl], t1[0][r9, :], t2[0][r9, :], ALU.add, [("t1", 0), ("t2", 0)], ["KR"])
        wvu, wku = self.wload(d["ukv"][li], 1, 512)
        for tb in range(NB):
            b = self.short()
            self.mm(self.pb[b][:, 0:256], kvg[:, tb * 128:(tb + 1) * 128], wvu[:, 0, 256:512], True, True,
                    ["kvg", wku], [("pb", b)])
            self.A(Vm[:, tb, :, 0:64], self.pb[b][:, 0:256].rearrange("p (h c) -> p h c", h=4), AF.Copy,
                   [("pb", b), "rkt"], ["Vm"], scale=rkt[:, tb:tb + 1])
        wvq, wkq = self.wload(d["uq"][li], 2, 768)
        scale = 96.0 ** -0.5
        for h in range(4):
            Q = QH[h % 2]
            K = KH[h % 2]
            qk = ("QH", h % 2)
            kk = ("KH", h % 2)
            for tt in range(4):
                sl = slice(tt * 512, (tt + 1) * 512)
                ba = self.proj_fm(wvq, wkq, (2 * h) * 96, 96, tt, src=qg, srckey="qg", kc=2)
                bb = self.proj_fm(wvq, wkq, (2 * h + 1) * 96, 96, tt, src=qg, srckey="qg", kc=2)
                c = tt % 2
                self.TT("dve", t1[c][0:96, :], self.pb[ba][0:96, :], CS[0:96, 0, sl], ALU.mult, [("pb", ba), "CS"], [("t1", c)])
                self.TT("dve", t2[c][0:96, :], self.pb[bb][0:96, :], CS[0:96, 1, sl], ALU.mult, [("pb", bb), "CS"], [("t2", c)])
                self.TT("pool", Q[0:96, sl], t1[c][0:96, :], t2[c][0:96, :], ALU.add, [("t1", c), ("t2", c)], [qk])
                bk = self.short()
                self.mm(self.pb[bk][0:64, :], wvu[:, 0, h * 64:(h + 1) * 64], kvg[:, sl], True, True, [wku, "kvg"], [("pb", bk)])
                self.TT("dve", K[0:64, sl], self.pb[bk][0:64, :], rkv[0:64, sl], ALU.mult, [("pb", bk), "rkv"], [kk])
            self.CP("pool", K[r9, :], KR[r9, :], ["KR"], [kk])
            for qb in range(NB):
                qs = slice(qb * 128, (qb + 1) * 128)
                sm = small[qb % 2]
                smk = ("small", qb % 2)
                kbs = [(kb, "le" if kb == qb else None) for kb in range(qb + 1)]
                ob = self.softmax_attn_block(
                    qb, kbs, lambda kb: (K[0:96, kb * 128:(kb + 1) * 128], [kk]), (Q[0:96, qs], [qk]),
                    lambda kb: (Vm[:, kb, h, :], ["Vm"]), scale)
                self.S.op("dve", lambda e, s_=sm, o_=self.pb[ob][:, 64:65]: e.reciprocal(out=s_[:, 0:1], in_=o_),
                          reads=[("pb", ob)], writes=[smk])
                self.A(self.ymla[:, qb, h * 64:(h + 1) * 64], self.pb[ob][:, 0:64], AF.Copy, [("pb", ob), smk],
                       [("ymla", qb)], scale=sm[:, 0:1])
        for qb in range(NB):
            self.y_to_yT(self.ymla[:, qb, :], ("ymla", qb), 2, qb)

    def sb_branch(self, li):
        d = self.dram
        win = d["win"]
        cst = d["cst"]
        QT = self.carve([2, T], BF16)
        KT = self.carve([2, T], BF16)
        V = self.carve([NB, 256], BF16)
        indt = self.carve([16, 16], BF16)
        selgt = self.carve([16, 128], BF16, parts=16)
        sp = [self.carve([NB * 128], F32) for _ in range(2)]
        lk = [self.carve([NB * 128], BF16) for _ in range(2)]
        ex = [self.carve([512], F32) for _ in range(2)]
        ar = [self.carve([512], F32) for _ in range(2)]
        aa = [self.carve([512], BF16) for _ in range(3)]
        ts_ = [self.carve([128], BF16, parts=16) for _ in range(2)]
        ytile = [self.carve([256], F32) for _ in range(2)]
        self.cast_load(indt, cst["indt"], 128, [16, 16], "indt")
        self.cast_load(selgt, cst["selgt"], 16, [16, 128], "selgt")
        wv, wk = self.wload(win[li][:, OFF_SB:OFF_SB + 512], 8, 512)
        for tt in range(4):
            sl = slice(tt * 512, (tt + 1) * 512)
            for c in range(2):
                b0 = self.proj_fm(wv, wk, c * 128, 128, tt)
                self.CP("act", QT[:, c, sl], self.pb[b0][:, :], [("pb", b0)], ["QT"])
                b1 = self.proj_fm(wv, wk, 256 + c * 128, 128, tt)
                self.CP("dve", KT[:, c, sl], self.pb[b1][:, :], [("pb", b1)], ["KT"])
        wvt, wkt = self.wload(win[li][:, OFF_TOK + 256:OFF_TOK + 512], 8, 256)
        for tb in range(NB):
            b = self.short()
            for k in range(8):
                self.mm(self.pb[b][:, 0:256], self.xT[:, k, tb * 128:(tb + 1) * 128], wvt[:, k, :], k == 0, k == 7,
                        [wkt, ("xT", tb // 4)], [("pb", b)])
            self.CP("act", V[:, tb, :], self.pb[b][:, 0:256], [("pb", b)], ["V"])
        it = 0
        ai_ = 0
        for qb in range(NB):
            qs = slice(qb * 128, (qb + 1) * 128)
            yt = ytile[qb % 2]
            yk = ("yt", qb % 2)
            for h in range(4):
                hp_ = slice((h % 2) * 64, (h % 2) * 64 + 64)
                spt, lkt, tst = sp[it % 2], lk[it % 2], ts_[it % 2]
                spk, lkk, tsk = ("sp", it % 2), ("lk", it % 2), ("ts", it % 2)
                it += 1
                nk = qb + 1
                for g0 in range(0, nk, 4):
                    n = min(4, nk - g0)
                    w = n * 128
                    sbk = self.short()
                    for i in range(n):
                        kb = g0 + i
                        self.mm(self.pb[sbk][:, i * 128:(i + 1) * 128], KT[hp_, h // 2, kb * 128:(kb + 1) * 128],
                                QT[hp_, h // 2, qs], True, True, ["KT", "QT"], [("pb", sbk)])
                    e_ = ex[(g0 // 4) % 2]
                    exk = ("ex", (g0 // 4) % 2)
                    self.A(e_[:, 0:w], self.pb[sbk][:, 0:w], AF.Exp, [("pb", sbk)], [exk], scale=-0.125)
                    self.A(spt[:, g0 * 128:g0 * 128 + w], e_[:, 0:w], AF.Ln, [exk], [spk], bias=1.0)
                    self.STT("dve", lkt[:, g0 * 128:g0 * 128 + w], self.pb[sbk][:, 0:w], -0.125, spt[:, g0 * 128:g0 * 128 + w],
                             ALU.mult, ALU.subtract, [("pb", sbk), spk], [lkk])
                self.TT("pool", lkt[:, qb * 128:(qb + 1) * 128], lkt[:, qb * 128:(qb + 1) * 128], self.cmask[:, 1, :],
                        ALU.mult, [lkk, "cmask"], [lkk])
                bts = self.short()
                for kb in range(nk):
                    self.mm(self.pb[bts][0:16, 0:128], indt[:, kb, :], lkt[:, kb * 128:(kb + 1) * 128], kb == 0, kb == nk - 1,
                            ["indt", lkk], [("pb", bts)])
                self.CP("act", tst, self.pb[bts][0:16, 0:128], [("pb", bts)], [tsk])
                ob = self.accb()
                for g0 in range(0, nk, 4):
                    n = min(4, nk - g0)
                    w = n * 128
                    lb = self.short()
                    for i in range(n):
                        kb = g0 + i
                        self.mm(self.pb[lb][:, i * 128:(i + 1) * 128], self.cmask[:, 2, :], lkt[:, kb * 128:(kb + 1) * 128],
                                True, False, ["cmask", lkk], [("pb", lb)])
                        self.mm(self.pb[lb][:, i * 128:(i + 1) * 128], selgt[:, kb, :], tst, False, True,
                                ["selgt", tsk], [("pb", lb)])
                    a_ = ar[(g0 // 4) % 2]
                    ark = ("ar", (g0 // 4) % 2)
                    self.TT("dve", a_[:, 0:w], self.pb[lb][:, 0:w], spt[:, g0 * 128:g0 * 128 + w], ALU.subtract,
                            [("pb", lb), spk], [ark])
                    at = aa[ai_ % 3]
                    ak = ("aa", ai_ % 3)
                    ai_ += 1
                    self.A(at[:, 0:w], a_[:, 0:w], AF.Exp, [ark], [ak])
                    if g0 + n == nk:
                        i = n - 1
                        self.TT("pool", at[:, i * 128:(i + 1) * 128], at[:, i * 128:(i + 1) * 128], self.cmask[:, 1, :],
                                ALU.mult, [ak, "cmask"], [ak])
                    for i in range(n):
                        kb = g0 + i
                        self.mm(self.pb[ob][:, 0:64], at[:, i * 128:(i + 1) * 128], V[:, kb, h * 64:(h + 1) * 64],
                                kb == 0, kb == nk - 1, [ak, "V"], [("pb", ob)])
                self.CP("act", yt[:, h * 64:(h + 1) * 64], self.pb[ob][:, 0:64], [("pb", ob)], [yk])
            self.y_to_yT(yt, yk, 3, qb)

    def layer_norm_block(self, h, hk, gb, tb, res_out, li, route):
        st = self.lnst[tb % 2]
        sk = ("lnst", tb % 2)
        junk = self.lnjunk
        self.A(junk, h, AF.Copy, [hk], ["lnjunk", sk], accum=st[:, 0:1])
        self.A(junk, h, AF.Square, [hk], ["lnjunk", sk], accum=st[:, 1:2])
        self.TS("dve", st[:, 2:3], st[:, 0:1], 1.0 / D, None, ALU.mult, None, [sk], [sk])
        self.TT("dve", st[:, 3:4], st[:, 2:3], st[:, 2:3], ALU.mult, [sk], [sk])
        self.STT("dve", st[:, 4:5], st[:, 1:2], 1.0 / D, st[:, 3:4], ALU.mult, ALU.subtract, [sk], [sk])
        self.A(st[:, 4:5], st[:, 4:5], AF.Sqrt, [sk], [sk], bias=LN_EPS)
        self.S.op("dve", lambda e, s_=st: e.reciprocal(out=s_[:, 5:6], in_=s_[:, 4:5]), reads=[sk], writes=[sk])
        self.STT("dve", st[:, 6:7], st[:, 2:3], -1.0, st[:, 5:6], ALU.mult, ALU.mult, [sk], [sk])
        self.A(h, h, AF.Identity, [hk, sk], [hk], scale=st[:, 5:6], bias=st[:, 6:7])
        self.TT("dve", h, h, gb[:, 0, :], ALU.mult, [hk, "lngb"], [hk])
        self.TT("dve", h, h, gb[:, 1, :], ALU.add, [hk, "lngb"], [hk])
        o = self.dma(res_out[tb * 128:(tb + 1) * 128, :], h, [hk], [("res", id(res_out), tb)])
        self.x_to_xT(h, hk, tb, rt=route)
        return o

    def merge_ln1(self, li, res_in, res_out):
        d = self.dram
        win = d["win"]
        HT = 1024
        mp = self.carve([8, HT], BF16)
        accm = self.carve([8, HT], F32)
        sgt = [self.carve([512], BF16) for _ in range(2)]
        prod = [self.carve([512], F32) for _ in range(2)]
        gb = self.carve([2, D], F32)
        hbuf = [self.carve([D], F32) for _ in range(2)]
        xin = [self.carve([D], F32) for _ in range(2)]
        self.lnst = [self.carve([8], F32) for _ in range(2)]
        self.lnjunk = self.carve([D], BF16)
        self.dma(gb[:, 0, :], d["ln1g"][li:li + 1, :].to_broadcast([128, D]), [], ["lngb"])
        self.dma(gb[:, 1, :], d["ln1b"][li:li + 1, :].to_broadcast([128, D]), [], ["lngb"])
        route = None
        if li == 1:
            route = self.make_router(li)
        for th in range(2):
            for n in range(4):
                wvb, wkb = self.wload(d["wbr"][li, n], 2, D)
                for q4 in range(2):
                    c0 = OFF_GATE + n * D + q4 * 512
                    wvg, wkg = self.wload(win[li][:, c0:c0 + 512], 8, 512)
                    for cc in range(4):
                        dc = q4 * 4 + cc
                        for t2 in range(2):
                            tt = th * 2 + t2
                            sl = slice(t2 * 512, (t2 + 1) * 512)
                            bg = self.proj_fm(wvg, wkg, cc * 128, 128, tt)
                            bp = self.proj_fm(wvb, wkb, dc * 128, 128, tt, src=self.yT[n], srckey="yT%d" % n, kc=2)
                            s_ = sgt[t2]
                            sk = ("sgt", t2)
                            self.A(s_, self.pb[bg][:, :], AF.Sigmoid, [("pb", bg)], [sk])
                            ak = ("accm", dc, t2)
                            if n == 0:
                                self.TT("dve", accm[:, dc, sl], self.pb[bp][:, :], s_, ALU.mult, [("pb", bp), sk], [ak])
                            else:
                                p_ = prod[t2]
                                pk = ("prod", t2)
                                self.TT("dve", p_, self.pb[bp][:, :], s_, ALU.mult, [("pb", bp), sk], [pk])
                                if n < 3:
                                    self.TT("dve", accm[:, dc, sl], accm[:, dc, sl], p_, ALU.add, [ak, pk], [ak])
                                else:
                                    self.TT("dve", mp[:, dc, sl], accm[:, dc, sl], p_, ALU.add, [ak, pk], [("mp", dc, t2)])
            wo = [self.wload(d["wout"][li][:, hh * 512:(hh + 1) * 512], 8, 512) for hh in range(2)]
            for j in range(8):
                tb = th * 8 + j
                xi = xin[tb % 2]
                xk = ("xin", tb % 2)
                self.dma(xi, res_in[tb * 128:(tb + 1) * 128, :], [("res", id(res_in), tb)], [xk])
                h = hbuf[tb % 2]
                hk = ("hbuf", tb % 2)
                for hh in range(2):
                    ob = self.accb()
                    for k in range(8):
                        self.mm(self.pb[ob][:, :], mp[:, k, j * 128:(j + 1) * 128], wo[hh][0][:, k, :], k == 0, k == 7,
                                [("mp", k, j // 4), wo[hh][1]], [("pb", ob)])
                    self.STT("dve", h[:, hh * 512:(hh + 1) * 512], xi[:, hh * 512:(hh + 1) * 512], ALPHA, self.pb[ob][:, :],
                             ALU.mult, ALU.add, [xk, ("pb", ob)], [hk])
                rt = (lambda half, b, tb=tb: route(tb, half, b)) if route else None
                o = self.layer_norm_block(h, hk, gb, tb, res_out, li, rt)
                if self.stop_after == ("mix", li):
                    self.finals.append(o)

    def layer_norm_block(self, h, hk, gb, tb, res_out, li, route):
        st = self.lnst[tb % 2]
        sk = ("lnst", tb % 2)
        junk = self.lnjunk
        self.MS("dve", st[:, 0:2], 0.0, [sk])
        self.A(junk, h, AF.Copy, [hk, sk], ["lnjunk", sk], accum=st[:, 0:1])
        self.A(junk, h, AF.Square, [hk, sk], ["lnjunk", sk], accum=st[:, 1:2])
        self.TS("dve", st[:, 2:3], st[:, 0:1], 1.0 / D, None, ALU.mult, None, [sk], [sk])
        self.TT("dve", st[:, 3:4], st[:, 2:3], st[:, 2:3], ALU.mult, [sk], [sk])
        self.STT("dve", st[:, 4:5], st[:, 1:2], 1.0 / D, st[:, 3:4], ALU.mult, ALU.subtract, [sk], [sk])
        self.A(st[:, 4:5], st[:, 4:5], AF.Sqrt, [sk], [sk], bias=LN_EPS)
        self.S.op("dve", lambda e, s_=st: e.reciprocal(out=s_[:, 5:6], in_=s_[:, 4:5]), reads=[sk], writes=[sk])
        self.STT("dve", st[:, 6:7], st[:, 2:3], -1.0, st[:, 5:6], ALU.mult, ALU.mult, [sk], [sk])
        self.A(h, h, AF.Identity, [hk, sk], [hk], scale=st[:, 5:6], bias=st[:, 6:7])
        self.TT("dve", h, h, gb[:, 0, :], ALU.mult, [hk, "lngb"], [hk])
        self.TT("dve", h, h, gb[:, 1, :], ALU.add, [hk, "lngb"], [hk])
        o = self.dma(res_out[tb * 128:(tb + 1) * 128, :], h, [hk], [("res", id(res_out), tb)])
        self.x_to_xT(h, hk, tb, rt=route)
        return o

    def make_router(self, li):
        d = self.dram
        rw = self.carve([8, NE], F32)
        self.dma(rw, d["router"].rearrange("(c p) e -> p c e", p=128), [], ["rw"])
        xf = [self.carve([512], F32) for _ in range(2)]
        lg = [self.carve([32], F32) for _ in range(2)]
        state = {}

        def route(tb, half, b):
            x_ = xf[half]
            xk = ("xf", half)
            self.CP("dve", x_, self.pb[b][:, :], [("pb", b)], [xk])
            if half == 0:
                state["bank"] = self.accb()
            rb = state["bank"]
            for c in range(4):
                k = half * 4 + c
                self.mm(self.pb[rb][:, 0:NE], x_[:, c * 128:(c + 1) * 128], rw[:, k, :], k == 0, k == 7,
                        [xk, "rw"], [("pb", rb)])
            if half == 1:
                l_ = lg[tb % 2]
                lk = ("lg", tb % 2)
                self.CP("dve", l_[:, 0:8], self.pb[rb][:, 0:NE], [("pb", rb)], [lk])
                self.S.op("dve", lambda e, l_=l_: e.max(out=l_[:, 8:16], in_=l_[:, 0:8]), reads=[lk], writes=[lk])
                self.TT("dve", l_[:, 16:17], l_[:, 9:10], l_[:, 8:9], ALU.subtract, [lk], [lk])
                self.A(l_[:, 16:17], l_[:, 16:17], AF.Exp, [lk], [lk])
                self.TS("dve", l_[:, 16:17], l_[:, 16:17], 1.0, None, ALU.add, None, [lk], [lk])
                self.S.op("dve", lambda e, l_=l_: e.reciprocal(out=l_[:, 17:18], in_=l_[:, 16:17]), reads=[lk], writes=[lk])
                self.TS("dve", l_[:, 18:19], l_[:, 8:9], -1.0, None, ALU.mult, None, [lk], [lk])
                self.A(l_[:, 24:32], l_[:, 0:8], AF.Exp, [lk], [lk], bias=l_[:, 18:19])
                self.TS("dve", l_[:, 0:8], l_[:, 0:8], l_[:, 9:10], l_[:, 17:18], ALU.is_ge, ALU.mult, [lk], [lk])
                self.TT("dve", self.gates[:, tb, :], l_[:, 0:8], l_[:, 24:32], ALU.mult, [lk], ["gates"])
        return route

    def ffn_phase(self, li, res_in, res_out):
        d = self.dram
        self.arena_reset()
        G = 1024
        moe = (li == 1)
        dff = D_FFE if moe else D_FF
        nfc = dff // 128
        hT = self.carve([nfc, G], BF16)
        facc = self.carve([8, D], F32)
        gb = self.carve([2, D], F32)
        sa = [self.carve([512], BF16) for _ in range(2)]
        pblk = [self.carve([256], F32) for _ in range(2)]
        pT = [self.carve([2, 128], BF16) for _ in range(2)]
        ple = self.carve([D], F32)
        hbuf = [self.carve([D], F32) for _ in range(2)]
        xin = self.carve([D], F32)
        self.lnst = [self.carve([8], F32) for _ in range(2)]
        self.lnjunk = self.carve([D], BF16)
        self.stage = self.stage[0:2] + [self.carve([2048], F32)]
        self.dma(gb[:, 0, :], d["ln2g"][li:li + 1, :].to_broadcast([128, D]), [], ["lngb"])
        self.dma(gb[:, 1, :], d["ln2b"][li:li + 1, :].to_broadcast([128, D]), [], ["lngb"])
        for g in range(T // G):
            experts = list(range(NE)) if moe else [None]
            for e in experts:
                w_in = d["moe_in"][e] if moe else d["ffn_in"]
                w_out = d["moe_out"][e] if moe else d["ffn_out"]
                for f0 in range(0, nfc, 4):
                    nf = min(4, nfc - f0)
                    wa, wak = self.wload(w_in[:, f0 * 128:(f0 + nf) * 128], 8, nf * 128)
                    wu, wuk = self.wload(w_in[:, dff + f0 * 128:dff + (f0 + nf) * 128], 8, nf * 128)
                    for fi in range(nf):
                        fc = f0 + fi
                        for t2 in range(G // 512):
                            tt = g * (G // 512) + t2
                            ba = self.proj_fm(wa, wak, fi * 128, 128, tt)
                            bu = self.proj_fm(wu, wuk, fi * 128, 128, tt)
                            s_ = sa[t2 % 2]
                            sk = ("sa", t2 % 2)
                            self.A(s_, self.pb[ba][:, :], AF.Silu, [("pb", ba)], [sk])
                            self.TT("dve", hT[:, fc, t2 * 512:(t2 + 1) * 512], self.pb[bu][:, :], s_, ALU.mult,
                                    [("pb", bu), sk], [("hT", fc)])
                for ps_ in range(2):
                    for f0 in range(0, nfc, 4):
                        nf = min(4, nfc - f0)
                        wo, wok = self.wload(w_out[f0 * 128:(f0 + nf) * 128, :], nf, D)
                        for fi in range(nf):
                            fc = f0 + fi
                            for j4 in range(4):
                                j = ps_ * 4 + j4
                                for hh in range(2):
                                    b = j4 * 2 + hh
                                    self.mm(self.pb[b][:, :], hT[:, fc, j * 128:(j + 1) * 128],
                                            wo[:, fi, hh * 512:(hh + 1) * 512], fc == 0, fc == nfc - 1,
                                            [("hT", fc), wok], [("pb", b)])
                    for j4 in range(4):
                        j = ps_ * 4 + j4
                        tb = g * 8 + j
                        for hh in range(2):
                            b = j4 * 2 + hh
                            dst = facc[:, j, hh * 512:(hh + 1) * 512]
                            fk = ("facc", j, hh)
                            if not moe:
                                self.CP("act" if hh == 0 else "dve", dst, self.pb[b][:, :], [("pb", b)], [fk])
                            elif e == 0:
                                self.TS("dve", dst, self.pb[b][:, :], self.gates[:, tb, e:e + 1], None, ALU.mult, None,
                                        [("pb", b), "gates"], [fk])
                            else:
                                self.STT("dve", dst, self.pb[b][:, :], self.gates[:, tb, e:e + 1], dst, ALU.mult, ALU.add,
                                         [("pb", b), "gates", fk], [fk])
            wg = [self.wload(d["pleg"][li][:, hh * 512:(hh + 1) * 512], 8, 512) for hh in range(2)]
            wp, wpk = self.wload(d["plep"][li], 2, D)
            for j in range(8):
                tb = g * 8 + j
                pb_ = pblk[j % 2]
                pk = ("pblk", j % 2)
                self.dma(pb_, d["p_in"][li, tb * 128:(tb + 1) * 128, :], [], [pk])
                b = self.short()
                for c in range(2):
                    self.tr(self.pb[b][:, c * 128:(c + 1) * 128], pb_[:, c * 128:(c + 1) * 128], [pk], [("pb", b)])
                pt = pT[j % 2]
                ptk = ("pT", j % 2)
                self.CP("act", pt, self.pb[b][:, 0:256].rearrange("p (c t) -> p c t", c=2), [("pb", b)], [ptk])
                for hh in range(2):
                    bg_ = self.short()
                    for k in range(8):
                        self.mm(self.pb[bg_][:, :], self.xT[:, k, tb * 128:(tb + 1) * 128], wg[hh][0][:, k, :], k == 0, k == 7,
                                [("xT", tb // 4), wg[hh][1]], [("pb", bg_)])
                    bp_ = self.short()
                    for c in range(2):
                        self.mm(self.pb[bp_][:, :], pt[:, c, :], wp[:, c, hh * 512:(hh + 1) * 512],
                                c == 0, c == 1, [ptk, wpk], [("pb", bp_)])
                    self.A(ple[:, hh * 512:(hh + 1) * 512], self.pb[bg_][:, :], AF.Sigmoid, [("pb", bg_)], ["ple"])
                    self.TT("dve", ple[:, hh * 512:(hh + 1) * 512], ple[:, hh * 512:(hh + 1) * 512], self.pb[bp_][:, :],
                            ALU.mult, ["ple", ("pb", bp_)], ["ple"])
                self.dma(xin, res_in[tb * 128:(tb + 1) * 128, :], [("res", id(res_in), tb)], ["xin"])
                h = hbuf[j % 2]
                hk = ("hbuf", j % 2)
                self.STT("dve", h, xin, ALPHA, ple, ALU.mult, ALU.add, ["xin", "ple"], [hk])
                self.TT("dve", h, h, facc[:, j, :], ALU.add, [hk, ("facc", j, 0), ("facc", j, 1)], [hk])
                o = self.layer_norm_block(h, hk, gb, tb, res_out, li, None)
                if li == self.layers[-1] or self.stop_after == ("ffn", li):
                    self.finals.append(o)


_IDX = _win_index()


def prepare_inputs(inputs):
    f = lambda a: np.ascontiguousarray(np.asarray(a))
    w_in = f(inputs["w_in"])
    win = np.zeros((2, D, NCOLS_R), np.float32)
    valid = _IDX >= 0
    win[:, :, valid] = w_in[:, :, _IDX[valid]]
    conv_w = f(inputs["conv_w"])
    convp = np.zeros((2, 128, 2, 34), np.float32)
    for c in range(2):
        convp[:, :, c, 0:31] = conv_w[:, :, c * 128:(c + 1) * 128].transpose(0, 2, 1)
        convp[:, :, c, 31] = f(inputs["conv_b"])[:, c * 128:(c + 1) * 128]
        convp[:, :, c, 32] = f(inputs["conv_ln_g"])[:, c * 128:(c + 1) * 128]
        convp[:, :, c, 33] = f(inputs["conv_ln_b"])[:, c * 128:(c + 1) * 128]
    pe = f(inputs["nsa_cmp_pe"])
    pe_r = np.ascontiguousarray(pe.transpose(0, 2, 3, 1).reshape(2, 128, 32))
    w2 = f(inputs["nsa_cmp_w2"])
    w2k = np.ascontiguousarray(np.concatenate([w2[:, 0], w2[:, 0]], axis=2))
    w2v = np.ascontiguousarray(w2[:, 1])
    qn = np.ascontiguousarray(f(inputs["mla_q_norm"]).reshape(2, 2, 128).transpose(0, 2, 1))
    kvn = np.ascontiguousarray(f(inputs["mla_kv_norm"]).reshape(2, 128, 1))
    wuq = f(inputs["mla_w_uq"])
    cols = []
    for h in range(4):
        b = h * 96
        cols += list(range(b, b + 96))
        cols += list(range(b, b + 64)) + list(range(b + 80, b + 96)) + list(range(b + 64, b + 80))
    uq = np.ascontiguousarray(wuq[:, :, cols])
    wukv = f(inputs["mla_w_ukv"])
    cols = []
    for h in range(4):
        cols += list(range(h * 128, h * 128 + 64))
    for h in range(4):
        cols += list(range(h * 128 + 64, h * 128 + 128))
    ukv = np.ascontiguousarray(wukv[:, :, cols])
    shared = {
        "win": win, "convp": convp, "pe_r": pe_r, "w1": f(inputs["nsa_cmp_w1"]), "w2k": w2k, "w2v": w2v,
        "qn": qn, "kvn": kvn, "uq": uq, "ukv": ukv, "wbr": f(inputs["w_branch"]), "wout": f(inputs["w_out"]),
        "ln1g": f(inputs["ln1_g"]), "ln1b": f(inputs["ln1_b"]), "ln2g": f(inputs["ln2_g"]), "ln2b": f(inputs["ln2_b"]),
        "ffn_in": f(inputs["ffn_w_in"])[0], "ffn_out": f(inputs["ffn_w_out"])[0], "router": f(inputs["moe_router"])[0],
        "moe_in": f(inputs["moe_w_in"])[0], "moe_out": f(inputs["moe_w_out"])[0],
        "pleg": f(inputs["ple_w_gate"]), "plep": f(inputs["ple_w_proj"]),
    }
    for k, v in _host_consts().items():
        shared["c_" + k] = v
    x = f(inputs["x"])
    p = f(inputs["p"])
    pos = f(inputs["positions"]).astype(np.int32)
    in_maps = []
    for b in range(8):
        m = dict(shared)
        m["x"] = x[b]
        m["p"] = np.ascontiguousarray(p[:, b])
        m["pos"] = pos[b:b + 1]
        in_maps.append(m)
    return in_maps


def kernel(**inputs):
    in_maps = prepare_inputs(inputs)
    nc = MK().build()
    res = run_bass_kernel_spmd(nc, in_maps, core_ids=list(range(8)))
    return np.stack([np.asarray(r["y"], dtype=np.float32) for r in res.results], axis=0)
```

```python
import math
import contextlib
import numpy as np
import concourse.bass as bass
import concourse.mybir as mybir
from concourse.bass_utils import run_bass_kernel_spmd

F32 = mybir.dt.float32
BF16 = mybir.dt.bfloat16
I32 = mybir.dt.int32
AF = mybir.ActivationFunctionType
ALU = mybir.AluOpType
AX = mybir.AxisListType

T = 2048
D = 1024
NB = 16
ALPHA = 4.0 ** 0.25
LN_EPS = 1e-5
RMS_EPS = 1e-6
THETA = 10000.0
D_FF = 2816
D_FFE = 3584
NE = 8

ENGS = ("pe", "act", "dve", "pool", "sp")
N_DMA_SEMS = 6


class Op:
    __slots__ = ("eng", "fn", "deps", "is_dma", "signal", "sig_val", "dma_sem", "dma_val",
                 "dma_prev", "idx")

    def __init__(self, eng, fn, is_dma):
        self.eng = eng
        self.fn = fn
        self.deps = []
        self.is_dma = is_dma
        self.signal = False
        self.sig_val = 0
        self.dma_sem = None
        self.dma_val = 0
        self.dma_prev = 0
        self.idx = 0


class Sched:
    def __init__(self):
        self.ops = {e: [] for e in ENGS}
        self.last_w = {}
        self.readers = {}
        self.all_ops = []

    def op(self, eng, fn, reads=(), writes=(), dma=False, acc=False):
        o = Op(eng, fn, dma)
        deps = []
        for k in reads:
            w = self.last_w.get(k)
            if w is not None:
                deps.append(w)
            if isinstance(k, tuple) and k[0] == "pb":
                for r in self.readers.get(k, ()):
                    if r.eng != eng:
                        deps.append(r)
        for k in writes:
            w = self.last_w.get(k)
            if w is not None and not (acc and w.eng == eng and not w.is_dma):
                deps.append(w)
            for r in self.readers.get(k, ()):
                deps.append(r)
        seen = set()
        for d in deps:
            if id(d) not in seen and d is not o:
                seen.add(id(d))
                o.deps.append(d)
        for k in reads:
            lst = self.readers.setdefault(k, [])
            if not dma:
                for i, r in enumerate(lst):
                    if r.eng == eng and not r.is_dma:
                        lst[i] = o
                        break
                else:
                    lst.append(o)
            else:
                lst.append(o)
        for k in writes:
            self.last_w[k] = o
            self.readers[k] = []
        o.idx = len(self.ops[eng])
        self.ops[eng].append(o)
        self.all_ops.append(o)
        return o

    def barrier(self):
        lasts = []
        for e in ENGS:
            ops = self.ops[e]
            nd = 0
            got_real = False
            for o in reversed(ops):
                if o.fn is None:
                    continue
                if o.is_dma:
                    if nd < N_DMA_SEMS:
                        lasts.append(o)
                        nd += 1
                elif not got_real:
                    lasts.append(o)
                    got_real = True
                if got_real and nd >= N_DMA_SEMS:
                    break
        for e in ENGS:
            o = Op(e, None, False)
            o.deps = [l for l in lasts]
            o.idx = len(self.ops[e])
            self.ops[e].append(o)
            self.all_ops.append(o)
        self.last_w = {}
        self.readers = {}

    def emit(self, nc, final_wait_ops=()):
        for fo in final_wait_ops:
            if not fo.is_dma:
                fo.signal = True
        for o in self.all_ops:
            for d in o.deps:
                if not d.is_dma:
                    d.signal = True
        cnt = {e: 0 for e in ENGS}
        for e in ENGS:
            for o in self.ops[e]:
                if o.signal and not o.is_dma:
                    cnt[e] += 1
                    o.sig_val = cnt[e]
        dma_count = {}
        for e in ENGS:
            k = 0
            for o in self.ops[e]:
                if o.is_dma:
                    j = k % N_DMA_SEMS
                    k += 1
                    key = (e, j)
                    prev = dma_count.get(key, 0)
                    o.dma_sem = key
                    o.dma_prev = prev
                    o.dma_val = prev + 16
                    dma_count[key] = prev + 16
        with contextlib.ExitStack() as st:
            sems = {e: st.enter_context(nc.semaphore("s_" + e)) for e in ENGS if cnt[e] > 0}
            dsems = {key: st.enter_context(nc.semaphore("d_%s%d" % key)) for key in dma_count}
            block = st.enter_context(nc.Block())
            regs = {"pe": block.tensor, "act": block.scalar, "dve": block.vector,
                    "pool": block.gpsimd, "sp": block.sync}

            def make(e):
                def body(eng):
                    known = {}
                    for o in self.ops[e]:
                        waits = {}
                        for d in o.deps:
                            if d.is_dma:
                                s, v = dsems[d.dma_sem], d.dma_val
                            else:
                                s, v = sems[d.eng], d.sig_val
                            kk = id(s)
                            if known.get(kk, 0) >= v:
                                continue
                            if kk not in waits or waits[kk][1] < v:
                                waits[kk] = (s, v)
                        if o.is_dma and o.dma_prev > 0:
                            s = dsems[o.dma_sem]
                            kk = id(s)
                            if known.get(kk, 0) < o.dma_prev:
                                if kk not in waits or waits[kk][1] < o.dma_prev:
                                    waits[kk] = (s, o.dma_prev)
                        for kk, (s, v) in waits.items():
                            eng.wait_ge(s, v)
                            known[kk] = v
                        if o.fn is None:
                            continue
                        ins = o.fn(eng)
                        if o.is_dma:
                            ins.then_inc(dsems[o.dma_sem], 16)
                        elif o.signal:
                            ins.then_inc(sems[e], 1)
                    if e == "sp":
                        for fo in final_wait_ops:
                            if fo.is_dma:
                                eng.wait_ge(dsems[fo.dma_sem], fo.dma_val)
                            else:
                                eng.wait_ge(sems[fo.eng], fo.sig_val)
                return body

            for e in ENGS:
                if self.ops[e] or e == "sp":
                    regs[e](make(e))


OFF_CONV, OFF_NQ, OFF_NK, OFF_MISC, OFF_KR, OFF_SB, OFF_TOK, OFF_GATE = 0, 512, 1024, 1536, 2048, 2304, 2816, 3328
NCOLS_R = 7424


def _win_index():
    sw64 = lambda b: list(range(b + 32, b + 64)) + list(range(b, b + 32))
    sw32 = lambda b: list(range(b + 16, b + 32)) + list(range(b, b + 16))
    idx = []
    idx += list(range(0, 512))
    idx += list(range(512, 768))
    for h in range(4):
        idx += sw64(512 + 64 * h)
    ks, kw = 896, 1024
    idx += list(range(ks, ks + 64)) * 2 + sw64(ks) * 2 + list(range(kw, kw + 64)) * 2 + sw64(kw) * 2
    idx += list(range(768, 896)) + list(range(1164, 1420)) + list(range(1420, 1548))
    idx += [-1] * 64 + list(range(1548, 1580)) + [-1] * 32
    idx += [-1] * 64 + sw32(1548) + [-1] * 32
    idx += list(range(1580, 1580 + 512))
    idx += list(range(960, 1024)) + list(range(1088, 1152)) + list(range(1152, 1164)) + [-1] * 116
    idx += list(range(1580 + 512, 1580 + 768))
    idx += list(range(2348, 6444))
    assert len(idx) == NCOLS_R
    return np.array(idx)


def _host_consts():
    c = {}
    c["ident"] = np.eye(128, dtype=np.float32)
    p = np.arange(128)[:, None]
    f = np.arange(128)[None, :]
    cm = np.zeros((128, 5, 128), np.float32)
    cm[:, 0] = (p <= f)
    cm[:, 1] = (p < f)
    cm[:, 2] = (p > f)
    cm[:, 3] = 1.0
    cm[:, 4] = (p == f)
    c["cmask"] = cm
    j = np.arange(128)[:, None]
    t = np.arange(T)[None, :]
    c["cmpvalid"] = ((16 * j + 31 <= t) & (j < 127)).astype(np.float32)
    n = np.arange(32)[:, None]
    c["eexp"] = ((t // 64) == n).astype(np.float32)
    jj = np.arange(127)
    nn = np.arange(32)
    ov = ((jj[:, None] * 16 < nn[None, :] * 64 + 64) & (jj[:, None] * 16 + 32 > nn[None, :] * 64)).astype(np.float32)
    ovl = np.zeros((128, 33), np.float32)
    ovl[:127, :32] = ov
    ovl[:127, 32] = 1.0
    c["ovl"] = ovl
    cur = (np.arange(T) // 64)[:, None]
    nid = np.arange(32)[None, :]
    forced = (nid == 0) | (nid == cur) | (nid == cur - 1)
    future = nid > cur
    keep = (~forced & ~future).astype(np.float32)
    add = np.where(future, -1e30, np.where(forced, 100.0, 0.0)).astype(np.float32)
    ka = np.zeros((128, 2, 16, 32), np.float32)
    ka[:, 0] = keep.reshape(16, 128, 32).transpose(1, 0, 2)
    ka[:, 1] = add.reshape(16, 128, 32).transpose(1, 0, 2)
    c["keepadd"] = ka
    rc = np.zeros((128, 4), np.float32)
    pp = np.arange(128)
    rc[:, 0] = THETA ** (-(pp % 32).astype(np.float64) / 32.0)
    rc[:, 1] = np.where((pp % 64) < 32, -1.0, 1.0)
    m = (pp >= 64) & (pp < 96)
    rc[m, 2] = THETA ** (-((pp[m] - 64) % 16).astype(np.float64) / 16.0)
    rc[m, 3] = np.where((pp[m] - 64) < 16, -1.0, 1.0)
    c["ropec"] = rc
    ind = np.zeros((128, 16, 16), np.float32)
    for kb in range(16):
        ind[:, kb, kb] = 1.0
    c["indt"] = ind
    c["iota"] = np.tile(np.arange(512, dtype=np.float32)[None, :], (128, 1))
    sg = np.zeros((16, 16, 128), np.float32)
    for kb in range(16):
        sg[kb + 1:, kb, :] = 1.0
    c["selgt"] = sg
    return c


CONST_SHAPES = {"iota": [128, 512], "ident": [128, 128], "cmask": [128, 5, 128], "cmpvalid": [128, T], "eexp": [32, T],
                "ovl": [128, 33], "keepadd": [128, 2, 16, 32], "ropec": [128, 4], "indt": [128, 16, 16],
                "selgt": [16, 16, 128]}


class MK:
    NW = 3

    def __init__(self, layers=(0, 1), debug=False, stop_after=None):
        self.layers = layers
        self.debug = debug
        self.stop_after = stop_after
        self.nc = bass.Bass("TRN2", target_bir_lowering=False)
        self.S = Sched()
        self.st = contextlib.ExitStack()
        self.st.enter_context(self.nc.allow_low_precision(reason="bf16 matmul operands / fp32 accumulation by design"))
        self.wi = 0
        self.stg_i = 0
        self.si = 0
        self.ai = 0
        self.finals = []
        self.dbg_outs = {}

    def din(self, name, shape, dt=F32):
        return self.nc.dram_tensor(name, list(shape), dt, kind="ExternalInput").ap()

    def dout(self, name, shape, dt=F32):
        return self.nc.dram_tensor(name, list(shape), dt, kind="ExternalOutput").ap()

    def sb(self, name, shape, dt):
        return self.st.enter_context(self.nc.sbuf_tensor(name, list(shape), dt))

    def arena_reset(self):
        self.S.barrier()
        self.aoff = 0

    def carve(self, shape, dt, parts=128):
        n = 1
        for s in shape:
            n *= s
        nbytes = n * (4 if dt in (F32, I32) else 2)
        nbytes = (nbytes + 63) // 64 * 64
        off = self.aoff
        self.aoff += nbytes
        assert self.aoff <= self.ARENA_BYTES, (self.aoff, self.ARENA_BYTES)
        v = self.arena[0:parts, off // 2:(off + n * (4 if dt in (F32, I32) else 2)) // 2]
        if dt != BF16:
            v = v.bitcast(dt)
        if len(shape) == 2:
            v = v.rearrange("p (a b) -> p a b", a=shape[0])
        elif len(shape) == 3:
            v = v.rearrange("p (a b c) -> p a b c", a=shape[0], b=shape[1])
        return v

    def short(self):
        b = self.si % 4
        self.si += 1
        return b

    def accb(self):
        b = 4 + self.ai % 4
        self.ai += 1
        return b

    def mm(self, out, lhsT, rhs, start, stop, r, w):
        self.S.op("pe", lambda e: e.matmul(out, lhsT=lhsT, rhs=rhs, start=start, stop=stop),
                  reads=r, writes=w, acc=True)

    def tr(self, out, in_, r, w, parts=128):
        idt = self.ident[0:parts, 0:parts]
        self.S.op("pe", lambda e: e.transpose(out, in_, idt), reads=list(r) + ["ident"], writes=w, acc=True)

    def A(self, out, in_, func, r, w, bias=None, scale=None, accum=None):
        kw = {}
        if bias is not None:
            kw["bias"] = bias
        if scale is not None:
            kw["scale"] = scale
        if accum is not None:
            kw["accum_out"] = accum
        self.S.op("act", lambda e: e.activation(out=out, in_=in_, func=func, **kw), reads=r, writes=w)

    def TT(self, eng, out, in0, in1, op, r, w):
        self.S.op(eng, lambda e: e.tensor_tensor(out=out, in0=in0, in1=in1, op=op), reads=r, writes=w)

    def TS(self, eng, out, in0, s1, s2, op0, op1, r, w):
        if op1 is None:
            self.S.op(eng, lambda e: e.tensor_scalar(out=out, in0=in0, scalar1=s1, scalar2=None, op0=op0),
                      reads=r, writes=w)
        else:
            self.S.op(eng, lambda e: e.tensor_scalar(out=out, in0=in0, scalar1=s1, scalar2=s2, op0=op0, op1=op1),
                      reads=r, writes=w)

    def STT(self, eng, out, in0, scalar, in1, op0, op1, r, w):
        self.S.op(eng, lambda e: e.scalar_tensor_tensor(out=out, in0=in0, scalar=scalar, in1=in1, op0=op0, op1=op1),
                  reads=r, writes=w)

    def CP(self, eng, out, in_, r, w):
        if eng == "act":
            self.S.op("act", lambda e: e.activation(out=out, in_=in_, func=AF.Copy), reads=r, writes=w)
        else:
            self.S.op(eng, lambda e: e.tensor_copy(out=out, in_=in_), reads=r, writes=w)

    def MS(self, eng, out, val, w):
        self.S.op(eng, lambda e: e.memset(out, val), writes=w)

    def dma(self, out, in_, r, w, eng="sp"):
        return self.S.op(eng, lambda e: e.dma_start(out=out, in_=in_), reads=r, writes=w, dma=True)

    CAST_ENGS = ("dve", "act", "dve", "act")

    def cast_load(self, dst, src, parts, free_shape, dkey):
        n = 1
        for x_ in free_shape:
            n *= x_
        assert n <= 2048, n
        si_ = self.stg_i % len(self.stage)
        ce = self.CAST_ENGS[self.stg_i % 4]
        self.stg_i += 1
        stg = self.stage[si_][0:parts, 0:n]
        if len(free_shape) == 2:
            stg = stg.rearrange("p (a b) -> p a b", a=free_shape[0])
        elif len(free_shape) == 3:
            stg = stg.rearrange("p (a b c) -> p a b c", a=free_shape[0], b=free_shape[1])
        sk = ("stg", si_)
        self.dma(stg, src, [], [sk])
        self.CP(ce, dst, stg, [sk], [dkey])

    def dma_w1(self, w1t, src, c):
        si_ = self.stg_i % len(self.stage)
        ce = self.CAST_ENGS[self.stg_i % 4]
        self.stg_i += 1
        ps_ = slice(c * 64, (c + 1) * 64)
        stg = self.stage[si_][ps_, 0:2048].rearrange("p (l e) -> p l e", l=32)
        sk = ("stg", si_)
        self.dma(stg, src.rearrange("(l d) e -> d l e", d=64), [], [sk])
        self.CP(ce, w1t[ps_, :, :], stg, [sk], ["w1t"])

    def wload(self, src, kc, n, rows=128):
        slot = self.wi % self.NW
        self.wi += 1
        assert kc * n <= 4096
        v = self.wring[slot][0:rows, 0:kc * n].rearrange("p (c n) -> p c n", c=kc)
        srcv = src.rearrange("(c p) n -> p c n", p=rows)
        key = ("w", slot)
        step = max(1, 2048 // n)
        for k0 in range(0, kc, step):
            k1 = min(kc, k0 + step)
            self.cast_load(v[:, k0:k1, :], srcv[:, k0:k1, :], rows, [k1 - k0, n], key)
        return v, key

    def proj_fm(self, wv, wkey, c0, M, tt, src=None, srckey=None, kc=8):
        b = self.short()
        src = self.xT if src is None else src
        srckey = ("xT", tt) if srckey is None else srckey
        for k in range(kc):
            self.mm(self.pb[b][0:M, :], wv[:, k, c0:c0 + M], src[:, k, tt * 512:(tt + 1) * 512],
                    k == 0, k == kc - 1, [wkey, srckey], [("pb", b)])
        return b

    def build(self):
        nc, S = self.nc, self.S
        L = self.layers
        x_in = self.din("x", [T, D])
        p_in = self.din("p", [2, T, 256])
        pos_in = self.din("pos", [1, T], I32)
        win = self.din("win", [2, D, NCOLS_R])
        convp = self.din("convp", [2, 128, 2, 34])
        pe_r = self.din("pe_r", [2, 128, 32])
        w1 = self.din("w1", [2, 2, 2048, 64])
        w2k = self.din("w2k", [2, 64, 128])
        w2v = self.din("w2v", [2, 64, 64])
        qn = self.din("qn", [2, 128, 2])
        kvn = self.din("kvn", [2, 128, 1])
        uq = self.din("uq", [2, 256, 768])
        ukv = self.din("ukv", [2, 128, 512])
        wbr = self.din("wbr", [2, 4, 256, D])
        wout = self.din("wout", [2, D, D])
        ln1g = self.din("ln1g", [2, D])
        ln1b = self.din("ln1b", [2, D])
        ln2g = self.din("ln2g", [2, D])
        ln2b = self.din("ln2b", [2, D])
        lite = self.stop_after is not None and self.stop_after[0] in ("conv", "nsa", "mla", "sb", "mix")
        need_ffn = (0 in L) and not lite
        need_moe = (1 in L) and not (lite and self.stop_after[1] == 0) and self.stop_after != ("ffn", 0)
        ffn_in = self.din("ffn_in", [D, 2 * D_FF]) if need_ffn else None
        ffn_out = self.din("ffn_out", [D_FF, D]) if need_ffn else None
        router = self.din("router", [D, NE])
        moe_in = self.din("moe_in", [NE, D, 2 * D_FFE]) if need_moe else None
        moe_out = self.din("moe_out", [NE, D_FFE, D]) if need_moe else None
        pleg = self.din("pleg", [2, D, D])
        plep = self.din("plep", [2, 256, D])
        cst = {k: self.din("c_" + k, v) for k, v in CONST_SHAPES.items()}
        y_out = self.dout("y", [T, D])
        xa = self.dout("xa", [T, D])
        xb = self.dout("xb", [T, D])
        tabs_d = self.dout("tabs_d", [2, 128, 2 * T], BF16)
        self.dram = dict(locals())

        self.xT = self.sb("xT", [128, 8, T], BF16)
        self.wring = [self.sb("wr%d" % i, [128, 4096], BF16) for i in range(self.NW)]
        self.stage = [self.sb("stg%d" % i, [128, 2048], F32) for i in range(2)]
        self.ident = self.sb("ident", [128, 128], F32)
        self.cmask = self.sb("cmask", [128, 5, 128], BF16)
        self.onesf = self.sb("onesf", [128, 128], F32)
        self.gates = self.sb("gates", [128, NB, NE], F32)
        self.pb = [self.st.enter_context(nc.psum_tensor("pb%d" % i, [128, 512], F32)) for i in range(8)]
        self.ARENA_BYTES = 133 * 1024
        self.arena = self.sb("arena", [128, self.ARENA_BYTES // 2], BF16)
        self.aoff = 0

        self.dma(self.ident[:], cst["ident"], [], ["ident"])
        self.cast_load(self.cmask[:], cst["cmask"], 128, [5, 128], "cmask")
        self.MS("dve", self.onesf[:], 1.0, ["onesf"])

        self.setup_rope(pos_in, cst)
        self.load_xT(x_in)

        res_in = x_in
        outs = [(xa, xb), (xa, y_out)]
        for li in L:
            mid, fin = outs[li]
            if li == 1:
                res_in = xb
            if self.mixer_phase(li, res_in, mid):
                break
            if self.stop_after == ("mix", li):
                break
            self.ffn_phase(li, mid, fin)
            if self.stop_after == ("ffn", li):
                break
        S.emit(nc, final_wait_ops=self.finals)
        self.st.close()
        return nc

    def setup_rope(self, pos_in, cst):
        self.arena_reset()
        self.tabN = self.carve([2, T], BF16)
        self.tabM = self.carve([2, T], BF16)
        pi = self.carve([T], I32)
        pf = self.carve([T], F32)
        ang = self.carve([T], F32)
        kf = self.carve([T], F32)
        ki = self.carve([T], I32)
        rc = self.carve([4], F32)
        self.dma(pi, pos_in[0:1, :].to_broadcast([128, T]), [], ["pi"])
        self.dma(rc, cst["ropec"], [], ["rc"])
        self.CP("dve", pf, pi, ["pi"], ["pf"])
        for tab, ic, sc in ((self.tabN, 0, 1), (self.tabM, 2, 3)):
            for which in range(2):
                shift = math.pi / 2 if which == 0 else 0.0
                self.TS("dve", ang, pf, rc[:, ic:ic + 1], shift, ALU.mult, ALU.add, ["pf", "rc"], ["ang"])
                self.TS("dve", kf, ang, 1.0 / (2 * math.pi), None, ALU.mult, None, ["ang"], ["kf"])
                self.CP("dve", ki, kf, ["kf"], ["ki"])
                self.CP("dve", kf, ki, ["ki"], ["kf"])
                self.STT("dve", ang, kf, -2 * math.pi, ang, ALU.mult, ALU.add, ["kf", "ang"], ["ang"])
                self.TS("dve", kf, ang, math.pi, -2 * math.pi, ALU.is_gt, ALU.mult, ["ang"], ["kf"])
                self.TT("dve", ang, ang, kf, ALU.add, ["ang", "kf"], ["ang"])
                self.TS("dve", kf, ang, -math.pi, 2 * math.pi, ALU.is_lt, ALU.mult, ["ang"], ["kf"])
                self.TT("dve", ang, ang, kf, ALU.add, ["ang", "kf"], ["ang"])
                self.A(ang, ang, AF.Sin, ["ang"], ["ang"])
                if which == 0:
                    self.CP("dve", tab[:, 0, :], ang, ["ang"], ["tab"])
                else:
                    self.TS("dve", tab[:, 1, :], ang, rc[:, sc:sc + 1], None, ALU.mult, None, ["ang", "rc"], ["tab"])
        td = self.dram["tabs_d"]
        self.dma(td[0], self.tabN.rearrange("p a t -> p (a t)"), ["tab"], ["tabs_d"])
        self.dma(td[1], self.tabM.rearrange("p a t -> p (a t)"), ["tab"], ["tabs_d"])

    def x_to_xT(self, xblk, xkey, tb, rt=None):
        tt = tb // 4
        for half in range(2):
            b = self.short()
            for c in range(4):
                cc = half * 4 + c
                self.tr(self.pb[b][:, c * 128:(c + 1) * 128], xblk[:, cc * 128:(cc + 1) * 128], [xkey], [("pb", b)])
            dst = self.xT[:, half * 4:half * 4 + 4, tb * 128:(tb + 1) * 128]
            src = self.pb[b][:, :].rearrange("p (c t) -> p c t", c=4)
            self.CP("act" if half == 0 else "dve", dst, src, [("pb", b)], [("xT", tt)])
            if rt is not None:
                rt(half, b)

    def load_xT(self, x_in):
        self.arena_reset()
        xbs = [self.carve([D], F32) for _ in range(2)]
        for tb in range(NB):
            xb_ = xbs[tb % 2]
            key = ("xblk", tb % 2)
            self.dma(xb_, x_in[tb * 128:(tb + 1) * 128, :], [], [key])
            self.x_to_xT(xb_, key, tb)

    def mixer_phase(self, li, res_in, res_out):
        d = self.dram
        self.arena_reset()
        self.stage = self.stage[0:2]
        self.yT = [self.carve([2, T], BF16) for _ in range(4)]
        self.mixer_base = self.aoff
        for n, (nm, fn) in enumerate((("conv", self.conv_branch), ("nsa", self.nsa_branch), ("mla", self.mla_branch),
                                     ("sb", self.sb_branch))):
            only = getattr(self, "only", None)
            if only is None or nm in only:
                fn(li)
                self.dbg("yT%d_%d" % (n, li), self.yT[n], [128, 2, T], ["yT%d" % n])
            self.aoff = self.mixer_base
            self.S.barrier()
            if self.stop_after == (nm, li):
                return True
        self.merge_ln1(li, res_in, res_out)

    def dbg(self, name, ap, shape, keys, dt=BF16):
        if not self.debug:
            return
        o = self.dout("dbg_" + name, shape, dt)
        self.finals.append(self.dma(o, ap, keys, []))

    def conv_branch(self, li):
        d = self.dram
        win = d["win"]
        cp = self.carve([2, 34], F32)
        self.dma(cp, d["convp"][li], [], ["cp"])
        hp = self.carve([2, 30 + T], F32)
        acc = self.carve([2, T], F32)
        sig = [self.carve([512], F32) for _ in range(2)]
        self.MS("pool", hp[:, :, 0:30], 0.0, ["hp"])
        wv, wk = self.wload(win[li][:, OFF_CONV:OFF_CONV + 512], 8, 512)
        for tt in range(4):
            for c in range(2):
                ba = self.proj_fm(wv, wk, c * 128, 128, tt)
                bg = self.proj_fm(wv, wk, 256 + c * 128, 128, tt)
                sg = sig[c]
                self.A(sg, self.pb[bg][:, :], AF.Sigmoid, [("pb", bg)], [("sig", c)])
                self.TT("dve", hp[:, c, 30 + tt * 512:30 + (tt + 1) * 512], self.pb[ba][:, :], sg, ALU.mult,
                        [("pb", ba), ("sig", c)], ["hp"])
        for c in range(2):
            eng = "dve"
            self.TS(eng, acc[:, c, :], hp[:, c, 0:T], cp[:, c, 0:1], cp[:, c, 31:32], ALU.mult, ALU.add,
                    ["hp", "cp"], [("acc", c)])
            for w in range(1, 31):
                self.STT(eng, acc[:, c, :], hp[:, c, w:w + T], cp[:, c, w:w + 1], acc[:, c, :], ALU.mult, ALU.add,
                         ["hp", "cp", ("acc", c)], [("acc", c)])
        sq = [self.carve([512], F32) for _ in range(2)]
        m2 = self.carve([512], F32)
        rstd = self.carve([512], F32)
        dd = [self.carve([512], F32) for _ in range(2)]
        for tt in range(4):
            sl = slice(tt * 512, (tt + 1) * 512)
            bm = self.short()
            for c in range(2):
                self.mm(self.pb[bm][:, :], self.onesf[:, :], acc[:, c, sl], c == 0, c == 1,
                        ["onesf", ("acc", c)], [("pb", bm)])
            bq = self.short()
            for c in range(2):
                self.A(sq[c], acc[:, c, sl], AF.Square, [("acc", c)], [("sq", c)])
            for c in range(2):
                self.mm(self.pb[bq][:, :], self.onesf[:, :], sq[c], c == 0, c == 1,
                        ["onesf", ("sq", c)], [("pb", bq)])
            self.A(m2, self.pb[bm][:, :], AF.Square, [("pb", bm)], ["m2"], scale=1.0 / 256)
            self.STT("dve", rstd, self.pb[bq][:, :], 1.0 / 256, m2, ALU.mult, ALU.subtract, [("pb", bq), "m2"], ["rstd"])
            self.A(rstd, rstd, AF.Sqrt, ["rstd"], ["rstd"], bias=LN_EPS)
            self.S.op("dve", lambda e, r=rstd: e.reciprocal(out=r, in_=r), reads=["rstd"], writes=["rstd"])
            for c in range(2):
                self.STT("dve", dd[c], self.pb[bm][:, :], -1.0 / 256, acc[:, c, sl], ALU.mult, ALU.add,
                         [("pb", bm), ("acc", c)], [("dd", c)])
                self.TT("dve", dd[c], dd[c], rstd, ALU.mult, [("dd", c), "rstd"], [("dd", c)])
                self.A(self.yT[0][:, c, sl], dd[c], AF.Silu, [("dd", c), "cp"], ["yT0"],
                       scale=cp[:, c, 32:33], bias=cp[:, c, 33:34])

    def y_to_yT(self, ytile, ykey, n, qb):
        b = self.short()
        for c in range(2):
            self.tr(self.pb[b][:, c * 128:(c + 1) * 128], ytile[:, c * 128:(c + 1) * 128], [ykey], [("pb", b)])
        self.CP("act", self.yT[n][:, :, qb * 128:(qb + 1) * 128],
                self.pb[b][:, 0:256].rearrange("p (c t) -> p c t", c=2), [("pb", b)], ["yT%d" % n])

    def softmax_attn_block(self, qb, kbs, kT_fn, qT_ap, v_fn, scale, extra_mask=None, nm=""):
        ob = self.accb()
        n = len(kbs)
        for g0 in range(0, n, 4):
            grp = kbs[g0:g0 + 4]
            sbk = self.short()
            for i, (kb, mt) in enumerate(grp):
                kT, kkeys = kT_fn(kb)
                self.mm(self.pb[sbk][:, i * 128:(i + 1) * 128], kT, qT_ap[0], True, True,
                        list(kkeys) + list(qT_ap[1]), [("pb", sbk)])
            ei = self.ei % 3
            self.ei += 1
            E = self.Ebuf[ei]
            ek = ("E", ei)
            w = len(grp) * 128
            self.A(E[:, 0:w], self.pb[sbk][:, 0:w], AF.Exp, [("pb", sbk)], [ek], scale=scale)
            if extra_mask is not None:
                mtile, mkey = extra_mask
                self.TT("pool", E[:, 0:w], E[:, 0:w], mtile[:, g0 * 128:g0 * 128 + w], ALU.mult, [ek, mkey], [ek])
            else:
                for i, (kb, mt) in enumerate(grp):
                    if mt is not None:
                        mi = {"le": 0, "lt": 1, "gt": 2}[mt]
                        self.TT("pool", E[:, i * 128:(i + 1) * 128], E[:, i * 128:(i + 1) * 128],
                                self.cmask[:, mi, :], ALU.mult, [ek, "cmask"], [ek])
            for i, (kb, mt) in enumerate(grp):
                v, vkeys = v_fn(kb)
                gi = g0 + i
                self.mm(self.pb[ob][:, 0:65], E[:, i * 128:(i + 1) * 128], v, gi == 0, gi == n - 1,
                        [ek] + list(vkeys), [("pb", ob)])
        return ob

    def nsa_branch(self, li):
        d = self.dram
        win = d["win"]
        cst = {k: d["cst"][k] for k in d["cst"]}
        self.tabN = self.carve([2, T], BF16)
        self.dma(self.tabN.rearrange("p a t -> p (a t)"), d["tabs_d"][0], [], ["tab"])
        QT = self.carve([2, T], BF16)
        QR = self.carve([2, T], BF16)
        KS = self.carve([T], BF16)
        KW = self.carve([T], BF16)
        KCV = self.carve([T], BF16)
        vS = self.carve([NB, 65], BF16)
        vW = self.carve([NB, 65], BF16)
        sg = self.carve([NB, 12], F32)
        cmpvalid = self.carve([T], BF16)
        eexp = self.carve([T], BF16, parts=32)
        keepadd = self.carve([2, NB, 32], F32)
        VC = self.carve([97], BF16)
        w1t = self.carve([32, 64], BF16)
        pet = self.carve([32], BF16)
        w2kt = self.carve([128], BF16, parts=64)
        w2vt = self.carve([64], BF16, parts=64)
        hid = [self.carve([127], BF16, parts=64) for _ in range(2)]
        hb = self.carve([2], F32, parts=64)
        kcT = self.carve([127], BF16)
        t1 = [self.carve([512], F32) for _ in range(2)]
        t2 = [self.carve([512], F32) for _ in range(2)]
        self.Ebuf = [self.carve([512], BF16) for _ in range(3)]
        self.ei = 0
        ytile = [self.carve([256], F32) for _ in range(2)]
        selmask = [self.carve([NB * 128], BF16) for _ in range(2)]
        small = [self.carve([64], F32) for _ in range(2)]
        imp = [self.carve([32], F32) for _ in range(2)]
        scr = [self.carve([32], F32) for _ in range(2)]
        selT = [self.carve([128], BF16, parts=32) for _ in range(2)]
        self.cast_load(cmpvalid, cst["cmpvalid"], 128, [T], "cmpvalid")
        self.cast_load(eexp, cst["eexp"], 32, [T], "eexp")
        self.dma(keepadd, cst["keepadd"], [], ["keepadd"])
        self.cast_load(VC[:, 64:97], cst["ovl"], 128, [33], "VCc")
        for c in range(2):
            self.dma_w1(w1t, d["w1"][li, c], c)
        self.cast_load(pet, d["pe_r"][li], 128, [32], "pet")
        self.cast_load(w2kt, d["w2k"][li], 64, [128], "w2kt")
        self.cast_load(w2vt, d["w2v"][li], 64, [64], "w2vt")
        self.MS("dve", vS[:, :, 64:65], 1.0, ["vS"])
        self.MS("dve", vW[:, :, 64:65], 1.0, ["vW"])
        wv, wk = self.wload(win[li][:, OFF_NQ:OFF_NQ + 512], 8, 512)
        for tt in range(4):
            sl = slice(tt * 512, (tt + 1) * 512)
            for c in range(2):
                b0 = self.proj_fm(wv, wk, c * 128, 128, tt)
                b1 = self.proj_fm(wv, wk, 256 + c * 128, 128, tt)
                self.CP("act", QT[:, c, sl], self.pb[b0][:, :], [("pb", b0)], ["QT"])
                self.TT("dve", t1[c], self.pb[b0][:, :], self.tabN[:, 0, sl], ALU.mult, [("pb", b0), "tab"], [("t1", c)])
                self.TT("dve", t2[c], self.pb[b1][:, :], self.tabN[:, 1, sl], ALU.mult, [("pb", b1), "tab"], [("t2", c)])
                self.TT("pool", QR[:, c, sl], t1[c], t2[c], ALU.add, [("t1", c), ("t2", c)], ["QR"])
        wv, wk = self.wload(win[li][:, OFF_NK:OFF_NK + 512], 8, 512)
        for tt in range(4):
            sl = slice(tt * 512, (tt + 1) * 512)
            for c, dst, dk in ((0, KS, "KS"), (1, KW, "KW")):
                b0 = self.proj_fm(wv, wk, c * 256, 128, tt)
                b1 = self.proj_fm(wv, wk, c * 256 + 128, 128, tt)
                self.TT("dve", t1[c], self.pb[b0][:, :], self.tabN[:, 0, sl], ALU.mult, [("pb", b0), "tab"], [("t1", c)])
                self.TT("dve", t2[c], self.pb[b1][:, :], self.tabN[:, 1, sl], ALU.mult, [("pb", b1), "tab"], [("t2", c)])
                self.TT("pool", dst[:, sl], t1[c], t2[c], ALU.add, [("t1", c), ("t2", c)], [dk])
        wvm, wkm = self.wload(win[li][:, OFF_MISC:OFF_MISC + 128], 8, 128)
        for tt in range(4):
            sl = slice(tt * 512, (tt + 1) * 512)
            b0 = self.proj_fm(wvm, wkm, 0, 128, tt)
            self.CP("act", KCV[:, sl], self.pb[b0][:, :], [("pb", b0)], ["KCV"])
        wvt, wkt = self.wload(win[li][:, OFF_TOK:OFF_TOK + 140], 8, 140)
        for tb in range(NB):
            b = self.short()
            for k in range(8):
                self.mm(self.pb[b][:, 0:140], self.xT[:, k, tb * 128:(tb + 1) * 128], wvt[:, k, :], k == 0, k == 7,
                        [wkt, ("xT", tb // 4)], [("pb", b)])
            self.CP("act", vS[:, tb, 0:64], self.pb[b][:, 0:64], [("pb", b)], ["vS"])
            self.CP("dve", vW[:, tb, 0:64], self.pb[b][:, 64:128], [("pb", b)], ["vW"])
            self.A(sg[:, tb, :], self.pb[b][:, 128:140], AF.Sigmoid, [("pb", b)], ["sg"])
        for c in range(2):
            ps_ = slice(c * 64, (c + 1) * 64)
            b = self.short()
            for l in range(32):
                self.mm(self.pb[b][0:64, 0:127], w1t[ps_, l, :], KCV[ps_, l:l + 16 * 126 + 1:16], l == 0, l == 31,
                        ["w1t", "KCV"], [("pb", b)])
            b2 = self.short()
            for l in range(32):
                self.mm(self.pb[b2][0:64, 0:1], w1t[ps_, l, :], pet[ps_, l:l + 1], l == 0, l == 31,
                        ["w1t", "pet"], [("pb", b2)])
            self.CP("dve", hb[:, c:c + 1], self.pb[b2][0:64, 0:1], [("pb", b2)], ["hb"])
            self.A(hid[c], self.pb[b][0:64, 0:127], AF.Gelu_apprx_tanh, [("pb", b), "hb"], [("hid", c)],
                   bias=hb[:, c:c + 1])
        b = self.short()
        self.mm(self.pb[b][:, 0:127], w2kt[:, :], hid[0], True, True, ["w2kt", ("hid", 0)], [("pb", b)])
        self.CP("act", kcT, self.pb[b][:, 0:127], [("pb", b)], ["kcT"])
        b = self.short()
        self.mm(self.pb[b][0:127, 0:64], hid[1], w2vt[:, :], True, True, ["w2vt", ("hid", 1)], [("pb", b)])
        self.CP("act", VC[0:127, 0:64], self.pb[b][0:127, 0:64], [("pb", b)], ["VCv"])
        for qb in range(NB):
            qs = slice(qb * 128, (qb + 1) * 128)
            yt = ytile[qb % 2]
            yk = ("yt", qb % 2)
            sm = small[qb % 2]
            smk = ("small", qb % 2)
            im = imp[qb % 2]
            imk = ("imp", qb % 2)
            sbk2 = [self.short(), self.short()]
            for h in range(4):
                hp_ = slice((h % 2) * 64, (h % 2) * 64 + 64)
                bb_ = sbk2[h % 2]
                self.mm(self.pb[bb_][0:127, (h // 2) * 128:(h // 2 + 1) * 128], kcT[hp_, :], QT[hp_, h // 2, qs], True, True,
                        ["kcT", "QT"], [("pb", bb_)])
            ei = self.ei % 3
            self.ei += 1
            E = self.Ebuf[ei]
            ek = ("E", ei)
            for h in range(4):
                bb_ = sbk2[h % 2]
                self.A(E[0:127, h * 128:(h + 1) * 128], self.pb[bb_][0:127, (h // 2) * 128:(h // 2 + 1) * 128], AF.Exp,
                       [("pb", bb_)], [ek], scale=0.125)
            for h in range(4):
                self.TT("pool", E[0:127, h * 128:(h + 1) * 128], E[0:127, h * 128:(h + 1) * 128], cmpvalid[0:127, qs],
                        ALU.mult, [ek, "cmpvalid"], [ek])
            ob = self.accb()
            for h in range(4):
                self.mm(self.pb[ob][:, h * 97:(h + 1) * 97], E[0:127, h * 128:(h + 1) * 128], VC[0:127, :], True, True,
                        [ek, "VCv", "VCc"], [("pb", ob)])
            P = self.pb[ob]
            for h in range(4):
                self.TS("dve", sm[:, h:h + 1], P[:, 97 * h + 96:97 * h + 97], 1e-30, None, ALU.max, None, [("pb", ob)], [smk])
            self.S.op("dve", lambda e, s_=sm: e.reciprocal(out=s_[:, 0:4], in_=s_[:, 0:4]), reads=[smk], writes=[smk])
            for h in range(4):
                if h == 0:
                    self.TS("dve", im, P[:, 64:96], sm[:, 0:1], None, ALU.mult, None, [("pb", ob), smk], [imk])
                else:
                    self.STT("dve", im, P[:, 97 * h + 64:97 * h + 96], sm[:, h:h + 1], im, ALU.mult, ALU.add,
                             [("pb", ob), smk, imk], [imk])
            for h in range(4):
                self.TT("dve", sm[:, 4 + h:5 + h], sm[:, h:h + 1], sg[:, qb, 3 * h:3 * h + 1], ALU.mult, [smk, "sg"], [smk])
                self.A(yt[:, h * 64:(h + 1) * 64], P[:, 97 * h:97 * h + 64], AF.Copy, [("pb", ob), smk], [yk],
                       scale=sm[:, 4 + h:5 + h])
            mk = None
            if qb >= 8:
                sc_ = scr[qb % 2]
                sck = ("scr", qb % 2)
                self.TT("dve", im, im, keepadd[:, 0, qb, :], ALU.mult, [imk, "keepadd"], [imk])
                self.TT("dve", im, im, keepadd[:, 1, qb, :], ALU.add, [imk, "keepadd"], [imk])
                self.S.op("dve", lambda e, s_=sm, i_=im: e.max(out=s_[:, 8:16], in_=i_), reads=[imk, smk], writes=[smk])
                self.S.op("dve", lambda e, s_=sm, i_=im, c_=sc_: e.match_replace(out=c_, in_to_replace=s_[:, 8:16],
                                                                                in_values=i_, imm_value=-1e30),
                          reads=[imk, smk], writes=[sck])
                self.S.op("dve", lambda e, s_=sm, c_=sc_: e.max(out=s_[:, 16:24], in_=c_), reads=[sck, smk], writes=[smk])
                self.TS("dve", sc_, im, sm[:, 23:24], None, ALU.is_ge, None, [imk, smk], [sck])
                b = self.short()
                self.tr(self.pb[b][0:32, 0:128], sc_, [sck], [("pb", b)])
                sT = selT[qb % 2]
                stk = ("selT", qb % 2)
                self.CP("act", sT, self.pb[b][0:32, 0:128], [("pb", b)], [stk])
                smt = selmask[qb % 2]
                mk = ("selmask", qb % 2)
                for g0 in range(0, qb + 1, 4):
                    n = min(4, qb + 1 - g0)
                    b = self.short()
                    for i in range(n):
                        kb = g0 + i
                        self.mm(self.pb[b][:, i * 128:(i + 1) * 128], eexp[:, kb * 128:(kb + 1) * 128], sT, True, True,
                                ["eexp", stk], [("pb", b)])
                    self.CP("act", smt[:, g0 * 128:(g0 + n) * 128], self.pb[b][:, 0:n * 128], [("pb", b)], [mk])
                self.TT("pool", smt[:, qb * 128:(qb + 1) * 128], smt[:, qb * 128:(qb + 1) * 128], self.cmask[:, 0, :],
                        ALU.mult, [mk, "cmask"], [mk])
            for h in range(4):
                hp_ = slice((h % 2) * 64, (h % 2) * 64 + 64)
                qT_ap = (QR[hp_, h // 2, qs], ["QR"])
                for br, KT, kkey, V, vkey, gcol in ((1, KS, "KS", vS, "vS", 3 * h + 1), (2, KW, "KW", vW, "vW", 3 * h + 2)):
                    if br == 1:
                        kbs = [(kb, "le" if kb == qb else None) for kb in range(qb + 1)]
                        em = (selmask[qb % 2], mk) if qb >= 8 else None
                    else:
                        kbs = [(kb, "le" if kb == qb else ("gt" if kb == qb - 4 else None))
                               for kb in range(max(0, qb - 4), qb + 1)]
                        em = None
                    ob = self.softmax_attn_block(
                        qb, kbs, lambda kb, KT=KT, kkey=kkey: (KT[hp_, kb * 128:(kb + 1) * 128], [kkey]), qT_ap,
                        lambda kb, V=V, vkey=vkey: (V[:, kb, :], [vkey]), 0.125, extra_mask=em)
                    fk = ("fac", qb % 2)
                    fac = sm[:, 24 + 2 * h + (br - 1):25 + 2 * h + (br - 1)]
                    self.S.op("dve", lambda e, f_=fac, o_=self.pb[ob][:, 64:65]: e.reciprocal(out=f_, in_=o_),
                              reads=[("pb", ob), smk], writes=[smk])
                    self.TT("dve", fac, fac, sg[:, qb, gcol:gcol + 1], ALU.mult, [smk, "sg"], [smk])
                    self.STT("dve", yt[:, h * 64:(h + 1) * 64], self.pb[ob][:, 0:64], fac, yt[:, h * 64:(h + 1) * 64],
                             ALU.mult, ALU.add, [("pb", ob), smk, yk], [yk])
            self.y_to_yT(yt, yk, 1, qb)

    def mla_branch(self, li):
        d = self.dram
        win = d["win"]
        self.tabM = self.carve([2, T], BF16)
        self.dma(self.tabM.rearrange("p a t -> p (a t)"), d["tabs_d"][1], [], ["tab"])
        qg = self.carve([2, T], BF16)
        kvg = self.carve([T], BF16)
        CS = self.carve([2, T], BF16)
        rkv = self.carve([T], BF16)
        rkt = self.carve([NB], F32)
        Vm = self.carve([NB, 4, 65], BF16)
        QH = [self.carve([T], BF16) for _ in range(2)]
        KH = [self.carve([T], BF16) for _ in range(2)]
        KR = self.carve([T], BF16)
        nrm = self.carve([4], F32)
        sq = [self.carve([512], F32) for _ in range(2)]
        t1 = [self.carve([512], F32) for _ in range(2)]
        t2 = [self.carve([512], F32) for _ in range(2)]
        rs = self.carve([512], F32)
        self.Ebuf = [self.carve([512], BF16) for _ in range(3)]
        self.ei = 0
        self.ymla = self.carve([NB, 256], F32)
        small = [self.carve([8], F32) for _ in range(2)]
        self.dma(nrm[:, 0:2], d["qn"][li], [], ["nrm"])
        self.dma(nrm[:, 2:3], d["kvn"][li], [], ["nrm"])
        self.MS("dve", Vm[:, :, :, 64:65], 1.0, ["Vm"])
        wvm, wkm = self.wload(win[li][:, OFF_MISC + 128:OFF_MISC + 512], 8, 384)
        for tt in range(4):
            sl = slice(tt * 512, (tt + 1) * 512)
            bq = []
            for c in range(2):
                b0 = self.proj_fm(wvm, wkm, c * 128, 128, tt)
                bq.append(b0)
                self.A(qg[:, c, sl], self.pb[b0][:, :], AF.Copy, [("pb", b0), "nrm"], ["qg"], scale=nrm[:, c:c + 1])
                self.A(sq[c], self.pb[b0][:, :], AF.Square, [("pb", b0)], [("sq", c)])
            bs = self.short()
            for c in range(2):
                self.mm(self.pb[bs][:, :], self.onesf[:, :], sq[c], c == 0, c == 1, ["onesf", ("sq", c)], [("pb", bs)])
            self.A(rs, self.pb[bs][:, :], AF.Sqrt, [("pb", bs)], ["rs"], scale=1.0 / 256, bias=RMS_EPS)
            self.S.op("dve", lambda e, r=rs: e.reciprocal(out=r, in_=r), reads=["rs"], writes=["rs"])
            for w in range(2):
                self.TT("dve", CS[:, w, sl], self.tabM[:, w, sl], rs, ALU.mult, ["tab", "rs"], ["CS"])
            b0 = self.proj_fm(wvm, wkm, 256, 128, tt)
            self.A(kvg[:, sl], self.pb[b0][:, :], AF.Copy, [("pb", b0), "nrm"], ["kvg"], scale=nrm[:, 2:3])
            self.A(sq[0], self.pb[b0][:, :], AF.Square, [("pb", b0)], [("sq", 0)])
            bs = self.short()
            self.mm(self.pb[bs][:, :], self.onesf[:, :], sq[0], True, True, ["onesf", ("sq", 0)], [("pb", bs)])
            self.A(rs, self.pb[bs][:, :], AF.Sqrt, [("pb", bs)], ["rs"], scale=1.0 / 128, bias=RMS_EPS)
            self.S.op("dve", lambda e, r=rs, o=rkv[:, sl]: e.reciprocal(out=o, in_=r), reads=["rs"], writes=["rkv"])
            bt = self.short()
            for i in range(4):
                self.mm(self.pb[bt][:, i:i + 1], sq[0][:, i * 128:(i + 1) * 128], self.onesf[:, 0:1], True, True,
                        [("sq", 0), "onesf"], [("pb", bt)])
            self.A(rkt[:, tt * 4:tt * 4 + 4], self.pb[bt][:, 0:4], AF.Sqrt, [("pb", bt)], ["rkt"], scale=1.0 / 128,
                   bias=RMS_EPS)
        self.S.op("dve", lambda e: e.reciprocal(out=rkt, in_=rkt), reads=["rkt"], writes=["rkt"])
        wvr, wkr = self.wload(win[li][:, OFF_KR:OFF_KR + 256], 8, 256)
        r9 = slice(64, 96)
        for tt in range(4):
            sl = slice(tt * 512, (tt + 1) * 512)
            b0 = self.proj_fm(wvr, wkr, 0, 96, tt)
            b1 = self.proj_fm(wvr, wkr, 128, 96, tt)
            self.TT("dve", t1[0][r9, :], self.pb[b0][r9, :], self.tabM[r9, 0, sl], ALU.mult, [("pb", b0), "tab"], [("t1", 0)])
            self.TT("dve", t2[0][r9, :], self.pb[b1][r9, :], self.tabM[r9, 1, sl], ALU.mult, [("pb", b1), "tab"], [("t2", 0)])
            self.TT("pool", KR[r9, sl], t1[0][r9, :], t2[0][r9, :], ALU.add, [("t1", 0), ("t2", 0)], ["KR"])
        wvu, wku = self.wload(d["ukv"][li], 1, 512)
        for tb in range(NB):
            b = self.short()
            self.mm(self.pb[b][:, 0:256], kvg[:, tb * 128:(tb + 1) * 128], wvu[:, 0, 256:512], True, True,
                    ["kvg", wku], [("pb", b)])
            self.A(Vm[:, tb, :, 0:64], self.pb[b][:, 0:256].rearrange("p (h c) -> p h c", h=4), AF.Copy,
                   [("pb", b), "rkt"], ["Vm"], scale=rkt[:, tb:tb + 1])
        wvq, wkq = self.wload(d["uq"][li], 2, 768)
        scale = 96.0 ** -0.5
        for h in range(4):
            Q = QH[h % 2]
            K = KH[h % 2]
            qk = ("QH", h % 2)
            kk = ("KH", h % 2)
            for tt in range(4):
                sl = slice(tt * 512, (tt + 1) * 512)
                ba = self.proj_fm(wvq, wkq, (2 * h) * 96, 96, tt, src=qg, srckey="qg", kc=2)
                bb = self.proj_fm(wvq, wkq, (2 * h + 1) * 96, 96, tt, src=qg, srckey="qg", kc=2)
                c = tt % 2
                self.TT("dve", t1[c][0:96, :], self.pb[ba][0:96, :], CS[0:96, 0, sl], ALU.mult, [("pb", ba), "CS"], [("t1", c)])
                self.TT("dve", t2[c][0:96, :], self.pb[bb][0:96, :], CS[0:96, 1, sl], ALU.mult, [("pb", bb), "CS"], [("t2", c)])
                self.TT("pool", Q[0:96, sl], t1[c][0:96, :], t2[c][0:96, :], ALU.add, [("t1", c), ("t2", c)], [qk])
                bk = self.short()
                self.mm(self.pb[bk][0:64, :], wvu[:, 0, h * 64:(h + 1) * 64], kvg[:, sl], True, True, [wku, "kvg"], [("pb", bk)])
                self.TT("dve", K[0:64, sl], self.pb[bk][0:64, :], rkv[0:64, sl], ALU.mult, [("pb", bk), "rkv"], [kk])
            self.CP("pool", K[r9, :], KR[r9, :], ["KR"], [kk])
            for qb in range(NB):
                qs = slice(qb * 128, (qb + 1) * 128)
                sm = small[qb % 2]
                smk = ("small", qb % 2)
                kbs = [(kb, "le" if kb == qb else None) for kb in range(qb + 1)]
                ob = self.softmax_attn_block(
                    qb, kbs, lambda kb: (K[0:96, kb * 128:(kb + 1) * 128], [kk]), (Q[0:96, qs], [qk]),
                    lambda kb: (Vm[:, kb, h, :], ["Vm"]), scale)
                self.S.op("dve", lambda e, s_=sm, o_=self.pb[ob][:, 64:65]: e.reciprocal(out=s_[:, 0:1], in_=o_),
                          reads=[("pb", ob)], writes=[smk])
                self.A(self.ymla[:, qb, h * 64:(h + 1) * 64], self.pb[ob][:, 0:64], AF.Copy, [("pb", ob), smk],
                       [("ymla", qb)], scale=sm[:, 0:1])
        for qb in range(NB):
            self.y_to_yT(self.ymla[:, qb, :], ("ymla", qb), 2, qb)

    def sb_branch(self, li):
        d = self.dram
        win = d["win"]
        cst = d["cst"]
        QT = self.carve([2, T], BF16)
        KT = self.carve([2, T], BF16)
        V = self.carve([NB, 256], BF16)
        indt = self.carve([16, 16], BF16)
        selgt = self.carve([16, 128], BF16, parts=16)
        sp = [self.carve([NB * 128], F32) for _ in range(2)]
        lk = [self.carve([NB * 128], BF16) for _ in range(2)]
        ex = [self.carve([512], F32) for _ in range(2)]
        ar = [self.carve([512], F32) for _ in range(2)]
        aa = [self.carve([512], BF16) for _ in range(3)]
        ts_ = [self.carve([128], BF16, parts=16) for _ in range(2)]
        ytile = [self.carve([256], F32) for _ in range(2)]
        self.cast_load(indt, cst["indt"], 128, [16, 16], "indt")
        self.cast_load(selgt, cst["selgt"], 16, [16, 128], "selgt")
        wv, wk = self.wload(win[li][:, OFF_SB:OFF_SB + 512], 8, 512)
        for tt in range(4):
            sl = slice(tt * 512, (tt + 1) * 512)
            for c in range(2):
                b0 = self.proj_fm(wv, wk, c * 128, 128, tt)
                self.CP("act", QT[:, c, sl], self.pb[b0][:, :], [("pb", b0)], ["QT"])
                b1 = self.proj_fm(wv, wk, 256 + c * 128, 128, tt)
                self.CP("dve", KT[:, c, sl], self.pb[b1][:, :], [("pb", b1)], ["KT"])
        wvt, wkt = self.wload(win[li][:, OFF_TOK + 256:OFF_TOK + 512], 8, 256)
        for tb in range(NB):
            b = self.short()
            for k in range(8):
                self.mm(self.pb[b][:, 0:256], self.xT[:, k, tb * 128:(tb + 1) * 128], wvt[:, k, :], k == 0, k == 7,
                        [wkt, ("xT", tb // 4)], [("pb", b)])
            self.CP("act", V[:, tb, :], self.pb[b][:, 0:256], [("pb", b)], ["V"])
        it = 0
        ai_ = 0
        for qb in range(NB):
            qs = slice(qb * 128, (qb + 1) * 128)
            yt = ytile[qb % 2]
            yk = ("yt", qb % 2)
            for h in range(4):
                hp_ = slice((h % 2) * 64, (h % 2) * 64 + 64)
                spt, lkt, tst = sp[it % 2], lk[it % 2], ts_[it % 2]
                spk, lkk, tsk = ("sp", it % 2), ("lk", it % 2), ("ts", it % 2)
                it += 1
                nk = qb + 1
                for g0 in range(0, nk, 4):
                    n = min(4, nk - g0)
                    w = n * 128
                    sbk = self.short()
                    for i in range(n):
                        kb = g0 + i
                        self.mm(self.pb[sbk][:, i * 128:(i + 1) * 128], KT[hp_, h // 2, kb * 128:(kb + 1) * 128],
                                QT[hp_, h // 2, qs], True, True, ["KT", "QT"], [("pb", sbk)])
                    e_ = ex[(g0 // 4) % 2]
                    exk = ("ex", (g0 // 4) % 2)
                    self.A(e_[:, 0:w], self.pb[sbk][:, 0:w], AF.Exp, [("pb", sbk)], [exk], scale=-0.125)
                    self.A(spt[:, g0 * 128:g0 * 128 + w], e_[:, 0:w], AF.Ln, [exk], [spk], bias=1.0)
                    self.STT("dve", lkt[:, g0 * 128:g0 * 128 + w], self.pb[sbk][:, 0:w], -0.125, spt[:, g0 * 128:g0 * 128 + w],
                             ALU.mult, ALU.subtract, [("pb", sbk), spk], [lkk])
                self.TT("pool", lkt[:, qb * 128:(qb + 1) * 128], lkt[:, qb * 128:(qb + 1) * 128], self.cmask[:, 1, :],
                        ALU.mult, [lkk, "cmask"], [lkk])
                bts = self.short()
                for kb in range(nk):
                    self.mm(self.pb[bts][0:16, 0:128], indt[:, kb, :], lkt[:, kb * 128:(kb + 1) * 128], kb == 0, kb == nk - 1,
                            ["indt", lkk], [("pb", bts)])
                self.CP("act", tst, self.pb[bts][0:16, 0:128], [("pb", bts)], [tsk])
                ob = self.accb()
                for g0 in range(0, nk, 4):
                    n = min(4, nk - g0)
                    w = n * 128
                    lb = self.short()
                    for i in range(n):
                        kb = g0 + i
                        self.mm(self.pb[lb][:, i * 128:(i + 1) * 128], self.cmask[:, 2, :], lkt[:, kb * 128:(kb + 1) * 128],
                                True, False, ["cmask", lkk], [("pb", lb)])
                        self.mm(self.pb[lb][:, i * 128:(i + 1) * 128], selgt[:, kb, :], tst, False, True,
                                ["selgt", tsk], [("pb", lb)])
                    a_ = ar[(g0 // 4) % 2]
                    ark = ("ar", (g0 // 4) % 2)
                    self.TT("dve", a_[:, 0:w], self.pb[lb][:, 0:w], spt[:, g0 * 128:g0 * 128 + w], ALU.subtract,
                            [("pb", lb), spk], [ark])
                    at = aa[ai_ % 3]
                    ak = ("aa", ai_ % 3)
                    ai_ += 1
                    self.A(at[:, 0:w], a_[:, 0:w], AF.Exp, [ark], [ak])
                    if g0 + n == nk:
                        i = n - 1
                        self.TT("pool", at[:, i * 128:(i + 1) * 128], at[:, i * 128:(i + 1) * 128], self.cmask[:, 1, :],
                                ALU.mult, [ak, "cmask"], [ak])
                    for i in range(n):
                        kb = g0 + i
                        self.mm(self.pb[ob][:, 0:64], at[:, i * 128:(i + 1) * 128], V[:, kb, h * 64:(h + 1) * 64],
                                kb == 0, kb == nk - 1, [ak, "V"], [("pb", ob)])
                self.CP("act", yt[:, h * 64:(h + 1) * 64], self.pb[ob][:, 0:64], [("pb", ob)], [yk])
            self.y_to_yT(yt, yk, 3, qb)

    def layer_norm_block(self, h, hk, gb, tb, res_out, li, route):
        st = self.lnst[tb % 2]
        sk = ("lnst", tb % 2)
        junk = self.lnjunk
        self.A(junk, h, AF.Copy, [hk], ["lnjunk", sk], accum=st[:, 0:1])
        self.A(junk, h, AF.Square, [hk], ["lnjunk", sk], accum=st[:, 1:2])
        self.TS("dve", st[:, 2:3], st[:, 0:1], 1.0 / D, None, ALU.mult, None, [sk], [sk])
        self.TT("dve", st[:, 3:4], st[:, 2:3], st[:, 2:3], ALU.mult, [sk], [sk])
        self.STT("dve", st[:, 4:5], st[:, 1:2], 1.0 / D, st[:, 3:4], ALU.mult, ALU.subtract, [sk], [sk])
        self.A(st[:, 4:5], st[:, 4:5], AF.Sqrt, [sk], [sk], bias=LN_EPS)
        self.S.op("dve", lambda e, s_=st: e.reciprocal(out=s_[:, 5:6], in_=s_[:, 4:5]), reads=[sk], writes=[sk])
        self.STT("dve", st[:, 6:7], st[:, 2:3], -1.0, st[:, 5:6], ALU.mult, ALU.mult, [sk], [sk])
        self.A(h, h, AF.Identity, [hk, sk], [hk], scale=st[:, 5:6], bias=st[:, 6:7])
        self.TT("dve", h, h, gb[:, 0, :], ALU.mult, [hk, "lngb"], [hk])
        self.TT("dve", h, h, gb[:, 1, :], ALU.add, [hk, "lngb"], [hk])
        o = self.dma(res_out[tb * 128:(tb + 1) * 128, :], h, [hk], [("res", id(res_out), tb)])
        self.x_to_xT(h, hk, tb, rt=route)
        return o

    def merge_ln1(self, li, res_in, res_out):
        d = self.dram
        win = d["win"]
        HT = 1024
        mp = self.carve([8, HT], BF16)
        accm = self.carve([8, HT], F32)
        sgt = [self.carve([512], BF16) for _ in range(2)]
        prod = [self.carve([512], F32) for _ in range(2)]
        gb = self.carve([2, D], F32)
        hbuf = [self.carve([D], F32) for _ in range(2)]
        xin = [self.carve([D], F32) for _ in range(2)]
        self.lnst = [self.carve([8], F32) for _ in range(2)]
        self.lnjunk = self.carve([D], BF16)
        self.dma(gb[:, 0, :], d["ln1g"][li:li + 1, :].to_broadcast([128, D]), [], ["lngb"])
        self.dma(gb[:, 1, :], d["ln1b"][li:li + 1, :].to_broadcast([128, D]), [], ["lngb"])
        route = None
        if li == 1:
            route = self.make_router(li)
        for th in range(2):
            for n in range(4):
                wvb, wkb = self.wload(d["wbr"][li, n], 2, D)
                for q4 in range(2):
                    c0 = OFF_GATE + n * D + q4 * 512
                    wvg, wkg = self.wload(win[li][:, c0:c0 + 512], 8, 512)
                    for cc in range(4):
                        dc = q4 * 4 + cc
                        for t2 in range(2):
                            tt = th * 2 + t2
                            sl = slice(t2 * 512, (t2 + 1) * 512)
                            bg = self.proj_fm(wvg, wkg, cc * 128, 128, tt)
                            bp = self.proj_fm(wvb, wkb, dc * 128, 128, tt, src=self.yT[n], srckey="yT%d" % n, kc=2)
                            s_ = sgt[t2]
                            sk = ("sgt", t2)
                            self.A(s_, self.pb[bg][:, :], AF.Sigmoid, [("pb", bg)], [sk])
                            ak = ("accm", dc, t2)
                            if n == 0:
                                self.TT("dve", accm[:, dc, sl], self.pb[bp][:, :], s_, ALU.mult, [("pb", bp), sk], [ak])
                            else:
                                p_ = prod[t2]
                                pk = ("prod", t2)
                                self.TT("dve", p_, self.pb[bp][:, :], s_, ALU.mult, [("pb", bp), sk], [pk])
                                if n < 3:
                                    self.TT("dve", accm[:, dc, sl], accm[:, dc, sl], p_, ALU.add, [ak, pk], [ak])
                                else:
                                    self.TT("dve", mp[:, dc, sl], accm[:, dc, sl], p_, ALU.add, [ak, pk], [("mp", dc, t2)])
            wo = [self.wload(d["wout"][li][:, hh * 512:(hh + 1) * 512], 8, 512) for hh in range(2)]
            for j in range(8):
                tb = th * 8 + j
                xi = xin[tb % 2]
                xk = ("xin", tb % 2)
                self.dma(xi, res_in[tb * 128:(tb + 1) * 128, :], [("res", id(res_in), tb)], [xk])
                h = hbuf[tb % 2]
                hk = ("hbuf", tb % 2)
                for hh in range(2):
                    ob = self.accb()
                    for k in range(8):
                        self.mm(self.pb[ob][:, :], mp[:, k, j * 128:(j + 1) * 128], wo[hh][0][:, k, :], k == 0, k == 7,
                                [("mp", k, j // 4), wo[hh][1]], [("pb", ob)])
                    self.STT("dve", h[:, hh * 512:(hh + 1) * 512], xi[:, hh * 512:(hh + 1) * 512], ALPHA, self.pb[ob][:, :],
                             ALU.mult, ALU.add, [xk, ("pb", ob)], [hk])
                rt = (lambda half, b, tb=tb: route(tb, half, b)) if route else None
                o = self.layer_norm_block(h, hk, gb, tb, res_out, li, rt)
                if self.stop_after == ("mix", li):
                    self.finals.append(o)

    def layer_norm_block(self, h, hk, gb, tb, res_out, li, route):
        st = self.lnst[tb % 2]
        sk = ("lnst", tb % 2)
        junk = self.lnjunk
        self.MS("dve", st[:, 0:2], 0.0, [sk])
        self.A(junk, h, AF.Copy, [hk, sk], ["lnjunk", sk], accum=st[:, 0:1])
        self.A(junk, h, AF.Square, [hk, sk], ["lnjunk", sk], accum=st[:, 1:2])
        self.TS("dve", st[:, 2:3], st[:, 0:1], 1.0 / D, None, ALU.mult, None, [sk], [sk])
        self.TT("dve", st[:, 3:4], st[:, 2:3], st[:, 2:3], ALU.mult, [sk], [sk])
        self.STT("dve", st[:, 4:5], st[:, 1:2], 1.0 / D, st[:, 3:4], ALU.mult, ALU.subtract, [sk], [sk])
        self.A(st[:, 4:5], st[:, 4:5], AF.Sqrt, [sk], [sk], bias=LN_EPS)
        self.S.op("dve", lambda e, s_=st: e.reciprocal(out=s_[:, 5:6], in_=s_[:, 4:5]), reads=[sk], writes=[sk])
        self.STT("dve", st[:, 6:7], st[:, 2:3], -1.0, st[:, 5:6], ALU.mult, ALU.mult, [sk], [sk])
        self.A(h, h, AF.Identity, [hk, sk], [hk], scale=st[:, 5:6], bias=st[:, 6:7])
        self.TT("dve", h, h, gb[:, 0, :], ALU.mult, [hk, "lngb"], [hk])
        self.TT("dve", h, h, gb[:, 1, :], ALU.add, [hk, "lngb"], [hk])
        o = self.dma(res_out[tb * 128:(tb + 1) * 128, :], h, [hk], [("res", id(res_out), tb)])
        self.x_to_xT(h, hk, tb, rt=route)
        return o

    def make_router(self, li):
        d = self.dram
        rw = self.carve([8, NE], F32)
        self.dma(rw, d["router"].rearrange("(c p) e -> p c e", p=128), [], ["rw"])
        xf = [self.carve([512], F32) for _ in range(2)]
        lg = [self.carve([32], F32) for _ in range(2)]
        state = {}

        def route(tb, half, b):
            x_ = xf[half]
            xk = ("xf", half)
            self.CP("dve", x_, self.pb[b][:, :], [("pb", b)], [xk])
            if half == 0:
                state["bank"] = self.accb()
            rb = state["bank"]
            for c in range(4):
                k = half * 4 + c
                self.mm(self.pb[rb][:, 0:NE], x_[:, c * 128:(c + 1) * 128], rw[:, k, :], k == 0, k == 7,
                        [xk, "rw"], [("pb", rb)])
            if half == 1:
                l_ = lg[tb % 2]
                lk = ("lg", tb % 2)
                self.CP("dve", l_[:, 0:8], self.pb[rb][:, 0:NE], [("pb", rb)], [lk])
                self.S.op("dve", lambda e, l_=l_: e.max(out=l_[:, 8:16], in_=l_[:, 0:8]), reads=[lk], writes=[lk])
                self.TT("dve", l_[:, 16:17], l_[:, 9:10], l_[:, 8:9], ALU.subtract, [lk], [lk])
                self.A(l_[:, 16:17], l_[:, 16:17], AF.Exp, [lk], [lk])
                self.TS("dve", l_[:, 16:17], l_[:, 16:17], 1.0, None, ALU.add, None, [lk], [lk])
                self.S.op("dve", lambda e, l_=l_: e.reciprocal(out=l_[:, 17:18], in_=l_[:, 16:17]), reads=[lk], writes=[lk])
                self.TS("dve", l_[:, 18:19], l_[:, 8:9], -1.0, None, ALU.mult, None, [lk], [lk])
                self.A(l_[:, 24:32], l_[:, 0:8], AF.Exp, [lk], [lk], bias=l_[:, 18:19])
                self.TS("dve", l_[:, 0:8], l_[:, 0:8], l_[:, 9:10], l_[:, 17:18], ALU.is_ge, ALU.mult, [lk], [lk])
                self.TT("dve", self.gates[:, tb, :], l_[:, 0:8], l_[:, 24:32], ALU.mult, [lk], ["gates"])
        return route

    MOE_CAP = 512

    def ffn_phase(self, li, res_in, res_out):
        self.arena_reset()
        G = 1024
        moe = (li == 1)
        dff = D_FFE if moe else D_FF
        nfc = dff // 128
        hT = self.carve([nfc, self.MOE_CAP if moe else G], BF16)
        facc = self.carve([8, D], F32)
        self.stage = self.stage[0:2] + [self.carve([2048], F32)]
        base = self.aoff
        for g in range(T // G):
            self.aoff = base
            if g > 0:
                self.S.barrier()
            if moe:
                self.moe_group(li, g, res_in, hT, facc, nfc, dff)
            else:
                self.dense_group(li, g, hT, facc, nfc, dff)
            self.S.barrier()
            self.aoff = base
            self.ple_ln2_group(li, g, res_in, res_out, facc)

    def hidden_fm(self, w_in, dff, nfc, hT, src, srckey, tiles, sa):
        for f0 in range(0, nfc, 4):
            nf = min(4, nfc - f0)
            wa, wak = self.wload(w_in[:, f0 * 128:(f0 + nf) * 128], 8, nf * 128)
            wu, wuk = self.wload(w_in[:, dff + f0 * 128:dff + (f0 + nf) * 128], 8, nf * 128)
            for fi in range(nf):
                fc = f0 + fi
                for t2, tt in enumerate(tiles):
                    ba = self.proj_fm(wa, wak, fi * 128, 128, tt, src=src, srckey=srckey)
                    bu = self.proj_fm(wu, wuk, fi * 128, 128, tt, src=src, srckey=srckey)
                    s_ = sa[t2 % 2]
                    sk = ("sa", t2 % 2)
                    self.A(s_, self.pb[ba][:, :], AF.Silu, [("pb", ba)], [sk])
                    self.TT("dve", hT[:, fc, t2 * 512:(t2 + 1) * 512], self.pb[bu][:, :], s_, ALU.mult,
                            [("pb", bu), sk], [("hT", fc)])

    def out_tm(self, w_out, nfc, hT, blocks, evac):
        for f0 in range(0, nfc, 4):
            nf = min(4, nfc - f0)
            wo, wok = self.wload(w_out[f0 * 128:(f0 + nf) * 128, :], nf, D)
            for fi in range(nf):
                fc = f0 + fi
                for i, j in enumerate(blocks):
                    for hh in range(2):
                        b = i * 2 + hh
                        self.mm(self.pb[b][:, :], hT[:, fc, j * 128:(j + 1) * 128], wo[:, fi, hh * 512:(hh + 1) * 512],
                                fc == 0, fc == nfc - 1, [("hT", fc), wok], [("pb", b)])
        for i, j in enumerate(blocks):
            for hh in range(2):
                evac(i, j, hh, i * 2 + hh)

    def dense_group(self, li, g, hT, facc, nfc, dff):
        d = self.dram
        sa = [self.carve([512], BF16) for _ in range(2)]
        self.hidden_fm(d["ffn_in"], dff, nfc, hT, None, None, [g * 2, g * 2 + 1], sa)
        for ps_ in range(2):
            def evac(i, j, hh, b):
                self.CP("act" if hh == 0 else "dve", facc[:, j, hh * 512:(hh + 1) * 512], self.pb[b][:, :],
                        [("pb", b)], [("facc", j, hh)])
            self.out_tm(d["ffn_out"], nfc, hT, [ps_ * 4 + i for i in range(4)], evac)

    def moe_group(self, li, g, res_in, hT, facc, nfc, dff):
        d = self.dram
        C = self.MOE_CAP
        NR = C // 128
        xtok = self.carve([8, D], BF16)
        Pb = self.carve([8, C], BF16)
        PT = self.carve([NR, 8, 128], BF16)
        xsT = self.carve([8, C], BF16)
        ys = self.carve([NR, D], BF16)
        iota = self.carve([C], F32)
        sa = [self.carve([512], BF16) for _ in range(2)]
        mf = self.carve([64], F32)
        mb = self.carve([64], BF16)
        rk = self.carve([64], F32)
        off = self.carve([64], F32)
        gs = self.carve([64, 2], BF16)
        gt = self.carve([64], F32)
        wr = self.carve([NR, 2], F32)
        gsl = self.gates[:, g * 8:(g + 1) * 8, :].rearrange("p j e -> p (j e)")
        self.dma(iota, d["cst"]["iota"], [], ["iota"])
        for j in range(8):
            tb = g * 8 + j
            self.cast_load(xtok[:, j, :], res_in[tb * 128:(tb + 1) * 128, :], 128, [D], ("xtok", j))
        self.TS("dve", mf, gsl, 0.0, None, ALU.is_gt, None, ["gates"], ["mf"])
        self.CP("dve", mb, mf, ["mf"], ["mb"])
        b1 = self.short()
        self.mm(self.pb[b1][:, 0:64], self.cmask[:, 0, :], mb, True, True, ["cmask", "mb"], [("pb", b1)])
        b2 = self.short()
        self.mm(self.pb[b2][:, 0:64], self.cmask[:, 3, :], mb, True, True, ["cmask", "mb"], [("pb", b2)])
        self.MS("dve", off[:, 0:8], 0.0, ["off"])
        for j in range(1, 8):
            self.TT("dve", off[:, j * 8:(j + 1) * 8], off[:, (j - 1) * 8:j * 8], self.pb[b2][:, (j - 1) * 8:j * 8], ALU.add,
                    ["off", ("pb", b2)], ["off"])
        self.TT("dve", rk, self.pb[b1][:, 0:64], off, ALU.add, [("pb", b1), "off"], ["rk"])
        self.TT("dve", rk, rk, mf, ALU.mult, ["rk", "mf"], ["rk"])
        self.TS("dve", rk, rk, -1.0, None, ALU.add, None, ["rk"], ["rk"])
        self.CP("dve", gs[:, :, 0], gsl, ["gates"], ["gs"])
        self.TT("dve", gt, gsl, gs[:, :, 0], ALU.subtract, ["gates", "gs"], ["gt"])
        self.CP("dve", gs[:, :, 1], gt, ["gt"], ["gs"])
        for e in range(NE):
            for j in range(8):
                self.TS("dve", Pb[:, j, :], iota, rk[:, j * 8 + e:j * 8 + e + 1], None, ALU.is_equal, None,
                        ["iota", "rk"], [("Pb", j)])
            for rb in range(NR):
                for j0 in range(0, 8, 4):
                    b = self.short()
                    for jj in range(4):
                        j = j0 + jj
                        self.mm(self.pb[b][:, jj * 128:(jj + 1) * 128], Pb[:, j, rb * 128:(rb + 1) * 128], self.cmask[:, 4, :],
                                True, True, [("Pb", j), "cmask"], [("pb", b)])
                    self.CP("act", PT[:, rb, j0:j0 + 4, :], self.pb[b][:, :].rearrange("p (j t) -> p j t", j=4),
                            [("pb", b)], [("PT", rb)])
            for dc in range(8):
                b = self.short()
                for j in range(8):
                    self.mm(self.pb[b][:, 0:C], xtok[:, j, dc * 128:(dc + 1) * 128], Pb[:, j, :], j == 0, j == 7,
                            [("xtok", j), ("Pb", j)], [("pb", b)])
                self.CP("dve" if dc % 2 else "act", xsT[:, dc, :], self.pb[b][:, 0:C], [("pb", b)], ["xsT"])
            bw = self.short()
            for rb in range(NR):
                for j in range(8):
                    self.mm(self.pb[bw][:, 2 * rb:2 * rb + 2], Pb[:, j, rb * 128:(rb + 1) * 128], gs[:, j * 8 + e, :],
                            j == 0, j == 7, [("Pb", j), "gs"], [("pb", bw)])
            self.CP("dve", wr, self.pb[bw][:, 0:2 * NR].rearrange("p (r c) -> p r c", c=2), [("pb", bw)], ["wr"])
            self.TT("dve", wr[:, :, 0], wr[:, :, 0], wr[:, :, 1], ALU.add, ["wr"], ["wr"])
            self.hidden_fm(d["moe_in"][e], dff, nfc, hT, xsT, "xsT", [0], sa)

            def evac(i, j, hh, b):
                self.A(ys[:, i, hh * 512:(hh + 1) * 512], self.pb[b][:, :], AF.Copy, [("pb", b), "wr"], [("ys", i)],
                       scale=wr[:, i, 0:1])
            self.out_tm(d["moe_out"][e], nfc, hT, list(range(NR)), evac)
            for j in range(8):
                for hh in range(2):
                    b = self.short()
                    for rb in range(NR):
                        self.mm(self.pb[b][:, :], PT[:, rb, j, :], ys[:, rb, hh * 512:(hh + 1) * 512], rb == 0, rb == NR - 1,
                                [("PT", rb), ("ys", rb)], [("pb", b)])
                    dst = facc[:, j, hh * 512:(hh + 1) * 512]
                    fk = ("facc", j, hh)
                    if e == 0:
                        self.CP("dve", dst, self.pb[b][:, :], [("pb", b)], [fk])
                    else:
                        self.TT("dve", dst, dst, self.pb[b][:, :], ALU.add, [fk, ("pb", b)], [fk])

    def ple_ln2_group(self, li, g, res_in, res_out, facc):
        d = self.dram
        gb = self.carve([2, D], F32)
        pblk = [self.carve([256], F32) for _ in range(2)]
        pT = [self.carve([2, 128], BF16) for _ in range(2)]
        ple = self.carve([D], F32)
        hbuf = [self.carve([D], F32) for _ in range(2)]
        xin = self.carve([D], F32)
        self.lnst = [self.carve([8], F32) for _ in range(2)]
        self.lnjunk = self.carve([D], BF16)
        self.dma(gb[:, 0, :], d["ln2g"][li:li + 1, :].to_broadcast([128, D]), [], ["lngb"])
        self.dma(gb[:, 1, :], d["ln2b"][li:li + 1, :].to_broadcast([128, D]), [], ["lngb"])
        wg = [self.wload(d["pleg"][li][:, hh * 512:(hh + 1) * 512], 8, 512) for hh in range(2)]
        wp, wpk = self.wload(d["plep"][li], 2, D)
        for j in range(8):
            tb = g * 8 + j
            pb_ = pblk[j % 2]
            pk = ("pblk", j % 2)
            self.dma(pb_, d["p_in"][li, tb * 128:(tb + 1) * 128, :], [], [pk])
            b = self.short()
            for c in range(2):
                self.tr(self.pb[b][:, c * 128:(c + 1) * 128], pb_[:, c * 128:(c + 1) * 128], [pk], [("pb", b)])
            pt = pT[j % 2]
            ptk = ("pT", j % 2)
            self.CP("act", pt, self.pb[b][:, 0:256].rearrange("p (c t) -> p c t", c=2), [("pb", b)], [ptk])
            for hh in range(2):
                bg_ = self.short()
                for k in range(8):
                    self.mm(self.pb[bg_][:, :], self.xT[:, k, tb * 128:(tb + 1) * 128], wg[hh][0][:, k, :], k == 0, k == 7,
                            [("xT", tb // 4), wg[hh][1]], [("pb", bg_)])
                bp_ = self.short()
                for c in range(2):
                    self.mm(self.pb[bp_][:, :], pt[:, c, :], wp[:, c, hh * 512:(hh + 1) * 512],
                            c == 0, c == 1, [ptk, wpk], [("pb", bp_)])
                self.A(ple[:, hh * 512:(hh + 1) * 512], self.pb[bg_][:, :], AF.Sigmoid, [("pb", bg_)], ["ple"])
                self.TT("dve", ple[:, hh * 512:(hh + 1) * 512], ple[:, hh * 512:(hh + 1) * 512], self.pb[bp_][:, :],
                        ALU.mult, ["ple", ("pb", bp_)], ["ple"])
            self.dma(xin, res_in[tb * 128:(tb + 1) * 128, :], [("res", id(res_in), tb)], ["xin"])
            h = hbuf[j % 2]
            hk = ("hbuf", j % 2)
            self.STT("dve", h, xin, ALPHA, ple, ALU.mult, ALU.add, ["xin", "ple"], [hk])
            self.TT("dve", h, h, facc[:, j, :], ALU.add, [hk], [hk])
            o = self.layer_norm_block(h, hk, gb, tb, res_out, li, None)
            if li == self.layers[-1] or self.stop_after == ("ffn", li):
                self.finals.append(o)


_IDX = _win_index()


def prepare_inputs(inputs):
    f = lambda a: np.ascontiguousarray(np.asarray(a))
    w_in = f(inputs["w_in"])
    win = np.zeros((2, D, NCOLS_R), np.float32)
    valid = _IDX >= 0
    win[:, :, valid] = w_in[:, :, _IDX[valid]]
    conv_w = f(inputs["conv_w"])
    convp = np.zeros((2, 128, 2, 34), np.float32)
    for c in range(2):
        convp[:, :, c, 0:31] = conv_w[:, :, c * 128:(c + 1) * 128].transpose(0, 2, 1)
        convp[:, :, c, 31] = f(inputs["conv_b"])[:, c * 128:(c + 1) * 128]
        convp[:, :, c, 32] = f(inputs["conv_ln_g"])[:, c * 128:(c + 1) * 128]
        convp[:, :, c, 33] = f(inputs["conv_ln_b"])[:, c * 128:(c + 1) * 128]
    pe = f(inputs["nsa_cmp_pe"])
    pe_r = np.ascontiguousarray(pe.transpose(0, 2, 3, 1).reshape(2, 128, 32))
    w2 = f(inputs["nsa_cmp_w2"])
    w2k = np.ascontiguousarray(np.concatenate([w2[:, 0], w2[:, 0]], axis=2))
    w2v = np.ascontiguousarray(w2[:, 1])
    qn = np.ascontiguousarray(f(inputs["mla_q_norm"]).reshape(2, 2, 128).transpose(0, 2, 1))
    kvn = np.ascontiguousarray(f(inputs["mla_kv_norm"]).reshape(2, 128, 1))
    wuq = f(inputs["mla_w_uq"])
    cols = []
    for h in range(4):
        b = h * 96
        cols += list(range(b, b + 96))
        cols += list(range(b, b + 64)) + list(range(b + 80, b + 96)) + list(range(b + 64, b + 80))
    uq = np.ascontiguousarray(wuq[:, :, cols])
    wukv = f(inputs["mla_w_ukv"])
    cols = []
    for h in range(4):
        cols += list(range(h * 128, h * 128 + 64))
    for h in range(4):
        cols += list(range(h * 128 + 64, h * 128 + 128))
    ukv = np.ascontiguousarray(wukv[:, :, cols])
    shared = {
        "win": win, "convp": convp, "pe_r": pe_r, "w1": f(inputs["nsa_cmp_w1"]), "w2k": w2k, "w2v": w2v,
        "qn": qn, "kvn": kvn, "uq": uq, "ukv": ukv, "wbr": f(inputs["w_branch"]), "wout": f(inputs["w_out"]),
        "ln1g": f(inputs["ln1_g"]), "ln1b": f(inputs["ln1_b"]), "ln2g": f(inputs["ln2_g"]), "ln2b": f(inputs["ln2_b"]),
        "ffn_in": f(inputs["ffn_w_in"])[0], "ffn_out": f(inputs["ffn_w_out"])[0], "router": f(inputs["moe_router"])[0],
        "moe_in": f(inputs["moe_w_in"])[0], "moe_out": f(inputs["moe_w_out"])[0],
        "pleg": f(inputs["ple_w_gate"]), "plep": f(inputs["ple_w_proj"]),
    }
    for k, v in _host_consts().items():
        shared["c_" + k] = v
    x = f(inputs["x"])
    p = f(inputs["p"])
    pos = f(inputs["positions"]).astype(np.int32)
    in_maps = []
    for b in range(8):
        m = dict(shared)
        m["x"] = x[b]
        m["p"] = np.ascontiguousarray(p[:, b])
        m["pos"] = pos[b:b + 1]
        in_maps.append(m)
    return in_maps


def kernel(**inputs):
    in_maps = prepare_inputs(inputs)
    nc = MK().build()
    res = run_bass_kernel_spmd(nc, in_maps, core_ids=list(range(8)))
    return np.stack([np.asarray(r["y"], dtype=np.float32) for r in res.results], axis=0)
```

```python
import math
import contextlib
import numpy as np
import concourse.bass as bass
import concourse.mybir as mybir
from concourse.bass_utils import run_bass_kernel_spmd

F32 = mybir.dt.float32
BF16 = mybir.dt.bfloat16
I32 = mybir.dt.int32
AF = mybir.ActivationFunctionType
ALU = mybir.AluOpType
AX = mybir.AxisListType

T = 2048
D = 1024
NB = 16
ALPHA = 4.0 ** 0.25
LN_EPS = 1e-5
RMS_EPS = 1e-6
THETA = 10000.0
D_FF = 2816
D_FFE = 3584
NE = 8

ENGS = ("pe", "act", "dve", "pool", "sp")
N_DMA_SEMS = 6


class Op:
    __slots__ = ("eng", "fn", "deps", "is_dma", "signal", "sig_val", "dma_sem", "dma_val",
                 "dma_prev", "idx")

    def __init__(self, eng, fn, is_dma):
        self.eng = eng
        self.fn = fn
        self.deps = []
        self.is_dma = is_dma
        self.signal = False
        self.sig_val = 0
        self.dma_sem = None
        self.dma_val = 0
        self.dma_prev = 0
        self.idx = 0


class Sched:
    def __init__(self):
        self.ops = {e: [] for e in ENGS}
        self.last_w = {}
        self.readers = {}
        self.all_ops = []

    def op(self, eng, fn, reads=(), writes=(), dma=False, acc=False):
        o = Op(eng, fn, dma)
        deps = []
        for k in reads:
            w = self.last_w.get(k)
            if w is not None:
                deps.append(w)
            if isinstance(k, tuple) and k[0] == "pb":
                for r in self.readers.get(k, ()):
                    if r.eng != eng:
                        deps.append(r)
        for k in writes:
            w = self.last_w.get(k)
            if w is not None and not (acc and w.eng == eng and not w.is_dma):
                deps.append(w)
            for r in self.readers.get(k, ()):
                deps.append(r)
        seen = set()
        for d in deps:
            if id(d) not in seen and d is not o:
                seen.add(id(d))
                o.deps.append(d)
        for k in reads:
            lst = self.readers.setdefault(k, [])
            if not dma:
                for i, r in enumerate(lst):
                    if r.eng == eng and not r.is_dma:
                        lst[i] = o
                        break
                else:
                    lst.append(o)
            else:
                lst.append(o)
        for k in writes:
            self.last_w[k] = o
            self.readers[k] = []
        o.idx = len(self.ops[eng])
        self.ops[eng].append(o)
        self.all_ops.append(o)
        return o

    def barrier(self):
        lasts = []
        for e in ENGS:
            ops = self.ops[e]
            nd = 0
            got_real = False
            for o in reversed(ops):
                if o.fn is None:
                    continue
                if o.is_dma:
                    if nd < N_DMA_SEMS:
                        lasts.append(o)
                        nd += 1
                elif not got_real:
                    lasts.append(o)
                    got_real = True
                if got_real and nd >= N_DMA_SEMS:
                    break
        for e in ENGS:
            o = Op(e, None, False)
            o.deps = [l for l in lasts]
            o.idx = len(self.ops[e])
            self.ops[e].append(o)
            self.all_ops.append(o)
        self.last_w = {}
        self.readers = {}

    def emit(self, nc, final_wait_ops=()):
        for fo in final_wait_ops:
            if not fo.is_dma:
                fo.signal = True
        for o in self.all_ops:
            for d in o.deps:
                if not d.is_dma:
                    d.signal = True
        cnt = {e: 0 for e in ENGS}
        for e in ENGS:
            for o in self.ops[e]:
                if o.signal and not o.is_dma:
                    cnt[e] += 1
                    o.sig_val = cnt[e]
        dma_count = {}
        for e in ENGS:
            k = 0
            for o in self.ops[e]:
                if o.is_dma:
                    j = k % N_DMA_SEMS
                    k += 1
                    key = (e, j)
                    prev = dma_count.get(key, 0)
                    o.dma_sem = key
                    o.dma_prev = prev
                    o.dma_val = prev + 16
                    dma_count[key] = prev + 16
        with contextlib.ExitStack() as st:
            sems = {e: st.enter_context(nc.semaphore("s_" + e)) for e in ENGS if cnt[e] > 0}
            dsems = {key: st.enter_context(nc.semaphore("d_%s%d" % key)) for key in dma_count}
            block = st.enter_context(nc.Block())
            regs = {"pe": block.tensor, "act": block.scalar, "dve": block.vector,
                    "pool": block.gpsimd, "sp": block.sync}

            def make(e):
                def body(eng):
                    known = {}
                    for o in self.ops[e]:
                        waits = {}
                        for d in o.deps:
                            if d.is_dma:
                                s, v = dsems[d.dma_sem], d.dma_val
                            else:
                                s, v = sems[d.eng], d.sig_val
                            kk = id(s)
                            if known.get(kk, 0) >= v:
                                continue
                            if kk not in waits or waits[kk][1] < v:
                                waits[kk] = (s, v)
                        if o.is_dma and o.dma_prev > 0:
                            s = dsems[o.dma_sem]
                            kk = id(s)
                            if known.get(kk, 0) < o.dma_prev:
                                if kk not in waits or waits[kk][1] < o.dma_prev:
                                    waits[kk] = (s, o.dma_prev)
                        for kk, (s, v) in waits.items():
                            eng.wait_ge(s, v)
                            known[kk] = v
                        if o.fn is None:
                            continue
                        ins = o.fn(eng)
                        if o.is_dma:
                            ins.then_inc(dsems[o.dma_sem], 16)
                        elif o.signal:
                            ins.then_inc(sems[e], 1)
                    if e == "sp":
                        for fo in final_wait_ops:
                            if fo.is_dma:
                                eng.wait_ge(dsems[fo.dma_sem], fo.dma_val)
                            else:
                                eng.wait_ge(sems[fo.eng], fo.sig_val)
                return body

            for e in ENGS:
                if self.ops[e] or e == "sp":
                    regs[e](make(e))


OFF_CONV, OFF_NQ, OFF_NK, OFF_MISC, OFF_KR, OFF_SB, OFF_TOK, OFF_GATE = 0, 512, 1024, 1536, 2048, 2304, 2816, 3328
NCOLS_R = 7424


def _win_index():
    sw64 = lambda b: list(range(b + 32, b + 64)) + list(range(b, b + 32))
    sw32 = lambda b: list(range(b + 16, b + 32)) + list(range(b, b + 16))
    idx = []
    idx += list(range(0, 512))
    idx += list(range(512, 768))
    for h in range(4):
        idx += sw64(512 + 64 * h)
    ks, kw = 896, 1024
    idx += list(range(ks, ks + 64)) * 2 + sw64(ks) * 2 + list(range(kw, kw + 64)) * 2 + sw64(kw) * 2
    idx += list(range(768, 896)) + list(range(1164, 1420)) + list(range(1420, 1548))
    idx += [-1] * 64 + list(range(1548, 1580)) + [-1] * 32
    idx += [-1] * 64 + sw32(1548) + [-1] * 32
    idx += list(range(1580, 1580 + 512))
    idx += list(range(960, 1024)) + list(range(1088, 1152)) + list(range(1152, 1164)) + [-1] * 116
    idx += list(range(1580 + 512, 1580 + 768))
    idx += list(range(2348, 6444))
    assert len(idx) == NCOLS_R
    return np.array(idx)


def _host_consts():
    c = {}
    c["ident"] = np.eye(128, dtype=np.float32)
    p = np.arange(128)[:, None]
    f = np.arange(128)[None, :]
    cm = np.zeros((128, 5, 128), np.float32)
    cm[:, 0] = (p <= f)
    cm[:, 1] = (p < f)
    cm[:, 2] = (p > f)
    cm[:, 3] = 1.0
    cm[:, 4] = (p == f)
    c["cmask"] = cm
    j = np.arange(128)[:, None]
    t = np.arange(T)[None, :]
    c["cmpvalid"] = ((16 * j + 31 <= t) & (j < 127)).astype(np.float32)
    n = np.arange(32)[:, None]
    c["eexp"] = ((t // 64) == n).astype(np.float32)
    jj = np.arange(127)
    nn = np.arange(32)
    ov = ((jj[:, None] * 16 < nn[None, :] * 64 + 64) & (jj[:, None] * 16 + 32 > nn[None, :] * 64)).astype(np.float32)
    ovl = np.zeros((128, 33), np.float32)
    ovl[:127, :32] = ov
    ovl[:127, 32] = 1.0
    c["ovl"] = ovl
    cur = (np.arange(T) // 64)[:, None]
    nid = np.arange(32)[None, :]
    forced = (nid == 0) | (nid == cur) | (nid == cur - 1)
    future = nid > cur
    keep = (~forced & ~future).astype(np.float32)
    add = np.where(future, -1e30, np.where(forced, 100.0, 0.0)).astype(np.float32)
    ka = np.zeros((128, 2, 16, 32), np.float32)
    ka[:, 0] = keep.reshape(16, 128, 32).transpose(1, 0, 2)
    ka[:, 1] = add.reshape(16, 128, 32).transpose(1, 0, 2)
    c["keepadd"] = ka
    rc = np.zeros((128, 4), np.float32)
    pp = np.arange(128)
    rc[:, 0] = THETA ** (-(pp % 32).astype(np.float64) / 32.0)
    rc[:, 1] = np.where((pp % 64) < 32, -1.0, 1.0)
    m = (pp >= 64) & (pp < 96)
    rc[m, 2] = THETA ** (-((pp[m] - 64) % 16).astype(np.float64) / 16.0)
    rc[m, 3] = np.where((pp[m] - 64) < 16, -1.0, 1.0)
    c["ropec"] = rc
    ind = np.zeros((128, 16, 16), np.float32)
    for kb in range(16):
        ind[:, kb, kb] = 1.0
    c["indt"] = ind
    c["iota"] = np.tile(np.arange(512, dtype=np.float32)[None, :], (128, 1))
    sg = np.zeros((16, 16, 128), np.float32)
    for kb in range(16):
        sg[kb + 1:, kb, :] = 1.0
    c["selgt"] = sg
    return c


CONST_SHAPES = {"iota": [128, 512], "ident": [128, 128], "cmask": [128, 5, 128], "cmpvalid": [128, T], "eexp": [32, T],
                "ovl": [128, 33], "keepadd": [128, 2, 16, 32], "ropec": [128, 4], "indt": [128, 16, 16],
                "selgt": [16, 16, 128]}


class MK:
    NW = 3

    def __init__(self, layers=(0, 1), debug=False, stop_after=None):
        self.layers = layers
        self.debug = debug
        self.stop_after = stop_after
        self.nc = bass.Bass("TRN2", target_bir_lowering=False)
        self.S = Sched()
        self.st = contextlib.ExitStack()
        self.st.enter_context(self.nc.allow_low_precision(reason="bf16 matmul operands / fp32 accumulation by design"))
        self.wi = 0
        self.stg_i = 0
        self.si = 0
        self.ai = 0
        self.finals = []
        self.dbg_outs = {}

    def din(self, name, shape, dt=F32):
        return self.nc.dram_tensor(name, list(shape), dt, kind="ExternalInput").ap()

    def dout(self, name, shape, dt=F32):
        return self.nc.dram_tensor(name, list(shape), dt, kind="ExternalOutput").ap()

    def sb(self, name, shape, dt):
        return self.st.enter_context(self.nc.sbuf_tensor(name, list(shape), dt))

    def arena_reset(self):
        self.S.barrier()
        self.aoff = 0

    def carve(self, shape, dt, parts=128):
        n = 1
        for s in shape:
            n *= s
        nbytes = n * (4 if dt in (F32, I32) else 2)
        nbytes = (nbytes + 63) // 64 * 64
        off = self.aoff
        self.aoff += nbytes
        assert self.aoff <= self.ARENA_BYTES, (self.aoff, self.ARENA_BYTES)
        v = self.arena[0:parts, off // 2:(off + n * (4 if dt in (F32, I32) else 2)) // 2]
        if dt != BF16:
            v = v.bitcast(dt)
        if len(shape) == 2:
            v = v.rearrange("p (a b) -> p a b", a=shape[0])
        elif len(shape) == 3:
            v = v.rearrange("p (a b c) -> p a b c", a=shape[0], b=shape[1])
        return v

    def short(self):
        b = self.si % 4
        self.si += 1
        return b

    def accb(self):
        b = 4 + self.ai % 4
        self.ai += 1
        return b

    def mm(self, out, lhsT, rhs, start, stop, r, w):
        self.S.op("pe", lambda e: e.matmul(out, lhsT=lhsT, rhs=rhs, start=start, stop=stop),
                  reads=r, writes=w, acc=True)

    def tr(self, out, in_, r, w, parts=128):
        idt = self.ident[0:parts, 0:parts]
        self.S.op("pe", lambda e: e.transpose(out, in_, idt), reads=list(r) + ["ident"], writes=w, acc=True)

    def A(self, out, in_, func, r, w, bias=None, scale=None, accum=None):
        kw = {}
        if bias is not None:
            kw["bias"] = bias
        if scale is not None:
            kw["scale"] = scale
        if accum is not None:
            kw["accum_out"] = accum
        self.S.op("act", lambda e: e.activation(out=out, in_=in_, func=func, **kw), reads=r, writes=w)

    def TT(self, eng, out, in0, in1, op, r, w):
        self.S.op(eng, lambda e: e.tensor_tensor(out=out, in0=in0, in1=in1, op=op), reads=r, writes=w)

    def TS(self, eng, out, in0, s1, s2, op0, op1, r, w):
        if op1 is None:
            self.S.op(eng, lambda e: e.tensor_scalar(out=out, in0=in0, scalar1=s1, scalar2=None, op0=op0),
                      reads=r, writes=w)
        else:
            self.S.op(eng, lambda e: e.tensor_scalar(out=out, in0=in0, scalar1=s1, scalar2=s2, op0=op0, op1=op1),
                      reads=r, writes=w)

    def STT(self, eng, out, in0, scalar, in1, op0, op1, r, w):
        self.S.op(eng, lambda e: e.scalar_tensor_tensor(out=out, in0=in0, scalar=scalar, in1=in1, op0=op0, op1=op1),
                  reads=r, writes=w)

    def CP(self, eng, out, in_, r, w):
        if eng == "act":
            self.S.op("act", lambda e: e.activation(out=out, in_=in_, func=AF.Copy), reads=r, writes=w)
        else:
            self.S.op(eng, lambda e: e.tensor_copy(out=out, in_=in_), reads=r, writes=w)

    def MS(self, eng, out, val, w):
        self.S.op(eng, lambda e: e.memset(out, val), writes=w)

    def dma(self, out, in_, r, w, eng="sp"):
        return self.S.op(eng, lambda e: e.dma_start(out=out, in_=in_), reads=r, writes=w, dma=True)

    CAST_ENGS = ("dve", "act", "dve", "act")

    def cast_load(self, dst, src, parts, free_shape, dkey):
        n = 1
        for x_ in free_shape:
            n *= x_
        assert n <= 2048, n
        si_ = self.stg_i % len(self.stage)
        ce = self.CAST_ENGS[self.stg_i % 4]
        self.stg_i += 1
        stg = self.stage[si_][0:parts, 0:n]
        if len(free_shape) == 2:
            stg = stg.rearrange("p (a b) -> p a b", a=free_shape[0])
        elif len(free_shape) == 3:
            stg = stg.rearrange("p (a b c) -> p a b c", a=free_shape[0], b=free_shape[1])
        sk = ("stg", si_)
        self.dma(stg, src, [], [sk])
        self.CP(ce, dst, stg, [sk], [dkey])

    def dma_w1(self, w1t, src, c):
        si_ = self.stg_i % len(self.stage)
        ce = self.CAST_ENGS[self.stg_i % 4]
        self.stg_i += 1
        ps_ = slice(c * 64, (c + 1) * 64)
        stg = self.stage[si_][ps_, 0:2048].rearrange("p (l e) -> p l e", l=32)
        sk = ("stg", si_)
        self.dma(stg, src.rearrange("(l d) e -> d l e", d=64), [], [sk])
        self.CP(ce, w1t[ps_, :, :], stg, [sk], ["w1t"])

    def wload(self, src, kc, n, rows=128):
        slot = self.wi % self.NW
        self.wi += 1
        assert kc * n <= 4096
        v = self.wring[slot][0:rows, 0:kc * n].rearrange("p (c n) -> p c n", c=kc)
        srcv = src.rearrange("(c p) n -> p c n", p=rows)
        key = ("w", slot)
        step = max(1, 2048 // n)
        for k0 in range(0, kc, step):
            k1 = min(kc, k0 + step)
            self.cast_load(v[:, k0:k1, :], srcv[:, k0:k1, :], rows, [k1 - k0, n], key)
        return v, key

    def proj_fm(self, wv, wkey, c0, M, tt, src=None, srckey=None, kc=8):
        b = self.short()
        src = self.xT if src is None else src
        srckey = ("xT", tt) if srckey is None else srckey
        for k in range(kc):
            self.mm(self.pb[b][0:M, :], wv[:, k, c0:c0 + M], src[:, k, tt * 512:(tt + 1) * 512],
                    k == 0, k == kc - 1, [wkey, srckey], [("pb", b)])
        return b

    def build(self):
        nc, S = self.nc, self.S
        L = self.layers
        x_in = self.din("x", [T, D])
        p_in = self.din("p", [2, T, 256])
        pos_in = self.din("pos", [1, T], I32)
        win = self.din("win", [2, D, NCOLS_R])
        convp = self.din("convp", [2, 128, 2, 34])
        pe_r = self.din("pe_r", [2, 128, 32])
        w1 = self.din("w1", [2, 2, 2048, 64])
        w2k = self.din("w2k", [2, 64, 128])
        w2v = self.din("w2v", [2, 64, 64])
        qn = self.din("qn", [2, 128, 2])
        kvn = self.din("kvn", [2, 128, 1])
        uq = self.din("uq", [2, 256, 768])
        ukv = self.din("ukv", [2, 128, 512])
        wbr = self.din("wbr", [2, 4, 256, D])
        wout = self.din("wout", [2, D, D])
        ln1g = self.din("ln1g", [2, D])
        ln1b = self.din("ln1b", [2, D])
        ln2g = self.din("ln2g", [2, D])
        ln2b = self.din("ln2b", [2, D])
        lite = self.stop_after is not None and self.stop_after[0] in ("conv", "nsa", "mla", "sb", "mix")
        need_ffn = (0 in L) and not lite
        need_moe = (1 in L) and not (lite and self.stop_after[1] == 0) and self.stop_after != ("ffn", 0)
        ffn_in = self.din("ffn_in", [D, 2 * D_FF]) if need_ffn else None
        ffn_out = self.din("ffn_out", [D_FF, D]) if need_ffn else None
        router = self.din("router", [D, NE])
        moe_in = self.din("moe_in", [NE, D, 2 * D_FFE]) if need_moe else None
        moe_out = self.din("moe_out", [NE, D_FFE, D]) if need_moe else None
        pleg = self.din("pleg", [2, D, D])
        plep = self.din("plep", [2, 256, D])
        cst = {k: self.din("c_" + k, v) for k, v in CONST_SHAPES.items()}
        y_out = self.dout("y", [T, D])
        xa = self.dout("xa", [T, D])
        xb = self.dout("xb", [T, D])
        tabs_d = self.dout("tabs_d", [2, 128, 2 * T], BF16)
        self.dram = dict(locals())

        self.xT = self.sb("xT", [128, 8, T], BF16)
        self.wring = [self.sb("wr%d" % i, [128, 4096], BF16) for i in range(self.NW)]
        self.stage = [self.sb("stg%d" % i, [128, 2048], F32) for i in range(2)]
        self.ident = self.sb("ident", [128, 128], F32)
        self.cmask = self.sb("cmask", [128, 5, 128], BF16)
        self.onesf = self.sb("onesf", [128, 128], F32)
        self.gates = self.sb("gates", [128, NB, NE], F32)
        self.pb = [self.st.enter_context(nc.psum_tensor("pb%d" % i, [128, 512], F32)) for i in range(8)]
        self.ARENA_BYTES = 133 * 1024
        self.arena = self.sb("arena", [128, self.ARENA_BYTES // 2], BF16)
        self.aoff = 0

        self.dma(self.ident[:], cst["ident"], [], ["ident"])
        self.cast_load(self.cmask[:], cst["cmask"], 128, [5, 128], "cmask")
        self.MS("dve", self.onesf[:], 1.0, ["onesf"])

        self.setup_rope(pos_in, cst)
        self.load_xT(x_in)

        res_in = x_in
        outs = [(xa, xb), (xa, y_out)]
        for li in L:
            mid, fin = outs[li]
            if li == 1:
                res_in = xb
            if self.mixer_phase(li, res_in, mid):
                break
            if self.stop_after == ("mix", li):
                break
            self.ffn_phase(li, mid, fin)
            if self.stop_after == ("ffn", li):
                break
        S.emit(nc, final_wait_ops=self.finals)
        self.st.close()
        return nc

    def setup_rope(self, pos_in, cst):
        self.arena_reset()
        self.tabN = self.carve([2, T], BF16)
        self.tabM = self.carve([2, T], BF16)
        pi = self.carve([T], I32)
        pf = self.carve([T], F32)
        ang = self.carve([T], F32)
        kf = self.carve([T], F32)
        ki = self.carve([T], I32)
        rc = self.carve([4], F32)
        self.dma(pi, pos_in[0:1, :].to_broadcast([128, T]), [], ["pi"])
        self.dma(rc, cst["ropec"], [], ["rc"])
        self.CP("dve", pf, pi, ["pi"], ["pf"])
        for tab, ic, sc in ((self.tabN, 0, 1), (self.tabM, 2, 3)):
            for which in range(2):
                shift = math.pi / 2 if which == 0 else 0.0
                self.TS("dve", ang, pf, rc[:, ic:ic + 1], shift, ALU.mult, ALU.add, ["pf", "rc"], ["ang"])
                self.TS("dve", kf, ang, 1.0 / (2 * math.pi), None, ALU.mult, None, ["ang"], ["kf"])
                self.CP("dve", ki, kf, ["kf"], ["ki"])
                self.CP("dve", kf, ki, ["ki"], ["kf"])
                self.STT("dve", ang, kf, -2 * math.pi, ang, ALU.mult, ALU.add, ["kf", "ang"], ["ang"])
                self.TS("dve", kf, ang, math.pi, -2 * math.pi, ALU.is_gt, ALU.mult, ["ang"], ["kf"])
                self.TT("dve", ang, ang, kf, ALU.add, ["ang", "kf"], ["ang"])
                self.TS("dve", kf, ang, -math.pi, 2 * math.pi, ALU.is_lt, ALU.mult, ["ang"], ["kf"])
                self.TT("dve", ang, ang, kf, ALU.add, ["ang", "kf"], ["ang"])
                self.A(ang, ang, AF.Sin, ["ang"], ["ang"])
                if which == 0:
                    self.CP("dve", tab[:, 0, :], ang, ["ang"], ["tab"])
                else:
                    self.TS("dve", tab[:, 1, :], ang, rc[:, sc:sc + 1], None, ALU.mult, None, ["ang", "rc"], ["tab"])
        td = self.dram["tabs_d"]
        self.dma(td[0], self.tabN.rearrange("p a t -> p (a t)"), ["tab"], ["tabs_d"])
        self.dma(td[1], self.tabM.rearrange("p a t -> p (a t)"), ["tab"], ["tabs_d"])

    def x_to_xT(self, xblk, xkey, tb, rt=None):
        tt = tb // 4
        for half in range(2):
            b = self.short()
            for c in range(4):
                cc = half * 4 + c
                self.tr(self.pb[b][:, c * 128:(c + 1) * 128], xblk[:, cc * 128:(cc + 1) * 128], [xkey], [("pb", b)])
            dst = self.xT[:, half * 4:half * 4 + 4, tb * 128:(tb + 1) * 128]
            src = self.pb[b][:, :].rearrange("p (c t) -> p c t", c=4)
            self.CP("act" if half == 0 else "dve", dst, src, [("pb", b)], [("xT", tt)])
            if rt is not None:
                rt(half, b)

    def load_xT(self, x_in):
        self.arena_reset()
        xbs = [self.carve([D], F32) for _ in range(2)]
        for tb in range(NB):
            xb_ = xbs[tb % 2]
            key = ("xblk", tb % 2)
            self.dma(xb_, x_in[tb * 128:(tb + 1) * 128, :], [], [key])
            self.x_to_xT(xb_, key, tb)

    def mixer_phase(self, li, res_in, res_out):
        d = self.dram
        self.arena_reset()
        self.stage = self.stage[0:2]
        self.yT = [self.carve([2, T], BF16) for _ in range(4)]
        self.mixer_base = self.aoff
        for n, (nm, fn) in enumerate((("conv", self.conv_branch), ("nsa", self.nsa_branch), ("mla", self.mla_branch),
                                     ("sb", self.sb_branch))):
            only = getattr(self, "only", None)
            if only is None or nm in only:
                fn(li)
                self.dbg("yT%d_%d" % (n, li), self.yT[n], [128, 2, T], ["yT%d" % n])
            self.aoff = self.mixer_base
            self.S.barrier()
            if self.stop_after == (nm, li):
                return True
        self.merge_ln1(li, res_in, res_out)

    def dbg(self, name, ap, shape, keys, dt=BF16):
        if not self.debug:
            return
        o = self.dout("dbg_" + name, shape, dt)
        self.finals.append(self.dma(o, ap, keys, []))

    def conv_branch(self, li):
        d = self.dram
        win = d["win"]
        cp = self.carve([2, 34], F32)
        self.dma(cp, d["convp"][li], [], ["cp"])
        hp = self.carve([2, 30 + T], F32)
        acc = self.carve([2, T], F32)
        sig = [self.carve([512], F32) for _ in range(2)]
        self.MS("pool", hp[:, :, 0:30], 0.0, ["hp"])
        wv, wk = self.wload(win[li][:, OFF_CONV:OFF_CONV + 512], 8, 512)
        for tt in range(4):
            for c in range(2):
                ba = self.proj_fm(wv, wk, c * 128, 128, tt)
                bg = self.proj_fm(wv, wk, 256 + c * 128, 128, tt)
                sg = sig[c]
                self.A(sg, self.pb[bg][:, :], AF.Sigmoid, [("pb", bg)], [("sig", c)])
                self.TT("dve", hp[:, c, 30 + tt * 512:30 + (tt + 1) * 512], self.pb[ba][:, :], sg, ALU.mult,
                        [("pb", ba), ("sig", c)], ["hp"])
        for c in range(2):
            eng = "dve"
            self.TS(eng, acc[:, c, :], hp[:, c, 0:T], cp[:, c, 0:1], cp[:, c, 31:32], ALU.mult, ALU.add,
                    ["hp", "cp"], [("acc", c)])
            for w in range(1, 31):
                self.STT(eng, acc[:, c, :], hp[:, c, w:w + T], cp[:, c, w:w + 1], acc[:, c, :], ALU.mult, ALU.add,
                         ["hp", "cp", ("acc", c)], [("acc", c)])
        sq = [self.carve([512], F32) for _ in range(2)]
        m2 = self.carve([512], F32)
        rstd = self.carve([512], F32)
        dd = [self.carve([512], F32) for _ in range(2)]
        for tt in range(4):
            sl = slice(tt * 512, (tt + 1) * 512)
            bm = self.short()
            for c in range(2):
                self.mm(self.pb[bm][:, :], self.onesf[:, :], acc[:, c, sl], c == 0, c == 1,
                        ["onesf", ("acc", c)], [("pb", bm)])
            bq = self.short()
            for c in range(2):
                self.A(sq[c], acc[:, c, sl], AF.Square, [("acc", c)], [("sq", c)])
            for c in range(2):
                self.mm(self.pb[bq][:, :], self.onesf[:, :], sq[c], c == 0, c == 1,
                        ["onesf", ("sq", c)], [("pb", bq)])
            self.A(m2, self.pb[bm][:, :], AF.Square, [("pb", bm)], ["m2"], scale=1.0 / 256)
            self.STT("dve", rstd, self.pb[bq][:, :], 1.0 / 256, m2, ALU.mult, ALU.subtract, [("pb", bq), "m2"], ["rstd"])
            self.A(rstd, rstd, AF.Sqrt, ["rstd"], ["rstd"], bias=LN_EPS)
            self.S.op("dve", lambda e, r=rstd: e.reciprocal(out=r, in_=r), reads=["rstd"], writes=["rstd"])
            for c in range(2):
                self.STT("dve", dd[c], self.pb[bm][:, :], -1.0 / 256, acc[:, c, sl], ALU.mult, ALU.add,
                         [("pb", bm), ("acc", c)], [("dd", c)])
                self.TT("dve", dd[c], dd[c], rstd, ALU.mult, [("dd", c), "rstd"], [("dd", c)])
                self.A(self.yT[0][:, c, sl], dd[c], AF.Silu, [("dd", c), "cp"], ["yT0"],
                       scale=cp[:, c, 32:33], bias=cp[:, c, 33:34])

    def y_to_yT(self, ytile, ykey, n, qb):
        b = self.short()
        for c in range(2):
            self.tr(self.pb[b][:, c * 128:(c + 1) * 128], ytile[:, c * 128:(c + 1) * 128], [ykey], [("pb", b)])
        self.CP("act", self.yT[n][:, :, qb * 128:(qb + 1) * 128],
                self.pb[b][:, 0:256].rearrange("p (c t) -> p c t", c=2), [("pb", b)], ["yT%d" % n])

    def softmax_attn_block(self, qb, kbs, kT_fn, qT_ap, v_fn, scale, extra_mask=None, nm=""):
        ob = self.accb()
        n = len(kbs)
        for g0 in range(0, n, 4):
            grp = kbs[g0:g0 + 4]
            sbk = self.short()
            for i, (kb, mt) in enumerate(grp):
                kT, kkeys = kT_fn(kb)
                self.mm(self.pb[sbk][:, i * 128:(i + 1) * 128], kT, qT_ap[0], True, True,
                        list(kkeys) + list(qT_ap[1]), [("pb", sbk)])
            ei = self.ei % 5
            self.ei += 1
            E = self.Ebuf[ei]
            ek = ("E", ei)
            w = len(grp) * 128
            self.A(E[:, 0:w], self.pb[sbk][:, 0:w], AF.Exp, [("pb", sbk)], [ek], scale=scale)
            if extra_mask is not None:
                mtile, mkey = extra_mask
                self.TT("dve", E[:, 0:w], E[:, 0:w], mtile[:, g0 * 128:g0 * 128 + w], ALU.mult, [ek, mkey], [ek])
            else:
                for i, (kb, mt) in enumerate(grp):
                    if mt is not None:
                        mi = {"le": 0, "lt": 1, "gt": 2}[mt]
                        self.TT("dve", E[:, i * 128:(i + 1) * 128], E[:, i * 128:(i + 1) * 128],
                                self.cmask[:, mi, :], ALU.mult, [ek, "cmask"], [ek])
            for i, (kb, mt) in enumerate(grp):
                v, vkeys = v_fn(kb)
                gi = g0 + i
                self.mm(self.pb[ob][:, 0:65], E[:, i * 128:(i + 1) * 128], v, gi == 0, gi == n - 1,
                        [ek] + list(vkeys), [("pb", ob)])
        return ob

    def nsa_branch(self, li):
        d = self.dram
        win = d["win"]
        cst = {k: d["cst"][k] for k in d["cst"]}
        self.tabN = self.carve([2, T], BF16)
        self.dma(self.tabN.rearrange("p a t -> p (a t)"), d["tabs_d"][0], [], ["tab"])
        QT = self.carve([2, T], BF16)
        QR = self.carve([2, T], BF16)
        KS = self.carve([T], BF16)
        KW = self.carve([T], BF16)
        KCV = self.carve([T], BF16)
        vS = self.carve([NB, 65], BF16)
        vW = self.carve([NB, 65], BF16)
        sg = self.carve([NB, 12], F32)
        cmpvalid = self.carve([T], BF16)
        eexp = self.carve([T], BF16, parts=32)
        keepadd = self.carve([2, NB, 32], F32)
        VC = self.carve([97], BF16)
        w1t = self.carve([32, 64], BF16)
        pet = self.carve([32], BF16)
        w2kt = self.carve([128], BF16, parts=64)
        w2vt = self.carve([64], BF16, parts=64)
        hid = [self.carve([127], BF16, parts=64) for _ in range(2)]
        hb = self.carve([2], F32, parts=64)
        kcT = self.carve([127], BF16)
        t1 = [self.carve([512], F32) for _ in range(2)]
        t2 = [self.carve([512], F32) for _ in range(2)]
        self.Ebuf = [self.carve([512], BF16) for _ in range(5)]
        self.ei = 0
        ytile = [self.carve([256], F32) for _ in range(2)]
        selmask = [self.carve([NB * 128], BF16) for _ in range(2)]
        small = [self.carve([64], F32) for _ in range(2)]
        imp = [self.carve([32], F32) for _ in range(2)]
        scr = [self.carve([32], F32) for _ in range(2)]
        selT = [self.carve([128], BF16, parts=32) for _ in range(2)]
        self.cast_load(cmpvalid, cst["cmpvalid"], 128, [T], "cmpvalid")
        self.cast_load(eexp, cst["eexp"], 32, [T], "eexp")
        self.dma(keepadd, cst["keepadd"], [], ["keepadd"])
        self.cast_load(VC[:, 64:97], cst["ovl"], 128, [33], "VCc")
        for c in range(2):
            self.dma_w1(w1t, d["w1"][li, c], c)
        self.cast_load(pet, d["pe_r"][li], 128, [32], "pet")
        self.cast_load(w2kt, d["w2k"][li], 64, [128], "w2kt")
        self.cast_load(w2vt, d["w2v"][li], 64, [64], "w2vt")
        self.MS("dve", vS[:, :, 64:65], 1.0, ["vS"])
        self.MS("dve", vW[:, :, 64:65], 1.0, ["vW"])
        wv, wk = self.wload(win[li][:, OFF_NQ:OFF_NQ + 512], 8, 512)
        for tt in range(4):
            sl = slice(tt * 512, (tt + 1) * 512)
            for c in range(2):
                b0 = self.proj_fm(wv, wk, c * 128, 128, tt)
                b1 = self.proj_fm(wv, wk, 256 + c * 128, 128, tt)
                self.CP("act", QT[:, c, sl], self.pb[b0][:, :], [("pb", b0)], ["QT"])
                self.TT("dve", t1[c], self.pb[b0][:, :], self.tabN[:, 0, sl], ALU.mult, [("pb", b0), "tab"], [("t1", c)])
                self.TT("dve", t2[c], self.pb[b1][:, :], self.tabN[:, 1, sl], ALU.mult, [("pb", b1), "tab"], [("t2", c)])
                self.TT("dve", QR[:, c, sl], t1[c], t2[c], ALU.add, [("t1", c), ("t2", c)], ["QR"])
        wv, wk = self.wload(win[li][:, OFF_NK:OFF_NK + 512], 8, 512)
        for tt in range(4):
            sl = slice(tt * 512, (tt + 1) * 512)
            for c, dst, dk in ((0, KS, "KS"), (1, KW, "KW")):
                b0 = self.proj_fm(wv, wk, c * 256, 128, tt)
                b1 = self.proj_fm(wv, wk, c * 256 + 128, 128, tt)
                self.TT("dve", t1[c], self.pb[b0][:, :], self.tabN[:, 0, sl], ALU.mult, [("pb", b0), "tab"], [("t1", c)])
                self.TT("dve", t2[c], self.pb[b1][:, :], self.tabN[:, 1, sl], ALU.mult, [("pb", b1), "tab"], [("t2", c)])
                self.TT("dve", dst[:, sl], t1[c], t2[c], ALU.add, [("t1", c), ("t2", c)], [dk])
        wvm, wkm = self.wload(win[li][:, OFF_MISC:OFF_MISC + 128], 8, 128)
        for tt in range(4):
            sl = slice(tt * 512, (tt + 1) * 512)
            b0 = self.proj_fm(wvm, wkm, 0, 128, tt)
            self.CP("act", KCV[:, sl], self.pb[b0][:, :], [("pb", b0)], ["KCV"])
        wvt, wkt = self.wload(win[li][:, OFF_TOK:OFF_TOK + 140], 8, 140)
        for tb in range(NB):
            b = self.short()
            for k in range(8):
                self.mm(self.pb[b][:, 0:140], self.xT[:, k, tb * 128:(tb + 1) * 128], wvt[:, k, :], k == 0, k == 7,
                        [wkt, ("xT", tb // 4)], [("pb", b)])
            self.CP("act", vS[:, tb, 0:64], self.pb[b][:, 0:64], [("pb", b)], ["vS"])
            self.CP("dve", vW[:, tb, 0:64], self.pb[b][:, 64:128], [("pb", b)], ["vW"])
            self.A(sg[:, tb, :], self.pb[b][:, 128:140], AF.Sigmoid, [("pb", b)], ["sg"])
        for c in range(2):
            ps_ = slice(c * 64, (c + 1) * 64)
            b = self.short()
            for l in range(32):
                self.mm(self.pb[b][0:64, 0:127], w1t[ps_, l, :], KCV[ps_, l:l + 16 * 126 + 1:16], l == 0, l == 31,
                        ["w1t", "KCV"], [("pb", b)])
            b2 = self.short()
            for l in range(32):
                self.mm(self.pb[b2][0:64, 0:1], w1t[ps_, l, :], pet[ps_, l:l + 1], l == 0, l == 31,
                        ["w1t", "pet"], [("pb", b2)])
            self.CP("dve", hb[:, c:c + 1], self.pb[b2][0:64, 0:1], [("pb", b2)], ["hb"])
            self.A(hid[c], self.pb[b][0:64, 0:127], AF.Gelu_apprx_tanh, [("pb", b), "hb"], [("hid", c)],
                   bias=hb[:, c:c + 1])
        b = self.short()
        self.mm(self.pb[b][:, 0:127], w2kt[:, :], hid[0], True, True, ["w2kt", ("hid", 0)], [("pb", b)])
        self.CP("act", kcT, self.pb[b][:, 0:127], [("pb", b)], ["kcT"])
        b = self.short()
        self.mm(self.pb[b][0:127, 0:64], hid[1], w2vt[:, :], True, True, ["w2vt", ("hid", 1)], [("pb", b)])
        self.CP("act", VC[0:127, 0:64], self.pb[b][0:127, 0:64], [("pb", b)], ["VCv"])
        for qb in range(NB):
            qs = slice(qb * 128, (qb + 1) * 128)
            yt = ytile[qb % 2]
            yk = ("yt", qb % 2)
            sm = small[qb % 2]
            smk = ("small", qb % 2)
            im = imp[qb % 2]
            imk = ("imp", qb % 2)
            sbk2 = [self.short(), self.short()]
            for h in range(4):
                hp_ = slice((h % 2) * 64, (h % 2) * 64 + 64)
                bb_ = sbk2[h % 2]
                self.mm(self.pb[bb_][0:127, (h // 2) * 128:(h // 2 + 1) * 128], kcT[hp_, :], QT[hp_, h // 2, qs], True, True,
                        ["kcT", "QT"], [("pb", bb_)])
            ei = self.ei % 5
            self.ei += 1
            E = self.Ebuf[ei]
            ek = ("E", ei)
            for h in range(4):
                bb_ = sbk2[h % 2]
                self.A(E[0:127, h * 128:(h + 1) * 128], self.pb[bb_][0:127, (h // 2) * 128:(h // 2 + 1) * 128], AF.Exp,
                       [("pb", bb_)], [ek], scale=0.125)
            for h in range(4):
                self.TT("dve", E[0:127, h * 128:(h + 1) * 128], E[0:127, h * 128:(h + 1) * 128], cmpvalid[0:127, qs],
                        ALU.mult, [ek, "cmpvalid"], [ek])
            ob = self.accb()
            for h in range(4):
                self.mm(self.pb[ob][:, h * 97:(h + 1) * 97], E[0:127, h * 128:(h + 1) * 128], VC[0:127, :], True, True,
                        [ek, "VCv", "VCc"], [("pb", ob)])
            P = self.pb[ob]
            for h in range(4):
                self.TS("dve", sm[:, h:h + 1], P[:, 97 * h + 96:97 * h + 97], 1e-30, None, ALU.max, None, [("pb", ob)], [smk])
            self.S.op("dve", lambda e, s_=sm: e.reciprocal(out=s_[:, 0:4], in_=s_[:, 0:4]), reads=[smk], writes=[smk])
            for h in range(4):
                if h == 0:
                    self.TS("dve", im, P[:, 64:96], sm[:, 0:1], None, ALU.mult, None, [("pb", ob), smk], [imk])
                else:
                    self.STT("dve", im, P[:, 97 * h + 64:97 * h + 96], sm[:, h:h + 1], im, ALU.mult, ALU.add,
                             [("pb", ob), smk, imk], [imk])
            for h in range(4):
                self.TT("dve", sm[:, 4 + h:5 + h], sm[:, h:h + 1], sg[:, qb, 3 * h:3 * h + 1], ALU.mult, [smk, "sg"], [smk])
                self.A(yt[:, h * 64:(h + 1) * 64], P[:, 97 * h:97 * h + 64], AF.Copy, [("pb", ob), smk], [yk],
                       scale=sm[:, 4 + h:5 + h])
            mk = None
            if qb >= 8:
                sc_ = scr[qb % 2]
                sck = ("scr", qb % 2)
                self.TT("dve", im, im, keepadd[:, 0, qb, :], ALU.mult, [imk, "keepadd"], [imk])
                self.TT("dve", im, im, keepadd[:, 1, qb, :], ALU.add, [imk, "keepadd"], [imk])
                self.S.op("dve", lambda e, s_=sm, i_=im: e.max(out=s_[:, 8:16], in_=i_), reads=[imk, smk], writes=[smk])
                self.S.op("dve", lambda e, s_=sm, i_=im, c_=sc_: e.match_replace(out=c_, in_to_replace=s_[:, 8:16],
                                                                                in_values=i_, imm_value=-1e30),
                          reads=[imk, smk], writes=[sck])
                self.S.op("dve", lambda e, s_=sm, c_=sc_: e.max(out=s_[:, 16:24], in_=c_), reads=[sck, smk], writes=[smk])
                self.TS("dve", sc_, im, sm[:, 23:24], None, ALU.is_ge, None, [imk, smk], [sck])
                b = self.short()
                self.tr(self.pb[b][0:32, 0:128], sc_, [sck], [("pb", b)])
                sT = selT[qb % 2]
                stk = ("selT", qb % 2)
                self.CP("act", sT, self.pb[b][0:32, 0:128], [("pb", b)], [stk])
                smt = selmask[qb % 2]
                mk = ("selmask", qb % 2)
                for g0 in range(0, qb + 1, 4):
                    n = min(4, qb + 1 - g0)
                    b = self.short()
                    for i in range(n):
                        kb = g0 + i
                        self.mm(self.pb[b][:, i * 128:(i + 1) * 128], eexp[:, kb * 128:(kb + 1) * 128], sT, True, True,
                                ["eexp", stk], [("pb", b)])
                    self.CP("act", smt[:, g0 * 128:(g0 + n) * 128], self.pb[b][:, 0:n * 128], [("pb", b)], [mk])
                self.TT("dve", smt[:, qb * 128:(qb + 1) * 128], smt[:, qb * 128:(qb + 1) * 128], self.cmask[:, 0, :],
                        ALU.mult, [mk, "cmask"], [mk])
            for h in range(4):
                hp_ = slice((h % 2) * 64, (h % 2) * 64 + 64)
                qT_ap = (QR[hp_, h // 2, qs], ["QR"])
                for br, KT, kkey, V, vkey, gcol in ((1, KS, "KS", vS, "vS", 3 * h + 1), (2, KW, "KW", vW, "vW", 3 * h + 2)):
                    if br == 1:
                        kbs = [(kb, "le" if kb == qb else None) for kb in range(qb + 1)]
                        em = (selmask[qb % 2], mk) if qb >= 8 else None
                    else:
                        kbs = [(kb, "le" if kb == qb else ("gt" if kb == qb - 4 else None))
                               for kb in range(max(0, qb - 4), qb + 1)]
                        em = None
                    ob = self.softmax_attn_block(
                        qb, kbs, lambda kb, KT=KT, kkey=kkey: (KT[hp_, kb * 128:(kb + 1) * 128], [kkey]), qT_ap,
                        lambda kb, V=V, vkey=vkey: (V[:, kb, :], [vkey]), 0.125, extra_mask=em)
                    fk = ("fac", qb % 2)
                    fac = sm[:, 24 + 2 * h + (br - 1):25 + 2 * h + (br - 1)]
                    self.S.op("dve", lambda e, f_=fac, o_=self.pb[ob][:, 64:65]: e.reciprocal(out=f_, in_=o_),
                              reads=[("pb", ob), smk], writes=[smk])
                    self.TT("dve", fac, fac, sg[:, qb, gcol:gcol + 1], ALU.mult, [smk, "sg"], [smk])
                    self.STT("dve", yt[:, h * 64:(h + 1) * 64], self.pb[ob][:, 0:64], fac, yt[:, h * 64:(h + 1) * 64],
                             ALU.mult, ALU.add, [("pb", ob), smk, yk], [yk])
            self.y_to_yT(yt, yk, 1, qb)

    def mla_branch(self, li):
        d = self.dram
        win = d["win"]
        self.tabM = self.carve([2, T], BF16)
        self.dma(self.tabM.rearrange("p a t -> p (a t)"), d["tabs_d"][1], [], ["tab"])
        qg = self.carve([2, T], BF16)
        kvg = self.carve([T], BF16)
        CS = self.carve([2, T], BF16)
        rkv = self.carve([T], BF16)
        rkt = self.carve([NB], F32)
        Vm = self.carve([NB, 4, 65], BF16)
        QH = [self.carve([T], BF16) for _ in range(2)]
        KH = [self.carve([T], BF16) for _ in range(2)]
        KR = self.carve([T], BF16)
        nrm = self.carve([4], F32)
        sq = [self.carve([512], F32) for _ in range(2)]
        t1 = [self.carve([512], F32) for _ in range(2)]
        t2 = [self.carve([512], F32) for _ in range(2)]
        rs = self.carve([512], F32)
        self.Ebuf = [self.carve([512], BF16) for _ in range(5)]
        self.ei = 0
        self.ymla = self.carve([NB, 256], F32)
        small = [self.carve([8], F32) for _ in range(2)]
        self.dma(nrm[:, 0:2], d["qn"][li], [], ["nrm"])
        self.dma(nrm[:, 2:3], d["kvn"][li], [], ["nrm"])
        self.MS("dve", Vm[:, :, :, 64:65], 1.0, ["Vm"])
        wvm, wkm = self.wload(win[li][:, OFF_MISC + 128:OFF_MISC + 512], 8, 384)
        for tt in range(4):
            sl = slice(tt * 512, (tt + 1) * 512)
            bq = []
            for c in range(2):
                b0 = self.proj_fm(wvm, wkm, c * 128, 128, tt)
                bq.append(b0)
                self.A(qg[:, c, sl], self.pb[b0][:, :], AF.Copy, [("pb", b0), "nrm"], ["qg"], scale=nrm[:, c:c + 1])
                self.A(sq[c], self.pb[b0][:, :], AF.Square, [("pb", b0)], [("sq", c)])
            bs = self.short()
            for c in range(2):
                self.mm(self.pb[bs][:, :], self.onesf[:, :], sq[c], c == 0, c == 1, ["onesf", ("sq", c)], [("pb", bs)])
            self.A(rs, self.pb[bs][:, :], AF.Sqrt, [("pb", bs)], ["rs"], scale=1.0 / 256, bias=RMS_EPS)
            self.S.op("dve", lambda e, r=rs: e.reciprocal(out=r, in_=r), reads=["rs"], writes=["rs"])
            for w in range(2):
                self.TT("dve", CS[:, w, sl], self.tabM[:, w, sl], rs, ALU.mult, ["tab", "rs"], ["CS"])
            b0 = self.proj_fm(wvm, wkm, 256, 128, tt)
            self.A(kvg[:, sl], self.pb[b0][:, :], AF.Copy, [("pb", b0), "nrm"], ["kvg"], scale=nrm[:, 2:3])
            self.A(sq[0], self.pb[b0][:, :], AF.Square, [("pb", b0)], [("sq", 0)])
            bs = self.short()
            self.mm(self.pb[bs][:, :], self.onesf[:, :], sq[0], True, True, ["onesf", ("sq", 0)], [("pb", bs)])
            self.A(rs, self.pb[bs][:, :], AF.Sqrt, [("pb", bs)], ["rs"], scale=1.0 / 128, bias=RMS_EPS)
            self.S.op("dve", lambda e, r=rs, o=rkv[:, sl]: e.reciprocal(out=o, in_=r), reads=["rs"], writes=["rkv"])
            bt = self.short()
            for i in range(4):
                self.mm(self.pb[bt][:, i:i + 1], sq[0][:, i * 128:(i + 1) * 128], self.onesf[:, 0:1], True, True,
                        [("sq", 0), "onesf"], [("pb", bt)])
            self.A(rkt[:, tt * 4:tt * 4 + 4], self.pb[bt][:, 0:4], AF.Sqrt, [("pb", bt)], ["rkt"], scale=1.0 / 128,
                   bias=RMS_EPS)
        self.S.op("dve", lambda e: e.reciprocal(out=rkt, in_=rkt), reads=["rkt"], writes=["rkt"])
        wvr, wkr = self.wload(win[li][:, OFF_KR:OFF_KR + 256], 8, 256)
        r9 = slice(64, 96)
        for tt in range(4):
            sl = slice(tt * 512, (tt + 1) * 512)
            b0 = self.proj_fm(wvr, wkr, 0, 96, tt)
            b1 = self.proj_fm(wvr, wkr, 128, 96, tt)
            self.TT("dve", t1[0][r9, :], self.pb[b0][r9, :], self.tabM[r9, 0, sl], ALU.mult, [("pb", b0), "tab"], [("t1", 0)])
            self.TT("dve", t2[0][r9, :], self.pb[b1][r9, :], self.tabM[r9, 1, sl], ALU.mult, [("pb", b1), "tab"], [("t2", 0)])
            self.TT("dve", KR[r9, sl], t1[0][r9, :], t2[0][r9, :], ALU.add, [("t1", 0), ("t2", 0)], ["KR"])
        wvu, wku = self.wload(d["ukv"][li], 1, 512)
        for tb in range(NB):
            b = self.short()
            self.mm(self.pb[b][:, 0:256], kvg[:, tb * 128:(tb + 1) * 128], wvu[:, 0, 256:512], True, True,
                    ["kvg", wku], [("pb", b)])
            self.A(Vm[:, tb, :, 0:64], self.pb[b][:, 0:256].rearrange("p (h c) -> p h c", h=4), AF.Copy,
                   [("pb", b), "rkt"], ["Vm"], scale=rkt[:, tb:tb + 1])
        wvq, wkq = self.wload(d["uq"][li], 2, 768)
        scale = 96.0 ** -0.5
        for h in range(4):
            Q = QH[h % 2]
            K = KH[h % 2]
            qk = ("QH", h % 2)
            kk = ("KH", h % 2)
            for tt in range(4):
                sl = slice(tt * 512, (tt + 1) * 512)
                ba = self.proj_fm(wvq, wkq, (2 * h) * 96, 96, tt, src=qg, srckey="qg", kc=2)
                bb = self.proj_fm(wvq, wkq, (2 * h + 1) * 96, 96, tt, src=qg, srckey="qg", kc=2)
                c = tt % 2
                self.TT("dve", t1[c][0:96, :], self.pb[ba][0:96, :], CS[0:96, 0, sl], ALU.mult, [("pb", ba), "CS"], [("t1", c)])
                self.TT("dve", t2[c][0:96, :], self.pb[bb][0:96, :], CS[0:96, 1, sl], ALU.mult, [("pb", bb), "CS"], [("t2", c)])
                self.TT("dve", Q[0:96, sl], t1[c][0:96, :], t2[c][0:96, :], ALU.add, [("t1", c), ("t2", c)], [qk])
                bk = self.short()
                self.mm(self.pb[bk][0:64, :], wvu[:, 0, h * 64:(h + 1) * 64], kvg[:, sl], True, True, [wku, "kvg"], [("pb", bk)])
                self.TT("dve", K[0:64, sl], self.pb[bk][0:64, :], rkv[0:64, sl], ALU.mult, [("pb", bk), "rkv"], [kk])
            self.CP("dve", K[r9, :], KR[r9, :], ["KR"], [kk])
            for qb in range(NB):
                qs = slice(qb * 128, (qb + 1) * 128)
                sm = small[qb % 2]
                smk = ("small", qb % 2)
                kbs = [(kb, "le" if kb == qb else None) for kb in range(qb + 1)]
                ob = self.softmax_attn_block(
                    qb, kbs, lambda kb: (K[0:96, kb * 128:(kb + 1) * 128], [kk]), (Q[0:96, qs], [qk]),
                    lambda kb: (Vm[:, kb, h, :], ["Vm"]), scale)
                self.S.op("dve", lambda e, s_=sm, o_=self.pb[ob][:, 64:65]: e.reciprocal(out=s_[:, 0:1], in_=o_),
                          reads=[("pb", ob)], writes=[smk])
                self.A(self.ymla[:, qb, h * 64:(h + 1) * 64], self.pb[ob][:, 0:64], AF.Copy, [("pb", ob), smk],
                       [("ymla", qb)], scale=sm[:, 0:1])
        for qb in range(NB):
            self.y_to_yT(self.ymla[:, qb, :], ("ymla", qb), 2, qb)

    def sb_branch(self, li):
        d = self.dram
        win = d["win"]
        cst = d["cst"]
        QT = self.carve([2, T], BF16)
        KT = self.carve([2, T], BF16)
        V = self.carve([NB, 256], BF16)
        indt = self.carve([16, 16], BF16)
        selgt = self.carve([16, 128], BF16, parts=16)
        sp = [self.carve([NB * 128], F32) for _ in range(2)]
        lk = [self.carve([NB * 128], BF16) for _ in range(2)]
        ex = [self.carve([512], F32) for _ in range(2)]
        ar = [self.carve([512], F32) for _ in range(2)]
        aa = [self.carve([512], BF16) for _ in range(5)]
        ts_ = [self.carve([128], BF16, parts=16) for _ in range(2)]
        ytile = [self.carve([256], F32) for _ in range(2)]
        self.cast_load(indt, cst["indt"], 128, [16, 16], "indt")
        self.cast_load(selgt, cst["selgt"], 16, [16, 128], "selgt")
        wv, wk = self.wload(win[li][:, OFF_SB:OFF_SB + 512], 8, 512)
        for tt in range(4):
            sl = slice(tt * 512, (tt + 1) * 512)
            for c in range(2):
                b0 = self.proj_fm(wv, wk, c * 128, 128, tt)
                self.CP("act", QT[:, c, sl], self.pb[b0][:, :], [("pb", b0)], ["QT"])
                b1 = self.proj_fm(wv, wk, 256 + c * 128, 128, tt)
                self.CP("dve", KT[:, c, sl], self.pb[b1][:, :], [("pb", b1)], ["KT"])
        wvt, wkt = self.wload(win[li][:, OFF_TOK + 256:OFF_TOK + 512], 8, 256)
        for tb in range(NB):
            b = self.short()
            for k in range(8):
                self.mm(self.pb[b][:, 0:256], self.xT[:, k, tb * 128:(tb + 1) * 128], wvt[:, k, :], k == 0, k == 7,
                        [wkt, ("xT", tb // 4)], [("pb", b)])
            self.CP("act", V[:, tb, :], self.pb[b][:, 0:256], [("pb", b)], ["V"])
        it = 0
        ai_ = 0
        for qb in range(NB):
            qs = slice(qb * 128, (qb + 1) * 128)
            yt = ytile[qb % 2]
            yk = ("yt", qb % 2)
            for h in range(4):
                hp_ = slice((h % 2) * 64, (h % 2) * 64 + 64)
                spt, lkt, tst = sp[it % 2], lk[it % 2], ts_[it % 2]
                spk, lkk, tsk = ("sp", it % 2), ("lk", it % 2), ("ts", it % 2)
                it += 1
                nk = qb + 1
                for g0 in range(0, nk, 4):
                    n = min(4, nk - g0)
                    w = n * 128
                    sbk = self.short()
                    for i in range(n):
                        kb = g0 + i
                        self.mm(self.pb[sbk][:, i * 128:(i + 1) * 128], KT[hp_, h // 2, kb * 128:(kb + 1) * 128],
                                QT[hp_, h // 2, qs], True, True, ["KT", "QT"], [("pb", sbk)])
                    e_ = ex[(g0 // 4) % 2]
                    exk = ("ex", (g0 // 4) % 2)
                    self.A(e_[:, 0:w], self.pb[sbk][:, 0:w], AF.Exp, [("pb", sbk)], [exk], scale=-0.125)
                    self.A(spt[:, g0 * 128:g0 * 128 + w], e_[:, 0:w], AF.Ln, [exk], [spk], bias=1.0)
                    self.STT("dve", lkt[:, g0 * 128:g0 * 128 + w], self.pb[sbk][:, 0:w], -0.125, spt[:, g0 * 128:g0 * 128 + w],
                             ALU.mult, ALU.subtract, [("pb", sbk), spk], [lkk])
                self.TT("dve", lkt[:, qb * 128:(qb + 1) * 128], lkt[:, qb * 128:(qb + 1) * 128], self.cmask[:, 1, :],
                        ALU.mult, [lkk, "cmask"], [lkk])
                bts = self.short()
                for kb in range(nk):
                    self.mm(self.pb[bts][0:16, 0:128], indt[:, kb, :], lkt[:, kb * 128:(kb + 1) * 128], kb == 0, kb == nk - 1,
                            ["indt", lkk], [("pb", bts)])
                self.CP("act", tst, self.pb[bts][0:16, 0:128], [("pb", bts)], [tsk])
                ob = self.accb()
                for g0 in range(0, nk, 4):
                    n = min(4, nk - g0)
                    w = n * 128
                    lb = self.short()
                    for i in range(n):
                        kb = g0 + i
                        self.mm(self.pb[lb][:, i * 128:(i + 1) * 128], self.cmask[:, 2, :], lkt[:, kb * 128:(kb + 1) * 128],
                                True, False, ["cmask", lkk], [("pb", lb)])
                        self.mm(self.pb[lb][:, i * 128:(i + 1) * 128], selgt[:, kb, :], tst, False, True,
                                ["selgt", tsk], [("pb", lb)])
                    a_ = ar[(g0 // 4) % 2]
                    ark = ("ar", (g0 // 4) % 2)
                    self.TT("dve", a_[:, 0:w], self.pb[lb][:, 0:w], spt[:, g0 * 128:g0 * 128 + w], ALU.subtract,
                            [("pb", lb), spk], [ark])
                    at = aa[ai_ % 5]
                    ak = ("aa", ai_ % 5)
                    ai_ += 1
                    self.A(at[:, 0:w], a_[:, 0:w], AF.Exp, [ark], [ak])
                    if g0 + n == nk:
                        i = n - 1
                        self.TT("dve", at[:, i * 128:(i + 1) * 128], at[:, i * 128:(i + 1) * 128], self.cmask[:, 1, :],
                                ALU.mult, [ak, "cmask"], [ak])
                    for i in range(n):
                        kb = g0 + i
                        self.mm(self.pb[ob][:, 0:64], at[:, i * 128:(i + 1) * 128], V[:, kb, h * 64:(h + 1) * 64],
                                kb == 0, kb == nk - 1, [ak, "V"], [("pb", ob)])
                self.CP("act", yt[:, h * 64:(h + 1) * 64], self.pb[ob][:, 0:64], [("pb", ob)], [yk])
            self.y_to_yT(yt, yk, 3, qb)

    def layer_norm_block(self, h, hk, gb, tb, res_out, li, route):
        st = self.lnst[tb % 2]
        sk = ("lnst", tb % 2)
        junk = self.lnjunk
        self.A(junk, h, AF.Copy, [hk], ["lnjunk", sk], accum=st[:, 0:1])
        self.A(junk, h, AF.Square, [hk], ["lnjunk", sk], accum=st[:, 1:2])
        self.TS("dve", st[:, 2:3], st[:, 0:1], 1.0 / D, None, ALU.mult, None, [sk], [sk])
        self.TT("dve", st[:, 3:4], st[:, 2:3], st[:, 2:3], ALU.mult, [sk], [sk])
        self.STT("dve", st[:, 4:5], st[:, 1:2], 1.0 / D, st[:, 3:4], ALU.mult, ALU.subtract, [sk], [sk])
        self.A(st[:, 4:5], st[:, 4:5], AF.Sqrt, [sk], [sk], bias=LN_EPS)
        self.S.op("dve", lambda e, s_=st: e.reciprocal(out=s_[:, 5:6], in_=s_[:, 4:5]), reads=[sk], writes=[sk])
        self.STT("dve", st[:, 6:7], st[:, 2:3], -1.0, st[:, 5:6], ALU.mult, ALU.mult, [sk], [sk])
        self.A(h, h, AF.Identity, [hk, sk], [hk], scale=st[:, 5:6], bias=st[:, 6:7])
        self.TT("dve", h, h, gb[:, 0, :], ALU.mult, [hk, "lngb"], [hk])
        self.TT("dve", h, h, gb[:, 1, :], ALU.add, [hk, "lngb"], [hk])
        o = self.dma(res_out[tb * 128:(tb + 1) * 128, :], h, [hk], [("res", id(res_out), tb)])
        self.x_to_xT(h, hk, tb, rt=route)
        return o

    def merge_ln1(self, li, res_in, res_out):
        d = self.dram
        win = d["win"]
        HT = 1024
        mp = self.carve([8, HT], BF16)
        accm = self.carve([8, HT], F32)
        sgt = [self.carve([512], BF16) for _ in range(2)]
        prod = [self.carve([512], F32) for _ in range(2)]
        gb = self.carve([2, D], F32)
        hbuf = [self.carve([D], F32) for _ in range(2)]
        xin = [self.carve([D], F32) for _ in range(2)]
        self.lnst = [self.carve([8], F32) for _ in range(2)]
        self.lnjunk = self.carve([D], BF16)
        self.dma(gb[:, 0, :], d["ln1g"][li:li + 1, :].to_broadcast([128, D]), [], ["lngb"])
        self.dma(gb[:, 1, :], d["ln1b"][li:li + 1, :].to_broadcast([128, D]), [], ["lngb"])
        route = None
        if li == 1:
            route = self.make_router(li)
        for th in range(2):
            for n in range(4):
                wvb, wkb = self.wload(d["wbr"][li, n], 2, D)
                for q4 in range(2):
                    c0 = OFF_GATE + n * D + q4 * 512
                    wvg, wkg = self.wload(win[li][:, c0:c0 + 512], 8, 512)
                    for cc in range(4):
                        dc = q4 * 4 + cc
                        for t2 in range(2):
                            tt = th * 2 + t2
                            sl = slice(t2 * 512, (t2 + 1) * 512)
                            bg = self.proj_fm(wvg, wkg, cc * 128, 128, tt)
                            bp = self.proj_fm(wvb, wkb, dc * 128, 128, tt, src=self.yT[n], srckey="yT%d" % n, kc=2)
                            s_ = sgt[t2]
                            sk = ("sgt", t2)
                            self.A(s_, self.pb[bg][:, :], AF.Sigmoid, [("pb", bg)], [sk])
                            ak = ("accm", dc, t2)
                            if n == 0:
                                self.TT("dve", accm[:, dc, sl], self.pb[bp][:, :], s_, ALU.mult, [("pb", bp), sk], [ak])
                            else:
                                p_ = prod[t2]
                                pk = ("prod", t2)
                                self.TT("dve", p_, self.pb[bp][:, :], s_, ALU.mult, [("pb", bp), sk], [pk])
                                if n < 3:
                                    self.TT("dve", accm[:, dc, sl], accm[:, dc, sl], p_, ALU.add, [ak, pk], [ak])
                                else:
                                    self.TT("dve", mp[:, dc, sl], accm[:, dc, sl], p_, ALU.add, [ak, pk], [("mp", dc, t2)])
            wo = [self.wload(d["wout"][li][:, hh * 512:(hh + 1) * 512], 8, 512) for hh in range(2)]
            for j in range(8):
                tb = th * 8 + j
                xi = xin[tb % 2]
                xk = ("xin", tb % 2)
                self.dma(xi, res_in[tb * 128:(tb + 1) * 128, :], [("res", id(res_in), tb)], [xk])
                h = hbuf[tb % 2]
                hk = ("hbuf", tb % 2)
                for hh in range(2):
                    ob = self.accb()
                    for k in range(8):
                        self.mm(self.pb[ob][:, :], mp[:, k, j * 128:(j + 1) * 128], wo[hh][0][:, k, :], k == 0, k == 7,
                                [("mp", k, j // 4), wo[hh][1]], [("pb", ob)])
                    self.STT("dve", h[:, hh * 512:(hh + 1) * 512], xi[:, hh * 512:(hh + 1) * 512], ALPHA, self.pb[ob][:, :],
                             ALU.mult, ALU.add, [xk, ("pb", ob)], [hk])
                rt = (lambda half, b, tb=tb: route(tb, half, b)) if route else None
                o = self.layer_norm_block(h, hk, gb, tb, res_out, li, rt)
                if self.stop_after == ("mix", li):
                    self.finals.append(o)

    def layer_norm_block(self, h, hk, gb, tb, res_out, li, route):
        st = self.lnst[tb % 2]
        sk = ("lnst", tb % 2)
        junk = self.lnjunk
        self.MS("dve", st[:, 0:2], 0.0, [sk])
        self.A(junk, h, AF.Copy, [hk, sk], ["lnjunk", sk], accum=st[:, 0:1])
        self.A(junk, h, AF.Square, [hk, sk], ["lnjunk", sk], accum=st[:, 1:2])
        self.TS("dve", st[:, 2:3], st[:, 0:1], 1.0 / D, None, ALU.mult, None, [sk], [sk])
        self.TT("dve", st[:, 3:4], st[:, 2:3], st[:, 2:3], ALU.mult, [sk], [sk])
        self.STT("dve", st[:, 4:5], st[:, 1:2], 1.0 / D, st[:, 3:4], ALU.mult, ALU.subtract, [sk], [sk])
        self.A(st[:, 4:5], st[:, 4:5], AF.Sqrt, [sk], [sk], bias=LN_EPS)
        self.S.op("dve", lambda e, s_=st: e.reciprocal(out=s_[:, 5:6], in_=s_[:, 4:5]), reads=[sk], writes=[sk])
        self.STT("dve", st[:, 6:7], st[:, 2:3], -1.0, st[:, 5:6], ALU.mult, ALU.mult, [sk], [sk])
        self.A(h, h, AF.Identity, [hk, sk], [hk], scale=st[:, 5:6], bias=st[:, 6:7])
        self.TT("dve", h, h, gb[:, 0, :], ALU.mult, [hk, "lngb"], [hk])
        self.TT("dve", h, h, gb[:, 1, :], ALU.add, [hk, "lngb"], [hk])
        o = self.dma(res_out[tb * 128:(tb + 1) * 128, :], h, [hk], [("res", id(res_out), tb)])
        self.x_to_xT(h, hk, tb, rt=route)
        return o

    def make_router(self, li):
        d = self.dram
        rw = self.carve([8, NE], F32)
        self.dma(rw, d["router"].rearrange("(c p) e -> p c e", p=128), [], ["rw"])
        xf = [self.carve([512], F32) for _ in range(2)]
        lg = [self.carve([32], F32) for _ in range(2)]
        state = {}

        def route(tb, half, b):
            x_ = xf[half]
            xk = ("xf", half)
            self.CP("dve", x_, self.pb[b][:, :], [("pb", b)], [xk])
            if half == 0:
                state["bank"] = self.accb()
            rb = state["bank"]
            for c in range(4):
                k = half * 4 + c
                self.mm(self.pb[rb][:, 0:NE], x_[:, c * 128:(c + 1) * 128], rw[:, k, :], k == 0, k == 7,
                        [xk, "rw"], [("pb", rb)])
            if half == 1:
                l_ = lg[tb % 2]
                lk = ("lg", tb % 2)
                self.CP("dve", l_[:, 0:8], self.pb[rb][:, 0:NE], [("pb", rb)], [lk])
                self.S.op("dve", lambda e, l_=l_: e.max(out=l_[:, 8:16], in_=l_[:, 0:8]), reads=[lk], writes=[lk])
                self.TT("dve", l_[:, 16:17], l_[:, 9:10], l_[:, 8:9], ALU.subtract, [lk], [lk])
                self.A(l_[:, 16:17], l_[:, 16:17], AF.Exp, [lk], [lk])
                self.TS("dve", l_[:, 16:17], l_[:, 16:17], 1.0, None, ALU.add, None, [lk], [lk])
                self.S.op("dve", lambda e, l_=l_: e.reciprocal(out=l_[:, 17:18], in_=l_[:, 16:17]), reads=[lk], writes=[lk])
                self.TS("dve", l_[:, 18:19], l_[:, 8:9], -1.0, None, ALU.mult, None, [lk], [lk])
                self.A(l_[:, 24:32], l_[:, 0:8], AF.Exp, [lk], [lk], bias=l_[:, 18:19])
                self.TS("dve", l_[:, 0:8], l_[:, 0:8], l_[:, 9:10], l_[:, 17:18], ALU.is_ge, ALU.mult, [lk], [lk])
                self.TT("dve", self.gates[:, tb, :], l_[:, 0:8], l_[:, 24:32], ALU.mult, [lk], ["gates"])
        return route

    MOE_CAP = 512

    def ffn_phase(self, li, res_in, res_out):
        self.arena_reset()
        G = 1024
        moe = (li == 1)
        dff = D_FFE if moe else D_FF
        nfc = dff // 128
        hT = self.carve([nfc, self.MOE_CAP if moe else G], BF16)
        facc = self.carve([8, D], F32)
        self.stage = self.stage[0:2] + [self.carve([2048], F32)]
        base = self.aoff
        for g in range(T // G):
            self.aoff = base
            if g > 0:
                self.S.barrier()
            if moe:
                self.moe_group(li, g, res_in, hT, facc, nfc, dff)
            else:
                self.dense_group(li, g, hT, facc, nfc, dff)
            self.S.barrier()
            self.aoff = base
            self.ple_ln2_group(li, g, res_in, res_out, facc)

    def hidden_fm(self, w_in, dff, nfc, hT, src, srckey, tiles, sa):
        for f0 in range(0, nfc, 4):
            nf = min(4, nfc - f0)
            wa, wak = self.wload(w_in[:, f0 * 128:(f0 + nf) * 128], 8, nf * 128)
            wu, wuk = self.wload(w_in[:, dff + f0 * 128:dff + (f0 + nf) * 128], 8, nf * 128)
            for fi in range(nf):
                fc = f0 + fi
                for t2, tt in enumerate(tiles):
                    ba = self.proj_fm(wa, wak, fi * 128, 128, tt, src=src, srckey=srckey)
                    bu = self.proj_fm(wu, wuk, fi * 128, 128, tt, src=src, srckey=srckey)
                    s_ = sa[t2 % 2]
                    sk = ("sa", t2 % 2)
                    self.A(s_, self.pb[ba][:, :], AF.Silu, [("pb", ba)], [sk])
                    self.TT("dve", hT[:, fc, t2 * 512:(t2 + 1) * 512], self.pb[bu][:, :], s_, ALU.mult,
                            [("pb", bu), sk], [("hT", fc)])

    def out_tm(self, w_out, nfc, hT, blocks, evac):
        for f0 in range(0, nfc, 4):
            nf = min(4, nfc - f0)
            wo, wok = self.wload(w_out[f0 * 128:(f0 + nf) * 128, :], nf, D)
            for fi in range(nf):
                fc = f0 + fi
                for i, j in enumerate(blocks):
                    for hh in range(2):
                        b = i * 2 + hh
                        self.mm(self.pb[b][:, :], hT[:, fc, j * 128:(j + 1) * 128], wo[:, fi, hh * 512:(hh + 1) * 512],
                                fc == 0, fc == nfc - 1, [("hT", fc), wok], [("pb", b)])
        for i, j in enumerate(blocks):
            for hh in range(2):
                evac(i, j, hh, i * 2 + hh)

    def dense_group(self, li, g, hT, facc, nfc, dff):
        d = self.dram
        sa = [self.carve([512], BF16) for _ in range(2)]
        self.hidden_fm(d["ffn_in"], dff, nfc, hT, None, None, [g * 2, g * 2 + 1], sa)
        for ps_ in range(2):
            def evac(i, j, hh, b):
                self.CP("act" if hh == 0 else "dve", facc[:, j, hh * 512:(hh + 1) * 512], self.pb[b][:, :],
                        [("pb", b)], [("facc", j, hh)])
            self.out_tm(d["ffn_out"], nfc, hT, [ps_ * 4 + i for i in range(4)], evac)

    def moe_group(self, li, g, res_in, hT, facc, nfc, dff):
        d = self.dram
        C = self.MOE_CAP
        NR = C // 128
        xtok = self.carve([8, D], BF16)
        Pb = self.carve([8, C], BF16)
        PT = self.carve([NR, 8, 128], BF16)
        xsT = self.carve([8, C], BF16)
        ys = self.carve([NR, D], BF16)
        iota = self.carve([C], F32)
        sa = [self.carve([512], BF16) for _ in range(2)]
        mf = self.carve([64], F32)
        mb = self.carve([64], BF16)
        rk = self.carve([64], F32)
        off = self.carve([64], F32)
        gs = self.carve([64, 2], BF16)
        gt = self.carve([64], F32)
        wr = self.carve([NR, 2], F32)
        gsl = self.gates[:, g * 8:(g + 1) * 8, :].rearrange("p j e -> p (j e)")
        self.dma(iota, d["cst"]["iota"], [], ["iota"])
        for j in range(8):
            tb = g * 8 + j
            self.cast_load(xtok[:, j, :], res_in[tb * 128:(tb + 1) * 128, :], 128, [D], ("xtok", j))
        self.TS("dve", mf, gsl, 0.0, None, ALU.is_gt, None, ["gates"], ["mf"])
        self.CP("dve", mb, mf, ["mf"], ["mb"])
        b1 = self.short()
        self.mm(self.pb[b1][:, 0:64], self.cmask[:, 0, :], mb, True, True, ["cmask", "mb"], [("pb", b1)])
        b2 = self.short()
        self.mm(self.pb[b2][:, 0:64], self.cmask[:, 3, :], mb, True, True, ["cmask", "mb"], [("pb", b2)])
        self.MS("dve", off[:, 0:8], 0.0, ["off"])
        for j in range(1, 8):
            self.TT("dve", off[:, j * 8:(j + 1) * 8], off[:, (j - 1) * 8:j * 8], self.pb[b2][:, (j - 1) * 8:j * 8], ALU.add,
                    ["off", ("pb", b2)], ["off"])
        self.TT("dve", rk, self.pb[b1][:, 0:64], off, ALU.add, [("pb", b1), "off"], ["rk"])
        self.TT("dve", rk, rk, mf, ALU.mult, ["rk", "mf"], ["rk"])
        self.TS("dve", rk, rk, -1.0, None, ALU.add, None, ["rk"], ["rk"])
        self.CP("dve", gs[:, :, 0], gsl, ["gates"], ["gs"])
        self.TT("dve", gt, gsl, gs[:, :, 0], ALU.subtract, ["gates", "gs"], ["gt"])
        self.CP("dve", gs[:, :, 1], gt, ["gt"], ["gs"])
        for e in range(NE):
            for j in range(8):
                self.TS("dve", Pb[:, j, :], iota, rk[:, j * 8 + e:j * 8 + e + 1], None, ALU.is_equal, None,
                        ["iota", "rk"], [("Pb", j)])
            for rb in range(NR):
                for j0 in range(0, 8, 4):
                    b = self.short()
                    for jj in range(4):
                        j = j0 + jj
                        self.mm(self.pb[b][:, jj * 128:(jj + 1) * 128], Pb[:, j, rb * 128:(rb + 1) * 128], self.cmask[:, 4, :],
                                True, True, [("Pb", j), "cmask"], [("pb", b)])
                    self.CP("act", PT[:, rb, j0:j0 + 4, :], self.pb[b][:, :].rearrange("p (j t) -> p j t", j=4),
                            [("pb", b)], [("PT", rb)])
            for dc in range(8):
                b = self.short()
                for j in range(8):
                    self.mm(self.pb[b][:, 0:C], xtok[:, j, dc * 128:(dc + 1) * 128], Pb[:, j, :], j == 0, j == 7,
                            [("xtok", j), ("Pb", j)], [("pb", b)])
                self.CP("dve" if dc % 2 else "act", xsT[:, dc, :], self.pb[b][:, 0:C], [("pb", b)], ["xsT"])
            bw = self.short()
            for rb in range(NR):
                for j in range(8):
                    self.mm(self.pb[bw][:, 2 * rb:2 * rb + 2], Pb[:, j, rb * 128:(rb + 1) * 128], gs[:, j * 8 + e, :],
                            j == 0, j == 7, [("Pb", j), "gs"], [("pb", bw)])
            self.CP("dve", wr, self.pb[bw][:, 0:2 * NR].rearrange("p (r c) -> p r c", c=2), [("pb", bw)], ["wr"])
            self.TT("dve", wr[:, :, 0], wr[:, :, 0], wr[:, :, 1], ALU.add, ["wr"], ["wr"])
            self.hidden_fm(d["moe_in"][e], dff, nfc, hT, xsT, "xsT", [0], sa)

            def evac(i, j, hh, b):
                self.A(ys[:, i, hh * 512:(hh + 1) * 512], self.pb[b][:, :], AF.Copy, [("pb", b), "wr"], [("ys", i)],
                       scale=wr[:, i, 0:1])
            self.out_tm(d["moe_out"][e], nfc, hT, list(range(NR)), evac)
            for j in range(8):
                for hh in range(2):
                    b = self.short()
                    for rb in range(NR):
                        self.mm(self.pb[b][:, :], PT[:, rb, j, :], ys[:, rb, hh * 512:(hh + 1) * 512], rb == 0, rb == NR - 1,
                                [("PT", rb), ("ys", rb)], [("pb", b)])
                    dst = facc[:, j, hh * 512:(hh + 1) * 512]
                    fk = ("facc", j, hh)
                    if e == 0:
                        self.CP("dve", dst, self.pb[b][:, :], [("pb", b)], [fk])
                    else:
                        self.TT("dve", dst, dst, self.pb[b][:, :], ALU.add, [fk, ("pb", b)], [fk])

    def ple_ln2_group(self, li, g, res_in, res_out, facc):
        d = self.dram
        gb = self.carve([2, D], F32)
        pblk = [self.carve([256], F32) for _ in range(2)]
        pT = [self.carve([2, 128], BF16) for _ in range(2)]
        ple = self.carve([D], F32)
        hbuf = [self.carve([D], F32) for _ in range(2)]
        xin = self.carve([D], F32)
        self.lnst = [self.carve([8], F32) for _ in range(2)]
        self.lnjunk = self.carve([D], BF16)
        self.dma(gb[:, 0, :], d["ln2g"][li:li + 1, :].to_broadcast([128, D]), [], ["lngb"])
        self.dma(gb[:, 1, :], d["ln2b"][li:li + 1, :].to_broadcast([128, D]), [], ["lngb"])
        wg = [self.wload(d["pleg"][li][:, hh * 512:(hh + 1) * 512], 8, 512) for hh in range(2)]
        wp, wpk = self.wload(d["plep"][li], 2, D)
        for j in range(8):
            tb = g * 8 + j
            pb_ = pblk[j % 2]
            pk = ("pblk", j % 2)
            self.dma(pb_, d["p_in"][li, tb * 128:(tb + 1) * 128, :], [], [pk])
            b = self.short()
            for c in range(2):
                self.tr(self.pb[b][:, c * 128:(c + 1) * 128], pb_[:, c * 128:(c + 1) * 128], [pk], [("pb", b)])
            pt = pT[j % 2]
            ptk = ("pT", j % 2)
            self.CP("act", pt, self.pb[b][:, 0:256].rearrange("p (c t) -> p c t", c=2), [("pb", b)], [ptk])
            for hh in range(2):
                bg_ = self.short()
                for k in range(8):
                    self.mm(self.pb[bg_][:, :], self.xT[:, k, tb * 128:(tb + 1) * 128], wg[hh][0][:, k, :], k == 0, k == 7,
                            [("xT", tb // 4), wg[hh][1]], [("pb", bg_)])
                bp_ = self.short()
                for c in range(2):
                    self.mm(self.pb[bp_][:, :], pt[:, c, :], wp[:, c, hh * 512:(hh + 1) * 512],
                            c == 0, c == 1, [ptk, wpk], [("pb", bp_)])
                self.A(ple[:, hh * 512:(hh + 1) * 512], self.pb[bg_][:, :], AF.Sigmoid, [("pb", bg_)], ["ple"])
                self.TT("dve", ple[:, hh * 512:(hh + 1) * 512], ple[:, hh * 512:(hh + 1) * 512], self.pb[bp_][:, :],
                        ALU.mult, ["ple", ("pb", bp_)], ["ple"])
            self.dma(xin, res_in[tb * 128:(tb + 1) * 128, :], [("res", id(res_in), tb)], ["xin"])
            h = hbuf[j % 2]
            hk = ("hbuf", j % 2)
            self.STT("dve", h, xin, ALPHA, ple, ALU.mult, ALU.add, ["xin", "ple"], [hk])
            self.TT("dve", h, h, facc[:, j, :], ALU.add, [hk], [hk])
            o = self.layer_norm_block(h, hk, gb, tb, res_out, li, None)
            if li == self.layers[-1] or self.stop_after == ("ffn", li):
                self.finals.append(o)


_IDX = _win_index()


def prepare_inputs(inputs):
    f = lambda a: np.ascontiguousarray(np.asarray(a))
    w_in = f(inputs["w_in"])
    win = np.zeros((2, D, NCOLS_R), np.float32)
    valid = _IDX >= 0
    win[:, :, valid] = w_in[:, :, _IDX[valid]]
    conv_w = f(inputs["conv_w"])
    convp = np.zeros((2, 128, 2, 34), np.float32)
    for c in range(2):
        convp[:, :, c, 0:31] = conv_w[:, :, c * 128:(c + 1) * 128].transpose(0, 2, 1)
        convp[:, :, c, 31] = f(inputs["conv_b"])[:, c * 128:(c + 1) * 128]
        convp[:, :, c, 32] = f(inputs["conv_ln_g"])[:, c * 128:(c + 1) * 128]
        convp[:, :, c, 33] = f(inputs["conv_ln_b"])[:, c * 128:(c + 1) * 128]
    pe = f(inputs["nsa_cmp_pe"])
    pe_r = np.ascontiguousarray(pe.transpose(0, 2, 3, 1).reshape(2, 128, 32))
    w2 = f(inputs["nsa_cmp_w2"])
    w2k = np.ascontiguousarray(np.concatenate([w2[:, 0], w2[:, 0]], axis=2))
    w2v = np.ascontiguousarray(w2[:, 1])
    qn = np.ascontiguousarray(f(inputs["mla_q_norm"]).reshape(2, 2, 128).transpose(0, 2, 1))
    kvn = np.ascontiguousarray(f(inputs["mla_kv_norm"]).reshape(2, 128, 1))
    wuq = f(inputs["mla_w_uq"])
    cols = []
    for h in range(4):
        b = h * 96
        cols += list(range(b, b + 96))
        cols += list(range(b, b + 64)) + list(range(b + 80, b + 96)) + list(range(b + 64, b + 80))
    uq = np.ascontiguousarray(wuq[:, :, cols])
    wukv = f(inputs["mla_w_ukv"])
    cols = []
    for h in range(4):
        cols += list(range(h * 128, h * 128 + 64))
    for h in range(4):
        cols += list(range(h * 128 + 64, h * 128 + 128))
    ukv = np.ascontiguousarray(wukv[:, :, cols])
    shared = {
        "win": win, "convp": convp, "pe_r": pe_r, "w1": f(inputs["nsa_cmp_w1"]), "w2k": w2k, "w2v": w2v,
        "qn": qn, "kvn": kvn, "uq": uq, "ukv": ukv, "wbr": f(inputs["w_branch"]), "wout": f(inputs["w_out"]),
        "ln1g": f(inputs["ln1_g"]), "ln1b": f(inputs["ln1_b"]), "ln2g": f(inputs["ln2_g"]), "ln2b": f(inputs["ln2_b"]),
        "ffn_in": f(inputs["ffn_w_in"])[0], "ffn_out": f(inputs["ffn_w_out"])[0], "router": f(inputs["moe_router"])[0],
        "moe_in": f(inputs["moe_w_in"])[0], "moe_out": f(inputs["moe_w_out"])[0],
        "pleg": f(inputs["ple_w_gate"]), "plep": f(inputs["ple_w_proj"]),
    }
    for k, v in _host_consts().items():
        shared["c_" + k] = v
    x = f(inputs["x"])
    p = f(inputs["p"])
    pos = f(inputs["positions"]).astype(np.int32)
    in_maps = []
    for b in range(8):
        m = dict(shared)
        m["x"] = x[b]
        m["p"] = np.ascontiguousarray(p[:, b])
        m["pos"] = pos[b:b + 1]
        in_maps.append(m)
    return in_maps


def kernel(**inputs):
    in_maps = prepare_inputs(inputs)
    nc = MK().build()
    res = run_bass_kernel_spmd(nc, in_maps, core_ids=list(range(8)))
    return np.stack([np.asarray(r["y"], dtype=np.float32) for r in res.results], axis=0)
```

```python
import math
import contextlib
import numpy as np
import concourse.bass as bass
import concourse.mybir as mybir
from concourse.bass_utils import run_bass_kernel_spmd

F32 = mybir.dt.float32
BF16 = mybir.dt.bfloat16
I32 = mybir.dt.int32
AF = mybir.ActivationFunctionType
ALU = mybir.AluOpType
AX = mybir.AxisListType

T = 2048
D = 1024
NB = 16
ALPHA = 4.0 ** 0.25
LN_EPS = 1e-5
RMS_EPS = 1e-6
THETA = 10000.0
D_FF = 2816
D_FFE = 3584
NE = 8

ENGS = ("pe", "act", "dve", "pool", "sp")
N_DMA_SEMS = 6


class Op:
    __slots__ = ("eng", "fn", "deps", "is_dma", "signal", "sig_val", "dma_sem", "dma_val",
                 "dma_prev", "idx")

    def __init__(self, eng, fn, is_dma):
        self.eng = eng
        self.fn = fn
        self.deps = []
        self.is_dma = is_dma
        self.signal = False
        self.sig_val = 0
        self.dma_sem = None
        self.dma_val = 0
        self.dma_prev = 0
        self.idx = 0


class Sched:
    def __init__(self):
        self.ops = {e: [] for e in ENGS}
        self.last_w = {}
        self.readers = {}
        self.all_ops = []

    def op(self, eng, fn, reads=(), writes=(), dma=False, acc=False):
        o = Op(eng, fn, dma)
        deps = []
        for k in reads:
            w = self.last_w.get(k)
            if w is not None:
                deps.append(w)
            if isinstance(k, tuple) and k[0] == "pb":
                for r in self.readers.get(k, ()):
                    if r.eng != eng:
                        deps.append(r)
        for k in writes:
            w = self.last_w.get(k)
            if w is not None and not (acc and w.eng == eng and not w.is_dma):
                deps.append(w)
            for r in self.readers.get(k, ()):
                deps.append(r)
        seen = set()
        for d in deps:
            if id(d) not in seen and d is not o:
                seen.add(id(d))
                o.deps.append(d)
        for k in reads:
            lst = self.readers.setdefault(k, [])
            if not dma:
                for i, r in enumerate(lst):
                    if r.eng == eng and not r.is_dma:
                        lst[i] = o
                        break
                else:
                    lst.append(o)
            else:
                lst.append(o)
        for k in writes:
            self.last_w[k] = o
            self.readers[k] = []
        o.idx = len(self.ops[eng])
        self.ops[eng].append(o)
        self.all_ops.append(o)
        return o

    def barrier(self):
        lasts = []
        for e in ENGS:
            ops = self.ops[e]
            nd = 0
            got_real = False
            for o in reversed(ops):
                if o.fn is None:
                    continue
                if o.is_dma:
                    if nd < N_DMA_SEMS:
                        lasts.append(o)
                        nd += 1
                elif not got_real:
                    lasts.append(o)
                    got_real = True
                if got_real and nd >= N_DMA_SEMS:
                    break
        for e in ENGS:
            o = Op(e, None, False)
            o.deps = [l for l in lasts]
            o.idx = len(self.ops[e])
            self.ops[e].append(o)
            self.all_ops.append(o)
        self.last_w = {}
        self.readers = {}

    def emit(self, nc, final_wait_ops=()):
        for fo in final_wait_ops:
            if not fo.is_dma:
                fo.signal = True
        for o in self.all_ops:
            for d in o.deps:
                if not d.is_dma:
                    d.signal = True
        cnt = {e: 0 for e in ENGS}
        for e in ENGS:
            for o in self.ops[e]:
                if o.signal and not o.is_dma:
                    cnt[e] += 1
                    o.sig_val = cnt[e]
        dma_count = {}
        for e in ENGS:
            k = 0
            for o in self.ops[e]:
                if o.is_dma:
                    j = k % N_DMA_SEMS
                    k += 1
                    key = (e, j)
                    prev = dma_count.get(key, 0)
                    o.dma_sem = key
                    o.dma_prev = prev
                    o.dma_val = prev + 16
                    dma_count[key] = prev + 16
        with contextlib.ExitStack() as st:
            sems = {e: st.enter_context(nc.semaphore("s_" + e)) for e in ENGS if cnt[e] > 0}
            dsems = {key: st.enter_context(nc.semaphore("d_%s%d" % key)) for key in dma_count}
            block = st.enter_context(nc.Block())
            regs = {"pe": block.tensor, "act": block.scalar, "dve": block.vector,
                    "pool": block.gpsimd, "sp": block.sync}

            def make(e):
                def body(eng):
                    known = {}
                    for o in self.ops[e]:
                        waits = {}
                        for d in o.deps:
                            if d.is_dma:
                                s, v = dsems[d.dma_sem], d.dma_val
                            else:
                                s, v = sems[d.eng], d.sig_val
                            kk = id(s)
                            if known.get(kk, 0) >= v:
                                continue
                            if kk not in waits or waits[kk][1] < v:
                                waits[kk] = (s, v)
                        if o.is_dma and o.dma_prev > 0:
                            s = dsems[o.dma_sem]
                            kk = id(s)
                            if known.get(kk, 0) < o.dma_prev:
                                if kk not in waits or waits[kk][1] < o.dma_prev:
                                    waits[kk] = (s, o.dma_prev)
                        for kk, (s, v) in waits.items():
                            eng.wait_ge(s, v)
                            known[kk] = v
                        if o.fn is None:
                            continue
                        ins = o.fn(eng)
                        if o.is_dma:
                            ins.then_inc(dsems[o.dma_sem], 16)
                        elif o.signal:
                            ins.then_inc(sems[e], 1)
                    if e == "sp":
                        for fo in final_wait_ops:
                            if fo.is_dma:
                                eng.wait_ge(dsems[fo.dma_sem], fo.dma_val)
                            else:
                                eng.wait_ge(sems[fo.eng], fo.sig_val)
                return body

            for e in ENGS:
                if self.ops[e] or e == "sp":
                    regs[e](make(e))


OFF_CONV, OFF_NQ, OFF_NK, OFF_MISC, OFF_KR, OFF_SB, OFF_TOK, OFF_GATE = 0, 512, 1024, 1536, 2048, 2304, 2816, 3328
NCOLS_R = 7424


def _win_index():
    sw64 = lambda b: list(range(b + 32, b + 64)) + list(range(b, b + 32))
    sw32 = lambda b: list(range(b + 16, b + 32)) + list(range(b, b + 16))
    idx = []
    idx += list(range(0, 512))
    idx += list(range(512, 768))
    for h in range(4):
        idx += sw64(512 + 64 * h)
    ks, kw = 896, 1024
    idx += list(range(ks, ks + 64)) * 2 + sw64(ks) * 2 + list(range(kw, kw + 64)) * 2 + sw64(kw) * 2
    idx += list(range(768, 896)) + list(range(1164, 1420)) + list(range(1420, 1548))
    idx += [-1] * 64 + list(range(1548, 1580)) + [-1] * 32
    idx += [-1] * 64 + sw32(1548) + [-1] * 32
    idx += list(range(1580, 1580 + 512))
    idx += list(range(960, 1024)) + list(range(1088, 1152)) + list(range(1152, 1164)) + [-1] * 116
    idx += list(range(1580 + 512, 1580 + 768))
    idx += list(range(2348, 6444))
    assert len(idx) == NCOLS_R
    return np.array(idx)


def _host_consts():
    c = {}
    c["ident"] = np.eye(128, dtype=np.float32)
    p = np.arange(128)[:, None]
    f = np.arange(128)[None, :]
    cm = np.zeros((128, 5, 128), np.float32)
    cm[:, 0] = (p <= f)
    cm[:, 1] = (p < f)
    cm[:, 2] = (p > f)
    cm[:, 3] = 1.0
    cm[:, 4] = (p == f)
    c["cmask"] = cm
    j = np.arange(128)[:, None]
    t = np.arange(T)[None, :]
    c["cmpvalid"] = ((16 * j + 31 <= t) & (j < 127)).astype(np.float32)
    n = np.arange(32)[:, None]
    c["eexp"] = ((t // 64) == n).astype(np.float32)
    jj = np.arange(127)
    nn = np.arange(32)
    ov = ((jj[:, None] * 16 < nn[None, :] * 64 + 64) & (jj[:, None] * 16 + 32 > nn[None, :] * 64)).astype(np.float32)
    ovl = np.zeros((128, 33), np.float32)
    ovl[:127, :32] = ov
    ovl[:127, 32] = 1.0
    c["ovl"] = ovl
    cur = (np.arange(T) // 64)[:, None]
    nid = np.arange(32)[None, :]
    forced = (nid == 0) | (nid == cur) | (nid == cur - 1)
    future = nid > cur
    keep = (~forced & ~future).astype(np.float32)
    add = np.where(future, -1e30, np.where(forced, 100.0, 0.0)).astype(np.float32)
    ka = np.zeros((128, 2, 16, 32), np.float32)
    ka[:, 0] = keep.reshape(16, 128, 32).transpose(1, 0, 2)
    ka[:, 1] = add.reshape(16, 128, 32).transpose(1, 0, 2)
    c["keepadd"] = ka
    rc = np.zeros((128, 4), np.float32)
    pp = np.arange(128)
    rc[:, 0] = THETA ** (-(pp % 32).astype(np.float64) / 32.0)
    rc[:, 1] = np.where((pp % 64) < 32, -1.0, 1.0)
    m = (pp >= 64) & (pp < 96)
    rc[m, 2] = THETA ** (-((pp[m] - 64) % 16).astype(np.float64) / 16.0)
    rc[m, 3] = np.where((pp[m] - 64) < 16, -1.0, 1.0)
    c["ropec"] = rc
    ind = np.zeros((128, 16, 16), np.float32)
    for kb in range(16):
        ind[:, kb, kb] = 1.0
    c["indt"] = ind
    c["iota"] = np.tile(np.arange(512, dtype=np.float32)[None, :], (128, 1))
    sg = np.zeros((16, 16, 128), np.float32)
    for kb in range(16):
        sg[kb + 1:, kb, :] = 1.0
    c["selgt"] = sg
    return c


CONST_SHAPES = {"iota": [128, 512], "ident": [128, 128], "cmask": [128, 5, 128], "cmpvalid": [128, T], "eexp": [32, T],
                "ovl": [128, 33], "keepadd": [128, 2, 16, 32], "ropec": [128, 4], "indt": [128, 16, 16],
                "selgt": [16, 16, 128]}


class MK:
    NW = 3

    def __init__(self, layers=(0, 1), debug=False, stop_after=None):
        self.layers = layers
        self.debug = debug
        self.stop_after = stop_after
        self.nc = bass.Bass("TRN2", target_bir_lowering=False)
        self.S = Sched()
        self.st = contextlib.ExitStack()
        self.st.enter_context(self.nc.allow_low_precision(reason="bf16 matmul operands / fp32 accumulation by design"))
        self.wi = 0
        self.stg_i = 0
        self.si = 0
        self.ai = 0
        self.finals = []
        self.dbg_outs = {}

    def din(self, name, shape, dt=F32):
        return self.nc.dram_tensor(name, list(shape), dt, kind="ExternalInput").ap()

    def dout(self, name, shape, dt=F32):
        return self.nc.dram_tensor(name, list(shape), dt, kind="ExternalOutput").ap()

    def sb(self, name, shape, dt):
        return self.st.enter_context(self.nc.sbuf_tensor(name, list(shape), dt))

    def arena_reset(self):
        self.S.barrier()
        self.aoff = 0

    def carve(self, shape, dt, parts=128):
        n = 1
        for s in shape:
            n *= s
        nbytes = n * (4 if dt in (F32, I32) else 2)
        nbytes = (nbytes + 63) // 64 * 64
        off = self.aoff
        self.aoff += nbytes
        assert self.aoff <= self.ARENA_BYTES, (self.aoff, self.ARENA_BYTES)
        v = self.arena[0:parts, off // 2:(off + n * (4 if dt in (F32, I32) else 2)) // 2]
        if dt != BF16:
            v = v.bitcast(dt)
        if len(shape) == 2:
            v = v.rearrange("p (a b) -> p a b", a=shape[0])
        elif len(shape) == 3:
            v = v.rearrange("p (a b c) -> p a b c", a=shape[0], b=shape[1])
        return v

    def short(self):
        b = self.si % 4
        self.si += 1
        return b

    def accb(self):
        b = 4 + self.ai % 4
        self.ai += 1
        return b

    def mm(self, out, lhsT, rhs, start, stop, r, w):
        self.S.op("pe", lambda e: e.matmul(out, lhsT=lhsT, rhs=rhs, start=start, stop=stop),
                  reads=r, writes=w, acc=True)

    def tr(self, out, in_, r, w, parts=128):
        idt = self.ident[0:parts, 0:parts]
        self.S.op("pe", lambda e: e.transpose(out, in_, idt), reads=list(r) + ["ident"], writes=w, acc=True)

    def A(self, out, in_, func, r, w, bias=None, scale=None, accum=None):
        kw = {}
        if bias is not None:
            kw["bias"] = bias
        if scale is not None:
            kw["scale"] = scale
        if accum is not None:
            kw["accum_out"] = accum
        self.S.op("act", lambda e: e.activation(out=out, in_=in_, func=func, **kw), reads=r, writes=w)

    def TT(self, eng, out, in0, in1, op, r, w):
        self.S.op(eng, lambda e: e.tensor_tensor(out=out, in0=in0, in1=in1, op=op), reads=r, writes=w)

    def TS(self, eng, out, in0, s1, s2, op0, op1, r, w):
        if op1 is None:
            self.S.op(eng, lambda e: e.tensor_scalar(out=out, in0=in0, scalar1=s1, scalar2=None, op0=op0),
                      reads=r, writes=w)
        else:
            self.S.op(eng, lambda e: e.tensor_scalar(out=out, in0=in0, scalar1=s1, scalar2=s2, op0=op0, op1=op1),
                      reads=r, writes=w)

    def STT(self, eng, out, in0, scalar, in1, op0, op1, r, w):
        self.S.op(eng, lambda e: e.scalar_tensor_tensor(out=out, in0=in0, scalar=scalar, in1=in1, op0=op0, op1=op1),
                  reads=r, writes=w)

    def CP(self, eng, out, in_, r, w):
        if eng == "act":
            self.S.op("act", lambda e: e.activation(out=out, in_=in_, func=AF.Copy), reads=r, writes=w)
        else:
            self.S.op(eng, lambda e: e.tensor_copy(out=out, in_=in_), reads=r, writes=w)

    def MS(self, eng, out, val, w):
        self.S.op(eng, lambda e: e.memset(out, val), writes=w)

    def dma(self, out, in_, r, w, eng="sp"):
        return self.S.op(eng, lambda e: e.dma_start(out=out, in_=in_), reads=r, writes=w, dma=True)

    CAST_ENGS = ("dve", "act", "dve", "act")

    def cast_load(self, dst, src, parts, free_shape, dkey):
        n = 1
        for x_ in free_shape:
            n *= x_
        assert n <= 2048, n
        si_ = self.stg_i % len(self.stage)
        ce = self.CAST_ENGS[self.stg_i % 4]
        self.stg_i += 1
        stg = self.stage[si_][0:parts, 0:n]
        if len(free_shape) == 2:
            stg = stg.rearrange("p (a b) -> p a b", a=free_shape[0])
        elif len(free_shape) == 3:
            stg = stg.rearrange("p (a b c) -> p a b c", a=free_shape[0], b=free_shape[1])
        sk = ("stg", si_)
        self.dma(stg, src, [], [sk])
        self.CP(ce, dst, stg, [sk], [dkey])

    def dma_w1(self, w1t, src, c):
        si_ = self.stg_i % len(self.stage)
        ce = self.CAST_ENGS[self.stg_i % 4]
        self.stg_i += 1
        ps_ = slice(c * 64, (c + 1) * 64)
        stg = self.stage[si_][ps_, 0:2048].rearrange("p (l e) -> p l e", l=32)
        sk = ("stg", si_)
        self.dma(stg, src.rearrange("(l d) e -> d l e", d=64), [], [sk])
        self.CP(ce, w1t[ps_, :, :], stg, [sk], ["w1t"])

    def wload(self, src, kc, n, rows=128):
        slot = self.wi % self.NW
        self.wi += 1
        assert kc * n <= 4096
        v = self.wring[slot][0:rows, 0:kc * n].rearrange("p (c n) -> p c n", c=kc)
        srcv = src.rearrange("(c p) n -> p c n", p=rows)
        key = ("w", slot)
        step = max(1, 2048 // n)
        for k0 in range(0, kc, step):
            k1 = min(kc, k0 + step)
            self.cast_load(v[:, k0:k1, :], srcv[:, k0:k1, :], rows, [k1 - k0, n], key)
        return v, key

    def proj_fm(self, wv, wkey, c0, M, tt, src=None, srckey=None, kc=8):
        b = self.short()
        src = self.xT if src is None else src
        srckey = ("xT", tt) if srckey is None else srckey
        for k in range(kc):
            self.mm(self.pb[b][0:M, :], wv[:, k, c0:c0 + M], src[:, k, tt * 512:(tt + 1) * 512],
                    k == 0, k == kc - 1, [wkey, srckey], [("pb", b)])
        return b

    def build(self):
        nc, S = self.nc, self.S
        L = self.layers
        x_in = self.din("x", [T, D])
        p_in = self.din("p", [2, T, 256])
        pos_in = self.din("pos", [1, T], I32)
        win = self.din("win", [2, D, NCOLS_R])
        convp = self.din("convp", [2, 128, 2, 34])
        pe_r = self.din("pe_r", [2, 128, 32])
        w1 = self.din("w1", [2, 2, 2048, 64])
        w2k = self.din("w2k", [2, 64, 128])
        w2v = self.din("w2v", [2, 64, 64])
        qn = self.din("qn", [2, 128, 2])
        kvn = self.din("kvn", [2, 128, 1])
        uq = self.din("uq", [2, 256, 768])
        ukv = self.din("ukv", [2, 128, 512])
        wbr = self.din("wbr", [2, 4, 256, D])
        wout = self.din("wout", [2, D, D])
        ln1g = self.din("ln1g", [2, D])
        ln1b = self.din("ln1b", [2, D])
        ln2g = self.din("ln2g", [2, D])
        ln2b = self.din("ln2b", [2, D])
        lite = self.stop_after is not None and self.stop_after[0] in ("conv", "nsa", "mla", "sb", "mix")
        need_ffn = (0 in L) and not lite
        need_moe = (1 in L) and not (lite and self.stop_after[1] == 0) and self.stop_after != ("ffn", 0)
        ffn_in = self.din("ffn_in", [D, 2 * D_FF]) if need_ffn else None
        ffn_out = self.din("ffn_out", [D_FF, D]) if need_ffn else None
        router = self.din("router", [D, NE])
        moe_in = self.din("moe_in", [NE, D, 2 * D_FFE]) if need_moe else None
        moe_out = self.din("moe_out", [NE, D_FFE, D]) if need_moe else None
        pleg = self.din("pleg", [2, D, D])
        plep = self.din("plep", [2, 256, D])
        cst = {k: self.din("c_" + k, v) for k, v in CONST_SHAPES.items()}
        y_out = self.dout("y", [T, D])
        xa = self.dout("xa", [T, D])
        xb = self.dout("xb", [T, D])
        tabs_d = self.dout("tabs_d", [2, 128, 2 * T], BF16)
        self.dram = dict(locals())

        self.xT = self.sb("xT", [128, 8, T], BF16)
        self.wring = [self.sb("wr%d" % i, [128, 4096], BF16) for i in range(self.NW)]
        self.stage = [self.sb("stg%d" % i, [128, 2048], F32) for i in range(2)]
        self.ident = self.sb("ident", [128, 128], F32)
        self.cmask = self.sb("cmask", [128, 5, 128], BF16)
        self.onesf = self.sb("onesf", [128, 128], F32)
        self.gates = self.sb("gates", [128, NB, NE], F32)
        self.pb = [self.st.enter_context(nc.psum_tensor("pb%d" % i, [128, 512], F32)) for i in range(8)]
        self.ARENA_BYTES = 133 * 1024
        self.arena = self.sb("arena", [128, self.ARENA_BYTES // 2], BF16)
        self.aoff = 0

        self.dma(self.ident[:], cst["ident"], [], ["ident"])
        self.cast_load(self.cmask[:], cst["cmask"], 128, [5, 128], "cmask")
        self.MS("dve", self.onesf[:], 1.0, ["onesf"])

        self.setup_rope(pos_in, cst)
        self.load_xT(x_in)

        res_in = x_in
        outs = [(xa, xb), (xa, y_out)]
        for li in L:
            mid, fin = outs[li]
            if li == 1:
                res_in = xb
            if self.mixer_phase(li, res_in, mid):
                break
            if self.stop_after == ("mix", li):
                break
            self.ffn_phase(li, mid, fin)
            if self.stop_after == ("ffn", li):
                break
        S.emit(nc, final_wait_ops=self.finals)
        self.st.close()
        return nc

    def setup_rope(self, pos_in, cst):
        self.arena_reset()
        self.tabN = self.carve([2, T], BF16)
        self.tabM = self.carve([2, T], BF16)
        pi = self.carve([T], I32)
        pf = self.carve([T], F32)
        ang = self.carve([T], F32)
        kf = self.carve([T], F32)
        ki = self.carve([T], I32)
        rc = self.carve([4], F32)
        self.dma(pi, pos_in[0:1, :].to_broadcast([128, T]), [], ["pi"])
        self.dma(rc, cst["ropec"], [], ["rc"])
        self.CP("dve", pf, pi, ["pi"], ["pf"])
        for tab, ic, sc in ((self.tabN, 0, 1), (self.tabM, 2, 3)):
            for which in range(2):
                shift = math.pi / 2 if which == 0 else 0.0
                self.TS("dve", ang, pf, rc[:, ic:ic + 1], shift, ALU.mult, ALU.add, ["pf", "rc"], ["ang"])
                self.TS("dve", kf, ang, 1.0 / (2 * math.pi), None, ALU.mult, None, ["ang"], ["kf"])
                self.CP("dve", ki, kf, ["kf"], ["ki"])
                self.CP("dve", kf, ki, ["ki"], ["kf"])
                self.STT("dve", ang, kf, -2 * math.pi, ang, ALU.mult, ALU.add, ["kf", "ang"], ["ang"])
                self.TS("dve", kf, ang, math.pi, -2 * math.pi, ALU.is_gt, ALU.mult, ["ang"], ["kf"])
                self.TT("dve", ang, ang, kf, ALU.add, ["ang", "kf"], ["ang"])
                self.TS("dve", kf, ang, -math.pi, 2 * math.pi, ALU.is_lt, ALU.mult, ["ang"], ["kf"])
                self.TT("dve", ang, ang, kf, ALU.add, ["ang", "kf"], ["ang"])
                self.A(ang, ang, AF.Sin, ["ang"], ["ang"])
                if which == 0:
                    self.CP("dve", tab[:, 0, :], ang, ["ang"], ["tab"])
                else:
                    self.TS("dve", tab[:, 1, :], ang, rc[:, sc:sc + 1], None, ALU.mult, None, ["ang", "rc"], ["tab"])
        td = self.dram["tabs_d"]
        self.dma(td[0], self.tabN.rearrange("p a t -> p (a t)"), ["tab"], ["tabs_d"])
        self.dma(td[1], self.tabM.rearrange("p a t -> p (a t)"), ["tab"], ["tabs_d"])

    def x_to_xT(self, xblk, xkey, tb, rt=None):
        tt = tb // 4
        for half in range(2):
            b = self.short()
            for c in range(4):
                cc = half * 4 + c
                self.tr(self.pb[b][:, c * 128:(c + 1) * 128], xblk[:, cc * 128:(cc + 1) * 128], [xkey], [("pb", b)])
            dst = self.xT[:, half * 4:half * 4 + 4, tb * 128:(tb + 1) * 128]
            src = self.pb[b][:, :].rearrange("p (c t) -> p c t", c=4)
            self.CP("act" if half == 0 else "dve", dst, src, [("pb", b)], [("xT", tt)])
            if rt is not None:
                rt(half, b)

    def load_xT(self, x_in):
        self.arena_reset()
        xbs = [self.carve([D], F32) for _ in range(2)]
        for tb in range(NB):
            xb_ = xbs[tb % 2]
            key = ("xblk", tb % 2)
            self.dma(xb_, x_in[tb * 128:(tb + 1) * 128, :], [], [key])
            self.x_to_xT(xb_, key, tb)

    def mixer_phase(self, li, res_in, res_out):
        d = self.dram
        self.arena_reset()
        self.stage = self.stage[0:2]
        self.yT = [self.carve([2, T], BF16) for _ in range(4)]
        self.mixer_base = self.aoff
        for n, (nm, fn) in enumerate((("conv", self.conv_branch), ("nsa", self.nsa_branch), ("mla", self.mla_branch),
                                     ("sb", self.sb_branch))):
            only = getattr(self, "only", None)
            if only is None or nm in only:
                fn(li)
                self.dbg("yT%d_%d" % (n, li), self.yT[n], [128, 2, T], ["yT%d" % n])
            self.aoff = self.mixer_base
            self.S.barrier()
            if self.stop_after == (nm, li):
                return True
        self.merge_ln1(li, res_in, res_out)

    def dbg(self, name, ap, shape, keys, dt=BF16):
        if not self.debug:
            return
        o = self.dout("dbg_" + name, shape, dt)
        self.finals.append(self.dma(o, ap, keys, []))

    def conv_branch(self, li):
        d = self.dram
        win = d["win"]
        cp = self.carve([2, 34], F32)
        self.dma(cp, d["convp"][li], [], ["cp"])
        hp = self.carve([2, 30 + T], F32)
        acc = self.carve([2, T], F32)
        sig = [self.carve([512], F32) for _ in range(2)]
        self.MS("pool", hp[:, :, 0:30], 0.0, ["hp"])
        wv, wk = self.wload(win[li][:, OFF_CONV:OFF_CONV + 512], 8, 512)
        for tt in range(4):
            for c in range(2):
                ba = self.proj_fm(wv, wk, c * 128, 128, tt)
                bg = self.proj_fm(wv, wk, 256 + c * 128, 128, tt)
                sg = sig[c]
                self.A(sg, self.pb[bg][:, :], AF.Sigmoid, [("pb", bg)], [("sig", c)])
                self.TT("dve", hp[:, c, 30 + tt * 512:30 + (tt + 1) * 512], self.pb[ba][:, :], sg, ALU.mult,
                        [("pb", ba), ("sig", c)], ["hp"])
        for c in range(2):
            eng = "dve"
            self.TS(eng, acc[:, c, :], hp[:, c, 0:T], cp[:, c, 0:1], cp[:, c, 31:32], ALU.mult, ALU.add,
                    ["hp", "cp"], [("acc", c)])
            for w in range(1, 31):
                self.STT(eng, acc[:, c, :], hp[:, c, w:w + T], cp[:, c, w:w + 1], acc[:, c, :], ALU.mult, ALU.add,
                         ["hp", "cp", ("acc", c)], [("acc", c)])
        sq = [self.carve([512], F32) for _ in range(2)]
        m2 = self.carve([512], F32)
        rstd = self.carve([512], F32)
        dd = [self.carve([512], F32) for _ in range(2)]
        for tt in range(4):
            sl = slice(tt * 512, (tt + 1) * 512)
            bm = self.short()
            for c in range(2):
                self.mm(self.pb[bm][:, :], self.onesf[:, :], acc[:, c, sl], c == 0, c == 1,
                        ["onesf", ("acc", c)], [("pb", bm)])
            bq = self.short()
            for c in range(2):
                self.A(sq[c], acc[:, c, sl], AF.Square, [("acc", c)], [("sq", c)])
            for c in range(2):
                self.mm(self.pb[bq][:, :], self.onesf[:, :], sq[c], c == 0, c == 1,
                        ["onesf", ("sq", c)], [("pb", bq)])
            self.A(m2, self.pb[bm][:, :], AF.Square, [("pb", bm)], ["m2"], scale=1.0 / 256)
            self.STT("dve", rstd, self.pb[bq][:, :], 1.0 / 256, m2, ALU.mult, ALU.subtract, [("pb", bq), "m2"], ["rstd"])
            self.A(rstd, rstd, AF.Sqrt, ["rstd"], ["rstd"], bias=LN_EPS)
            self.S.op("dve", lambda e, r=rstd: e.reciprocal(out=r, in_=r), reads=["rstd"], writes=["rstd"])
            for c in range(2):
                self.STT("dve", dd[c], self.pb[bm][:, :], -1.0 / 256, acc[:, c, sl], ALU.mult, ALU.add,
                         [("pb", bm), ("acc", c)], [("dd", c)])
                self.TT("dve", dd[c], dd[c], rstd, ALU.mult, [("dd", c), "rstd"], [("dd", c)])
                self.A(self.yT[0][:, c, sl], dd[c], AF.Silu, [("dd", c), "cp"], ["yT0"],
                       scale=cp[:, c, 32:33], bias=cp[:, c, 33:34])

    def y_to_yT(self, ytile, ykey, n, qb):
        b = self.short()
        for c in range(2):
            self.tr(self.pb[b][:, c * 128:(c + 1) * 128], ytile[:, c * 128:(c + 1) * 128], [ykey], [("pb", b)])
        self.CP("act", self.yT[n][:, :, qb * 128:(qb + 1) * 128],
                self.pb[b][:, 0:256].rearrange("p (c t) -> p c t", c=2), [("pb", b)], ["yT%d" % n])

    def softmax_attn_block(self, qb, kbs, kT_fn, qT_ap, v_fn, scale, extra_mask=None, nm=""):
        ob = self.accb()
        n = len(kbs)

        def front(g0):
            grp = kbs[g0:g0 + 4]
            sbk = self.short()
            for i, (kb, mt) in enumerate(grp):
                kT, kkeys = kT_fn(kb)
                self.mm(self.pb[sbk][:, i * 128:(i + 1) * 128], kT, qT_ap[0], True, True,
                        list(kkeys) + list(qT_ap[1]), [("pb", sbk)])
            ei = self.ei % 5
            self.ei += 1
            E = self.Ebuf[ei]
            ek = ("E", ei)
            w = len(grp) * 128
            self.A(E[:, 0:w], self.pb[sbk][:, 0:w], AF.Exp, [("pb", sbk)], [ek], scale=scale)
            if extra_mask is not None:
                mtile, mkey = extra_mask
                self.TT("dve", E[:, 0:w], E[:, 0:w], mtile[:, g0 * 128:g0 * 128 + w], ALU.mult, [ek, mkey], [ek])
            else:
                for i, (kb, mt) in enumerate(grp):
                    if mt is not None:
                        mi = {"le": 0, "lt": 1, "gt": 2}[mt]
                        self.TT("dve", E[:, i * 128:(i + 1) * 128], E[:, i * 128:(i + 1) * 128],
                                self.cmask[:, mi, :], ALU.mult, [ek, "cmask"], [ek])
            return g0, grp, E, ek

        def back(st_):
            g0, grp, E, ek = st_
            for i, (kb, mt) in enumerate(grp):
                v, vkeys = v_fn(kb)
                gi = g0 + i
                self.mm(self.pb[ob][:, 0:65], E[:, i * 128:(i + 1) * 128], v, gi == 0, gi == n - 1,
                        [ek] + list(vkeys), [("pb", ob)])

        prev = None
        for g0 in range(0, n, 4):
            cur = front(g0)
            if prev is not None:
                back(prev)
            prev = cur
        back(prev)
        return ob

    def nsa_branch(self, li):
        d = self.dram
        win = d["win"]
        cst = {k: d["cst"][k] for k in d["cst"]}
        self.tabN = self.carve([2, T], BF16)
        self.dma(self.tabN.rearrange("p a t -> p (a t)"), d["tabs_d"][0], [], ["tab"])
        QT = self.carve([2, T], BF16)
        QR = self.carve([2, T], BF16)
        KS = self.carve([T], BF16)
        KW = self.carve([T], BF16)
        KCV = self.carve([T], BF16)
        vS = self.carve([NB, 65], BF16)
        vW = self.carve([NB, 65], BF16)
        sg = self.carve([NB, 12], F32)
        cmpvalid = self.carve([T], BF16)
        eexp = self.carve([T], BF16, parts=32)
        keepadd = self.carve([2, NB, 32], F32)
        VC = self.carve([97], BF16)
        w1t = self.carve([32, 64], BF16)
        pet = self.carve([32], BF16)
        w2kt = self.carve([128], BF16, parts=64)
        w2vt = self.carve([64], BF16, parts=64)
        hid = [self.carve([127], BF16, parts=64) for _ in range(2)]
        hb = self.carve([2], F32, parts=64)
        kcT = self.carve([127], BF16)
        t1 = [self.carve([512], F32) for _ in range(2)]
        t2 = [self.carve([512], F32) for _ in range(2)]
        self.Ebuf = [self.carve([512], BF16) for _ in range(5)]
        self.ei = 0
        ytile = [self.carve([256], F32) for _ in range(2)]
        selmask = [self.carve([NB * 128], BF16) for _ in range(2)]
        small = [self.carve([64], F32) for _ in range(2)]
        imp = [self.carve([32], F32) for _ in range(2)]
        scr = [self.carve([32], F32) for _ in range(2)]
        selT = [self.carve([128], BF16, parts=32) for _ in range(2)]
        self.cast_load(cmpvalid, cst["cmpvalid"], 128, [T], "cmpvalid")
        self.cast_load(eexp, cst["eexp"], 32, [T], "eexp")
        self.dma(keepadd, cst["keepadd"], [], ["keepadd"])
        self.cast_load(VC[:, 64:97], cst["ovl"], 128, [33], "VCc")
        for c in range(2):
            self.dma_w1(w1t, d["w1"][li, c], c)
        self.cast_load(pet, d["pe_r"][li], 128, [32], "pet")
        self.cast_load(w2kt, d["w2k"][li], 64, [128], "w2kt")
        self.cast_load(w2vt, d["w2v"][li], 64, [64], "w2vt")
        self.MS("dve", vS[:, :, 64:65], 1.0, ["vS"])
        self.MS("dve", vW[:, :, 64:65], 1.0, ["vW"])
        wv, wk = self.wload(win[li][:, OFF_NQ:OFF_NQ + 512], 8, 512)
        for tt in range(4):
            sl = slice(tt * 512, (tt + 1) * 512)
            for c in range(2):
                b0 = self.proj_fm(wv, wk, c * 128, 128, tt)
                b1 = self.proj_fm(wv, wk, 256 + c * 128, 128, tt)
                self.CP("act", QT[:, c, sl], self.pb[b0][:, :], [("pb", b0)], ["QT"])
                self.TT("dve", t1[c], self.pb[b0][:, :], self.tabN[:, 0, sl], ALU.mult, [("pb", b0), "tab"], [("t1", c)])
                self.TT("dve", t2[c], self.pb[b1][:, :], self.tabN[:, 1, sl], ALU.mult, [("pb", b1), "tab"], [("t2", c)])
                self.TT("dve", QR[:, c, sl], t1[c], t2[c], ALU.add, [("t1", c), ("t2", c)], ["QR"])
        wv, wk = self.wload(win[li][:, OFF_NK:OFF_NK + 512], 8, 512)
        for tt in range(4):
            sl = slice(tt * 512, (tt + 1) * 512)
            for c, dst, dk in ((0, KS, "KS"), (1, KW, "KW")):
                b0 = self.proj_fm(wv, wk, c * 256, 128, tt)
                b1 = self.proj_fm(wv, wk, c * 256 + 128, 128, tt)
                self.TT("dve", t1[c], self.pb[b0][:, :], self.tabN[:, 0, sl], ALU.mult, [("pb", b0), "tab"], [("t1", c)])
                self.TT("dve", t2[c], self.pb[b1][:, :], self.tabN[:, 1, sl], ALU.mult, [("pb", b1), "tab"], [("t2", c)])
                self.TT("dve", dst[:, sl], t1[c], t2[c], ALU.add, [("t1", c), ("t2", c)], [dk])
        wvm, wkm = self.wload(win[li][:, OFF_MISC:OFF_MISC + 128], 8, 128)
        for tt in range(4):
            sl = slice(tt * 512, (tt + 1) * 512)
            b0 = self.proj_fm(wvm, wkm, 0, 128, tt)
            self.CP("act", KCV[:, sl], self.pb[b0][:, :], [("pb", b0)], ["KCV"])
        wvt, wkt = self.wload(win[li][:, OFF_TOK:OFF_TOK + 140], 8, 140)
        for tb in range(NB):
            b = self.short()
            for k in range(8):
                self.mm(self.pb[b][:, 0:140], self.xT[:, k, tb * 128:(tb + 1) * 128], wvt[:, k, :], k == 0, k == 7,
                        [wkt, ("xT", tb // 4)], [("pb", b)])
            self.CP("act", vS[:, tb, 0:64], self.pb[b][:, 0:64], [("pb", b)], ["vS"])
            self.CP("dve", vW[:, tb, 0:64], self.pb[b][:, 64:128], [("pb", b)], ["vW"])
            self.A(sg[:, tb, :], self.pb[b][:, 128:140], AF.Sigmoid, [("pb", b)], ["sg"])
        for c in range(2):
            ps_ = slice(c * 64, (c + 1) * 64)
            b = self.short()
            for l in range(32):
                self.mm(self.pb[b][0:64, 0:127], w1t[ps_, l, :], KCV[ps_, l:l + 16 * 126 + 1:16], l == 0, l == 31,
                        ["w1t", "KCV"], [("pb", b)])
            b2 = self.short()
            for l in range(32):
                self.mm(self.pb[b2][0:64, 0:1], w1t[ps_, l, :], pet[ps_, l:l + 1], l == 0, l == 31,
                        ["w1t", "pet"], [("pb", b2)])
            self.CP("dve", hb[:, c:c + 1], self.pb[b2][0:64, 0:1], [("pb", b2)], ["hb"])
            self.A(hid[c], self.pb[b][0:64, 0:127], AF.Gelu_apprx_tanh, [("pb", b), "hb"], [("hid", c)],
                   bias=hb[:, c:c + 1])
        b = self.short()
        self.mm(self.pb[b][:, 0:127], w2kt[:, :], hid[0], True, True, ["w2kt", ("hid", 0)], [("pb", b)])
        self.CP("act", kcT, self.pb[b][:, 0:127], [("pb", b)], ["kcT"])
        b = self.short()
        self.mm(self.pb[b][0:127, 0:64], hid[1], w2vt[:, :], True, True, ["w2vt", ("hid", 1)], [("pb", b)])
        self.CP("act", VC[0:127, 0:64], self.pb[b][0:127, 0:64], [("pb", b)], ["VCv"])
        for qb in range(NB):
            qs = slice(qb * 128, (qb + 1) * 128)
            yt = ytile[qb % 2]
            yk = ("yt", qb % 2)
            sm = small[qb % 2]
            smk = ("small", qb % 2)
            im = imp[qb % 2]
            imk = ("imp", qb % 2)
            sbk2 = [self.short(), self.short()]
            for h in range(4):
                hp_ = slice((h % 2) * 64, (h % 2) * 64 + 64)
                bb_ = sbk2[h % 2]
                self.mm(self.pb[bb_][0:127, (h // 2) * 128:(h // 2 + 1) * 128], kcT[hp_, :], QT[hp_, h // 2, qs], True, True,
                        ["kcT", "QT"], [("pb", bb_)])
            ei = self.ei % 5
            self.ei += 1
            E = self.Ebuf[ei]
            ek = ("E", ei)
            for h in range(4):
                bb_ = sbk2[h % 2]
                self.A(E[0:127, h * 128:(h + 1) * 128], self.pb[bb_][0:127, (h // 2) * 128:(h // 2 + 1) * 128], AF.Exp,
                       [("pb", bb_)], [ek], scale=0.125)
            for h in range(4):
                self.TT("dve", E[0:127, h * 128:(h + 1) * 128], E[0:127, h * 128:(h + 1) * 128], cmpvalid[0:127, qs],
                        ALU.mult, [ek, "cmpvalid"], [ek])
            ob = self.accb()
            for h in range(4):
                self.mm(self.pb[ob][:, h * 97:(h + 1) * 97], E[0:127, h * 128:(h + 1) * 128], VC[0:127, :], True, True,
                        [ek, "VCv", "VCc"], [("pb", ob)])
            P = self.pb[ob]
            for h in range(4):
                self.TS("dve", sm[:, h:h + 1], P[:, 97 * h + 96:97 * h + 97], 1e-30, None, ALU.max, None, [("pb", ob)], [smk])
            self.S.op("dve", lambda e, s_=sm: e.reciprocal(out=s_[:, 0:4], in_=s_[:, 0:4]), reads=[smk], writes=[smk])
            for h in range(4):
                if h == 0:
                    self.TS("dve", im, P[:, 64:96], sm[:, 0:1], None, ALU.mult, None, [("pb", ob), smk], [imk])
                else:
                    self.STT("dve", im, P[:, 97 * h + 64:97 * h + 96], sm[:, h:h + 1], im, ALU.mult, ALU.add,
                             [("pb", ob), smk, imk], [imk])
            for h in range(4):
                self.TT("dve", sm[:, 4 + h:5 + h], sm[:, h:h + 1], sg[:, qb, 3 * h:3 * h + 1], ALU.mult, [smk, "sg"], [smk])
                self.A(yt[:, h * 64:(h + 1) * 64], P[:, 97 * h:97 * h + 64], AF.Copy, [("pb", ob), smk], [yk],
                       scale=sm[:, 4 + h:5 + h])
            mk = None
            if qb >= 8:
                sc_ = scr[qb % 2]
                sck = ("scr", qb % 2)
                self.TT("dve", im, im, keepadd[:, 0, qb, :], ALU.mult, [imk, "keepadd"], [imk])
                self.TT("dve", im, im, keepadd[:, 1, qb, :], ALU.add, [imk, "keepadd"], [imk])
                self.S.op("dve", lambda e, s_=sm, i_=im: e.max(out=s_[:, 8:16], in_=i_), reads=[imk, smk], writes=[smk])
                self.S.op("dve", lambda e, s_=sm, i_=im, c_=sc_: e.match_replace(out=c_, in_to_replace=s_[:, 8:16],
                                                                                in_values=i_, imm_value=-1e30),
                          reads=[imk, smk], writes=[sck])
                self.S.op("dve", lambda e, s_=sm, c_=sc_: e.max(out=s_[:, 16:24], in_=c_), reads=[sck, smk], writes=[smk])
                self.TS("dve", sc_, im, sm[:, 23:24], None, ALU.is_ge, None, [imk, smk], [sck])
                b = self.short()
                self.tr(self.pb[b][0:32, 0:128], sc_, [sck], [("pb", b)])
                sT = selT[qb % 2]
                stk = ("selT", qb % 2)
                self.CP("act", sT, self.pb[b][0:32, 0:128], [("pb", b)], [stk])
                smt = selmask[qb % 2]
                mk = ("selmask", qb % 2)
                for g0 in range(0, qb + 1, 4):
                    n = min(4, qb + 1 - g0)
                    b = self.short()
                    for i in range(n):
                        kb = g0 + i
                        self.mm(self.pb[b][:, i * 128:(i + 1) * 128], eexp[:, kb * 128:(kb + 1) * 128], sT, True, True,
                                ["eexp", stk], [("pb", b)])
                    self.CP("act", smt[:, g0 * 128:(g0 + n) * 128], self.pb[b][:, 0:n * 128], [("pb", b)], [mk])
                self.TT("dve", smt[:, qb * 128:(qb + 1) * 128], smt[:, qb * 128:(qb + 1) * 128], self.cmask[:, 0, :],
                        ALU.mult, [mk, "cmask"], [mk])
            for h in range(4):
                hp_ = slice((h % 2) * 64, (h % 2) * 64 + 64)
                qT_ap = (QR[hp_, h // 2, qs], ["QR"])
                for br, KT, kkey, V, vkey, gcol in ((1, KS, "KS", vS, "vS", 3 * h + 1), (2, KW, "KW", vW, "vW", 3 * h + 2)):
                    if br == 1:
                        kbs = [(kb, "le" if kb == qb else None) for kb in range(qb + 1)]
                        em = (selmask[qb % 2], mk) if qb >= 8 else None
                    else:
                        kbs = [(kb, "le" if kb == qb else ("gt" if kb == qb - 4 else None))
                               for kb in range(max(0, qb - 4), qb + 1)]
                        em = None
                    ob = self.softmax_attn_block(
                        qb, kbs, lambda kb, KT=KT, kkey=kkey: (KT[hp_, kb * 128:(kb + 1) * 128], [kkey]), qT_ap,
                        lambda kb, V=V, vkey=vkey: (V[:, kb, :], [vkey]), 0.125, extra_mask=em)
                    fk = ("fac", qb % 2)
                    fac = sm[:, 24 + 2 * h + (br - 1):25 + 2 * h + (br - 1)]
                    self.S.op("dve", lambda e, f_=fac, o_=self.pb[ob][:, 64:65]: e.reciprocal(out=f_, in_=o_),
                              reads=[("pb", ob), smk], writes=[smk])
                    self.TT("dve", fac, fac, sg[:, qb, gcol:gcol + 1], ALU.mult, [smk, "sg"], [smk])
                    self.STT("dve", yt[:, h * 64:(h + 1) * 64], self.pb[ob][:, 0:64], fac, yt[:, h * 64:(h + 1) * 64],
                             ALU.mult, ALU.add, [("pb", ob), smk, yk], [yk])
            self.y_to_yT(yt, yk, 1, qb)

    def mla_branch(self, li):
        d = self.dram
        win = d["win"]
        self.tabM = self.carve([2, T], BF16)
        self.dma(self.tabM.rearrange("p a t -> p (a t)"), d["tabs_d"][1], [], ["tab"])
        qg = self.carve([2, T], BF16)
        kvg = self.carve([T], BF16)
        CS = self.carve([2, T], BF16)
        rkv = self.carve([T], BF16)
        rkt = self.carve([NB], F32)
        Vm = self.carve([NB, 4, 65], BF16)
        QH = [self.carve([T], BF16) for _ in range(2)]
        KH = [self.carve([T], BF16) for _ in range(2)]
        KR = self.carve([T], BF16)
        nrm = self.carve([4], F32)
        sq = [self.carve([512], F32) for _ in range(2)]
        t1 = [self.carve([512], F32) for _ in range(2)]
        t2 = [self.carve([512], F32) for _ in range(2)]
        rs = self.carve([512], F32)
        self.Ebuf = [self.carve([512], BF16) for _ in range(5)]
        self.ei = 0
        self.ymla = self.carve([NB, 256], F32)
        small = [self.carve([8], F32) for _ in range(2)]
        self.dma(nrm[:, 0:2], d["qn"][li], [], ["nrm"])
        self.dma(nrm[:, 2:3], d["kvn"][li], [], ["nrm"])
        self.MS("dve", Vm[:, :, :, 64:65], 1.0, ["Vm"])
        wvm, wkm = self.wload(win[li][:, OFF_MISC + 128:OFF_MISC + 512], 8, 384)
        for tt in range(4):
            sl = slice(tt * 512, (tt + 1) * 512)
            bq = []
            for c in range(2):
                b0 = self.proj_fm(wvm, wkm, c * 128, 128, tt)
                bq.append(b0)
                self.A(qg[:, c, sl], self.pb[b0][:, :], AF.Copy, [("pb", b0), "nrm"], ["qg"], scale=nrm[:, c:c + 1])
                self.A(sq[c], self.pb[b0][:, :], AF.Square, [("pb", b0)], [("sq", c)])
            bs = self.short()
            for c in range(2):
                self.mm(self.pb[bs][:, :], self.onesf[:, :], sq[c], c == 0, c == 1, ["onesf", ("sq", c)], [("pb", bs)])
            self.A(rs, self.pb[bs][:, :], AF.Sqrt, [("pb", bs)], ["rs"], scale=1.0 / 256, bias=RMS_EPS)
            self.S.op("dve", lambda e, r=rs: e.reciprocal(out=r, in_=r), reads=["rs"], writes=["rs"])
            for w in range(2):
                self.TT("dve", CS[:, w, sl], self.tabM[:, w, sl], rs, ALU.mult, ["tab", "rs"], ["CS"])
            b0 = self.proj_fm(wvm, wkm, 256, 128, tt)
            self.A(kvg[:, sl], self.pb[b0][:, :], AF.Copy, [("pb", b0), "nrm"], ["kvg"], scale=nrm[:, 2:3])
            self.A(sq[0], self.pb[b0][:, :], AF.Square, [("pb", b0)], [("sq", 0)])
            bs = self.short()
            self.mm(self.pb[bs][:, :], self.onesf[:, :], sq[0], True, True, ["onesf", ("sq", 0)], [("pb", bs)])
            self.A(rs, self.pb[bs][:, :], AF.Sqrt, [("pb", bs)], ["rs"], scale=1.0 / 128, bias=RMS_EPS)
            self.S.op("dve", lambda e, r=rs, o=rkv[:, sl]: e.reciprocal(out=o, in_=r), reads=["rs"], writes=["rkv"])
            bt = self.short()
            for i in range(4):
                self.mm(self.pb[bt][:, i:i + 1], sq[0][:, i * 128:(i + 1) * 128], self.onesf[:, 0:1], True, True,
                        [("sq", 0), "onesf"], [("pb", bt)])
            self.A(rkt[:, tt * 4:tt * 4 + 4], self.pb[bt][:, 0:4], AF.Sqrt, [("pb", bt)], ["rkt"], scale=1.0 / 128,
                   bias=RMS_EPS)
        self.S.op("dve", lambda e: e.reciprocal(out=rkt, in_=rkt), reads=["rkt"], writes=["rkt"])
        wvr, wkr = self.wload(win[li][:, OFF_KR:OFF_KR + 256], 8, 256)
        r9 = slice(64, 96)
        for tt in range(4):
            sl = slice(tt * 512, (tt + 1) * 512)
            b0 = self.proj_fm(wvr, wkr, 0, 96, tt)
            b1 = self.proj_fm(wvr, wkr, 128, 96, tt)
            self.TT("dve", t1[0][r9, :], self.pb[b0][r9, :], self.tabM[r9, 0, sl], ALU.mult, [("pb", b0), "tab"], [("t1", 0)])
            self.TT("dve", t2[0][r9, :], self.pb[b1][r9, :], self.tabM[r9, 1, sl], ALU.mult, [("pb", b1), "tab"], [("t2", 0)])
            self.TT("dve", KR[r9, sl], t1[0][r9, :], t2[0][r9, :], ALU.add, [("t1", 0), ("t2", 0)], ["KR"])
        wvu, wku = self.wload(d["ukv"][li], 1, 512)
        for tb in range(NB):
            b = self.short()
            self.mm(self.pb[b][:, 0:256], kvg[:, tb * 128:(tb + 1) * 128], wvu[:, 0, 256:512], True, True,
                    ["kvg", wku], [("pb", b)])
            self.A(Vm[:, tb, :, 0:64], self.pb[b][:, 0:256].rearrange("p (h c) -> p h c", h=4), AF.Copy,
                   [("pb", b), "rkt"], ["Vm"], scale=rkt[:, tb:tb + 1])
        wvq, wkq = self.wload(d["uq"][li], 2, 768)
        scale = 96.0 ** -0.5
        for h in range(4):
            Q = QH[h % 2]
            K = KH[h % 2]
            qk = ("QH", h % 2)
            kk = ("KH", h % 2)
            for tt in range(4):
                sl = slice(tt * 512, (tt + 1) * 512)
                ba = self.proj_fm(wvq, wkq, (2 * h) * 96, 96, tt, src=qg, srckey="qg", kc=2)
                bb = self.proj_fm(wvq, wkq, (2 * h + 1) * 96, 96, tt, src=qg, srckey="qg", kc=2)
                c = tt % 2
                self.TT("dve", t1[c][0:96, :], self.pb[ba][0:96, :], CS[0:96, 0, sl], ALU.mult, [("pb", ba), "CS"], [("t1", c)])
                self.TT("dve", t2[c][0:96, :], self.pb[bb][0:96, :], CS[0:96, 1, sl], ALU.mult, [("pb", bb), "CS"], [("t2", c)])
                self.TT("dve", Q[0:96, sl], t1[c][0:96, :], t2[c][0:96, :], ALU.add, [("t1", c), ("t2", c)], [qk])
                bk = self.short()
                self.mm(self.pb[bk][0:64, :], wvu[:, 0, h * 64:(h + 1) * 64], kvg[:, sl], True, True, [wku, "kvg"], [("pb", bk)])
                self.TT("dve", K[0:64, sl], self.pb[bk][0:64, :], rkv[0:64, sl], ALU.mult, [("pb", bk), "rkv"], [kk])
            self.CP("dve", K[r9, :], KR[r9, :], ["KR"], [kk])
            for qb in range(NB):
                qs = slice(qb * 128, (qb + 1) * 128)
                sm = small[qb % 2]
                smk = ("small", qb % 2)
                kbs = [(kb, "le" if kb == qb else None) for kb in range(qb + 1)]
                ob = self.softmax_attn_block(
                    qb, kbs, lambda kb: (K[0:96, kb * 128:(kb + 1) * 128], [kk]), (Q[0:96, qs], [qk]),
                    lambda kb: (Vm[:, kb, h, :], ["Vm"]), scale)
                self.S.op("dve", lambda e, s_=sm, o_=self.pb[ob][:, 64:65]: e.reciprocal(out=s_[:, 0:1], in_=o_),
                          reads=[("pb", ob)], writes=[smk])
                self.A(self.ymla[:, qb, h * 64:(h + 1) * 64], self.pb[ob][:, 0:64], AF.Copy, [("pb", ob), smk],
                       [("ymla", qb)], scale=sm[:, 0:1])
        for qb in range(NB):
            self.y_to_yT(self.ymla[:, qb, :], ("ymla", qb), 2, qb)

    def sb_branch(self, li):
        d = self.dram
        win = d["win"]
        cst = d["cst"]
        QT = self.carve([2, T], BF16)
        KT = self.carve([2, T], BF16)
        V = self.carve([NB, 256], BF16)
        indt = self.carve([16, 16], BF16)
        selgt = self.carve([16, 128], BF16, parts=16)
        sp = [self.carve([NB * 128], F32) for _ in range(2)]
        lk = [self.carve([NB * 128], BF16) for _ in range(2)]
        ex = [self.carve([512], F32) for _ in range(2)]
        ar = [self.carve([512], F32) for _ in range(2)]
        aa = [self.carve([512], BF16) for _ in range(5)]
        ts_ = [self.carve([128], BF16, parts=16) for _ in range(2)]
        ytile = [self.carve([256], F32) for _ in range(2)]
        self.cast_load(indt, cst["indt"], 128, [16, 16], "indt")
        self.cast_load(selgt, cst["selgt"], 16, [16, 128], "selgt")
        wv, wk = self.wload(win[li][:, OFF_SB:OFF_SB + 512], 8, 512)
        for tt in range(4):
            sl = slice(tt * 512, (tt + 1) * 512)
            for c in range(2):
                b0 = self.proj_fm(wv, wk, c * 128, 128, tt)
                self.CP("act", QT[:, c, sl], self.pb[b0][:, :], [("pb", b0)], ["QT"])
                b1 = self.proj_fm(wv, wk, 256 + c * 128, 128, tt)
                self.CP("dve", KT[:, c, sl], self.pb[b1][:, :], [("pb", b1)], ["KT"])
        wvt, wkt = self.wload(win[li][:, OFF_TOK + 256:OFF_TOK + 512], 8, 256)
        for tb in range(NB):
            b = self.short()
            for k in range(8):
                self.mm(self.pb[b][:, 0:256], self.xT[:, k, tb * 128:(tb + 1) * 128], wvt[:, k, :], k == 0, k == 7,
                        [wkt, ("xT", tb // 4)], [("pb", b)])
            self.CP("act", V[:, tb, :], self.pb[b][:, 0:256], [("pb", b)], ["V"])
        it = 0
        ai_ = 0
        for qb in range(NB):
            qs = slice(qb * 128, (qb + 1) * 128)
            yt = ytile[qb % 2]
            yk = ("yt", qb % 2)
            for h in range(4):
                hp_ = slice((h % 2) * 64, (h % 2) * 64 + 64)
                spt, lkt, tst = sp[it % 2], lk[it % 2], ts_[it % 2]
                spk, lkk, tsk = ("sp", it % 2), ("lk", it % 2), ("ts", it % 2)
                it += 1
                nk = qb + 1
                for g0 in range(0, nk, 4):
                    n = min(4, nk - g0)
                    w = n * 128
                    sbk = self.short()
                    for i in range(n):
                        kb = g0 + i
                        self.mm(self.pb[sbk][:, i * 128:(i + 1) * 128], KT[hp_, h // 2, kb * 128:(kb + 1) * 128],
                                QT[hp_, h // 2, qs], True, True, ["KT", "QT"], [("pb", sbk)])
                    e_ = ex[(g0 // 4) % 2]
                    exk = ("ex", (g0 // 4) % 2)
                    self.A(e_[:, 0:w], self.pb[sbk][:, 0:w], AF.Exp, [("pb", sbk)], [exk], scale=-0.125)
                    self.A(spt[:, g0 * 128:g0 * 128 + w], e_[:, 0:w], AF.Ln, [exk], [spk], bias=1.0)
                    self.STT("dve", lkt[:, g0 * 128:g0 * 128 + w], self.pb[sbk][:, 0:w], -0.125, spt[:, g0 * 128:g0 * 128 + w],
                             ALU.mult, ALU.subtract, [("pb", sbk), spk], [lkk])
                self.TT("dve", lkt[:, qb * 128:(qb + 1) * 128], lkt[:, qb * 128:(qb + 1) * 128], self.cmask[:, 1, :],
                        ALU.mult, [lkk, "cmask"], [lkk])
                bts = self.short()
                for kb in range(nk):
                    self.mm(self.pb[bts][0:16, 0:128], indt[:, kb, :], lkt[:, kb * 128:(kb + 1) * 128], kb == 0, kb == nk - 1,
                            ["indt", lkk], [("pb", bts)])
                self.CP("act", tst, self.pb[bts][0:16, 0:128], [("pb", bts)], [tsk])
                ob = self.accb()

                def front3(g0):
                    nonlocal ai_
                    n = min(4, nk - g0)
                    w = n * 128
                    lb = self.short()
                    for i in range(n):
                        kb = g0 + i
                        self.mm(self.pb[lb][:, i * 128:(i + 1) * 128], self.cmask[:, 2, :], lkt[:, kb * 128:(kb + 1) * 128],
                                True, False, ["cmask", lkk], [("pb", lb)])
                        self.mm(self.pb[lb][:, i * 128:(i + 1) * 128], selgt[:, kb, :], tst, False, True,
                                ["selgt", tsk], [("pb", lb)])
                    a_ = ar[(g0 // 4) % 2]
                    ark = ("ar", (g0 // 4) % 2)
                    self.TT("dve", a_[:, 0:w], self.pb[lb][:, 0:w], spt[:, g0 * 128:g0 * 128 + w], ALU.subtract,
                            [("pb", lb), spk], [ark])
                    at = aa[ai_ % 5]
                    ak = ("aa", ai_ % 5)
                    ai_ += 1
                    self.A(at[:, 0:w], a_[:, 0:w], AF.Exp, [ark], [ak])
                    if g0 + n == nk:
                        i = n - 1
                        self.TT("dve", at[:, i * 128:(i + 1) * 128], at[:, i * 128:(i + 1) * 128], self.cmask[:, 1, :],
                                ALU.mult, [ak, "cmask"], [ak])
                    return g0, n, at, ak

                def back3(st_):
                    g0, n, at, ak = st_
                    for i in range(n):
                        kb = g0 + i
                        self.mm(self.pb[ob][:, 0:64], at[:, i * 128:(i + 1) * 128], V[:, kb, h * 64:(h + 1) * 64],
                                kb == 0, kb == nk - 1, [ak, "V"], [("pb", ob)])

                prev = None
                for g0 in range(0, nk, 4):
                    cur = front3(g0)
                    if prev is not None:
                        back3(prev)
                    prev = cur
                back3(prev)
                self.CP("act", yt[:, h * 64:(h + 1) * 64], self.pb[ob][:, 0:64], [("pb", ob)], [yk])
            self.y_to_yT(yt, yk, 3, qb)

    def layer_norm_block(self, h, hk, gb, tb, res_out, li, route):
        st = self.lnst[tb % 2]
        sk = ("lnst", tb % 2)
        junk = self.lnjunk
        self.A(junk, h, AF.Copy, [hk], ["lnjunk", sk], accum=st[:, 0:1])
        self.A(junk, h, AF.Square, [hk], ["lnjunk", sk], accum=st[:, 1:2])
        self.TS("dve", st[:, 2:3], st[:, 0:1], 1.0 / D, None, ALU.mult, None, [sk], [sk])
        self.TT("dve", st[:, 3:4], st[:, 2:3], st[:, 2:3], ALU.mult, [sk], [sk])
        self.STT("dve", st[:, 4:5], st[:, 1:2], 1.0 / D, st[:, 3:4], ALU.mult, ALU.subtract, [sk], [sk])
        self.A(st[:, 4:5], st[:, 4:5], AF.Sqrt, [sk], [sk], bias=LN_EPS)
        self.S.op("dve", lambda e, s_=st: e.reciprocal(out=s_[:, 5:6], in_=s_[:, 4:5]), reads=[sk], writes=[sk])
        self.STT("dve", st[:, 6:7], st[:, 2:3], -1.0, st[:, 5:6], ALU.mult, ALU.mult, [sk], [sk])
        self.A(h, h, AF.Identity, [hk, sk], [hk], scale=st[:, 5:6], bias=st[:, 6:7])
        self.TT("dve", h, h, gb[:, 0, :], ALU.mult, [hk, "lngb"], [hk])
        self.TT("dve", h, h, gb[:, 1, :], ALU.add, [hk, "lngb"], [hk])
        o = self.dma(res_out[tb * 128:(tb + 1) * 128, :], h, [hk], [("res", id(res_out), tb)])
        self.x_to_xT(h, hk, tb, rt=route)
        return o

    def merge_ln1(self, li, res_in, res_out):
        d = self.dram
        win = d["win"]
        HT = 1024
        mp = self.carve([8, HT], BF16)
        accm = self.carve([8, HT], F32)
        sgt = [self.carve([512], BF16) for _ in range(2)]
        prod = [self.carve([512], F32) for _ in range(2)]
        gb = self.carve([2, D], F32)
        hbuf = [self.carve([D], F32) for _ in range(2)]
        xin = [self.carve([D], F32) for _ in range(2)]
        self.lnst = [self.carve([8], F32) for _ in range(2)]
        self.lnjunk = self.carve([D], BF16)
        self.dma(gb[:, 0, :], d["ln1g"][li:li + 1, :].to_broadcast([128, D]), [], ["lngb"])
        self.dma(gb[:, 1, :], d["ln1b"][li:li + 1, :].to_broadcast([128, D]), [], ["lngb"])
        route = None
        if li == 1:
            route = self.make_router(li)
        for th in range(2):
            for n in range(4):
                wvb, wkb = self.wload(d["wbr"][li, n], 2, D)
                for q4 in range(2):
                    c0 = OFF_GATE + n * D + q4 * 512
                    wvg, wkg = self.wload(win[li][:, c0:c0 + 512], 8, 512)
                    for cc in range(4):
                        dc = q4 * 4 + cc
                        for t2 in range(2):
                            tt = th * 2 + t2
                            sl = slice(t2 * 512, (t2 + 1) * 512)
                            bg = self.proj_fm(wvg, wkg, cc * 128, 128, tt)
                            bp = self.proj_fm(wvb, wkb, dc * 128, 128, tt, src=self.yT[n], srckey="yT%d" % n, kc=2)
                            s_ = sgt[t2]
                            sk = ("sgt", t2)
                            self.A(s_, self.pb[bg][:, :], AF.Sigmoid, [("pb", bg)], [sk])
                            ak = ("accm", dc, t2)
                            if n == 0:
                                self.TT("dve", accm[:, dc, sl], self.pb[bp][:, :], s_, ALU.mult, [("pb", bp), sk], [ak])
                            else:
                                p_ = prod[t2]
                                pk = ("prod", t2)
                                self.TT("dve", p_, self.pb[bp][:, :], s_, ALU.mult, [("pb", bp), sk], [pk])
                                if n < 3:
                                    self.TT("dve", accm[:, dc, sl], accm[:, dc, sl], p_, ALU.add, [ak, pk], [ak])
                                else:
                                    self.TT("dve", mp[:, dc, sl], accm[:, dc, sl], p_, ALU.add, [ak, pk], [("mp", dc, t2)])
            wo = [self.wload(d["wout"][li][:, hh * 512:(hh + 1) * 512], 8, 512) for hh in range(2)]
            for j in range(8):
                tb = th * 8 + j
                xi = xin[tb % 2]
                xk = ("xin", tb % 2)
                self.dma(xi, res_in[tb * 128:(tb + 1) * 128, :], [("res", id(res_in), tb)], [xk])
                h = hbuf[tb % 2]
                hk = ("hbuf", tb % 2)
                for hh in range(2):
                    ob = self.accb()
                    for k in range(8):
                        self.mm(self.pb[ob][:, :], mp[:, k, j * 128:(j + 1) * 128], wo[hh][0][:, k, :], k == 0, k == 7,
                                [("mp", k, j // 4), wo[hh][1]], [("pb", ob)])
                    self.STT("dve", h[:, hh * 512:(hh + 1) * 512], xi[:, hh * 512:(hh + 1) * 512], ALPHA, self.pb[ob][:, :],
                             ALU.mult, ALU.add, [xk, ("pb", ob)], [hk])
                rt = (lambda half, b, tb=tb: route(tb, half, b)) if route else None
                o = self.layer_norm_block(h, hk, gb, tb, res_out, li, rt)
                if self.stop_after == ("mix", li):
                    self.finals.append(o)

    def layer_norm_block(self, h, hk, gb, tb, res_out, li, route):
        st = self.lnst[tb % 2]
        sk = ("lnst", tb % 2)
        junk = self.lnjunk
        self.MS("dve", st[:, 0:2], 0.0, [sk])
        self.A(junk, h, AF.Copy, [hk, sk], ["lnjunk", sk], accum=st[:, 0:1])
        self.A(junk, h, AF.Square, [hk, sk], ["lnjunk", sk], accum=st[:, 1:2])
        self.TS("dve", st[:, 2:3], st[:, 0:1], 1.0 / D, None, ALU.mult, None, [sk], [sk])
        self.TT("dve", st[:, 3:4], st[:, 2:3], st[:, 2:3], ALU.mult, [sk], [sk])
        self.STT("dve", st[:, 4:5], st[:, 1:2], 1.0 / D, st[:, 3:4], ALU.mult, ALU.subtract, [sk], [sk])
        self.A(st[:, 4:5], st[:, 4:5], AF.Sqrt, [sk], [sk], bias=LN_EPS)
        self.S.op("dve", lambda e, s_=st: e.reciprocal(out=s_[:, 5:6], in_=s_[:, 4:5]), reads=[sk], writes=[sk])
        self.STT("dve", st[:, 6:7], st[:, 2:3], -1.0, st[:, 5:6], ALU.mult, ALU.mult, [sk], [sk])
        self.A(h, h, AF.Identity, [hk, sk], [hk], scale=st[:, 5:6], bias=st[:, 6:7])
        self.TT("dve", h, h, gb[:, 0, :], ALU.mult, [hk, "lngb"], [hk])
        self.TT("dve", h, h, gb[:, 1, :], ALU.add, [hk, "lngb"], [hk])
        o = self.dma(res_out[tb * 128:(tb + 1) * 128, :], h, [hk], [("res", id(res_out), tb)])
        self.x_to_xT(h, hk, tb, rt=route)
        return o

    def make_router(self, li):
        d = self.dram
        rw = self.carve([8, NE], F32)
        self.dma(rw, d["router"].rearrange("(c p) e -> p c e", p=128), [], ["rw"])
        xf = [self.carve([512], F32) for _ in range(2)]
        lg = [self.carve([32], F32) for _ in range(2)]
        state = {}

        def route(tb, half, b):
            x_ = xf[half]
            xk = ("xf", half)
            self.CP("dve", x_, self.pb[b][:, :], [("pb", b)], [xk])
            if half == 0:
                state["bank"] = self.accb()
            rb = state["bank"]
            for c in range(4):
                k = half * 4 + c
                self.mm(self.pb[rb][:, 0:NE], x_[:, c * 128:(c + 1) * 128], rw[:, k, :], k == 0, k == 7,
                        [xk, "rw"], [("pb", rb)])
            if half == 1:
                l_ = lg[tb % 2]
                lk = ("lg", tb % 2)
                self.CP("dve", l_[:, 0:8], self.pb[rb][:, 0:NE], [("pb", rb)], [lk])
                self.S.op("dve", lambda e, l_=l_: e.max(out=l_[:, 8:16], in_=l_[:, 0:8]), reads=[lk], writes=[lk])
                self.TT("dve", l_[:, 16:17], l_[:, 9:10], l_[:, 8:9], ALU.subtract, [lk], [lk])
                self.A(l_[:, 16:17], l_[:, 16:17], AF.Exp, [lk], [lk])
                self.TS("dve", l_[:, 16:17], l_[:, 16:17], 1.0, None, ALU.add, None, [lk], [lk])
                self.S.op("dve", lambda e, l_=l_: e.reciprocal(out=l_[:, 17:18], in_=l_[:, 16:17]), reads=[lk], writes=[lk])
                self.TS("dve", l_[:, 18:19], l_[:, 8:9], -1.0, None, ALU.mult, None, [lk], [lk])
                self.A(l_[:, 24:32], l_[:, 0:8], AF.Exp, [lk], [lk], bias=l_[:, 18:19])
                self.TS("dve", l_[:, 0:8], l_[:, 0:8], l_[:, 9:10], l_[:, 17:18], ALU.is_ge, ALU.mult, [lk], [lk])
                self.TT("dve", self.gates[:, tb, :], l_[:, 0:8], l_[:, 24:32], ALU.mult, [lk], ["gates"])
        return route

    MOE_CAP = 512

    def ffn_phase(self, li, res_in, res_out):
        self.arena_reset()
        G = 1024
        moe = (li == 1)
        dff = D_FFE if moe else D_FF
        nfc = dff // 128
        hT = self.carve([nfc, self.MOE_CAP if moe else G], BF16)
        facc = self.carve([8, D], F32)
        self.stage = self.stage[0:2] + [self.carve([2048], F32)]
        base = self.aoff
        for g in range(T // G):
            self.aoff = base
            if g > 0:
                self.S.barrier()
            if moe:
                self.moe_group(li, g, res_in, hT, facc, nfc, dff)
            else:
                self.dense_group(li, g, hT, facc, nfc, dff)
            self.S.barrier()
            self.aoff = base
            self.ple_ln2_group(li, g, res_in, res_out, facc)

    def hidden_fm(self, w_in, dff, nfc, hT, src, srckey, tiles, sa):
        for f0 in range(0, nfc, 4):
            nf = min(4, nfc - f0)
            wa, wak = self.wload(w_in[:, f0 * 128:(f0 + nf) * 128], 8, nf * 128)
            wu, wuk = self.wload(w_in[:, dff + f0 * 128:dff + (f0 + nf) * 128], 8, nf * 128)
            for fi in range(nf):
                fc = f0 + fi
                for t2, tt in enumerate(tiles):
                    ba = self.proj_fm(wa, wak, fi * 128, 128, tt, src=src, srckey=srckey)
                    bu = self.proj_fm(wu, wuk, fi * 128, 128, tt, src=src, srckey=srckey)
                    s_ = sa[t2 % 2]
                    sk = ("sa", t2 % 2)
                    self.A(s_, self.pb[ba][:, :], AF.Silu, [("pb", ba)], [sk])
                    self.TT("dve", hT[:, fc, t2 * 512:(t2 + 1) * 512], self.pb[bu][:, :], s_, ALU.mult,
                            [("pb", bu), sk], [("hT", fc)])

    def out_tm(self, w_out, nfc, hT, blocks, evac):
        for f0 in range(0, nfc, 4):
            nf = min(4, nfc - f0)
            wo, wok = self.wload(w_out[f0 * 128:(f0 + nf) * 128, :], nf, D)
            for fi in range(nf):
                fc = f0 + fi
                for i, j in enumerate(blocks):
                    for hh in range(2):
                        b = i * 2 + hh
                        self.mm(self.pb[b][:, :], hT[:, fc, j * 128:(j + 1) * 128], wo[:, fi, hh * 512:(hh + 1) * 512],
                                fc == 0, fc == nfc - 1, [("hT", fc), wok], [("pb", b)])
        for i, j in enumerate(blocks):
            for hh in range(2):
                evac(i, j, hh, i * 2 + hh)

    def dense_group(self, li, g, hT, facc, nfc, dff):
        d = self.dram
        sa = [self.carve([512], BF16) for _ in range(2)]
        self.hidden_fm(d["ffn_in"], dff, nfc, hT, None, None, [g * 2, g * 2 + 1], sa)
        for ps_ in range(2):
            def evac(i, j, hh, b):
                self.CP("act" if hh == 0 else "dve", facc[:, j, hh * 512:(hh + 1) * 512], self.pb[b][:, :],
                        [("pb", b)], [("facc", j, hh)])
            self.out_tm(d["ffn_out"], nfc, hT, [ps_ * 4 + i for i in range(4)], evac)

    def moe_group(self, li, g, res_in, hT, facc, nfc, dff):
        d = self.dram
        C = self.MOE_CAP
        NR = C // 128
        xtok = self.carve([8, D], BF16)
        Pb = self.carve([8, C], BF16)
        PT = self.carve([NR, 8, 128], BF16)
        xsT = self.carve([8, C], BF16)
        ys = self.carve([NR, D], BF16)
        iota = self.carve([C], F32)
        sa = [self.carve([512], BF16) for _ in range(2)]
        mf = self.carve([64], F32)
        mb = self.carve([64], BF16)
        rk = self.carve([64], F32)
        off = self.carve([64], F32)
        gs = self.carve([64, 2], BF16)
        gt = self.carve([64], F32)
        wr = self.carve([NR, 2], F32)
        gsl = self.gates[:, g * 8:(g + 1) * 8, :].rearrange("p j e -> p (j e)")
        self.dma(iota, d["cst"]["iota"], [], ["iota"])
        for j in range(8):
            tb = g * 8 + j
            self.cast_load(xtok[:, j, :], res_in[tb * 128:(tb + 1) * 128, :], 128, [D], ("xtok", j))
        self.TS("dve", mf, gsl, 0.0, None, ALU.is_gt, None, ["gates"], ["mf"])
        self.CP("dve", mb, mf, ["mf"], ["mb"])
        b1 = self.short()
        self.mm(self.pb[b1][:, 0:64], self.cmask[:, 0, :], mb, True, True, ["cmask", "mb"], [("pb", b1)])
        b2 = self.short()
        self.mm(self.pb[b2][:, 0:64], self.cmask[:, 3, :], mb, True, True, ["cmask", "mb"], [("pb", b2)])
        self.MS("dve", off[:, 0:8], 0.0, ["off"])
        for j in range(1, 8):
            self.TT("dve", off[:, j * 8:(j + 1) * 8], off[:, (j - 1) * 8:j * 8], self.pb[b2][:, (j - 1) * 8:j * 8], ALU.add,
                    ["off", ("pb", b2)], ["off"])
        self.TT("dve", rk, self.pb[b1][:, 0:64], off, ALU.add, [("pb", b1), "off"], ["rk"])
        self.TT("dve", rk, rk, mf, ALU.mult, ["rk", "mf"], ["rk"])
        self.TS("dve", rk, rk, -1.0, None, ALU.add, None, ["rk"], ["rk"])
        self.CP("dve", gs[:, :, 0], gsl, ["gates"], ["gs"])
        self.TT("dve", gt, gsl, gs[:, :, 0], ALU.subtract, ["gates", "gs"], ["gt"])
        self.CP("dve", gs[:, :, 1], gt, ["gt"], ["gs"])
        for e in range(NE):
            for j in range(8):
                self.TS("dve", Pb[:, j, :], iota, rk[:, j * 8 + e:j * 8 + e + 1], None, ALU.is_equal, None,
                        ["iota", "rk"], [("Pb", j)])
            for rb in range(NR):
                for j0 in range(0, 8, 4):
                    b = self.short()
                    for jj in range(4):
                        j = j0 + jj
                        self.mm(self.pb[b][:, jj * 128:(jj + 1) * 128], Pb[:, j, rb * 128:(rb + 1) * 128], self.cmask[:, 4, :],
                                True, True, [("Pb", j), "cmask"], [("pb", b)])
                    self.CP("act", PT[:, rb, j0:j0 + 4, :], self.pb[b][:, :].rearrange("p (j t) -> p j t", j=4),
                            [("pb", b)], [("PT", rb)])
            for dc in range(8):
                b = self.short()
                for j in range(8):
                    self.mm(self.pb[b][:, 0:C], xtok[:, j, dc * 128:(dc + 1) * 128], Pb[:, j, :], j == 0, j == 7,
                            [("xtok", j), ("Pb", j)], [("pb", b)])
                self.CP("dve" if dc % 2 else "act", xsT[:, dc, :], self.pb[b][:, 0:C], [("pb", b)], ["xsT"])
            bw = self.short()
            for rb in range(NR):
                for j in range(8):
                    self.mm(self.pb[bw][:, 2 * rb:2 * rb + 2], Pb[:, j, rb * 128:(rb + 1) * 128], gs[:, j * 8 + e, :],
                            j == 0, j == 7, [("Pb", j), "gs"], [("pb", bw)])
            self.CP("dve", wr, self.pb[bw][:, 0:2 * NR].rearrange("p (r c) -> p r c", c=2), [("pb", bw)], ["wr"])
            self.TT("dve", wr[:, :, 0], wr[:, :, 0], wr[:, :, 1], ALU.add, ["wr"], ["wr"])
            self.hidden_fm(d["moe_in"][e], dff, nfc, hT, xsT, "xsT", [0], sa)

            def evac(i, j, hh, b):
                self.A(ys[:, i, hh * 512:(hh + 1) * 512], self.pb[b][:, :], AF.Copy, [("pb", b), "wr"], [("ys", i)],
                       scale=wr[:, i, 0:1])
            self.out_tm(d["moe_out"][e], nfc, hT, list(range(NR)), evac)
            for j in range(8):
                for hh in range(2):
                    b = self.short()
                    for rb in range(NR):
                        self.mm(self.pb[b][:, :], PT[:, rb, j, :], ys[:, rb, hh * 512:(hh + 1) * 512], rb == 0, rb == NR - 1,
                                [("PT", rb), ("ys", rb)], [("pb", b)])
                    dst = facc[:, j, hh * 512:(hh + 1) * 512]
                    fk = ("facc", j, hh)
                    if e == 0:
                        self.CP("dve", dst, self.pb[b][:, :], [("pb", b)], [fk])
                    else:
                        self.TT("dve", dst, dst, self.pb[b][:, :], ALU.add, [fk, ("pb", b)], [fk])

    def ple_ln2_group(self, li, g, res_in, res_out, facc):
        d = self.dram
        gb = self.carve([2, D], F32)
        pblk = [self.carve([256], F32) for _ in range(2)]
        pT = [self.carve([2, 128], BF16) for _ in range(2)]
        ple = self.carve([D], F32)
        hbuf = [self.carve([D], F32) for _ in range(2)]
        xin = self.carve([D], F32)
        self.lnst = [self.carve([8], F32) for _ in range(2)]
        self.lnjunk = self.carve([D], BF16)
        self.dma(gb[:, 0, :], d["ln2g"][li:li + 1, :].to_broadcast([128, D]), [], ["lngb"])
        self.dma(gb[:, 1, :], d["ln2b"][li:li + 1, :].to_broadcast([128, D]), [], ["lngb"])
        wg = [self.wload(d["pleg"][li][:, hh * 512:(hh + 1) * 512], 8, 512) for hh in range(2)]
        wp, wpk = self.wload(d["plep"][li], 2, D)
        for j in range(8):
            tb = g * 8 + j
            pb_ = pblk[j % 2]
            pk = ("pblk", j % 2)
            self.dma(pb_, d["p_in"][li, tb * 128:(tb + 1) * 128, :], [], [pk])
            b = self.short()
            for c in range(2):
                self.tr(self.pb[b][:, c * 128:(c + 1) * 128], pb_[:, c * 128:(c + 1) * 128], [pk], [("pb", b)])
            pt = pT[j % 2]
            ptk = ("pT", j % 2)
            self.CP("act", pt, self.pb[b][:, 0:256].rearrange("p (c t) -> p c t", c=2), [("pb", b)], [ptk])
            for hh in range(2):
                bg_ = self.short()
                for k in range(8):
                    self.mm(self.pb[bg_][:, :], self.xT[:, k, tb * 128:(tb + 1) * 128], wg[hh][0][:, k, :], k == 0, k == 7,
                            [("xT", tb // 4), wg[hh][1]], [("pb", bg_)])
                bp_ = self.short()
                for c in range(2):
                    self.mm(self.pb[bp_][:, :], pt[:, c, :], wp[:, c, hh * 512:(hh + 1) * 512],
                            c == 0, c == 1, [ptk, wpk], [("pb", bp_)])
                self.A(ple[:, hh * 512:(hh + 1) * 512], self.pb[bg_][:, :], AF.Sigmoid, [("pb", bg_)], ["ple"])
                self.TT("dve", ple[:, hh * 512:(hh + 1) * 512], ple[:, hh * 512:(hh + 1) * 512], self.pb[bp_][:, :],
                        ALU.mult, ["ple", ("pb", bp_)], ["ple"])
            self.dma(xin, res_in[tb * 128:(tb + 1) * 128, :], [("res", id(res_in), tb)], ["xin"])
            h = hbuf[j % 2]
            hk = ("hbuf", j % 2)
            self.STT("dve", h, xin, ALPHA, ple, ALU.mult, ALU.add, ["xin", "ple"], [hk])
            self.TT("dve", h, h, facc[:, j, :], ALU.add, [hk], [hk])
            o = self.layer_norm_block(h, hk, gb, tb, res_out, li, None)
            if li == self.layers[-1] or self.stop_after == ("ffn", li):
                self.finals.append(o)


_IDX = _win_index()


def prepare_inputs(inputs):
    f = lambda a: np.ascontiguousarray(np.asarray(a))
    w_in = f(inputs["w_in"])
    win = np.zeros((2, D, NCOLS_R), np.float32)
    valid = _IDX >= 0
    win[:, :, valid] = w_in[:, :, _IDX[valid]]
    conv_w = f(inputs["conv_w"])
    convp = np.zeros((2, 128, 2, 34), np.float32)
    for c in range(2):
        convp[:, :, c, 0:31] = conv_w[:, :, c * 128:(c + 1) * 128].transpose(0, 2, 1)
        convp[:, :, c, 31] = f(inputs["conv_b"])[:, c * 128:(c + 1) * 128]
        convp[:, :, c, 32] = f(inputs["conv_ln_g"])[:, c * 128:(c + 1) * 128]
        convp[:, :, c, 33] = f(inputs["conv_ln_b"])[:, c * 128:(c + 1) * 128]
    pe = f(inputs["nsa_cmp_pe"])
    pe_r = np.ascontiguousarray(pe.transpose(0, 2, 3, 1).reshape(2, 128, 32))
    w2 = f(inputs["nsa_cmp_w2"])
    w2k = np.ascontiguousarray(np.concatenate([w2[:, 0], w2[:, 0]], axis=2))
    w2v = np.ascontiguousarray(w2[:, 1])
    qn = np.ascontiguousarray(f(inputs["mla_q_norm"]).reshape(2, 2, 128).transpose(0, 2, 1))
    kvn = np.ascontiguousarray(f(inputs["mla_kv_norm"]).reshape(2, 128, 1))
    wuq = f(inputs["mla_w_uq"])
    cols = []
    for h in range(4):
        b = h * 96
        cols += list(range(b, b + 96))
        cols += list(range(b, b + 64)) + list(range(b + 80, b + 96)) + list(range(b + 64, b + 80))
    uq = np.ascontiguousarray(wuq[:, :, cols])
    wukv = f(inputs["mla_w_ukv"])
    cols = []
    for h in range(4):
        cols += list(range(h * 128, h * 128 + 64))
    for h in range(4):
        cols += list(range(h * 128 + 64, h * 128 + 128))
    ukv = np.ascontiguousarray(wukv[:, :, cols])
    shared = {
        "win": win, "convp": convp, "pe_r": pe_r, "w1": f(inputs["nsa_cmp_w1"]), "w2k": w2k, "w2v": w2v,
        "qn": qn, "kvn": kvn, "uq": uq, "ukv": ukv, "wbr": f(inputs["w_branch"]), "wout": f(inputs["w_out"]),
        "ln1g": f(inputs["ln1_g"]), "ln1b": f(inputs["ln1_b"]), "ln2g": f(inputs["ln2_g"]), "ln2b": f(inputs["ln2_b"]),
        "ffn_in": f(inputs["ffn_w_in"])[0], "ffn_out": f(inputs["ffn_w_out"])[0], "router": f(inputs["moe_router"])[0],
        "moe_in": f(inputs["moe_w_in"])[0], "moe_out": f(inputs["moe_w_out"])[0],
        "pleg": f(inputs["ple_w_gate"]), "plep": f(inputs["ple_w_proj"]),
    }
    for k, v in _host_consts().items():
        shared["c_" + k] = v
    x = f(inputs["x"])
    p = f(inputs["p"])
    pos = f(inputs["positions"]).astype(np.int32)
    in_maps = []
    for b in range(8):
        m = dict(shared)
        m["x"] = x[b]
        m["p"] = np.ascontiguousarray(p[:, b])
        m["pos"] = pos[b:b + 1]
        in_maps.append(m)
    return in_maps


def kernel(**inputs):
    in_maps = prepare_inputs(inputs)
    nc = MK().build()
    res = run_bass_kernel_spmd(nc, in_maps, core_ids=list(range(8)))
    return np.stack([np.asarray(r["y"], dtype=np.float32) for r in res.results], axis=0)
```

```python
import math
import contextlib
import numpy as np
import concourse.bass as bass
import concourse.mybir as mybir
from concourse.bass_utils import run_bass_kernel_spmd

F32 = mybir.dt.float32
BF16 = mybir.dt.bfloat16
I32 = mybir.dt.int32
AF = mybir.ActivationFunctionType
ALU = mybir.AluOpType
AX = mybir.AxisListType

T = 2048
D = 1024
NB = 16
ALPHA = 4.0 ** 0.25
LN_EPS = 1e-5
RMS_EPS = 1e-6
THETA = 10000.0
D_FF = 2816
D_FFE = 3584
NE = 8

ENGS = ("pe", "act", "dve", "pool", "sp")
N_DMA_SEMS = 6


class Op:
    __slots__ = ("eng", "fn", "deps", "is_dma", "signal", "sig_val", "dma_sem", "dma_val",
                 "dma_prev", "idx")

    def __init__(self, eng, fn, is_dma):
        self.eng = eng
        self.fn = fn
        self.deps = []
        self.is_dma = is_dma
        self.signal = False
        self.sig_val = 0
        self.dma_sem = None
        self.dma_val = 0
        self.dma_prev = 0
        self.idx = 0


class Sched:
    def __init__(self):
        self.ops = {e: [] for e in ENGS}
        self.last_w = {}
        self.readers = {}
        self.all_ops = []

    def op(self, eng, fn, reads=(), writes=(), dma=False, acc=False):
        o = Op(eng, fn, dma)
        deps = []
        for k in reads:
            w = self.last_w.get(k)
            if w is not None:
                deps.append(w)
            if isinstance(k, tuple) and k[0] == "pb":
                for r in self.readers.get(k, ()):
                    if r.eng != eng:
                        deps.append(r)
        for k in writes:
            w = self.last_w.get(k)
            if w is not None and not (acc and w.eng == eng and not w.is_dma):
                deps.append(w)
            for r in self.readers.get(k, ()):
                deps.append(r)
        seen = set()
        for d in deps:
            if id(d) not in seen and d is not o:
                seen.add(id(d))
                o.deps.append(d)
        for k in reads:
            lst = self.readers.setdefault(k, [])
            if not dma:
                for i, r in enumerate(lst):
                    if r.eng == eng and not r.is_dma:
                        lst[i] = o
                        break
                else:
                    lst.append(o)
            else:
                lst.append(o)
        for k in writes:
            self.last_w[k] = o
            self.readers[k] = []
        o.idx = len(self.ops[eng])
        self.ops[eng].append(o)
        self.all_ops.append(o)
        return o

    def barrier(self):
        lasts = []
        for e in ENGS:
            ops = self.ops[e]
            nd = 0
            got_real = False
            for o in reversed(ops):
                if o.fn is None:
                    continue
                if o.is_dma:
                    if nd < N_DMA_SEMS:
                        lasts.append(o)
                        nd += 1
                elif not got_real:
                    lasts.append(o)
                    got_real = True
                if got_real and nd >= N_DMA_SEMS:
                    break
        for e in ENGS:
            o = Op(e, None, False)
            o.deps = [l for l in lasts]
            o.idx = len(self.ops[e])
            self.ops[e].append(o)
            self.all_ops.append(o)
        self.last_w = {}
        self.readers = {}

    def emit(self, nc, final_wait_ops=()):
        for fo in final_wait_ops:
            if not fo.is_dma:
                fo.signal = True
        for o in self.all_ops:
            for d in o.deps:
                if not d.is_dma:
                    d.signal = True
        cnt = {e: 0 for e in ENGS}
        for e in ENGS:
            for o in self.ops[e]:
                if o.signal and not o.is_dma:
                    cnt[e] += 1
                    o.sig_val = cnt[e]
        dma_count = {}
        for e in ENGS:
            k = 0
            for o in self.ops[e]:
                if o.is_dma:
                    j = k % N_DMA_SEMS
                    k += 1
                    key = (e, j)
                    prev = dma_count.get(key, 0)
                    o.dma_sem = key
                    o.dma_prev = prev
                    o.dma_val = prev + 16
                    dma_count[key] = prev + 16
        with contextlib.ExitStack() as st:
            sems = {e: st.enter_context(nc.semaphore("s_" + e)) for e in ENGS if cnt[e] > 0}
            dsems = {key: st.enter_context(nc.semaphore("d_%s%d" % key)) for key in dma_count}
            block = st.enter_context(nc.Block())
            regs = {"pe": block.tensor, "act": block.scalar, "dve": block.vector,
                    "pool": block.gpsimd, "sp": block.sync}

            def make(e):
                def body(eng):
                    known = {}
                    for o in self.ops[e]:
                        waits = {}
                        for d in o.deps:
                            if d.is_dma:
                                s, v = dsems[d.dma_sem], d.dma_val
                            else:
                                s, v = sems[d.eng], d.sig_val
                            kk = id(s)
                            if known.get(kk, 0) >= v:
                                continue
                            if kk not in waits or waits[kk][1] < v:
                                waits[kk] = (s, v)
                        if o.is_dma and o.dma_prev > 0:
                            s = dsems[o.dma_sem]
                            kk = id(s)
                            if known.get(kk, 0) < o.dma_prev:
                                if kk not in waits or waits[kk][1] < o.dma_prev:
                                    waits[kk] = (s, o.dma_prev)
                        for kk, (s, v) in waits.items():
                            eng.wait_ge(s, v)
                            known[kk] = v
                        if o.fn is None:
                            continue
                        ins = o.fn(eng)
                        if o.is_dma:
                            ins.then_inc(dsems[o.dma_sem], 16)
                        elif o.signal:
                            ins.then_inc(sems[e], 1)
                    if e == "sp":
                        for fo in final_wait_ops:
                            if fo.is_dma:
                                eng.wait_ge(dsems[fo.dma_sem], fo.dma_val)
                            else:
                                eng.wait_ge(sems[fo.eng], fo.sig_val)
                return body

            for e in ENGS:
                if self.ops[e] or e == "sp":
                    regs[e](make(e))


OFF_CONV, OFF_NQ, OFF_NK, OFF_MISC, OFF_KR, OFF_SB, OFF_TOK, OFF_GATE = 0, 512, 1024, 1536, 2048, 2304, 2816, 3328
NCOLS_R = 7424


def _win_index():
    sw64 = lambda b: list(range(b + 32, b + 64)) + list(range(b, b + 32))
    sw32 = lambda b: list(range(b + 16, b + 32)) + list(range(b, b + 16))
    idx = []
    idx += list(range(0, 512))
    idx += list(range(512, 768))
    for h in range(4):
        idx += sw64(512 + 64 * h)
    ks, kw = 896, 1024
    idx += list(range(ks, ks + 64)) * 2 + sw64(ks) * 2 + list(range(kw, kw + 64)) * 2 + sw64(kw) * 2
    idx += list(range(768, 896)) + list(range(1164, 1420)) + list(range(1420, 1548))
    idx += [-1] * 64 + list(range(1548, 1580)) + [-1] * 32
    idx += [-1] * 64 + sw32(1548) + [-1] * 32
    idx += list(range(1580, 1580 + 512))
    idx += list(range(960, 1024)) + list(range(1088, 1152)) + list(range(1152, 1164)) + [-1] * 116
    idx += list(range(1580 + 512, 1580 + 768))
    idx += list(range(2348, 6444))
    assert len(idx) == NCOLS_R
    return np.array(idx)


def _host_consts():
    c = {}
    c["ident"] = np.eye(128, dtype=np.float32)
    p = np.arange(128)[:, None]
    f = np.arange(128)[None, :]
    cm = np.zeros((128, 5, 128), np.float32)
    cm[:, 0] = (p <= f)
    cm[:, 1] = (p < f)
    cm[:, 2] = (p > f)
    cm[:, 3] = 1.0
    cm[:, 4] = (p == f)
    c["cmask"] = cm
    j = np.arange(128)[:, None]
    t = np.arange(T)[None, :]
    c["cmpvalid"] = ((16 * j + 31 <= t) & (j < 127)).astype(np.float32)
    n = np.arange(32)[:, None]
    c["eexp"] = ((t // 64) == n).astype(np.float32)
    jj = np.arange(127)
    nn = np.arange(32)
    ov = ((jj[:, None] * 16 < nn[None, :] * 64 + 64) & (jj[:, None] * 16 + 32 > nn[None, :] * 64)).astype(np.float32)
    ovl = np.zeros((128, 33), np.float32)
    ovl[:127, :32] = ov
    ovl[:127, 32] = 1.0
    c["ovl"] = ovl
    cur = (np.arange(T) // 64)[:, None]
    nid = np.arange(32)[None, :]
    forced = (nid == 0) | (nid == cur) | (nid == cur - 1)
    future = nid > cur
    keep = (~forced & ~future).astype(np.float32)
    add = np.where(future, -1e30, np.where(forced, 100.0, 0.0)).astype(np.float32)
    ka = np.zeros((128, 2, 16, 32), np.float32)
    ka[:, 0] = keep.reshape(16, 128, 32).transpose(1, 0, 2)
    ka[:, 1] = add.reshape(16, 128, 32).transpose(1, 0, 2)
    c["keepadd"] = ka
    rc = np.zeros((128, 4), np.float32)
    pp = np.arange(128)
    rc[:, 0] = THETA ** (-(pp % 32).astype(np.float64) / 32.0)
    rc[:, 1] = np.where((pp % 64) < 32, -1.0, 1.0)
    m = (pp >= 64) & (pp < 96)
    rc[m, 2] = THETA ** (-((pp[m] - 64) % 16).astype(np.float64) / 16.0)
    rc[m, 3] = np.where((pp[m] - 64) < 16, -1.0, 1.0)
    c["ropec"] = rc
    ind = np.zeros((128, 16, 16), np.float32)
    for kb in range(16):
        ind[:, kb, kb] = 1.0
    c["indt"] = ind
    c["iota"] = np.tile(np.arange(512, dtype=np.float32)[None, :], (128, 1))
    sg = np.zeros((16, 16, 128), np.float32)
    for kb in range(16):
        sg[kb + 1:, kb, :] = 1.0
    c["selgt"] = sg
    return c


CONST_SHAPES = {"iota": [128, 512], "ident": [128, 128], "cmask": [128, 5, 128], "cmpvalid": [128, T], "eexp": [32, T],
                "ovl": [128, 33], "keepadd": [128, 2, 16, 32], "ropec": [128, 4], "indt": [128, 16, 16],
                "selgt": [16, 16, 128]}


class MK:
    NW = 3

    def __init__(self, layers=(0, 1), debug=False, stop_after=None):
        self.layers = layers
        self.debug = debug
        self.stop_after = stop_after
        self.nc = bass.Bass("TRN2", target_bir_lowering=False)
        self.S = Sched()
        self.st = contextlib.ExitStack()
        self.st.enter_context(self.nc.allow_low_precision(reason="bf16 matmul operands / fp32 accumulation by design"))
        self.wi = 0
        self.stg_i = 0
        self.deferred = []
        self.si = 0
        self.ai = 0
        self.finals = []
        self.dbg_outs = {}

    def din(self, name, shape, dt=F32):
        return self.nc.dram_tensor(name, list(shape), dt, kind="ExternalInput").ap()

    def dout(self, name, shape, dt=F32):
        return self.nc.dram_tensor(name, list(shape), dt, kind="ExternalOutput").ap()

    def sb(self, name, shape, dt):
        return self.st.enter_context(self.nc.sbuf_tensor(name, list(shape), dt))

    def arena_reset(self):
        self.S.barrier()
        self.aoff = 0

    def carve(self, shape, dt, parts=128):
        n = 1
        for s in shape:
            n *= s
        nbytes = n * (4 if dt in (F32, I32) else 2)
        nbytes = (nbytes + 63) // 64 * 64
        off = self.aoff
        self.aoff += nbytes
        assert self.aoff <= self.ARENA_BYTES, (self.aoff, self.ARENA_BYTES)
        v = self.arena[0:parts, off // 2:(off + n * (4 if dt in (F32, I32) else 2)) // 2]
        if dt != BF16:
            v = v.bitcast(dt)
        if len(shape) == 2:
            v = v.rearrange("p (a b) -> p a b", a=shape[0])
        elif len(shape) == 3:
            v = v.rearrange("p (a b c) -> p a b c", a=shape[0], b=shape[1])
        return v

    def flush_deferred(self):
        dl, self.deferred = self.deferred, []
        for fn in dl:
            fn()

    def short(self):
        b = self.si % 4
        self.si += 1
        return b

    def accb(self):
        b = 4 + self.ai % 4
        self.ai += 1
        return b

    def mm(self, out, lhsT, rhs, start, stop, r, w):
        self.S.op("pe", lambda e: e.matmul(out, lhsT=lhsT, rhs=rhs, start=start, stop=stop),
                  reads=r, writes=w, acc=True)

    def tr(self, out, in_, r, w, parts=128):
        idt = self.ident[0:parts, 0:parts]
        self.S.op("pe", lambda e: e.transpose(out, in_, idt), reads=list(r) + ["ident"], writes=w, acc=True)

    def A(self, out, in_, func, r, w, bias=None, scale=None, accum=None):
        kw = {}
        if bias is not None:
            kw["bias"] = bias
        if scale is not None:
            kw["scale"] = scale
        if accum is not None:
            kw["accum_out"] = accum
        self.S.op("act", lambda e: e.activation(out=out, in_=in_, func=func, **kw), reads=r, writes=w)

    def TT(self, eng, out, in0, in1, op, r, w):
        self.S.op(eng, lambda e: e.tensor_tensor(out=out, in0=in0, in1=in1, op=op), reads=r, writes=w)

    def TS(self, eng, out, in0, s1, s2, op0, op1, r, w):
        if op1 is None:
            self.S.op(eng, lambda e: e.tensor_scalar(out=out, in0=in0, scalar1=s1, scalar2=None, op0=op0),
                      reads=r, writes=w)
        else:
            self.S.op(eng, lambda e: e.tensor_scalar(out=out, in0=in0, scalar1=s1, scalar2=s2, op0=op0, op1=op1),
                      reads=r, writes=w)

    def STT(self, eng, out, in0, scalar, in1, op0, op1, r, w):
        self.S.op(eng, lambda e: e.scalar_tensor_tensor(out=out, in0=in0, scalar=scalar, in1=in1, op0=op0, op1=op1),
                  reads=r, writes=w)

    def CP(self, eng, out, in_, r, w):
        if eng == "act":
            self.S.op("act", lambda e: e.activation(out=out, in_=in_, func=AF.Copy), reads=r, writes=w)
        else:
            self.S.op(eng, lambda e: e.tensor_copy(out=out, in_=in_), reads=r, writes=w)

    def MS(self, eng, out, val, w):
        self.S.op(eng, lambda e: e.memset(out, val), writes=w)

    def dma(self, out, in_, r, w, eng="sp"):
        return self.S.op(eng, lambda e: e.dma_start(out=out, in_=in_), reads=r, writes=w, dma=True)

    CAST_ENGS = ("dve", "act", "dve", "act")

    def cast_load(self, dst, src, parts, free_shape, dkey):
        n = 1
        for x_ in free_shape:
            n *= x_
        assert n <= 2048, n
        si_ = self.stg_i % len(self.stage)
        ce = self.CAST_ENGS[self.stg_i % 4]
        self.stg_i += 1
        stg = self.stage[si_][0:parts, 0:n]
        if len(free_shape) == 2:
            stg = stg.rearrange("p (a b) -> p a b", a=free_shape[0])
        elif len(free_shape) == 3:
            stg = stg.rearrange("p (a b c) -> p a b c", a=free_shape[0], b=free_shape[1])
        sk = ("stg", si_)
        self.dma(stg, src, [], [sk])
        self.CP(ce, dst, stg, [sk], [dkey])

    def dma_w1(self, w1t, src, c):
        si_ = self.stg_i % len(self.stage)
        ce = self.CAST_ENGS[self.stg_i % 4]
        self.stg_i += 1
        ps_ = slice(c * 64, (c + 1) * 64)
        stg = self.stage[si_][ps_, 0:2048].rearrange("p (l e) -> p l e", l=32)
        sk = ("stg", si_)
        self.dma(stg, src.rearrange("(l d) e -> d l e", d=64), [], [sk])
        self.CP(ce, w1t[ps_, :, :], stg, [sk], ["w1t"])

    def wload(self, src, kc, n, rows=128):
        slot = self.wi % self.NW
        self.wi += 1
        assert kc * n <= 4096
        v = self.wring[slot][0:rows, 0:kc * n].rearrange("p (c n) -> p c n", c=kc)
        srcv = src.rearrange("(c p) n -> p c n", p=rows)
        key = ("w", slot)
        step = max(1, 2048 // n)
        for k0 in range(0, kc, step):
            k1 = min(kc, k0 + step)
            self.cast_load(v[:, k0:k1, :], srcv[:, k0:k1, :], rows, [k1 - k0, n], key)
        return v, key

    def proj_fm(self, wv, wkey, c0, M, tt, src=None, srckey=None, kc=8):
        b = self.short()
        src = self.xT if src is None else src
        srckey = ("xT", tt) if srckey is None else srckey
        for k in range(kc):
            self.mm(self.pb[b][0:M, :], wv[:, k, c0:c0 + M], src[:, k, tt * 512:(tt + 1) * 512],
                    k == 0, k == kc - 1, [wkey, srckey], [("pb", b)])
        return b

    def build(self):
        nc, S = self.nc, self.S
        L = self.layers
        x_in = self.din("x", [T, D])
        p_in = self.din("p", [2, T, 256])
        pos_in = self.din("pos", [1, T], I32)
        win = self.din("win", [2, D, NCOLS_R])
        convp = self.din("convp", [2, 128, 2, 34])
        pe_r = self.din("pe_r", [2, 128, 32])
        w1 = self.din("w1", [2, 2, 2048, 64])
        w2k = self.din("w2k", [2, 64, 128])
        w2v = self.din("w2v", [2, 64, 64])
        qn = self.din("qn", [2, 128, 2])
        kvn = self.din("kvn", [2, 128, 1])
        uq = self.din("uq", [2, 256, 768])
        ukv = self.din("ukv", [2, 128, 512])
        wbr = self.din("wbr", [2, 4, 256, D])
        wout = self.din("wout", [2, D, D])
        ln1g = self.din("ln1g", [2, D])
        ln1b = self.din("ln1b", [2, D])
        ln2g = self.din("ln2g", [2, D])
        ln2b = self.din("ln2b", [2, D])
        lite = self.stop_after is not None and self.stop_after[0] in ("conv", "nsa", "mla", "sb", "mix")
        need_ffn = (0 in L) and not lite
        need_moe = (1 in L) and not (lite and self.stop_after[1] == 0) and self.stop_after != ("ffn", 0)
        ffn_in = self.din("ffn_in", [D, 2 * D_FF]) if need_ffn else None
        ffn_out = self.din("ffn_out", [D_FF, D]) if need_ffn else None
        router = self.din("router", [D, NE])
        moe_in = self.din("moe_in", [NE, D, 2 * D_FFE]) if need_moe else None
        moe_out = self.din("moe_out", [NE, D_FFE, D]) if need_moe else None
        pleg = self.din("pleg", [2, D, D])
        plep = self.din("plep", [2, 256, D])
        cst = {k: self.din("c_" + k, v) for k, v in CONST_SHAPES.items()}
        y_out = self.dout("y", [T, D])
        xa = self.dout("xa", [T, D])
        xb = self.dout("xb", [T, D])
        tabs_d = self.dout("tabs_d", [2, 128, 2 * T], BF16)
        self.dram = dict(locals())

        self.xT = self.sb("xT", [128, 8, T], BF16)
        self.wring = [self.sb("wr%d" % i, [128, 4096], BF16) for i in range(self.NW)]
        self.stage = [self.sb("stg%d" % i, [128, 2048], F32) for i in range(2)]
        self.ident = self.sb("ident", [128, 128], F32)
        self.cmask = self.sb("cmask", [128, 5, 128], BF16)
        self.onesf = self.sb("onesf", [128, 128], F32)
        self.gates = self.sb("gates", [128, NB, NE], F32)
        self.pb = [self.st.enter_context(nc.psum_tensor("pb%d" % i, [128, 512], F32)) for i in range(8)]
        self.ARENA_BYTES = 133 * 1024
        self.arena = self.sb("arena", [128, self.ARENA_BYTES // 2], BF16)
        self.aoff = 0

        self.dma(self.ident[:], cst["ident"], [], ["ident"])
        self.cast_load(self.cmask[:], cst["cmask"], 128, [5, 128], "cmask")
        self.MS("dve", self.onesf[:], 1.0, ["onesf"])

        self.setup_rope(pos_in, cst)
        self.load_xT(x_in)

        res_in = x_in
        outs = [(xa, xb), (xa, y_out)]
        for li in L:
            mid, fin = outs[li]
            if li == 1:
                res_in = xb
            if self.mixer_phase(li, res_in, mid):
                break
            if self.stop_after == ("mix", li):
                break
            self.ffn_phase(li, mid, fin)
            if self.stop_after == ("ffn", li):
                break
        S.emit(nc, final_wait_ops=self.finals)
        self.st.close()
        return nc

    def setup_rope(self, pos_in, cst):
        self.arena_reset()
        self.tabN = self.carve([2, T], BF16)
        self.tabM = self.carve([2, T], BF16)
        pi = self.carve([T], I32)
        pf = self.carve([T], F32)
        ang = self.carve([T], F32)
        kf = self.carve([T], F32)
        ki = self.carve([T], I32)
        rc = self.carve([4], F32)
        self.dma(pi, pos_in[0:1, :].to_broadcast([128, T]), [], ["pi"])
        self.dma(rc, cst["ropec"], [], ["rc"])
        self.CP("dve", pf, pi, ["pi"], ["pf"])
        for tab, ic, sc in ((self.tabN, 0, 1), (self.tabM, 2, 3)):
            for which in range(2):
                shift = math.pi / 2 if which == 0 else 0.0
                self.TS("dve", ang, pf, rc[:, ic:ic + 1], shift, ALU.mult, ALU.add, ["pf", "rc"], ["ang"])
                self.TS("dve", kf, ang, 1.0 / (2 * math.pi), None, ALU.mult, None, ["ang"], ["kf"])
                self.CP("dve", ki, kf, ["kf"], ["ki"])
                self.CP("dve", kf, ki, ["ki"], ["kf"])
                self.STT("dve", ang, kf, -2 * math.pi, ang, ALU.mult, ALU.add, ["kf", "ang"], ["ang"])
                self.TS("dve", kf, ang, math.pi, -2 * math.pi, ALU.is_gt, ALU.mult, ["ang"], ["kf"])
                self.TT("dve", ang, ang, kf, ALU.add, ["ang", "kf"], ["ang"])
                self.TS("dve", kf, ang, -math.pi, 2 * math.pi, ALU.is_lt, ALU.mult, ["ang"], ["kf"])
                self.TT("dve", ang, ang, kf, ALU.add, ["ang", "kf"], ["ang"])
                self.A(ang, ang, AF.Sin, ["ang"], ["ang"])
                if which == 0:
                    self.CP("dve", tab[:, 0, :], ang, ["ang"], ["tab"])
                else:
                    self.TS("dve", tab[:, 1, :], ang, rc[:, sc:sc + 1], None, ALU.mult, None, ["ang", "rc"], ["tab"])
        td = self.dram["tabs_d"]
        self.dma(td[0], self.tabN.rearrange("p a t -> p (a t)"), ["tab"], ["tabs_d"])
        self.dma(td[1], self.tabM.rearrange("p a t -> p (a t)"), ["tab"], ["tabs_d"])

    def x_to_xT(self, xblk, xkey, tb, rt=None):
        tt = tb // 4
        for half in range(2):
            b = self.short()
            for c in range(4):
                cc = half * 4 + c
                self.tr(self.pb[b][:, c * 128:(c + 1) * 128], xblk[:, cc * 128:(cc + 1) * 128], [xkey], [("pb", b)])
            dst = self.xT[:, half * 4:half * 4 + 4, tb * 128:(tb + 1) * 128]
            src = self.pb[b][:, :].rearrange("p (c t) -> p c t", c=4)
            self.CP("act" if half == 0 else "dve", dst, src, [("pb", b)], [("xT", tt)])
            if rt is not None:
                rt(half, b)

    def load_xT(self, x_in):
        self.arena_reset()
        xbs = [self.carve([D], F32) for _ in range(2)]
        for tb in range(NB):
            xb_ = xbs[tb % 2]
            key = ("xblk", tb % 2)
            self.dma(xb_, x_in[tb * 128:(tb + 1) * 128, :], [], [key])
            self.x_to_xT(xb_, key, tb)

    def mixer_phase(self, li, res_in, res_out):
        d = self.dram
        self.arena_reset()
        self.stage = self.stage[0:2]
        self.yT = [self.carve([2, T], BF16) for _ in range(4)]
        self.mixer_base = self.aoff
        for n, (nm, fn) in enumerate((("conv", self.conv_branch), ("nsa", self.nsa_branch), ("mla", self.mla_branch),
                                     ("sb", self.sb_branch))):
            only = getattr(self, "only", None)
            if only is None or nm in only:
                fn(li)
                self.dbg("yT%d_%d" % (n, li), self.yT[n], [128, 2, T], ["yT%d" % n])
            self.aoff = self.mixer_base
            self.S.barrier()
            if self.stop_after == (nm, li):
                return True
        self.merge_ln1(li, res_in, res_out)

    def dbg(self, name, ap, shape, keys, dt=BF16):
        if not self.debug:
            return
        o = self.dout("dbg_" + name, shape, dt)
        self.finals.append(self.dma(o, ap, keys, []))

    def conv_branch(self, li):
        d = self.dram
        win = d["win"]
        cp = self.carve([2, 34], F32)
        self.dma(cp, d["convp"][li], [], ["cp"])
        hp = self.carve([2, 30 + T], F32)
        acc = self.carve([2, T], F32)
        sig = [self.carve([512], F32) for _ in range(2)]
        self.MS("pool", hp[:, :, 0:30], 0.0, ["hp"])
        wv, wk = self.wload(win[li][:, OFF_CONV:OFF_CONV + 512], 8, 512)
        for tt in range(4):
            for c in range(2):
                ba = self.proj_fm(wv, wk, c * 128, 128, tt)
                bg = self.proj_fm(wv, wk, 256 + c * 128, 128, tt)
                sg = sig[c]
                self.A(sg, self.pb[bg][:, :], AF.Sigmoid, [("pb", bg)], [("sig", c)])
                self.TT("dve", hp[:, c, 30 + tt * 512:30 + (tt + 1) * 512], self.pb[ba][:, :], sg, ALU.mult,
                        [("pb", ba), ("sig", c)], ["hp"])
        for c in range(2):
            eng = "dve"
            self.TS(eng, acc[:, c, :], hp[:, c, 0:T], cp[:, c, 0:1], cp[:, c, 31:32], ALU.mult, ALU.add,
                    ["hp", "cp"], [("acc", c)])
            for w in range(1, 31):
                self.STT(eng, acc[:, c, :], hp[:, c, w:w + T], cp[:, c, w:w + 1], acc[:, c, :], ALU.mult, ALU.add,
                         ["hp", "cp", ("acc", c)], [("acc", c)])
        sq = [self.carve([512], F32) for _ in range(2)]
        m2 = self.carve([512], F32)
        rstd = self.carve([512], F32)
        dd = [self.carve([512], F32) for _ in range(2)]
        for tt in range(4):
            sl = slice(tt * 512, (tt + 1) * 512)
            bm = self.short()
            for c in range(2):
                self.mm(self.pb[bm][:, :], self.onesf[:, :], acc[:, c, sl], c == 0, c == 1,
                        ["onesf", ("acc", c)], [("pb", bm)])
            bq = self.short()
            for c in range(2):
                self.A(sq[c], acc[:, c, sl], AF.Square, [("acc", c)], [("sq", c)])
            for c in range(2):
                self.mm(self.pb[bq][:, :], self.onesf[:, :], sq[c], c == 0, c == 1,
                        ["onesf", ("sq", c)], [("pb", bq)])
            self.A(m2, self.pb[bm][:, :], AF.Square, [("pb", bm)], ["m2"], scale=1.0 / 256)
            self.STT("dve", rstd, self.pb[bq][:, :], 1.0 / 256, m2, ALU.mult, ALU.subtract, [("pb", bq), "m2"], ["rstd"])
            self.A(rstd, rstd, AF.Sqrt, ["rstd"], ["rstd"], bias=LN_EPS)
            self.S.op("dve", lambda e, r=rstd: e.reciprocal(out=r, in_=r), reads=["rstd"], writes=["rstd"])
            for c in range(2):
                self.STT("dve", dd[c], self.pb[bm][:, :], -1.0 / 256, acc[:, c, sl], ALU.mult, ALU.add,
                         [("pb", bm), ("acc", c)], [("dd", c)])
                self.TT("dve", dd[c], dd[c], rstd, ALU.mult, [("dd", c), "rstd"], [("dd", c)])
                self.A(self.yT[0][:, c, sl], dd[c], AF.Silu, [("dd", c), "cp"], ["yT0"],
                       scale=cp[:, c, 32:33], bias=cp[:, c, 33:34])

    def y_to_yT(self, ytile, ykey, n, qb):
        b = self.short()
        for c in range(2):
            self.tr(self.pb[b][:, c * 128:(c + 1) * 128], ytile[:, c * 128:(c + 1) * 128], [ykey], [("pb", b)])
        self.CP("act", self.yT[n][:, :, qb * 128:(qb + 1) * 128],
                self.pb[b][:, 0:256].rearrange("p (c t) -> p c t", c=2), [("pb", b)], ["yT%d" % n])

    def softmax_attn_block(self, qb, kbs, kT_fn, qT_ap, v_fn, scale, extra_mask=None, finalize=None):
        ob = self.accb()
        n = len(kbs)

        def front(g0):
            grp = kbs[g0:g0 + 4]
            sbk = self.short()
            for i, (kb, mt) in enumerate(grp):
                kT, kkeys = kT_fn(kb)
                self.mm(self.pb[sbk][:, i * 128:(i + 1) * 128], kT, qT_ap[0], True, True,
                        list(kkeys) + list(qT_ap[1]), [("pb", sbk)])
            ei = self.ei % 5
            self.ei += 1
            E = self.Ebuf[ei]
            ek = ("E", ei)
            w = len(grp) * 128
            self.A(E[:, 0:w], self.pb[sbk][:, 0:w], AF.Exp, [("pb", sbk)], [ek], scale=scale)
            if extra_mask is not None:
                mtile, mkey = extra_mask
                self.TT("dve", E[:, 0:w], E[:, 0:w], mtile[:, g0 * 128:g0 * 128 + w], ALU.mult, [ek, mkey], [ek])
            else:
                for i, (kb, mt) in enumerate(grp):
                    if mt is not None:
                        mi = {"le": 0, "lt": 1, "gt": 2}[mt]
                        self.TT("dve", E[:, i * 128:(i + 1) * 128], E[:, i * 128:(i + 1) * 128],
                                self.cmask[:, mi, :], ALU.mult, [ek, "cmask"], [ek])
            return g0, grp, E, ek

        def back(st_):
            g0, grp, E, ek = st_
            for i, (kb, mt) in enumerate(grp):
                v, vkeys = v_fn(kb)
                gi = g0 + i
                self.mm(self.pb[ob][:, 0:65], E[:, i * 128:(i + 1) * 128], v, gi == 0, gi == n - 1,
                        [ek] + list(vkeys), [("pb", ob)])

        prev = None
        for g0 in range(0, n, 4):
            cur = front(g0)
            if g0 == 0:
                self.flush_deferred()
            if prev is not None:
                back(prev)
            prev = cur

        def tail(prev=prev):
            back(prev)
            finalize(ob)
        self.deferred.append(tail)
        return ob

    def nsa_branch(self, li):
        d = self.dram
        win = d["win"]
        cst = {k: d["cst"][k] for k in d["cst"]}
        self.tabN = self.carve([2, T], BF16)
        self.dma(self.tabN.rearrange("p a t -> p (a t)"), d["tabs_d"][0], [], ["tab"])
        QT = self.carve([2, T], BF16)
        QR = self.carve([2, T], BF16)
        KS = self.carve([T], BF16)
        KW = self.carve([T], BF16)
        KCV = self.carve([T], BF16)
        vS = self.carve([NB, 65], BF16)
        vW = self.carve([NB, 65], BF16)
        sg = self.carve([NB, 12], F32)
        cmpvalid = self.carve([T], BF16)
        eexp = self.carve([T], BF16, parts=32)
        keepadd = self.carve([2, NB, 32], F32)
        VC = self.carve([97], BF16)
        w1t = self.carve([32, 64], BF16)
        pet = self.carve([32], BF16)
        w2kt = self.carve([128], BF16, parts=64)
        w2vt = self.carve([64], BF16, parts=64)
        hid = [self.carve([127], BF16, parts=64) for _ in range(2)]
        hb = self.carve([2], F32, parts=64)
        kcT = self.carve([127], BF16)
        t1 = [self.carve([512], F32) for _ in range(2)]
        t2 = [self.carve([512], F32) for _ in range(2)]
        self.Ebuf = [self.carve([512], BF16) for _ in range(5)]
        self.ei = 0
        ytile = [self.carve([256], F32) for _ in range(2)]
        selmask = [self.carve([NB * 128], BF16) for _ in range(2)]
        small = [self.carve([64], F32) for _ in range(2)]
        imp = [self.carve([32], F32) for _ in range(2)]
        scr = [self.carve([32], F32) for _ in range(2)]
        selT = [self.carve([128], BF16, parts=32) for _ in range(2)]
        self.cast_load(cmpvalid, cst["cmpvalid"], 128, [T], "cmpvalid")
        self.cast_load(eexp, cst["eexp"], 32, [T], "eexp")
        self.dma(keepadd, cst["keepadd"], [], ["keepadd"])
        self.cast_load(VC[:, 64:97], cst["ovl"], 128, [33], "VCc")
        for c in range(2):
            self.dma_w1(w1t, d["w1"][li, c], c)
        self.cast_load(pet, d["pe_r"][li], 128, [32], "pet")
        self.cast_load(w2kt, d["w2k"][li], 64, [128], "w2kt")
        self.cast_load(w2vt, d["w2v"][li], 64, [64], "w2vt")
        self.MS("dve", vS[:, :, 64:65], 1.0, ["vS"])
        self.MS("dve", vW[:, :, 64:65], 1.0, ["vW"])
        wv, wk = self.wload(win[li][:, OFF_NQ:OFF_NQ + 512], 8, 512)
        for tt in range(4):
            sl = slice(tt * 512, (tt + 1) * 512)
            for c in range(2):
                b0 = self.proj_fm(wv, wk, c * 128, 128, tt)
                b1 = self.proj_fm(wv, wk, 256 + c * 128, 128, tt)
                self.CP("act", QT[:, c, sl], self.pb[b0][:, :], [("pb", b0)], ["QT"])
                self.TT("dve", t1[c], self.pb[b0][:, :], self.tabN[:, 0, sl], ALU.mult, [("pb", b0), "tab"], [("t1", c)])
                self.TT("dve", t2[c], self.pb[b1][:, :], self.tabN[:, 1, sl], ALU.mult, [("pb", b1), "tab"], [("t2", c)])
                self.TT("dve", QR[:, c, sl], t1[c], t2[c], ALU.add, [("t1", c), ("t2", c)], ["QR"])
        wv, wk = self.wload(win[li][:, OFF_NK:OFF_NK + 512], 8, 512)
        for tt in range(4):
            sl = slice(tt * 512, (tt + 1) * 512)
            for c, dst, dk in ((0, KS, "KS"), (1, KW, "KW")):
                b0 = self.proj_fm(wv, wk, c * 256, 128, tt)
                b1 = self.proj_fm(wv, wk, c * 256 + 128, 128, tt)
                self.TT("dve", t1[c], self.pb[b0][:, :], self.tabN[:, 0, sl], ALU.mult, [("pb", b0), "tab"], [("t1", c)])
                self.TT("dve", t2[c], self.pb[b1][:, :], self.tabN[:, 1, sl], ALU.mult, [("pb", b1), "tab"], [("t2", c)])
                self.TT("dve", dst[:, sl], t1[c], t2[c], ALU.add, [("t1", c), ("t2", c)], [dk])
        wvm, wkm = self.wload(win[li][:, OFF_MISC:OFF_MISC + 128], 8, 128)
        for tt in range(4):
            sl = slice(tt * 512, (tt + 1) * 512)
            b0 = self.proj_fm(wvm, wkm, 0, 128, tt)
            self.CP("act", KCV[:, sl], self.pb[b0][:, :], [("pb", b0)], ["KCV"])
        wvt, wkt = self.wload(win[li][:, OFF_TOK:OFF_TOK + 140], 8, 140)
        for tb in range(NB):
            b = self.short()
            for k in range(8):
                self.mm(self.pb[b][:, 0:140], self.xT[:, k, tb * 128:(tb + 1) * 128], wvt[:, k, :], k == 0, k == 7,
                        [wkt, ("xT", tb // 4)], [("pb", b)])
            self.CP("act", vS[:, tb, 0:64], self.pb[b][:, 0:64], [("pb", b)], ["vS"])
            self.CP("dve", vW[:, tb, 0:64], self.pb[b][:, 64:128], [("pb", b)], ["vW"])
            self.A(sg[:, tb, :], self.pb[b][:, 128:140], AF.Sigmoid, [("pb", b)], ["sg"])
        for c in range(2):
            ps_ = slice(c * 64, (c + 1) * 64)
            b = self.short()
            for l in range(32):
                self.mm(self.pb[b][0:64, 0:127], w1t[ps_, l, :], KCV[ps_, l:l + 16 * 126 + 1:16], l == 0, l == 31,
                        ["w1t", "KCV"], [("pb", b)])
            b2 = self.short()
            for l in range(32):
                self.mm(self.pb[b2][0:64, 0:1], w1t[ps_, l, :], pet[ps_, l:l + 1], l == 0, l == 31,
                        ["w1t", "pet"], [("pb", b2)])
            self.CP("dve", hb[:, c:c + 1], self.pb[b2][0:64, 0:1], [("pb", b2)], ["hb"])
            self.A(hid[c], self.pb[b][0:64, 0:127], AF.Gelu_apprx_tanh, [("pb", b), "hb"], [("hid", c)],
                   bias=hb[:, c:c + 1])
        b = self.short()
        self.mm(self.pb[b][:, 0:127], w2kt[:, :], hid[0], True, True, ["w2kt", ("hid", 0)], [("pb", b)])
        self.CP("act", kcT, self.pb[b][:, 0:127], [("pb", b)], ["kcT"])
        b = self.short()
        self.mm(self.pb[b][0:127, 0:64], hid[1], w2vt[:, :], True, True, ["w2vt", ("hid", 1)], [("pb", b)])
        self.CP("act", VC[0:127, 0:64], self.pb[b][0:127, 0:64], [("pb", b)], ["VCv"])
        for qb in range(NB):
            qs = slice(qb * 128, (qb + 1) * 128)
            yt = ytile[qb % 2]
            yk = ("yt", qb % 2)
            sm = small[qb % 2]
            smk = ("small", qb % 2)
            im = imp[qb % 2]
            imk = ("imp", qb % 2)
            sbk2 = [self.short(), self.short()]
            for h in range(4):
                hp_ = slice((h % 2) * 64, (h % 2) * 64 + 64)
                bb_ = sbk2[h % 2]
                self.mm(self.pb[bb_][0:127, (h // 2) * 128:(h // 2 + 1) * 128], kcT[hp_, :], QT[hp_, h // 2, qs], True, True,
                        ["kcT", "QT"], [("pb", bb_)])
            ei = self.ei % 5
            self.ei += 1
            E = self.Ebuf[ei]
            ek = ("E", ei)
            for h in range(4):
                bb_ = sbk2[h % 2]
                self.A(E[0:127, h * 128:(h + 1) * 128], self.pb[bb_][0:127, (h // 2) * 128:(h // 2 + 1) * 128], AF.Exp,
                       [("pb", bb_)], [ek], scale=0.125)
            for h in range(4):
                self.TT("dve", E[0:127, h * 128:(h + 1) * 128], E[0:127, h * 128:(h + 1) * 128], cmpvalid[0:127, qs],
                        ALU.mult, [ek, "cmpvalid"], [ek])
            ob = self.accb()
            for h in range(4):
                self.mm(self.pb[ob][:, h * 97:(h + 1) * 97], E[0:127, h * 128:(h + 1) * 128], VC[0:127, :], True, True,
                        [ek, "VCv", "VCc"], [("pb", ob)])
            P = self.pb[ob]
            for h in range(4):
                self.TS("dve", sm[:, h:h + 1], P[:, 97 * h + 96:97 * h + 97], 1e-30, None, ALU.max, None, [("pb", ob)], [smk])
            self.S.op("dve", lambda e, s_=sm: e.reciprocal(out=s_[:, 0:4], in_=s_[:, 0:4]), reads=[smk], writes=[smk])
            for h in range(4):
                if h == 0:
                    self.TS("dve", im, P[:, 64:96], sm[:, 0:1], None, ALU.mult, None, [("pb", ob), smk], [imk])
                else:
                    self.STT("dve", im, P[:, 97 * h + 64:97 * h + 96], sm[:, h:h + 1], im, ALU.mult, ALU.add,
                             [("pb", ob), smk, imk], [imk])
            for h in range(4):
                self.TT("dve", sm[:, 4 + h:5 + h], sm[:, h:h + 1], sg[:, qb, 3 * h:3 * h + 1], ALU.mult, [smk, "sg"], [smk])
                self.A(yt[:, h * 64:(h + 1) * 64], P[:, 97 * h:97 * h + 64], AF.Copy, [("pb", ob), smk], [yk],
                       scale=sm[:, 4 + h:5 + h])
            mk = None
            if qb >= 8:
                sc_ = scr[qb % 2]
                sck = ("scr", qb % 2)
                self.TT("dve", im, im, keepadd[:, 0, qb, :], ALU.mult, [imk, "keepadd"], [imk])
                self.TT("dve", im, im, keepadd[:, 1, qb, :], ALU.add, [imk, "keepadd"], [imk])
                self.S.op("dve", lambda e, s_=sm, i_=im: e.max(out=s_[:, 8:16], in_=i_), reads=[imk, smk], writes=[smk])
                self.S.op("dve", lambda e, s_=sm, i_=im, c_=sc_: e.match_replace(out=c_, in_to_replace=s_[:, 8:16],
                                                                                in_values=i_, imm_value=-1e30),
                          reads=[imk, smk], writes=[sck])
                self.S.op("dve", lambda e, s_=sm, c_=sc_: e.max(out=s_[:, 16:24], in_=c_), reads=[sck, smk], writes=[smk])
                self.TS("dve", sc_, im, sm[:, 23:24], None, ALU.is_ge, None, [imk, smk], [sck])
                b = self.short()
                self.tr(self.pb[b][0:32, 0:128], sc_, [sck], [("pb", b)])
                sT = selT[qb % 2]
                stk = ("selT", qb % 2)
                self.CP("act", sT, self.pb[b][0:32, 0:128], [("pb", b)], [stk])
                smt = selmask[qb % 2]
                mk = ("selmask", qb % 2)
                for g0 in range(0, qb + 1, 4):
                    n = min(4, qb + 1 - g0)
                    b = self.short()
                    for i in range(n):
                        kb = g0 + i
                        self.mm(self.pb[b][:, i * 128:(i + 1) * 128], eexp[:, kb * 128:(kb + 1) * 128], sT, True, True,
                                ["eexp", stk], [("pb", b)])
                    self.CP("act", smt[:, g0 * 128:(g0 + n) * 128], self.pb[b][:, 0:n * 128], [("pb", b)], [mk])
                self.TT("dve", smt[:, qb * 128:(qb + 1) * 128], smt[:, qb * 128:(qb + 1) * 128], self.cmask[:, 0, :],
                        ALU.mult, [mk, "cmask"], [mk])
            for h in range(4):
                hp_ = slice((h % 2) * 64, (h % 2) * 64 + 64)
                qT_ap = (QR[hp_, h // 2, qs], ["QR"])
                for br, KT, kkey, V, vkey, gcol in ((1, KS, "KS", vS, "vS", 3 * h + 1), (2, KW, "KW", vW, "vW", 3 * h + 2)):
                    if br == 1:
                        kbs = [(kb, "le" if kb == qb else None) for kb in range(qb + 1)]
                        em = (selmask[qb % 2], mk) if qb >= 8 else None
                    else:
                        kbs = [(kb, "le" if kb == qb else ("gt" if kb == qb - 4 else None))
                               for kb in range(max(0, qb - 4), qb + 1)]
                        em = None
                    def fin(ob, h=h, br=br, gcol=gcol, yt=yt, yk=yk, sm=sm, smk=smk, qb=qb):
                        fac = sm[:, 24 + 2 * h + (br - 1):25 + 2 * h + (br - 1)]
                        self.S.op("dve", lambda e, f_=fac, o_=self.pb[ob][:, 64:65]: e.reciprocal(out=f_, in_=o_),
                                  reads=[("pb", ob), smk], writes=[smk])
                        self.TT("dve", fac, fac, sg[:, qb, gcol:gcol + 1], ALU.mult, [smk, "sg"], [smk])
                        self.STT("dve", yt[:, h * 64:(h + 1) * 64], self.pb[ob][:, 0:64], fac, yt[:, h * 64:(h + 1) * 64],
                                 ALU.mult, ALU.add, [("pb", ob), smk, yk], [yk])
                    self.softmax_attn_block(
                        qb, kbs, lambda kb, KT=KT, kkey=kkey: (KT[hp_, kb * 128:(kb + 1) * 128], [kkey]), qT_ap,
                        lambda kb, V=V, vkey=vkey: (V[:, kb, :], [vkey]), 0.125, extra_mask=em, finalize=fin)
            self.flush_deferred()
            self.y_to_yT(yt, yk, 1, qb)

    def mla_branch(self, li):
        d = self.dram
        win = d["win"]
        self.tabM = self.carve([2, T], BF16)
        self.dma(self.tabM.rearrange("p a t -> p (a t)"), d["tabs_d"][1], [], ["tab"])
        qg = self.carve([2, T], BF16)
        kvg = self.carve([T], BF16)
        CS = self.carve([2, T], BF16)
        rkv = self.carve([T], BF16)
        rkt = self.carve([NB], F32)
        Vm = self.carve([NB, 4, 65], BF16)
        QH = [self.carve([T], BF16) for _ in range(2)]
        KH = [self.carve([T], BF16) for _ in range(2)]
        KR = self.carve([T], BF16)
        nrm = self.carve([4], F32)
        sq = [self.carve([512], F32) for _ in range(2)]
        t1 = [self.carve([512], F32) for _ in range(2)]
        t2 = [self.carve([512], F32) for _ in range(2)]
        rs = self.carve([512], F32)
        self.Ebuf = [self.carve([512], BF16) for _ in range(5)]
        self.ei = 0
        self.ymla = self.carve([NB, 256], F32)
        small = [self.carve([8], F32) for _ in range(2)]
        self.dma(nrm[:, 0:2], d["qn"][li], [], ["nrm"])
        self.dma(nrm[:, 2:3], d["kvn"][li], [], ["nrm"])
        self.MS("dve", Vm[:, :, :, 64:65], 1.0, ["Vm"])
        wvm, wkm = self.wload(win[li][:, OFF_MISC + 128:OFF_MISC + 512], 8, 384)
        for tt in range(4):
            sl = slice(tt * 512, (tt + 1) * 512)
            bq = []
            for c in range(2):
                b0 = self.proj_fm(wvm, wkm, c * 128, 128, tt)
                bq.append(b0)
                self.A(qg[:, c, sl], self.pb[b0][:, :], AF.Copy, [("pb", b0), "nrm"], ["qg"], scale=nrm[:, c:c + 1])
                self.A(sq[c], self.pb[b0][:, :], AF.Square, [("pb", b0)], [("sq", c)])
            bs = self.short()
            for c in range(2):
                self.mm(self.pb[bs][:, :], self.onesf[:, :], sq[c], c == 0, c == 1, ["onesf", ("sq", c)], [("pb", bs)])
            self.A(rs, self.pb[bs][:, :], AF.Sqrt, [("pb", bs)], ["rs"], scale=1.0 / 256, bias=RMS_EPS)
            self.S.op("dve", lambda e, r=rs: e.reciprocal(out=r, in_=r), reads=["rs"], writes=["rs"])
            for w in range(2):
                self.TT("dve", CS[:, w, sl], self.tabM[:, w, sl], rs, ALU.mult, ["tab", "rs"], ["CS"])
            b0 = self.proj_fm(wvm, wkm, 256, 128, tt)
            self.A(kvg[:, sl], self.pb[b0][:, :], AF.Copy, [("pb", b0), "nrm"], ["kvg"], scale=nrm[:, 2:3])
            self.A(sq[0], self.pb[b0][:, :], AF.Square, [("pb", b0)], [("sq", 0)])
            bs = self.short()
            self.mm(self.pb[bs][:, :], self.onesf[:, :], sq[0], True, True, ["onesf", ("sq", 0)], [("pb", bs)])
            self.A(rs, self.pb[bs][:, :], AF.Sqrt, [("pb", bs)], ["rs"], scale=1.0 / 128, bias=RMS_EPS)
            self.S.op("dve", lambda e, r=rs, o=rkv[:, sl]: e.reciprocal(out=o, in_=r), reads=["rs"], writes=["rkv"])
            bt = self.short()
            for i in range(4):
                self.mm(self.pb[bt][:, i:i + 1], sq[0][:, i * 128:(i + 1) * 128], self.onesf[:, 0:1], True, True,
                        [("sq", 0), "onesf"], [("pb", bt)])
            self.A(rkt[:, tt * 4:tt * 4 + 4], self.pb[bt][:, 0:4], AF.Sqrt, [("pb", bt)], ["rkt"], scale=1.0 / 128,
                   bias=RMS_EPS)
        self.S.op("dve", lambda e: e.reciprocal(out=rkt, in_=rkt), reads=["rkt"], writes=["rkt"])
        wvr, wkr = self.wload(win[li][:, OFF_KR:OFF_KR + 256], 8, 256)
        r9 = slice(64, 96)
        for tt in range(4):
            sl = slice(tt * 512, (tt + 1) * 512)
            b0 = self.proj_fm(wvr, wkr, 0, 96, tt)
            b1 = self.proj_fm(wvr, wkr, 128, 96, tt)
            self.TT("dve", t1[0][r9, :], self.pb[b0][r9, :], self.tabM[r9, 0, sl], ALU.mult, [("pb", b0), "tab"], [("t1", 0)])
            self.TT("dve", t2[0][r9, :], self.pb[b1][r9, :], self.tabM[r9, 1, sl], ALU.mult, [("pb", b1), "tab"], [("t2", 0)])
            self.TT("dve", KR[r9, sl], t1[0][r9, :], t2[0][r9, :], ALU.add, [("t1", 0), ("t2", 0)], ["KR"])
        wvu, wku = self.wload(d["ukv"][li], 1, 512)
        for tb in range(NB):
            b = self.short()
            self.mm(self.pb[b][:, 0:256], kvg[:, tb * 128:(tb + 1) * 128], wvu[:, 0, 256:512], True, True,
                    ["kvg", wku], [("pb", b)])
            self.A(Vm[:, tb, :, 0:64], self.pb[b][:, 0:256].rearrange("p (h c) -> p h c", h=4), AF.Copy,
                   [("pb", b), "rkt"], ["Vm"], scale=rkt[:, tb:tb + 1])
        wvq, wkq = self.wload(d["uq"][li], 2, 768)
        scale = 96.0 ** -0.5
        for h in range(4):
            Q = QH[h % 2]
            K = KH[h % 2]
            qk = ("QH", h % 2)
            kk = ("KH", h % 2)
            for tt in range(4):
                sl = slice(tt * 512, (tt + 1) * 512)
                ba = self.proj_fm(wvq, wkq, (2 * h) * 96, 96, tt, src=qg, srckey="qg", kc=2)
                bb = self.proj_fm(wvq, wkq, (2 * h + 1) * 96, 96, tt, src=qg, srckey="qg", kc=2)
                c = tt % 2
                self.TT("dve", t1[c][0:96, :], self.pb[ba][0:96, :], CS[0:96, 0, sl], ALU.mult, [("pb", ba), "CS"], [("t1", c)])
                self.TT("dve", t2[c][0:96, :], self.pb[bb][0:96, :], CS[0:96, 1, sl], ALU.mult, [("pb", bb), "CS"], [("t2", c)])
                self.TT("dve", Q[0:96, sl], t1[c][0:96, :], t2[c][0:96, :], ALU.add, [("t1", c), ("t2", c)], [qk])
                bk = self.short()
                self.mm(self.pb[bk][0:64, :], wvu[:, 0, h * 64:(h + 1) * 64], kvg[:, sl], True, True, [wku, "kvg"], [("pb", bk)])
                self.TT("dve", K[0:64, sl], self.pb[bk][0:64, :], rkv[0:64, sl], ALU.mult, [("pb", bk), "rkv"], [kk])
            self.CP("dve", K[r9, :], KR[r9, :], ["KR"], [kk])
            for qb in range(NB):
                qs = slice(qb * 128, (qb + 1) * 128)
                sm = small[qb % 2]
                smk = ("small", qb % 2)
                kbs = [(kb, "le" if kb == qb else None) for kb in range(qb + 1)]
                def fin(ob, sm=sm, smk=smk, qb=qb, h=h):
                    self.S.op("dve", lambda e, s_=sm, o_=self.pb[ob][:, 64:65]: e.reciprocal(out=s_[:, 0:1], in_=o_),
                              reads=[("pb", ob)], writes=[smk])
                    self.A(self.ymla[:, qb, h * 64:(h + 1) * 64], self.pb[ob][:, 0:64], AF.Copy, [("pb", ob), smk],
                           [("ymla", qb)], scale=sm[:, 0:1])
                self.softmax_attn_block(
                    qb, kbs, lambda kb, K=K, kk=kk: (K[0:96, kb * 128:(kb + 1) * 128], [kk]), (Q[0:96, qs], [qk]),
                    lambda kb, h=h: (Vm[:, kb, h, :], ["Vm"]), scale, finalize=fin)
            self.flush_deferred()
        for qb in range(NB):
            self.y_to_yT(self.ymla[:, qb, :], ("ymla", qb), 2, qb)

    def sb_branch(self, li):
        d = self.dram
        win = d["win"]
        cst = d["cst"]
        QT = self.carve([2, T], BF16)
        KT = self.carve([2, T], BF16)
        V = self.carve([NB, 256], BF16)
        indt = self.carve([16, 16], BF16)
        selgt = self.carve([16, 128], BF16, parts=16)
        sp = [self.carve([NB * 128], F32) for _ in range(2)]
        lk = [self.carve([NB * 128], BF16) for _ in range(2)]
        ex = [self.carve([512], F32) for _ in range(2)]
        ar = [self.carve([512], F32) for _ in range(2)]
        aa = [self.carve([512], BF16) for _ in range(5)]
        ts_ = [self.carve([128], BF16, parts=16) for _ in range(2)]
        ytile = [self.carve([256], F32) for _ in range(2)]
        self.cast_load(indt, cst["indt"], 128, [16, 16], "indt")
        self.cast_load(selgt, cst["selgt"], 16, [16, 128], "selgt")
        wv, wk = self.wload(win[li][:, OFF_SB:OFF_SB + 512], 8, 512)
        for tt in range(4):
            sl = slice(tt * 512, (tt + 1) * 512)
            for c in range(2):
                b0 = self.proj_fm(wv, wk, c * 128, 128, tt)
                self.CP("act", QT[:, c, sl], self.pb[b0][:, :], [("pb", b0)], ["QT"])
                b1 = self.proj_fm(wv, wk, 256 + c * 128, 128, tt)
                self.CP("dve", KT[:, c, sl], self.pb[b1][:, :], [("pb", b1)], ["KT"])
        wvt, wkt = self.wload(win[li][:, OFF_TOK + 256:OFF_TOK + 512], 8, 256)
        for tb in range(NB):
            b = self.short()
            for k in range(8):
                self.mm(self.pb[b][:, 0:256], self.xT[:, k, tb * 128:(tb + 1) * 128], wvt[:, k, :], k == 0, k == 7,
                        [wkt, ("xT", tb // 4)], [("pb", b)])
            self.CP("act", V[:, tb, :], self.pb[b][:, 0:256], [("pb", b)], ["V"])
        it = 0
        ai_ = 0
        for qb in range(NB):
            qs = slice(qb * 128, (qb + 1) * 128)
            yt = ytile[qb % 2]
            yk = ("yt", qb % 2)
            for h in range(4):
                hp_ = slice((h % 2) * 64, (h % 2) * 64 + 64)
                spt, lkt, tst = sp[it % 2], lk[it % 2], ts_[it % 2]
                spk, lkk, tsk = ("sp", it % 2), ("lk", it % 2), ("ts", it % 2)
                it += 1
                nk = qb + 1
                for g0 in range(0, nk, 4):
                    n = min(4, nk - g0)
                    w = n * 128
                    sbk = self.short()
                    for i in range(n):
                        kb = g0 + i
                        self.mm(self.pb[sbk][:, i * 128:(i + 1) * 128], KT[hp_, h // 2, kb * 128:(kb + 1) * 128],
                                QT[hp_, h // 2, qs], True, True, ["KT", "QT"], [("pb", sbk)])
                    e_ = ex[(g0 // 4) % 2]
                    exk = ("ex", (g0 // 4) % 2)
                    self.A(e_[:, 0:w], self.pb[sbk][:, 0:w], AF.Exp, [("pb", sbk)], [exk], scale=-0.125)
                    self.A(spt[:, g0 * 128:g0 * 128 + w], e_[:, 0:w], AF.Ln, [exk], [spk], bias=1.0)
                    self.STT("dve", lkt[:, g0 * 128:g0 * 128 + w], self.pb[sbk][:, 0:w], -0.125, spt[:, g0 * 128:g0 * 128 + w],
                             ALU.mult, ALU.subtract, [("pb", sbk), spk], [lkk])
                self.TT("dve", lkt[:, qb * 128:(qb + 1) * 128], lkt[:, qb * 128:(qb + 1) * 128], self.cmask[:, 1, :],
                        ALU.mult, [lkk, "cmask"], [lkk])
                bts = self.short()
                for kb in range(nk):
                    self.mm(self.pb[bts][0:16, 0:128], indt[:, kb, :], lkt[:, kb * 128:(kb + 1) * 128], kb == 0, kb == nk - 1,
                            ["indt", lkk], [("pb", bts)])
                self.CP("act", tst, self.pb[bts][0:16, 0:128], [("pb", bts)], [tsk])
                ob = self.accb()

                def front3(g0):
                    nonlocal ai_
                    n = min(4, nk - g0)
                    w = n * 128
                    lb = self.short()
                    for i in range(n):
                        kb = g0 + i
                        self.mm(self.pb[lb][:, i * 128:(i + 1) * 128], self.cmask[:, 2, :], lkt[:, kb * 128:(kb + 1) * 128],
                                True, False, ["cmask", lkk], [("pb", lb)])
                        self.mm(self.pb[lb][:, i * 128:(i + 1) * 128], selgt[:, kb, :], tst, False, True,
                                ["selgt", tsk], [("pb", lb)])
                    a_ = ar[(g0 // 4) % 2]
                    ark = ("ar", (g0 // 4) % 2)
                    self.TT("dve", a_[:, 0:w], self.pb[lb][:, 0:w], spt[:, g0 * 128:g0 * 128 + w], ALU.subtract,
                            [("pb", lb), spk], [ark])
                    at = aa[ai_ % 5]
                    ak = ("aa", ai_ % 5)
                    ai_ += 1
                    self.A(at[:, 0:w], a_[:, 0:w], AF.Exp, [ark], [ak])
                    if g0 + n == nk:
                        i = n - 1
                        self.TT("dve", at[:, i * 128:(i + 1) * 128], at[:, i * 128:(i + 1) * 128], self.cmask[:, 1, :],
                                ALU.mult, [ak, "cmask"], [ak])
                    return g0, n, at, ak

                def back3(st_):
                    g0, n, at, ak = st_
                    for i in range(n):
                        kb = g0 + i
                        self.mm(self.pb[ob][:, 0:64], at[:, i * 128:(i + 1) * 128], V[:, kb, h * 64:(h + 1) * 64],
                                kb == 0, kb == nk - 1, [ak, "V"], [("pb", ob)])

                prev = None
                for g0 in range(0, nk, 4):
                    cur = front3(g0)
                    if prev is not None:
                        back3(prev)
                    prev = cur
                back3(prev)
                self.CP("act", yt[:, h * 64:(h + 1) * 64], self.pb[ob][:, 0:64], [("pb", ob)], [yk])
            self.y_to_yT(yt, yk, 3, qb)

    def layer_norm_block(self, h, hk, gb, tb, res_out, li, route):
        st = self.lnst[tb % 2]
        sk = ("lnst", tb % 2)
        junk = self.lnjunk
        self.A(junk, h, AF.Copy, [hk], ["lnjunk", sk], accum=st[:, 0:1])
        self.A(junk, h, AF.Square, [hk], ["lnjunk", sk], accum=st[:, 1:2])
        self.TS("dve", st[:, 2:3], st[:, 0:1], 1.0 / D, None, ALU.mult, None, [sk], [sk])
        self.TT("dve", st[:, 3:4], st[:, 2:3], st[:, 2:3], ALU.mult, [sk], [sk])
        self.STT("dve", st[:, 4:5], st[:, 1:2], 1.0 / D, st[:, 3:4], ALU.mult, ALU.subtract, [sk], [sk])
        self.A(st[:, 4:5], st[:, 4:5], AF.Sqrt, [sk], [sk], bias=LN_EPS)
        self.S.op("dve", lambda e, s_=st: e.reciprocal(out=s_[:, 5:6], in_=s_[:, 4:5]), reads=[sk], writes=[sk])
        self.STT("dve", st[:, 6:7], st[:, 2:3], -1.0, st[:, 5:6], ALU.mult, ALU.mult, [sk], [sk])
        self.A(h, h, AF.Identity, [hk, sk], [hk], scale=st[:, 5:6], bias=st[:, 6:7])
        self.TT("dve", h, h, gb[:, 0, :], ALU.mult, [hk, "lngb"], [hk])
        self.TT("dve", h, h, gb[:, 1, :], ALU.add, [hk, "lngb"], [hk])
        o = self.dma(res_out[tb * 128:(tb + 1) * 128, :], h, [hk], [("res", id(res_out), tb)])
        self.x_to_xT(h, hk, tb, rt=route)
        return o

    def merge_ln1(self, li, res_in, res_out):
        d = self.dram
        win = d["win"]
        HT = 1024
        mp = self.carve([8, HT], BF16)
        accm = self.carve([8, HT], F32)
        sgt = [self.carve([512], BF16) for _ in range(2)]
        prod = [self.carve([512], F32) for _ in range(2)]
        gb = self.carve([2, D], F32)
        hbuf = [self.carve([D], F32) for _ in range(2)]
        xin = [self.carve([D], F32) for _ in range(2)]
        self.lnst = [self.carve([8], F32) for _ in range(2)]
        self.lnjunk = self.carve([D], BF16)
        self.dma(gb[:, 0, :], d["ln1g"][li:li + 1, :].to_broadcast([128, D]), [], ["lngb"])
        self.dma(gb[:, 1, :], d["ln1b"][li:li + 1, :].to_broadcast([128, D]), [], ["lngb"])
        route = None
        if li == 1:
            route = self.make_router(li)
        for th in range(2):
            for n in range(4):
                wvb, wkb = self.wload(d["wbr"][li, n], 2, D)
                for q4 in range(2):
                    c0 = OFF_GATE + n * D + q4 * 512
                    wvg, wkg = self.wload(win[li][:, c0:c0 + 512], 8, 512)
                    for cc in range(4):
                        dc = q4 * 4 + cc
                        for t2 in range(2):
                            tt = th * 2 + t2
                            sl = slice(t2 * 512, (t2 + 1) * 512)
                            bg = self.proj_fm(wvg, wkg, cc * 128, 128, tt)
                            bp = self.proj_fm(wvb, wkb, dc * 128, 128, tt, src=self.yT[n], srckey="yT%d" % n, kc=2)
                            s_ = sgt[t2]
                            sk = ("sgt", t2)
                            self.A(s_, self.pb[bg][:, :], AF.Sigmoid, [("pb", bg)], [sk])
                            ak = ("accm", dc, t2)
                            if n == 0:
                                self.TT("dve", accm[:, dc, sl], self.pb[bp][:, :], s_, ALU.mult, [("pb", bp), sk], [ak])
                            else:
                                p_ = prod[t2]
                                pk = ("prod", t2)
                                self.TT("dve", p_, self.pb[bp][:, :], s_, ALU.mult, [("pb", bp), sk], [pk])
                                if n < 3:
                                    self.TT("dve", accm[:, dc, sl], accm[:, dc, sl], p_, ALU.add, [ak, pk], [ak])
                                else:
                                    self.TT("dve", mp[:, dc, sl], accm[:, dc, sl], p_, ALU.add, [ak, pk], [("mp", dc, t2)])
            wo = [self.wload(d["wout"][li][:, hh * 512:(hh + 1) * 512], 8, 512) for hh in range(2)]
            for j in range(8):
                tb = th * 8 + j
                xi = xin[tb % 2]
                xk = ("xin", tb % 2)
                self.dma(xi, res_in[tb * 128:(tb + 1) * 128, :], [("res", id(res_in), tb)], [xk])
                h = hbuf[tb % 2]
                hk = ("hbuf", tb % 2)
                for hh in range(2):
                    ob = self.accb()
                    for k in range(8):
                        self.mm(self.pb[ob][:, :], mp[:, k, j * 128:(j + 1) * 128], wo[hh][0][:, k, :], k == 0, k == 7,
                                [("mp", k, j // 4), wo[hh][1]], [("pb", ob)])
                    self.STT("dve", h[:, hh * 512:(hh + 1) * 512], xi[:, hh * 512:(hh + 1) * 512], ALPHA, self.pb[ob][:, :],
                             ALU.mult, ALU.add, [xk, ("pb", ob)], [hk])
                rt = (lambda half, b, tb=tb: route(tb, half, b)) if route else None
                o = self.layer_norm_block(h, hk, gb, tb, res_out, li, rt)
                if self.stop_after == ("mix", li):
                    self.finals.append(o)

    def layer_norm_block(self, h, hk, gb, tb, res_out, li, route):
        st = self.lnst[tb % 2]
        sk = ("lnst", tb % 2)
        junk = self.lnjunk
        self.MS("dve", st[:, 0:2], 0.0, [sk])
        self.A(junk, h, AF.Copy, [hk, sk], ["lnjunk", sk], accum=st[:, 0:1])
        self.A(junk, h, AF.Square, [hk, sk], ["lnjunk", sk], accum=st[:, 1:2])
        self.TS("dve", st[:, 2:3], st[:, 0:1], 1.0 / D, None, ALU.mult, None, [sk], [sk])
        self.TT("dve", st[:, 3:4], st[:, 2:3], st[:, 2:3], ALU.mult, [sk], [sk])
        self.STT("dve", st[:, 4:5], st[:, 1:2], 1.0 / D, st[:, 3:4], ALU.mult, ALU.subtract, [sk], [sk])
        self.A(st[:, 4:5], st[:, 4:5], AF.Sqrt, [sk], [sk], bias=LN_EPS)
        self.S.op("dve", lambda e, s_=st: e.reciprocal(out=s_[:, 5:6], in_=s_[:, 4:5]), reads=[sk], writes=[sk])
        self.STT("dve", st[:, 6:7], st[:, 2:3], -1.0, st[:, 5:6], ALU.mult, ALU.mult, [sk], [sk])
        self.A(h, h, AF.Identity, [hk, sk], [hk], scale=st[:, 5:6], bias=st[:, 6:7])
        self.TT("dve", h, h, gb[:, 0, :], ALU.mult, [hk, "lngb"], [hk])
        self.TT("dve", h, h, gb[:, 1, :], ALU.add, [hk, "lngb"], [hk])
        o = self.dma(res_out[tb * 128:(tb + 1) * 128, :], h, [hk], [("res", id(res_out), tb)])
        self.x_to_xT(h, hk, tb, rt=route)
        return o

    def make_router(self, li):
        d = self.dram
        rw = self.carve([8, NE], F32)
        self.dma(rw, d["router"].rearrange("(c p) e -> p c e", p=128), [], ["rw"])
        xf = [self.carve([512], F32) for _ in range(2)]
        lg = [self.carve([32], F32) for _ in range(2)]
        state = {}

        def route(tb, half, b):
            x_ = xf[half]
            xk = ("xf", half)
            self.CP("dve", x_, self.pb[b][:, :], [("pb", b)], [xk])
            if half == 0:
                state["bank"] = self.accb()
            rb = state["bank"]
            for c in range(4):
                k = half * 4 + c
                self.mm(self.pb[rb][:, 0:NE], x_[:, c * 128:(c + 1) * 128], rw[:, k, :], k == 0, k == 7,
                        [xk, "rw"], [("pb", rb)])
            if half == 1:
                l_ = lg[tb % 2]
                lk = ("lg", tb % 2)
                self.CP("dve", l_[:, 0:8], self.pb[rb][:, 0:NE], [("pb", rb)], [lk])
                self.S.op("dve", lambda e, l_=l_: e.max(out=l_[:, 8:16], in_=l_[:, 0:8]), reads=[lk], writes=[lk])
                self.TT("dve", l_[:, 16:17], l_[:, 9:10], l_[:, 8:9], ALU.subtract, [lk], [lk])
                self.A(l_[:, 16:17], l_[:, 16:17], AF.Exp, [lk], [lk])
                self.TS("dve", l_[:, 16:17], l_[:, 16:17], 1.0, None, ALU.add, None, [lk], [lk])
                self.S.op("dve", lambda e, l_=l_: e.reciprocal(out=l_[:, 17:18], in_=l_[:, 16:17]), reads=[lk], writes=[lk])
                self.TS("dve", l_[:, 18:19], l_[:, 8:9], -1.0, None, ALU.mult, None, [lk], [lk])
                self.A(l_[:, 24:32], l_[:, 0:8], AF.Exp, [lk], [lk], bias=l_[:, 18:19])
                self.TS("dve", l_[:, 0:8], l_[:, 0:8], l_[:, 9:10], l_[:, 17:18], ALU.is_ge, ALU.mult, [lk], [lk])
                self.TT("dve", self.gates[:, tb, :], l_[:, 0:8], l_[:, 24:32], ALU.mult, [lk], ["gates"])
        return route

    MOE_CAP = 512

    def ffn_phase(self, li, res_in, res_out):
        self.arena_reset()
        G = 1024
        moe = (li == 1)
        dff = D_FFE if moe else D_FF
        nfc = dff // 128
        hT = self.carve([nfc, self.MOE_CAP if moe else G], BF16)
        facc = self.carve([8, D], F32)
        self.stage = self.stage[0:2] + [self.carve([2048], F32)]
        base = self.aoff
        for g in range(T // G):
            self.aoff = base
            if g > 0:
                self.S.barrier()
            if moe:
                self.moe_group(li, g, res_in, hT, facc, nfc, dff)
            else:
                self.dense_group(li, g, hT, facc, nfc, dff)
            self.S.barrier()
            self.aoff = base
            self.ple_ln2_group(li, g, res_in, res_out, facc)

    def hidden_fm(self, w_in, dff, nfc, hT, src, srckey, tiles, sa):
        for f0 in range(0, nfc, 4):
            nf = min(4, nfc - f0)
            wa, wak = self.wload(w_in[:, f0 * 128:(f0 + nf) * 128], 8, nf * 128)
            wu, wuk = self.wload(w_in[:, dff + f0 * 128:dff + (f0 + nf) * 128], 8, nf * 128)
            for fi in range(nf):
                fc = f0 + fi
                for t2, tt in enumerate(tiles):
                    ba = self.proj_fm(wa, wak, fi * 128, 128, tt, src=src, srckey=srckey)
                    bu = self.proj_fm(wu, wuk, fi * 128, 128, tt, src=src, srckey=srckey)
                    s_ = sa[t2 % 2]
                    sk = ("sa", t2 % 2)
                    self.A(s_, self.pb[ba][:, :], AF.Silu, [("pb", ba)], [sk])
                    self.TT("dve", hT[:, fc, t2 * 512:(t2 + 1) * 512], self.pb[bu][:, :], s_, ALU.mult,
                            [("pb", bu), sk], [("hT", fc)])

    def out_tm(self, w_out, nfc, hT, blocks, evac):
        for f0 in range(0, nfc, 4):
            nf = min(4, nfc - f0)
            wo, wok = self.wload(w_out[f0 * 128:(f0 + nf) * 128, :], nf, D)
            for fi in range(nf):
                fc = f0 + fi
                for i, j in enumerate(blocks):
                    for hh in range(2):
                        b = i * 2 + hh
                        self.mm(self.pb[b][:, :], hT[:, fc, j * 128:(j + 1) * 128], wo[:, fi, hh * 512:(hh + 1) * 512],
                                fc == 0, fc == nfc - 1, [("hT", fc), wok], [("pb", b)])
        for i, j in enumerate(blocks):
            for hh in range(2):
                evac(i, j, hh, i * 2 + hh)

    def dense_group(self, li, g, hT, facc, nfc, dff):
        d = self.dram
        sa = [self.carve([512], BF16) for _ in range(2)]
        self.hidden_fm(d["ffn_in"], dff, nfc, hT, None, None, [g * 2, g * 2 + 1], sa)
        for ps_ in range(2):
            def evac(i, j, hh, b):
                self.CP("act" if hh == 0 else "dve", facc[:, j, hh * 512:(hh + 1) * 512], self.pb[b][:, :],
                        [("pb", b)], [("facc", j, hh)])
            self.out_tm(d["ffn_out"], nfc, hT, [ps_ * 4 + i for i in range(4)], evac)

    def moe_group(self, li, g, res_in, hT, facc, nfc, dff):
        d = self.dram
        C = self.MOE_CAP
        NR = C // 128
        xtok = self.carve([8, D], BF16)
        Pb = self.carve([8, C], BF16)
        PT = self.carve([NR, 8, 128], BF16)
        xsT = self.carve([8, C], BF16)
        ys = self.carve([NR, D], BF16)
        iota = self.carve([C], F32)
        sa = [self.carve([512], BF16) for _ in range(2)]
        mf = self.carve([64], F32)
        mb = self.carve([64], BF16)
        rk = self.carve([64], F32)
        off = self.carve([64], F32)
        gs = self.carve([64, 2], BF16)
        gt = self.carve([64], F32)
        wr = self.carve([NR, 2], F32)
        gsl = self.gates[:, g * 8:(g + 1) * 8, :].rearrange("p j e -> p (j e)")
        self.dma(iota, d["cst"]["iota"], [], ["iota"])
        for j in range(8):
            tb = g * 8 + j
            self.cast_load(xtok[:, j, :], res_in[tb * 128:(tb + 1) * 128, :], 128, [D], ("xtok", j))
        self.TS("dve", mf, gsl, 0.0, None, ALU.is_gt, None, ["gates"], ["mf"])
        self.CP("dve", mb, mf, ["mf"], ["mb"])
        b1 = self.short()
        self.mm(self.pb[b1][:, 0:64], self.cmask[:, 0, :], mb, True, True, ["cmask", "mb"], [("pb", b1)])
        b2 = self.short()
        self.mm(self.pb[b2][:, 0:64], self.cmask[:, 3, :], mb, True, True, ["cmask", "mb"], [("pb", b2)])
        self.MS("dve", off[:, 0:8], 0.0, ["off"])
        for j in range(1, 8):
            self.TT("dve", off[:, j * 8:(j + 1) * 8], off[:, (j - 1) * 8:j * 8], self.pb[b2][:, (j - 1) * 8:j * 8], ALU.add,
                    ["off", ("pb", b2)], ["off"])
        self.TT("dve", rk, self.pb[b1][:, 0:64], off, ALU.add, [("pb", b1), "off"], ["rk"])
        self.TT("dve", rk, rk, mf, ALU.mult, ["rk", "mf"], ["rk"])
        self.TS("dve", rk, rk, -1.0, None, ALU.add, None, ["rk"], ["rk"])
        self.CP("dve", gs[:, :, 0], gsl, ["gates"], ["gs"])
        self.TT("dve", gt, gsl, gs[:, :, 0], ALU.subtract, ["gates", "gs"], ["gt"])
        self.CP("dve", gs[:, :, 1], gt, ["gt"], ["gs"])
        for e in range(NE):
            for j in range(8):
                self.TS("dve", Pb[:, j, :], iota, rk[:, j * 8 + e:j * 8 + e + 1], None, ALU.is_equal, None,
                        ["iota", "rk"], [("Pb", j)])
            for rb in range(NR):
                for j0 in range(0, 8, 4):
                    b = self.short()
                    for jj in range(4):
                        j = j0 + jj
                        self.mm(self.pb[b][:, jj * 128:(jj + 1) * 128], Pb[:, j, rb * 128:(rb + 1) * 128], self.cmask[:, 4, :],
                                True, True, [("Pb", j), "cmask"], [("pb", b)])
                    self.CP("act", PT[:, rb, j0:j0 + 4, :], self.pb[b][:, :].rearrange("p (j t) -> p j t", j=4),
                            [("pb", b)], [("PT", rb)])
            for dc in range(8):
                b = self.short()
                for j in range(8):
                    self.mm(self.pb[b][:, 0:C], xtok[:, j, dc * 128:(dc + 1) * 128], Pb[:, j, :], j == 0, j == 7,
                            [("xtok", j), ("Pb", j)], [("pb", b)])
                self.CP("dve" if dc % 2 else "act", xsT[:, dc, :], self.pb[b][:, 0:C], [("pb", b)], ["xsT"])
            bw = self.short()
            for rb in range(NR):
                for j in range(8):
                    self.mm(self.pb[bw][:, 2 * rb:2 * rb + 2], Pb[:, j, rb * 128:(rb + 1) * 128], gs[:, j * 8 + e, :],
                            j == 0, j == 7, [("Pb", j), "gs"], [("pb", bw)])
            self.CP("dve", wr, self.pb[bw][:, 0:2 * NR].rearrange("p (r c) -> p r c", c=2), [("pb", bw)], ["wr"])
            self.TT("dve", wr[:, :, 0], wr[:, :, 0], wr[:, :, 1], ALU.add, ["wr"], ["wr"])
            self.hidden_fm(d["moe_in"][e], dff, nfc, hT, xsT, "xsT", [0], sa)

            def evac(i, j, hh, b):
                self.A(ys[:, i, hh * 512:(hh + 1) * 512], self.pb[b][:, :], AF.Copy, [("pb", b), "wr"], [("ys", i)],
                       scale=wr[:, i, 0:1])
            self.out_tm(d["moe_out"][e], nfc, hT, list(range(NR)), evac)
            for j in range(8):
                for hh in range(2):
                    b = self.short()
                    for rb in range(NR):
                        self.mm(self.pb[b][:, :], PT[:, rb, j, :], ys[:, rb, hh * 512:(hh + 1) * 512], rb == 0, rb == NR - 1,
                                [("PT", rb), ("ys", rb)], [("pb", b)])
                    dst = facc[:, j, hh * 512:(hh + 1) * 512]
                    fk = ("facc", j, hh)
                    if e == 0:
                        self.CP("dve", dst, self.pb[b][:, :], [("pb", b)], [fk])
                    else:
                        self.TT("dve", dst, dst, self.pb[b][:, :], ALU.add, [fk, ("pb", b)], [fk])

    def ple_ln2_group(self, li, g, res_in, res_out, facc):
        d = self.dram
        gb = self.carve([2, D], F32)
        pblk = [self.carve([256], F32) for _ in range(2)]
        pT = [self.carve([2, 128], BF16) for _ in range(2)]
        ple = self.carve([D], F32)
        hbuf = [self.carve([D], F32) for _ in range(2)]
        xin = self.carve([D], F32)
        self.lnst = [self.carve([8], F32) for _ in range(2)]
        self.lnjunk = self.carve([D], BF16)
        self.dma(gb[:, 0, :], d["ln2g"][li:li + 1, :].to_broadcast([128, D]), [], ["lngb"])
        self.dma(gb[:, 1, :], d["ln2b"][li:li + 1, :].to_broadcast([128, D]), [], ["lngb"])
        wg = [self.wload(d["pleg"][li][:, hh * 512:(hh + 1) * 512], 8, 512) for hh in range(2)]
        wp, wpk = self.wload(d["plep"][li], 2, D)
        for j in range(8):
            tb = g * 8 + j
            pb_ = pblk[j % 2]
            pk = ("pblk", j % 2)
            self.dma(pb_, d["p_in"][li, tb * 128:(tb + 1) * 128, :], [], [pk])
            b = self.short()
            for c in range(2):
                self.tr(self.pb[b][:, c * 128:(c + 1) * 128], pb_[:, c * 128:(c + 1) * 128], [pk], [("pb", b)])
            pt = pT[j % 2]
            ptk = ("pT", j % 2)
            self.CP("act", pt, self.pb[b][:, 0:256].rearrange("p (c t) -> p c t", c=2), [("pb", b)], [ptk])
            for hh in range(2):
                bg_ = self.short()
                for k in range(8):
                    self.mm(self.pb[bg_][:, :], self.xT[:, k, tb * 128:(tb + 1) * 128], wg[hh][0][:, k, :], k == 0, k == 7,
                            [("xT", tb // 4), wg[hh][1]], [("pb", bg_)])
                bp_ = self.short()
                for c in range(2):
                    self.mm(self.pb[bp_][:, :], pt[:, c, :], wp[:, c, hh * 512:(hh + 1) * 512],
                            c == 0, c == 1, [ptk, wpk], [("pb", bp_)])
                self.A(ple[:, hh * 512:(hh + 1) * 512], self.pb[bg_][:, :], AF.Sigmoid, [("pb", bg_)], ["ple"])
                self.TT("dve", ple[:, hh * 512:(hh + 1) * 512], ple[:, hh * 512:(hh + 1) * 512], self.pb[bp_][:, :],
                        ALU.mult, ["ple", ("pb", bp_)], ["ple"])
            self.dma(xin, res_in[tb * 128:(tb + 1) * 128, :], [("res", id(res_in), tb)], ["xin"])
            h = hbuf[j % 2]
            hk = ("hbuf", j % 2)
            self.STT("dve", h, xin, ALPHA, ple, ALU.mult, ALU.add, ["xin", "ple"], [hk])
            self.TT("dve", h, h, facc[:, j, :], ALU.add, [hk], [hk])
            o = self.layer_norm_block(h, hk, gb, tb, res_out, li, None)
            if li == self.layers[-1] or self.stop_after == ("ffn", li):
                self.finals.append(o)


_IDX = _win_index()


def prepare_inputs(inputs):
    f = lambda a: np.ascontiguousarray(np.asarray(a))
    w_in = f(inputs["w_in"])
    win = np.zeros((2, D, NCOLS_R), np.float32)
    valid = _IDX >= 0
    win[:, :, valid] = w_in[:, :, _IDX[valid]]
    conv_w = f(inputs["conv_w"])
    convp = np.zeros((2, 128, 2, 34), np.float32)
    for c in range(2):
        convp[:, :, c, 0:31] = conv_w[:, :, c * 128:(c + 1) * 128].transpose(0, 2, 1)
        convp[:, :, c, 31] = f(inputs["conv_b"])[:, c * 128:(c + 1) * 128]
        convp[:, :, c, 32] = f(inputs["conv_ln_g"])[:, c * 128:(c + 1) * 128]
        convp[:, :, c, 33] = f(inputs["conv_ln_b"])[:, c * 128:(c + 1) * 128]
    pe = f(inputs["nsa_cmp_pe"])
    pe_r = np.ascontiguousarray(pe.transpose(0, 2, 3, 1).reshape(2, 128, 32))
    w2 = f(inputs["nsa_cmp_w2"])
    w2k = np.ascontiguousarray(np.concatenate([w2[:, 0], w2[:, 0]], axis=2))
    w2v = np.ascontiguousarray(w2[:, 1])
    qn = np.ascontiguousarray(f(inputs["mla_q_norm"]).reshape(2, 2, 128).transpose(0, 2, 1))
    kvn = np.ascontiguousarray(f(inputs["mla_kv_norm"]).reshape(2, 128, 1))
    wuq = f(inputs["mla_w_uq"])
    cols = []
    for h in range(4):
        b = h * 96
        cols += list(range(b, b + 96))
        cols += list(range(b, b + 64)) + list(range(b + 80, b + 96)) + list(range(b + 64, b + 80))
    uq = np.ascontiguousarray(wuq[:, :, cols])
    wukv = f(inputs["mla_w_ukv"])
    cols = []
    for h in range(4):
        cols += list(range(h * 128, h * 128 + 64))
    for h in range(4):
        cols += list(range(h * 128 + 64, h * 128 + 128))
    ukv = np.ascontiguousarray(wukv[:, :, cols])
    shared = {
        "win": win, "convp": convp, "pe_r": pe_r, "w1": f(inputs["nsa_cmp_w1"]), "w2k": w2k, "w2v": w2v,
        "qn": qn, "kvn": kvn, "uq": uq, "ukv": ukv, "wbr": f(inputs["w_branch"]), "wout": f(inputs["w_out"]),
        "ln1g": f(inputs["ln1_g"]), "ln1b": f(inputs["ln1_b"]), "ln2g": f(inputs["ln2_g"]), "ln2b": f(inputs["ln2_b"]),
        "ffn_in": f(inputs["ffn_w_in"])[0], "ffn_out": f(inputs["ffn_w_out"])[0], "router": f(inputs["moe_router"])[0],
        "moe_in": f(inputs["moe_w_in"])[0], "moe_out": f(inputs["moe_w_out"])[0],
        "pleg": f(inputs["ple_w_gate"]), "plep": f(inputs["ple_w_proj"]),
    }
    for k, v in _host_consts().items():
        shared["c_" + k] = v
    x = f(inputs["x"])
    p = f(inputs["p"])
    pos = f(inputs["positions"]).astype(np.int32)
    in_maps = []
    for b in range(8):
        m = dict(shared)
        m["x"] = x[b]
        m["p"] = np.ascontiguousarray(p[:, b])
        m["pos"] = pos[b:b + 1]
        in_maps.append(m)
    return in_maps


def kernel(**inputs):
    in_maps = prepare_inputs(inputs)
    nc = MK().build()
    res = run_bass_kernel_spmd(nc, in_maps, core_ids=list(range(8)))
    return np.stack([np.asarray(r["y"], dtype=np.float32) for r in res.results], axis=0)
```

```python
import math
import contextlib
import numpy as np
import concourse.bass as bass
import concourse.mybir as mybir
from concourse.bass_utils import run_bass_kernel_spmd

F32 = mybir.dt.float32
BF16 = mybir.dt.bfloat16
I32 = mybir.dt.int32
AF = mybir.ActivationFunctionType
ALU = mybir.AluOpType
AX = mybir.AxisListType

T = 2048
D = 1024
NB = 16
ALPHA = 4.0 ** 0.25
LN_EPS = 1e-5
RMS_EPS = 1e-6
THETA = 10000.0
D_FF = 2816
D_FFE = 3584
NE = 8

ENGS = ("pe", "act", "dve", "pool", "sp")
N_DMA_SEMS = 6


class Op:
    __slots__ = ("eng", "fn", "deps", "is_dma", "signal", "sig_val", "dma_sem", "dma_val",
                 "dma_prev", "idx")

    def __init__(self, eng, fn, is_dma):
        self.eng = eng
        self.fn = fn
        self.deps = []
        self.is_dma = is_dma
        self.signal = False
        self.sig_val = 0
        self.dma_sem = None
        self.dma_val = 0
        self.dma_prev = 0
        self.idx = 0


class Sched:
    def __init__(self):
        self.ops = {e: [] for e in ENGS}
        self.last_w = {}
        self.readers = {}
        self.all_ops = []

    def op(self, eng, fn, reads=(), writes=(), dma=False, acc=False):
        o = Op(eng, fn, dma)
        deps = []
        for k in reads:
            w = self.last_w.get(k)
            if w is not None:
                deps.append(w)
            if isinstance(k, tuple) and k[0] == "pb":
                for r in self.readers.get(k, ()):
                    if r.eng != eng:
                        deps.append(r)
        for k in writes:
            w = self.last_w.get(k)
            if w is not None and not (acc and w.eng == eng and not w.is_dma):
                deps.append(w)
            for r in self.readers.get(k, ()):
                deps.append(r)
        seen = set()
        for d in deps:
            if id(d) not in seen and d is not o:
                seen.add(id(d))
                o.deps.append(d)
        for k in reads:
            lst = self.readers.setdefault(k, [])
            if not dma:
                for i, r in enumerate(lst):
                    if r.eng == eng and not r.is_dma:
                        lst[i] = o
                        break
                else:
                    lst.append(o)
            else:
                lst.append(o)
        for k in writes:
            self.last_w[k] = o
            self.readers[k] = []
        o.idx = len(self.ops[eng])
        self.ops[eng].append(o)
        self.all_ops.append(o)
        return o

    def barrier(self):
        lasts = []
        for e in ENGS:
            ops = self.ops[e]
            nd = 0
            got_real = False
            for o in reversed(ops):
                if o.fn is None:
                    continue
                if o.is_dma:
                    if nd < N_DMA_SEMS:
                        lasts.append(o)
                        nd += 1
                elif not got_real:
                    lasts.append(o)
                    got_real = True
                if got_real and nd >= N_DMA_SEMS:
                    break
        for e in ENGS:
            o = Op(e, None, False)
            o.deps = [l for l in lasts]
            o.idx = len(self.ops[e])
            self.ops[e].append(o)
            self.all_ops.append(o)
        self.last_w = {}
        self.readers = {}

    def emit(self, nc, final_wait_ops=()):
        for fo in final_wait_ops:
            if not fo.is_dma:
                fo.signal = True
        for o in self.all_ops:
            for d in o.deps:
                if not d.is_dma:
                    d.signal = True
        cnt = {e: 0 for e in ENGS}
        for e in ENGS:
            for o in self.ops[e]:
                if o.signal and not o.is_dma:
                    cnt[e] += 1
                    o.sig_val = cnt[e]
        dma_count = {}
        for e in ENGS:
            k = 0
            for o in self.ops[e]:
                if o.is_dma:
                    j = k % N_DMA_SEMS
                    k += 1
                    key = (e, j)
                    prev = dma_count.get(key, 0)
                    o.dma_sem = key
                    o.dma_prev = prev
                    o.dma_val = prev + 16
                    dma_count[key] = prev + 16
        with contextlib.ExitStack() as st:
            sems = {e: st.enter_context(nc.semaphore("s_" + e)) for e in ENGS if cnt[e] > 0}
            dsems = {key: st.enter_context(nc.semaphore("d_%s%d" % key)) for key in dma_count}
            block = st.enter_context(nc.Block())
            regs = {"pe": block.tensor, "act": block.scalar, "dve": block.vector,
                    "pool": block.gpsimd, "sp": block.sync}

            def make(e):
                def body(eng):
                    known = {}
                    for o in self.ops[e]:
                        waits = {}
                        for d in o.deps:
                            if d.is_dma:
                                s, v = dsems[d.dma_sem], d.dma_val
                            else:
                                s, v = sems[d.eng], d.sig_val
                            kk = id(s)
                            if known.get(kk, 0) >= v:
                                continue
                            if kk not in waits or waits[kk][1] < v:
                                waits[kk] = (s, v)
                        if o.is_dma and o.dma_prev > 0:
                            s = dsems[o.dma_sem]
                            kk = id(s)
                            if known.get(kk, 0) < o.dma_prev:
                                if kk not in waits or waits[kk][1] < o.dma_prev:
                                    waits[kk] = (s, o.dma_prev)
                        for kk, (s, v) in waits.items():
                            eng.wait_ge(s, v)
                            known[kk] = v
                        if o.fn is None:
                            continue
                        ins = o.fn(eng)
                        if o.is_dma:
                            ins.then_inc(dsems[o.dma_sem], 16)
                        elif o.signal:
                            ins.then_inc(sems[e], 1)
                    if e == "sp":
                        for fo in final_wait_ops:
                            if fo.is_dma:
                                eng.wait_ge(dsems[fo.dma_sem], fo.dma_val)
                            else:
                                eng.wait_ge(sems[fo.eng], fo.sig_val)
                return body

            for e in ENGS:
                if self.ops[e] or e == "sp":
                    regs[e](make(e))


OFF_CONV, OFF_NQ, OFF_NK, OFF_MISC, OFF_KR, OFF_SB, OFF_TOK, OFF_GATE = 0, 512, 1024, 1536, 2048, 2304, 2816, 3328
NCOLS_R = 7424


def _win_index():
    sw64 = lambda b: list(range(b + 32, b + 64)) + list(range(b, b + 32))
    sw32 = lambda b: list(range(b + 16, b + 32)) + list(range(b, b + 16))
    idx = []
    idx += list(range(0, 512))
    idx += list(range(512, 768))
    for h in range(4):
        idx += sw64(512 + 64 * h)
    ks, kw = 896, 1024
    idx += list(range(ks, ks + 64)) * 2 + sw64(ks) * 2 + list(range(kw, kw + 64)) * 2 + sw64(kw) * 2
    idx += list(range(768, 896)) + list(range(1164, 1420)) + list(range(1420, 1548))
    idx += [-1] * 64 + list(range(1548, 1580)) + [-1] * 32
    idx += [-1] * 64 + sw32(1548) + [-1] * 32
    idx += list(range(1580, 1580 + 512))
    idx += list(range(960, 1024)) + list(range(1088, 1152)) + list(range(1152, 1164)) + [-1] * 116
    idx += list(range(1580 + 512, 1580 + 768))
    idx += list(range(2348, 6444))
    assert len(idx) == NCOLS_R
    return np.array(idx)


def _host_consts():
    c = {}
    c["ident"] = np.eye(128, dtype=np.float32)
    p = np.arange(128)[:, None]
    f = np.arange(128)[None, :]
    cm = np.zeros((128, 5, 128), np.float32)
    cm[:, 0] = (p <= f)
    cm[:, 1] = (p < f)
    cm[:, 2] = (p > f)
    cm[:, 3] = 1.0
    cm[:, 4] = (p == f)
    c["cmask"] = cm
    j = np.arange(128)[:, None]
    t = np.arange(T)[None, :]
    c["cmpvalid"] = ((16 * j + 31 <= t) & (j < 127)).astype(np.float32)
    n = np.arange(32)[:, None]
    c["eexp"] = ((t // 64) == n).astype(np.float32)
    jj = np.arange(127)
    nn = np.arange(32)
    ov = ((jj[:, None] * 16 < nn[None, :] * 64 + 64) & (jj[:, None] * 16 + 32 > nn[None, :] * 64)).astype(np.float32)
    ovl = np.zeros((128, 33), np.float32)
    ovl[:127, :32] = ov
    ovl[:127, 32] = 1.0
    c["ovl"] = ovl
    cur = (np.arange(T) // 64)[:, None]
    nid = np.arange(32)[None, :]
    forced = (nid == 0) | (nid == cur) | (nid == cur - 1)
    future = nid > cur
    keep = (~forced & ~future).astype(np.float32)
    add = np.where(future, -1e30, np.where(forced, 100.0, 0.0)).astype(np.float32)
    ka = np.zeros((128, 2, 16, 32), np.float32)
    ka[:, 0] = keep.reshape(16, 128, 32).transpose(1, 0, 2)
    ka[:, 1] = add.reshape(16, 128, 32).transpose(1, 0, 2)
    c["keepadd"] = ka
    rc = np.zeros((128, 4), np.float32)
    pp = np.arange(128)
    rc[:, 0] = THETA ** (-(pp % 32).astype(np.float64) / 32.0)
    rc[:, 1] = np.where((pp % 64) < 32, -1.0, 1.0)
    m = (pp >= 64) & (pp < 96)
    rc[m, 2] = THETA ** (-((pp[m] - 64) % 16).astype(np.float64) / 16.0)
    rc[m, 3] = np.where((pp[m] - 64) < 16, -1.0, 1.0)
    c["ropec"] = rc
    ind = np.zeros((128, 16, 16), np.float32)
    for kb in range(16):
        ind[:, kb, kb] = 1.0
    c["indt"] = ind
    c["iota"] = np.tile(np.arange(512, dtype=np.float32)[None, :], (128, 1))
    sg = np.zeros((16, 16, 128), np.float32)
    for kb in range(16):
        sg[kb + 1:, kb, :] = 1.0
    c["selgt"] = sg
    return c


CONST_SHAPES = {"iota": [128, 512], "ident": [128, 128], "cmask": [128, 5, 128], "cmpvalid": [128, T], "eexp": [32, T],
                "ovl": [128, 33], "keepadd": [128, 2, 16, 32], "ropec": [128, 4], "indt": [128, 16, 16],
                "selgt": [16, 16, 128]}


class MK:
    NW = 3

    def __init__(self, layers=(0, 1), debug=False, stop_after=None):
        self.layers = layers
        self.debug = debug
        self.stop_after = stop_after
        self.nc = bass.Bass("TRN2", target_bir_lowering=False)
        self.S = Sched()
        self.st = contextlib.ExitStack()
        self.st.enter_context(self.nc.allow_low_precision(reason="bf16 matmul operands / fp32 accumulation by design"))
        self.wi = 0
        self.stg_i = 0
        self.deferred = []
        self.si = 0
        self.ai = 0
        self.finals = []
        self.dbg_outs = {}

    def din(self, name, shape, dt=F32):
        return self.nc.dram_tensor(name, list(shape), dt, kind="ExternalInput").ap()

    def dout(self, name, shape, dt=F32):
        return self.nc.dram_tensor(name, list(shape), dt, kind="ExternalOutput").ap()

    def sb(self, name, shape, dt):
        return self.st.enter_context(self.nc.sbuf_tensor(name, list(shape), dt))

    def arena_reset(self):
        self.S.barrier()
        self.aoff = 0

    def carve(self, shape, dt, parts=128):
        n = 1
        for s in shape:
            n *= s
        nbytes = n * (4 if dt in (F32, I32) else 2)
        nbytes = (nbytes + 63) // 64 * 64
        off = self.aoff
        self.aoff += nbytes
        assert self.aoff <= self.ARENA_BYTES, (self.aoff, self.ARENA_BYTES)
        v = self.arena[0:parts, off // 2:(off + n * (4 if dt in (F32, I32) else 2)) // 2]
        if dt != BF16:
            v = v.bitcast(dt)
        if len(shape) == 2:
            v = v.rearrange("p (a b) -> p a b", a=shape[0])
        elif len(shape) == 3:
            v = v.rearrange("p (a b c) -> p a b c", a=shape[0], b=shape[1])
        return v

    def flush_deferred(self):
        dl, self.deferred = self.deferred, []
        for fn in dl:
            fn()

    def short(self):
        b = self.si % 4
        self.si += 1
        return b

    def accb(self):
        b = 4 + self.ai % 4
        self.ai += 1
        return b

    def mm(self, out, lhsT, rhs, start, stop, r, w):
        self.S.op("pe", lambda e: e.matmul(out, lhsT=lhsT, rhs=rhs, start=start, stop=stop),
                  reads=r, writes=w, acc=True)

    def tr(self, out, in_, r, w, parts=128):
        idt = self.ident[0:parts, 0:parts]
        self.S.op("pe", lambda e: e.transpose(out, in_, idt), reads=list(r) + ["ident"], writes=w, acc=True)

    def A(self, out, in_, func, r, w, bias=None, scale=None, accum=None):
        kw = {}
        if bias is not None:
            kw["bias"] = bias
        if scale is not None:
            kw["scale"] = scale
        if accum is not None:
            kw["accum_out"] = accum
        self.S.op("act", lambda e: e.activation(out=out, in_=in_, func=func, **kw), reads=r, writes=w)

    def TT(self, eng, out, in0, in1, op, r, w):
        self.S.op(eng, lambda e: e.tensor_tensor(out=out, in0=in0, in1=in1, op=op), reads=r, writes=w)

    def TS(self, eng, out, in0, s1, s2, op0, op1, r, w):
        if op1 is None:
            self.S.op(eng, lambda e: e.tensor_scalar(out=out, in0=in0, scalar1=s1, scalar2=None, op0=op0),
                      reads=r, writes=w)
        else:
            self.S.op(eng, lambda e: e.tensor_scalar(out=out, in0=in0, scalar1=s1, scalar2=s2, op0=op0, op1=op1),
                      reads=r, writes=w)

    def STT(self, eng, out, in0, scalar, in1, op0, op1, r, w):
        self.S.op(eng, lambda e: e.scalar_tensor_tensor(out=out, in0=in0, scalar=scalar, in1=in1, op0=op0, op1=op1),
                  reads=r, writes=w)

    def CP(self, eng, out, in_, r, w):
        if eng == "act":
            self.S.op("act", lambda e: e.activation(out=out, in_=in_, func=AF.Copy), reads=r, writes=w)
        else:
            self.S.op(eng, lambda e: e.tensor_copy(out=out, in_=in_), reads=r, writes=w)

    def MS(self, eng, out, val, w):
        self.S.op(eng, lambda e: e.memset(out, val), writes=w)

    def dma(self, out, in_, r, w, eng="sp"):
        return self.S.op(eng, lambda e: e.dma_start(out=out, in_=in_), reads=r, writes=w, dma=True)

    CAST_ENGS = ("dve", "act", "dve", "act")

    def cast_load(self, dst, src, parts, free_shape, dkey):
        n = 1
        for x_ in free_shape:
            n *= x_
        assert n <= 2048, n
        si_ = self.stg_i % len(self.stage)
        ce = self.CAST_ENGS[self.stg_i % 4]
        self.stg_i += 1
        stg = self.stage[si_][0:parts, 0:n]
        if len(free_shape) == 2:
            stg = stg.rearrange("p (a b) -> p a b", a=free_shape[0])
        elif len(free_shape) == 3:
            stg = stg.rearrange("p (a b c) -> p a b c", a=free_shape[0], b=free_shape[1])
        sk = ("stg", si_)
        self.dma(stg, src, [], [sk])
        self.CP(ce, dst, stg, [sk], [dkey])

    def dma_w1(self, w1t, src, c):
        si_ = self.stg_i % len(self.stage)
        ce = self.CAST_ENGS[self.stg_i % 4]
        self.stg_i += 1
        ps_ = slice(c * 64, (c + 1) * 64)
        stg = self.stage[si_][ps_, 0:2048].rearrange("p (l e) -> p l e", l=32)
        sk = ("stg", si_)
        self.dma(stg, src.rearrange("(l d) e -> d l e", d=64), [], [sk])
        self.CP(ce, w1t[ps_, :, :], stg, [sk], ["w1t"])

    def wload(self, src, kc, n, rows=128):
        slot = self.wi % self.NW
        self.wi += 1
        assert kc * n <= 4096
        v = self.wring[slot][0:rows, 0:kc * n].rearrange("p (c n) -> p c n", c=kc)
        srcv = src.rearrange("(c p) n -> p c n", p=rows)
        key = ("w", slot)
        step = max(1, 2048 // n)
        for k0 in range(0, kc, step):
            k1 = min(kc, k0 + step)
            self.cast_load(v[:, k0:k1, :], srcv[:, k0:k1, :], rows, [k1 - k0, n], key)
        return v, key

    def proj_fm(self, wv, wkey, c0, M, tt, src=None, srckey=None, kc=8):
        b = self.short()
        src = self.xT if src is None else src
        srckey = ("xT", tt) if srckey is None else srckey
        for k in range(kc):
            self.mm(self.pb[b][0:M, :], wv[:, k, c0:c0 + M], src[:, k, tt * 512:(tt + 1) * 512],
                    k == 0, k == kc - 1, [wkey, srckey], [("pb", b)])
        return b

    def build(self):
        nc, S = self.nc, self.S
        L = self.layers
        x_in = self.din("x", [T, D])
        p_in = self.din("p", [2, T, 256])
        pos_in = self.din("pos", [1, T], I32)
        win = self.din("win", [2, D, NCOLS_R])
        convp = self.din("convp", [2, 128, 2, 34])
        pe_r = self.din("pe_r", [2, 128, 32])
        w1 = self.din("w1", [2, 2, 2048, 64])
        w2k = self.din("w2k", [2, 64, 128])
        w2v = self.din("w2v", [2, 64, 64])
        qn = self.din("qn", [2, 128, 2])
        kvn = self.din("kvn", [2, 128, 1])
        uq = self.din("uq", [2, 256, 768])
        ukv = self.din("ukv", [2, 128, 512])
        wbr = self.din("wbr", [2, 4, 256, D])
        wout = self.din("wout", [2, D, D])
        ln1g = self.din("ln1g", [2, D])
        ln1b = self.din("ln1b", [2, D])
        ln2g = self.din("ln2g", [2, D])
        ln2b = self.din("ln2b", [2, D])
        lite = self.stop_after is not None and self.stop_after[0] in ("conv", "nsa", "mla", "sb", "mix")
        need_ffn = (0 in L) and not lite
        need_moe = (1 in L) and not (lite and self.stop_after[1] == 0) and self.stop_after != ("ffn", 0)
        ffn_in = self.din("ffn_in", [D, 2 * D_FF]) if need_ffn else None
        ffn_out = self.din("ffn_out", [D_FF, D]) if need_ffn else None
        router = self.din("router", [D, NE])
        moe_in = self.din("moe_in", [NE, D, 2 * D_FFE]) if need_moe else None
        moe_out = self.din("moe_out", [NE, D_FFE, D]) if need_moe else None
        pleg = self.din("pleg", [2, D, D])
        plep = self.din("plep", [2, 256, D])
        cst = {k: self.din("c_" + k, v) for k, v in CONST_SHAPES.items()}
        y_out = self.dout("y", [T, D])
        xa = self.dout("xa", [T, D])
        xb = self.dout("xb", [T, D])
        tabs_d = self.dout("tabs_d", [2, 128, 2 * T], BF16)
        self.dram = dict(locals())

        self.xT = self.sb("xT", [128, 8, T], BF16)
        self.wring = [self.sb("wr%d" % i, [128, 4096], BF16) for i in range(self.NW)]
        self.stage = [self.sb("stg%d" % i, [128, 2048], F32) for i in range(2)]
        self.ident = self.sb("ident", [128, 128], F32)
        self.cmask = self.sb("cmask", [128, 5, 128], BF16)
        self.onesf = self.sb("onesf", [128, 128], F32)
        self.gates = self.sb("gates", [128, NB, NE], F32)
        self.pb = [self.st.enter_context(nc.psum_tensor("pb%d" % i, [128, 512], F32)) for i in range(8)]
        self.ARENA_BYTES = 133 * 1024
        self.arena = self.sb("arena", [128, self.ARENA_BYTES // 2], BF16)
        self.aoff = 0

        self.dma(self.ident[:], cst["ident"], [], ["ident"])
        self.cast_load(self.cmask[:], cst["cmask"], 128, [5, 128], "cmask")
        self.MS("dve", self.onesf[:], 1.0, ["onesf"])

        self.setup_rope(pos_in, cst)
        self.load_xT(x_in)

        res_in = x_in
        outs = [(xa, xb), (xa, y_out)]
        for li in L:
            mid, fin = outs[li]
            if li == 1:
                res_in = xb
            if self.mixer_phase(li, res_in, mid):
                break
            if self.stop_after == ("mix", li):
                break
            self.ffn_phase(li, mid, fin)
            if self.stop_after == ("ffn", li):
                break
        S.emit(nc, final_wait_ops=self.finals)
        self.st.close()
        return nc

    def setup_rope(self, pos_in, cst):
        self.arena_reset()
        self.tabN = self.carve([2, T], BF16)
        self.tabM = self.carve([2, T], BF16)
        pi = self.carve([T], I32)
        pf = self.carve([T], F32)
        ang = self.carve([T], F32)
        kf = self.carve([T], F32)
        ki = self.carve([T], I32)
        rc = self.carve([4], F32)
        self.dma(pi, pos_in[0:1, :].to_broadcast([128, T]), [], ["pi"])
        self.dma(rc, cst["ropec"], [], ["rc"])
        self.CP("dve", pf, pi, ["pi"], ["pf"])
        for tab, ic, sc in ((self.tabN, 0, 1), (self.tabM, 2, 3)):
            for which in range(2):
                shift = math.pi / 2 if which == 0 else 0.0
                self.TS("dve", ang, pf, rc[:, ic:ic + 1], shift, ALU.mult, ALU.add, ["pf", "rc"], ["ang"])
                self.TS("dve", kf, ang, 1.0 / (2 * math.pi), None, ALU.mult, None, ["ang"], ["kf"])
                self.CP("dve", ki, kf, ["kf"], ["ki"])
                self.CP("dve", kf, ki, ["ki"], ["kf"])
                self.STT("dve", ang, kf, -2 * math.pi, ang, ALU.mult, ALU.add, ["kf", "ang"], ["ang"])
                self.TS("dve", kf, ang, math.pi, -2 * math.pi, ALU.is_gt, ALU.mult, ["ang"], ["kf"])
                self.TT("dve", ang, ang, kf, ALU.add, ["ang", "kf"], ["ang"])
                self.TS("dve", kf, ang, -math.pi, 2 * math.pi, ALU.is_lt, ALU.mult, ["ang"], ["kf"])
                self.TT("dve", ang, ang, kf, ALU.add, ["ang", "kf"], ["ang"])
                self.A(ang, ang, AF.Sin, ["ang"], ["ang"])
                if which == 0:
                    self.CP("dve", tab[:, 0, :], ang, ["ang"], ["tab"])
                else:
                    self.TS("dve", tab[:, 1, :], ang, rc[:, sc:sc + 1], None, ALU.mult, None, ["ang", "rc"], ["tab"])
        td = self.dram["tabs_d"]
        self.dma(td[0], self.tabN.rearrange("p a t -> p (a t)"), ["tab"], ["tabs_d"])
        self.dma(td[1], self.tabM.rearrange("p a t -> p (a t)"), ["tab"], ["tabs_d"])

    def x_to_xT(self, xblk, xkey, tb, rt=None):
        tt = tb // 4
        for half in range(2):
            b = self.short()
            for c in range(4):
                cc = half * 4 + c
                self.tr(self.pb[b][:, c * 128:(c + 1) * 128], xblk[:, cc * 128:(cc + 1) * 128], [xkey], [("pb", b)])
            dst = self.xT[:, half * 4:half * 4 + 4, tb * 128:(tb + 1) * 128]
            src = self.pb[b][:, :].rearrange("p (c t) -> p c t", c=4)
            self.CP("act" if half == 0 else "dve", dst, src, [("pb", b)], [("xT", tt)])
            if rt is not None:
                rt(half, b)

    def load_xT(self, x_in):
        self.arena_reset()
        xbs = [self.carve([D], F32) for _ in range(2)]
        for tb in range(NB):
            xb_ = xbs[tb % 2]
            key = ("xblk", tb % 2)
            self.dma(xb_, x_in[tb * 128:(tb + 1) * 128, :], [], [key])
            self.x_to_xT(xb_, key, tb)

    def mixer_phase(self, li, res_in, res_out):
        d = self.dram
        self.arena_reset()
        self.stage = self.stage[0:2]
        self.yT = [self.carve([2, T], BF16) for _ in range(4)]
        self.mixer_base = self.aoff
        for n, (nm, fn) in enumerate((("conv", self.conv_branch), ("nsa", self.nsa_branch), ("mla", self.mla_branch),
                                     ("sb", self.sb_branch))):
            only = getattr(self, "only", None)
            if only is None or nm in only:
                fn(li)
                self.dbg("yT%d_%d" % (n, li), self.yT[n], [128, 2, T], ["yT%d" % n])
            self.aoff = self.mixer_base
            self.S.barrier()
            if self.stop_after == (nm, li):
                return True
        self.merge_ln1(li, res_in, res_out)

    def dbg(self, name, ap, shape, keys, dt=BF16):
        if not self.debug:
            return
        o = self.dout("dbg_" + name, shape, dt)
        self.finals.append(self.dma(o, ap, keys, []))

    def conv_branch(self, li):
        d = self.dram
        win = d["win"]
        cp = self.carve([2, 34], F32)
        self.dma(cp, d["convp"][li], [], ["cp"])
        hp = self.carve([2, 30 + T], F32)
        acc = self.carve([2, T], F32)
        sig = [self.carve([512], F32) for _ in range(2)]
        self.MS("pool", hp[:, :, 0:30], 0.0, ["hp"])
        wv, wk = self.wload(win[li][:, OFF_CONV:OFF_CONV + 512], 8, 512)
        for tt in range(4):
            for c in range(2):
                ba = self.proj_fm(wv, wk, c * 128, 128, tt)
                bg = self.proj_fm(wv, wk, 256 + c * 128, 128, tt)
                sg = sig[c]
                self.A(sg, self.pb[bg][:, :], AF.Sigmoid, [("pb", bg)], [("sig", c)])
                self.TT("dve", hp[:, c, 30 + tt * 512:30 + (tt + 1) * 512], self.pb[ba][:, :], sg, ALU.mult,
                        [("pb", ba), ("sig", c)], ["hp"])
        for c in range(2):
            eng = "dve"
            self.TS(eng, acc[:, c, :], hp[:, c, 0:T], cp[:, c, 0:1], cp[:, c, 31:32], ALU.mult, ALU.add,
                    ["hp", "cp"], [("acc", c)])
            for w in range(1, 31):
                self.STT(eng, acc[:, c, :], hp[:, c, w:w + T], cp[:, c, w:w + 1], acc[:, c, :], ALU.mult, ALU.add,
                         ["hp", "cp", ("acc", c)], [("acc", c)])
        sq = [self.carve([512], F32) for _ in range(2)]
        m2 = self.carve([512], F32)
        rstd = self.carve([512], F32)
        dd = [self.carve([512], F32) for _ in range(2)]
        for tt in range(4):
            sl = slice(tt * 512, (tt + 1) * 512)
            bm = self.short()
            for c in range(2):
                self.mm(self.pb[bm][:, :], self.onesf[:, :], acc[:, c, sl], c == 0, c == 1,
                        ["onesf", ("acc", c)], [("pb", bm)])
            bq = self.short()
            for c in range(2):
                self.A(sq[c], acc[:, c, sl], AF.Square, [("acc", c)], [("sq", c)])
            for c in range(2):
                self.mm(self.pb[bq][:, :], self.onesf[:, :], sq[c], c == 0, c == 1,
                        ["onesf", ("sq", c)], [("pb", bq)])
            self.A(m2, self.pb[bm][:, :], AF.Square, [("pb", bm)], ["m2"], scale=1.0 / 256)
            self.STT("dve", rstd, self.pb[bq][:, :], 1.0 / 256, m2, ALU.mult, ALU.subtract, [("pb", bq), "m2"], ["rstd"])
            self.A(rstd, rstd, AF.Sqrt, ["rstd"], ["rstd"], bias=LN_EPS)
            self.S.op("dve", lambda e, r=rstd: e.reciprocal(out=r, in_=r), reads=["rstd"], writes=["rstd"])
            for c in range(2):
                self.STT("dve", dd[c], self.pb[bm][:, :], -1.0 / 256, acc[:, c, sl], ALU.mult, ALU.add,
                         [("pb", bm), ("acc", c)], [("dd", c)])
                self.TT("dve", dd[c], dd[c], rstd, ALU.mult, [("dd", c), "rstd"], [("dd", c)])
                self.A(self.yT[0][:, c, sl], dd[c], AF.Silu, [("dd", c), "cp"], ["yT0"],
                       scale=cp[:, c, 32:33], bias=cp[:, c, 33:34])

    def y_to_yT(self, ytile, ykey, n, qb):
        b = self.short()
        for c in range(2):
            self.tr(self.pb[b][:, c * 128:(c + 1) * 128], ytile[:, c * 128:(c + 1) * 128], [ykey], [("pb", b)])
        self.CP("act", self.yT[n][:, :, qb * 128:(qb + 1) * 128],
                self.pb[b][:, 0:256].rearrange("p (c t) -> p c t", c=2), [("pb", b)], ["yT%d" % n])

    def softmax_attn_block(self, qb, kbs, kT_fn, qT_ap, v_fn, scale, extra_mask=None, finalize=None):
        ob = self.accb()
        n = len(kbs)

        def front(g0):
            grp = kbs[g0:g0 + 4]
            sbk = self.short()
            for i, (kb, mt) in enumerate(grp):
                kT, kkeys = kT_fn(kb)
                self.mm(self.pb[sbk][:, i * 128:(i + 1) * 128], kT, qT_ap[0], True, True,
                        list(kkeys) + list(qT_ap[1]), [("pb", sbk)])
            ei = self.ei % 5
            self.ei += 1
            E = self.Ebuf[ei]
            ek = ("E", ei)
            w = len(grp) * 128
            self.A(E[:, 0:w], self.pb[sbk][:, 0:w], AF.Exp, [("pb", sbk)], [ek], scale=scale)
            if extra_mask is not None:
                mtile, mkey = extra_mask
                self.TT("dve", E[:, 0:w], E[:, 0:w], mtile[:, g0 * 128:g0 * 128 + w], ALU.mult, [ek, mkey], [ek])
            else:
                for i, (kb, mt) in enumerate(grp):
                    if mt is not None:
                        mi = {"le": 0, "lt": 1, "gt": 2}[mt]
                        self.TT("dve", E[:, i * 128:(i + 1) * 128], E[:, i * 128:(i + 1) * 128],
                                self.cmask[:, mi, :], ALU.mult, [ek, "cmask"], [ek])
            return g0, grp, E, ek

        def back(st_):
            g0, grp, E, ek = st_
            for i, (kb, mt) in enumerate(grp):
                v, vkeys = v_fn(kb)
                gi = g0 + i
                self.mm(self.pb[ob][:, 0:65], E[:, i * 128:(i + 1) * 128], v, gi == 0, gi == n - 1,
                        [ek] + list(vkeys), [("pb", ob)])

        prev = None
        for g0 in range(0, n, 4):
            cur = front(g0)
            if g0 == 0:
                self.flush_deferred()
            if prev is not None:
                back(prev)
            prev = cur

        def tail(prev=prev):
            back(prev)
            finalize(ob)
        self.deferred.append(tail)
        return ob

    def nsa_branch(self, li):
        d = self.dram
        win = d["win"]
        cst = {k: d["cst"][k] for k in d["cst"]}
        self.tabN = self.carve([2, T], BF16)
        self.dma(self.tabN.rearrange("p a t -> p (a t)"), d["tabs_d"][0], [], ["tab"])
        QT = self.carve([2, T], BF16)
        QR = self.carve([2, T], BF16)
        KS = self.carve([T], BF16)
        KW = self.carve([T], BF16)
        KCV = self.carve([T], BF16)
        vS = self.carve([NB, 65], BF16)
        vW = self.carve([NB, 65], BF16)
        sg = self.carve([NB, 12], F32)
        cmpvalid = self.carve([T], BF16)
        eexp = self.carve([T], BF16, parts=32)
        keepadd = self.carve([2, NB, 32], F32)
        VC = self.carve([97], BF16)
        w1t = self.carve([32, 64], BF16)
        pet = self.carve([32], BF16)
        w2kt = self.carve([128], BF16, parts=64)
        w2vt = self.carve([64], BF16, parts=64)
        hid = [self.carve([127], BF16, parts=64) for _ in range(2)]
        hb = self.carve([2], F32, parts=64)
        kcT = self.carve([127], BF16)
        t1 = [self.carve([512], F32) for _ in range(2)]
        t2 = [self.carve([512], F32) for _ in range(2)]
        self.Ebuf = [self.carve([512], BF16) for _ in range(5)]
        self.ei = 0
        ytile = [self.carve([256], F32) for _ in range(2)]
        selmask = [self.carve([NB * 128], BF16) for _ in range(2)]
        small = [self.carve([64], F32) for _ in range(2)]
        imp = [self.carve([32], F32) for _ in range(2)]
        scr = [self.carve([32], F32) for _ in range(2)]
        selT = [self.carve([128], BF16, parts=32) for _ in range(2)]
        self.cast_load(cmpvalid, cst["cmpvalid"], 128, [T], "cmpvalid")
        self.cast_load(eexp, cst["eexp"], 32, [T], "eexp")
        self.dma(keepadd, cst["keepadd"], [], ["keepadd"])
        self.cast_load(VC[:, 64:97], cst["ovl"], 128, [33], "VCc")
        for c in range(2):
            self.dma_w1(w1t, d["w1"][li, c], c)
        self.cast_load(pet, d["pe_r"][li], 128, [32], "pet")
        self.cast_load(w2kt, d["w2k"][li], 64, [128], "w2kt")
        self.cast_load(w2vt, d["w2v"][li], 64, [64], "w2vt")
        self.MS("dve", vS[:, :, 64:65], 1.0, ["vS"])
        self.MS("dve", vW[:, :, 64:65], 1.0, ["vW"])
        wv, wk = self.wload(win[li][:, OFF_NQ:OFF_NQ + 512], 8, 512)
        for tt in range(4):
            sl = slice(tt * 512, (tt + 1) * 512)
            for c in range(2):
                b0 = self.proj_fm(wv, wk, c * 128, 128, tt)
                b1 = self.proj_fm(wv, wk, 256 + c * 128, 128, tt)
                self.CP("act", QT[:, c, sl], self.pb[b0][:, :], [("pb", b0)], ["QT"])
                self.TT("dve", t1[c], self.pb[b0][:, :], self.tabN[:, 0, sl], ALU.mult, [("pb", b0), "tab"], [("t1", c)])
                self.TT("dve", t2[c], self.pb[b1][:, :], self.tabN[:, 1, sl], ALU.mult, [("pb", b1), "tab"], [("t2", c)])
                self.TT("dve", QR[:, c, sl], t1[c], t2[c], ALU.add, [("t1", c), ("t2", c)], ["QR"])
        wv, wk = self.wload(win[li][:, OFF_NK:OFF_NK + 512], 8, 512)
        for tt in range(4):
            sl = slice(tt * 512, (tt + 1) * 512)
            for c, dst, dk in ((0, KS, "KS"), (1, KW, "KW")):
                b0 = self.proj_fm(wv, wk, c * 256, 128, tt)
                b1 = self.proj_fm(wv, wk, c * 256 + 128, 128, tt)
                self.TT("dve", t1[c], self.pb[b0][:, :], self.tabN[:, 0, sl], ALU.mult, [("pb", b0), "tab"], [("t1", c)])
                self.TT("dve", t2[c], self.pb[b1][:, :], self.tabN[:, 1, sl], ALU.mult, [("pb", b1), "tab"], [("t2", c)])
                self.TT("dve", dst[:, sl], t1[c], t2[c], ALU.add, [("t1", c), ("t2", c)], [dk])
        wvm, wkm = self.wload(win[li][:, OFF_MISC:OFF_MISC + 128], 8, 128)
        for tt in range(4):
            sl = slice(tt * 512, (tt + 1) * 512)
            b0 = self.proj_fm(wvm, wkm, 0, 128, tt)
            self.CP("act", KCV[:, sl], self.pb[b0][:, :], [("pb", b0)], ["KCV"])
        wvt, wkt = self.wload(win[li][:, OFF_TOK:OFF_TOK + 140], 8, 140)
        for tb in range(NB):
            b = self.short()
            for k in range(8):
                self.mm(self.pb[b][:, 0:140], self.xT[:, k, tb * 128:(tb + 1) * 128], wvt[:, k, :], k == 0, k == 7,
                        [wkt, ("xT", tb // 4)], [("pb", b)])
            self.CP("act", vS[:, tb, 0:64], self.pb[b][:, 0:64], [("pb", b)], ["vS"])
            self.CP("dve", vW[:, tb, 0:64], self.pb[b][:, 64:128], [("pb", b)], ["vW"])
            self.A(sg[:, tb, :], self.pb[b][:, 128:140], AF.Sigmoid, [("pb", b)], ["sg"])
        for c in range(2):
            ps_ = slice(c * 64, (c + 1) * 64)
            b = self.short()
            for l in range(32):
                self.mm(self.pb[b][0:64, 0:127], w1t[ps_, l, :], KCV[ps_, l:l + 16 * 126 + 1:16], l == 0, l == 31,
                        ["w1t", "KCV"], [("pb", b)])
            b2 = self.short()
            for l in range(32):
                self.mm(self.pb[b2][0:64, 0:1], w1t[ps_, l, :], pet[ps_, l:l + 1], l == 0, l == 31,
                        ["w1t", "pet"], [("pb", b2)])
            self.CP("dve", hb[:, c:c + 1], self.pb[b2][0:64, 0:1], [("pb", b2)], ["hb"])
            self.A(hid[c], self.pb[b][0:64, 0:127], AF.Gelu_apprx_tanh, [("pb", b), "hb"], [("hid", c)],
                   bias=hb[:, c:c + 1])
        b = self.short()
        self.mm(self.pb[b][:, 0:127], w2kt[:, :], hid[0], True, True, ["w2kt", ("hid", 0)], [("pb", b)])
        self.CP("act", kcT, self.pb[b][:, 0:127], [("pb", b)], ["kcT"])
        b = self.short()
        self.mm(self.pb[b][0:127, 0:64], hid[1], w2vt[:, :], True, True, ["w2vt", ("hid", 1)], [("pb", b)])
        self.CP("act", VC[0:127, 0:64], self.pb[b][0:127, 0:64], [("pb", b)], ["VCv"])
        for qb in range(NB):
            qs = slice(qb * 128, (qb + 1) * 128)
            yt = ytile[qb % 2]
            yk = ("yt", qb % 2)
            sm = small[qb % 2]
            smk = ("small", qb % 2)
            im = imp[qb % 2]
            imk = ("imp", qb % 2)
            sbk2 = [self.short(), self.short()]
            for h in range(4):
                hp_ = slice((h % 2) * 64, (h % 2) * 64 + 64)
                bb_ = sbk2[h % 2]
                self.mm(self.pb[bb_][0:127, (h // 2) * 128:(h // 2 + 1) * 128], kcT[hp_, :], QT[hp_, h // 2, qs], True, True,
                        ["kcT", "QT"], [("pb", bb_)])
            ei = self.ei % 5
            self.ei += 1
            E = self.Ebuf[ei]
            ek = ("E", ei)
            for h in range(4):
                bb_ = sbk2[h % 2]
                self.A(E[0:127, h * 128:(h + 1) * 128], self.pb[bb_][0:127, (h // 2) * 128:(h // 2 + 1) * 128], AF.Exp,
                       [("pb", bb_)], [ek], scale=0.125)
            for h in range(4):
                self.TT("dve", E[0:127, h * 128:(h + 1) * 128], E[0:127, h * 128:(h + 1) * 128], cmpvalid[0:127, qs],
                        ALU.mult, [ek, "cmpvalid"], [ek])
            ob = self.accb()
            for h in range(4):
                self.mm(self.pb[ob][:, h * 97:(h + 1) * 97], E[0:127, h * 128:(h + 1) * 128], VC[0:127, :], True, True,
                        [ek, "VCv", "VCc"], [("pb", ob)])
            P = self.pb[ob]
            for h in range(4):
                self.TS("dve", sm[:, h:h + 1], P[:, 97 * h + 96:97 * h + 97], 1e-30, None, ALU.max, None, [("pb", ob)], [smk])
            self.S.op("dve", lambda e, s_=sm: e.reciprocal(out=s_[:, 0:4], in_=s_[:, 0:4]), reads=[smk], writes=[smk])
            for h in range(4):
                if h == 0:
                    self.TS("dve", im, P[:, 64:96], sm[:, 0:1], None, ALU.mult, None, [("pb", ob), smk], [imk])
                else:
                    self.STT("dve", im, P[:, 97 * h + 64:97 * h + 96], sm[:, h:h + 1], im, ALU.mult, ALU.add,
                             [("pb", ob), smk, imk], [imk])
            for h in range(4):
                self.TT("dve", sm[:, 4 + h:5 + h], sm[:, h:h + 1], sg[:, qb, 3 * h:3 * h + 1], ALU.mult, [smk, "sg"], [smk])
                self.A(yt[:, h * 64:(h + 1) * 64], P[:, 97 * h:97 * h + 64], AF.Copy, [("pb", ob), smk], [yk],
                       scale=sm[:, 4 + h:5 + h])
            mk = None
            if qb >= 8:
                sc_ = scr[qb % 2]
                sck = ("scr", qb % 2)
                self.TT("dve", im, im, keepadd[:, 0, qb, :], ALU.mult, [imk, "keepadd"], [imk])
                self.TT("dve", im, im, keepadd[:, 1, qb, :], ALU.add, [imk, "keepadd"], [imk])
                self.S.op("dve", lambda e, s_=sm, i_=im: e.max(out=s_[:, 8:16], in_=i_), reads=[imk, smk], writes=[smk])
                self.S.op("dve", lambda e, s_=sm, i_=im, c_=sc_: e.match_replace(out=c_, in_to_replace=s_[:, 8:16],
                                                                                in_values=i_, imm_value=-1e30),
                          reads=[imk, smk], writes=[sck])
                self.S.op("dve", lambda e, s_=sm, c_=sc_: e.max(out=s_[:, 16:24], in_=c_), reads=[sck, smk], writes=[smk])
                self.TS("dve", sc_, im, sm[:, 23:24], None, ALU.is_ge, None, [imk, smk], [sck])
                b = self.short()
                self.tr(self.pb[b][0:32, 0:128], sc_, [sck], [("pb", b)])
                sT = selT[qb % 2]
                stk = ("selT", qb % 2)
                self.CP("act", sT, self.pb[b][0:32, 0:128], [("pb", b)], [stk])
                smt = selmask[qb % 2]
                mk = ("selmask", qb % 2)
                for g0 in range(0, qb + 1, 4):
                    n = min(4, qb + 1 - g0)
                    b = self.short()
                    for i in range(n):
                        kb = g0 + i
                        self.mm(self.pb[b][:, i * 128:(i + 1) * 128], eexp[:, kb * 128:(kb + 1) * 128], sT, True, True,
                                ["eexp", stk], [("pb", b)])
                    self.CP("act", smt[:, g0 * 128:(g0 + n) * 128], self.pb[b][:, 0:n * 128], [("pb", b)], [mk])
                self.TT("dve", smt[:, qb * 128:(qb + 1) * 128], smt[:, qb * 128:(qb + 1) * 128], self.cmask[:, 0, :],
                        ALU.mult, [mk, "cmask"], [mk])
            for h in range(4):
                hp_ = slice((h % 2) * 64, (h % 2) * 64 + 64)
                qT_ap = (QR[hp_, h // 2, qs], ["QR"])
                for br, KT, kkey, V, vkey, gcol in ((1, KS, "KS", vS, "vS", 3 * h + 1), (2, KW, "KW", vW, "vW", 3 * h + 2)):
                    if br == 1:
                        kbs = [(kb, "le" if kb == qb else None) for kb in range(qb + 1)]
                        em = (selmask[qb % 2], mk) if qb >= 8 else None
                    else:
                        kbs = [(kb, "le" if kb == qb else ("gt" if kb == qb - 4 else None))
                               for kb in range(max(0, qb - 4), qb + 1)]
                        em = None
                    def fin(ob, h=h, br=br, gcol=gcol, yt=yt, yk=yk, sm=sm, smk=smk, qb=qb):
                        fac = sm[:, 24 + 2 * h + (br - 1):25 + 2 * h + (br - 1)]
                        self.S.op("dve", lambda e, f_=fac, o_=self.pb[ob][:, 64:65]: e.reciprocal(out=f_, in_=o_),
                                  reads=[("pb", ob), smk], writes=[smk])
                        self.TT("dve", fac, fac, sg[:, qb, gcol:gcol + 1], ALU.mult, [smk, "sg"], [smk])
                        self.STT("dve", yt[:, h * 64:(h + 1) * 64], self.pb[ob][:, 0:64], fac, yt[:, h * 64:(h + 1) * 64],
                                 ALU.mult, ALU.add, [("pb", ob), smk, yk], [yk])
                    self.softmax_attn_block(
                        qb, kbs, lambda kb, KT=KT, kkey=kkey: (KT[hp_, kb * 128:(kb + 1) * 128], [kkey]), qT_ap,
                        lambda kb, V=V, vkey=vkey: (V[:, kb, :], [vkey]), 0.125, extra_mask=em, finalize=fin)
            self.flush_deferred()
            self.y_to_yT(yt, yk, 1, qb)

    def mla_branch(self, li):
        d = self.dram
        win = d["win"]
        self.tabM = self.carve([2, T], BF16)
        self.dma(self.tabM.rearrange("p a t -> p (a t)"), d["tabs_d"][1], [], ["tab"])
        qg = self.carve([2, T], BF16)
        kvg = self.carve([T], BF16)
        CS = self.carve([2, T], BF16)
        rkv = self.carve([T], BF16)
        rkt = self.carve([NB], F32)
        Vm = self.carve([NB, 4, 65], BF16)
        QH = [self.carve([T], BF16) for _ in range(2)]
        KH = [self.carve([T], BF16) for _ in range(2)]
        KR = self.carve([T], BF16)
        nrm = self.carve([4], F32)
        sq = [self.carve([512], F32) for _ in range(2)]
        t1 = [self.carve([512], F32) for _ in range(2)]
        t2 = [self.carve([512], F32) for _ in range(2)]
        rs = self.carve([512], F32)
        self.Ebuf = [self.carve([512], BF16) for _ in range(5)]
        self.ei = 0
        self.ymla = self.carve([NB, 256], F32)
        small = [self.carve([8], F32) for _ in range(2)]
        self.dma(nrm[:, 0:2], d["qn"][li], [], ["nrm"])
        self.dma(nrm[:, 2:3], d["kvn"][li], [], ["nrm"])
        self.MS("dve", Vm[:, :, :, 64:65], 1.0, ["Vm"])
        wvm, wkm = self.wload(win[li][:, OFF_MISC + 128:OFF_MISC + 512], 8, 384)
        for tt in range(4):
            sl = slice(tt * 512, (tt + 1) * 512)
            bq = []
            for c in range(2):
                b0 = self.proj_fm(wvm, wkm, c * 128, 128, tt)
                bq.append(b0)
                self.A(qg[:, c, sl], self.pb[b0][:, :], AF.Copy, [("pb", b0), "nrm"], ["qg"], scale=nrm[:, c:c + 1])
                self.A(sq[c], self.pb[b0][:, :], AF.Square, [("pb", b0)], [("sq", c)])
            bs = self.short()
            for c in range(2):
                self.mm(self.pb[bs][:, :], self.onesf[:, :], sq[c], c == 0, c == 1, ["onesf", ("sq", c)], [("pb", bs)])
            self.A(rs, self.pb[bs][:, :], AF.Sqrt, [("pb", bs)], ["rs"], scale=1.0 / 256, bias=RMS_EPS)
            self.S.op("dve", lambda e, r=rs: e.reciprocal(out=r, in_=r), reads=["rs"], writes=["rs"])
            for w in range(2):
                self.TT("dve", CS[:, w, sl], self.tabM[:, w, sl], rs, ALU.mult, ["tab", "rs"], ["CS"])
            b0 = self.proj_fm(wvm, wkm, 256, 128, tt)
            self.A(kvg[:, sl], self.pb[b0][:, :], AF.Copy, [("pb", b0), "nrm"], ["kvg"], scale=nrm[:, 2:3])
            self.A(sq[0], self.pb[b0][:, :], AF.Square, [("pb", b0)], [("sq", 0)])
            bs = self.short()
            self.mm(self.pb[bs][:, :], self.onesf[:, :], sq[0], True, True, ["onesf", ("sq", 0)], [("pb", bs)])
            self.A(rs, self.pb[bs][:, :], AF.Sqrt, [("pb", bs)], ["rs"], scale=1.0 / 128, bias=RMS_EPS)
            self.S.op("dve", lambda e, r=rs, o=rkv[:, sl]: e.reciprocal(out=o, in_=r), reads=["rs"], writes=["rkv"])
            bt = self.short()
            for i in range(4):
                self.mm(self.pb[bt][:, i:i + 1], sq[0][:, i * 128:(i + 1) * 128], self.onesf[:, 0:1], True, True,
                        [("sq", 0), "onesf"], [("pb", bt)])
            self.A(rkt[:, tt * 4:tt * 4 + 4], self.pb[bt][:, 0:4], AF.Sqrt, [("pb", bt)], ["rkt"], scale=1.0 / 128,
                   bias=RMS_EPS)
        self.S.op("dve", lambda e: e.reciprocal(out=rkt, in_=rkt), reads=["rkt"], writes=["rkt"])
        wvr, wkr = self.wload(win[li][:, OFF_KR:OFF_KR + 256], 8, 256)
        r9 = slice(64, 96)
        for tt in range(4):
            sl = slice(tt * 512, (tt + 1) * 512)
            b0 = self.proj_fm(wvr, wkr, 0, 96, tt)
            b1 = self.proj_fm(wvr, wkr, 128, 96, tt)
            self.TT("dve", t1[0][r9, :], self.pb[b0][r9, :], self.tabM[r9, 0, sl], ALU.mult, [("pb", b0), "tab"], [("t1", 0)])
            self.TT("dve", t2[0][r9, :], self.pb[b1][r9, :], self.tabM[r9, 1, sl], ALU.mult, [("pb", b1), "tab"], [("t2", 0)])
            self.TT("dve", KR[r9, sl], t1[0][r9, :], t2[0][r9, :], ALU.add, [("t1", 0), ("t2", 0)], ["KR"])
        wvu, wku = self.wload(d["ukv"][li], 1, 512)
        for tb in range(NB):
            b = self.short()
            self.mm(self.pb[b][:, 0:256], kvg[:, tb * 128:(tb + 1) * 128], wvu[:, 0, 256:512], True, True,
                    ["kvg", wku], [("pb", b)])
            self.A(Vm[:, tb, :, 0:64], self.pb[b][:, 0:256].rearrange("p (h c) -> p h c", h=4), AF.Copy,
                   [("pb", b), "rkt"], ["Vm"], scale=rkt[:, tb:tb + 1])
        wvq, wkq = self.wload(d["uq"][li], 2, 768)
        scale = 96.0 ** -0.5
        for h in range(4):
            Q = QH[h % 2]
            K = KH[h % 2]
            qk = ("QH", h % 2)
            kk = ("KH", h % 2)
            for tt in range(4):
                sl = slice(tt * 512, (tt + 1) * 512)
                ba = self.proj_fm(wvq, wkq, (2 * h) * 96, 96, tt, src=qg, srckey="qg", kc=2)
                bb = self.proj_fm(wvq, wkq, (2 * h + 1) * 96, 96, tt, src=qg, srckey="qg", kc=2)
                c = tt % 2
                self.TT("dve", t1[c][0:96, :], self.pb[ba][0:96, :], CS[0:96, 0, sl], ALU.mult, [("pb", ba), "CS"], [("t1", c)])
                self.TT("dve", t2[c][0:96, :], self.pb[bb][0:96, :], CS[0:96, 1, sl], ALU.mult, [("pb", bb), "CS"], [("t2", c)])
                self.TT("dve", Q[0:96, sl], t1[c][0:96, :], t2[c][0:96, :], ALU.add, [("t1", c), ("t2", c)], [qk])
                bk = self.short()
                self.mm(self.pb[bk][0:64, :], wvu[:, 0, h * 64:(h + 1) * 64], kvg[:, sl], True, True, [wku, "kvg"], [("pb", bk)])
                self.TT("dve", K[0:64, sl], self.pb[bk][0:64, :], rkv[0:64, sl], ALU.mult, [("pb", bk), "rkv"], [kk])
            self.CP("dve", K[r9, :], KR[r9, :], ["KR"], [kk])
            for qb in range(NB):
                qs = slice(qb * 128, (qb + 1) * 128)
                sm = small[qb % 2]
                smk = ("small", qb % 2)
                kbs = [(kb, "le" if kb == qb else None) for kb in range(qb + 1)]
                def fin(ob, sm=sm, smk=smk, qb=qb, h=h):
                    self.S.op("dve", lambda e, s_=sm, o_=self.pb[ob][:, 64:65]: e.reciprocal(out=s_[:, 0:1], in_=o_),
                              reads=[("pb", ob)], writes=[smk])
                    self.A(self.ymla[:, qb, h * 64:(h + 1) * 64], self.pb[ob][:, 0:64], AF.Copy, [("pb", ob), smk],
                           [("ymla", qb)], scale=sm[:, 0:1])
                self.softmax_attn_block(
                    qb, kbs, lambda kb, K=K, kk=kk: (K[0:96, kb * 128:(kb + 1) * 128], [kk]), (Q[0:96, qs], [qk]),
                    lambda kb, h=h: (Vm[:, kb, h, :], ["Vm"]), scale, finalize=fin)
            self.flush_deferred()
        for qb in range(NB):
            self.y_to_yT(self.ymla[:, qb, :], ("ymla", qb), 2, qb)

    def sb_branch(self, li):
        d = self.dram
        win = d["win"]
        cst = d["cst"]
        QT = self.carve([2, T], BF16)
        KT = self.carve([2, T], BF16)
        V = self.carve([NB, 256], BF16)
        indt = self.carve([16, 16], BF16)
        selgt = self.carve([16, 128], BF16, parts=16)
        sp = [self.carve([NB * 128], F32) for _ in range(2)]
        lk = [self.carve([NB * 128], BF16) for _ in range(2)]
        ex = [self.carve([512], F32) for _ in range(2)]
        ar = [self.carve([512], F32) for _ in range(2)]
        aa = [self.carve([512], BF16) for _ in range(5)]
        ts_ = [self.carve([128], BF16, parts=16) for _ in range(2)]
        ytile = [self.carve([256], F32) for _ in range(2)]
        self.cast_load(indt, cst["indt"], 128, [16, 16], "indt")
        self.cast_load(selgt, cst["selgt"], 16, [16, 128], "selgt")
        wv, wk = self.wload(win[li][:, OFF_SB:OFF_SB + 512], 8, 512)
        for tt in range(4):
            sl = slice(tt * 512, (tt + 1) * 512)
            for c in range(2):
                b0 = self.proj_fm(wv, wk, c * 128, 128, tt)
                self.CP("act", QT[:, c, sl], self.pb[b0][:, :], [("pb", b0)], ["QT"])
                b1 = self.proj_fm(wv, wk, 256 + c * 128, 128, tt)
                self.CP("dve", KT[:, c, sl], self.pb[b1][:, :], [("pb", b1)], ["KT"])
        wvt, wkt = self.wload(win[li][:, OFF_TOK + 256:OFF_TOK + 512], 8, 256)
        for tb in range(NB):
            b = self.short()
            for k in range(8):
                self.mm(self.pb[b][:, 0:256], self.xT[:, k, tb * 128:(tb + 1) * 128], wvt[:, k, :], k == 0, k == 7,
                        [wkt, ("xT", tb // 4)], [("pb", b)])
            self.CP("act", V[:, tb, :], self.pb[b][:, 0:256], [("pb", b)], ["V"])
        ai_ = 0
        iters = [(qb, h) for qb in range(NB) for h in range(4)]

        def ctx(idx):
            qb, h = iters[idx]
            return dict(qb=qb, h=h, qs=slice(qb * 128, (qb + 1) * 128), yt=ytile[qb % 2], yk=("yt", qb % 2),
                        hp_=slice((h % 2) * 64, (h % 2) * 64 + 64), spt=sp[idx % 2], lkt=lk[idx % 2], tst=ts_[idx % 2],
                        spk=("sp", idx % 2), lkk=("lk", idx % 2), tsk=("ts", idx % 2), nk=qb + 1)

        def p1(idx):
            c_ = ctx(idx)
            qb, h, qs, hp_, spt, lkt, spk, lkk, nk = (c_[k] for k in ("qb", "h", "qs", "hp_", "spt", "lkt", "spk", "lkk", "nk"))
            for g0 in range(0, nk, 4):
                n = min(4, nk - g0)
                w = n * 128
                sbk = self.short()
                for i in range(n):
                    kb = g0 + i
                    self.mm(self.pb[sbk][:, i * 128:(i + 1) * 128], KT[hp_, h // 2, kb * 128:(kb + 1) * 128],
                            QT[hp_, h // 2, qs], True, True, ["KT", "QT"], [("pb", sbk)])
                e_ = ex[(g0 // 4) % 2]
                exk = ("ex", (g0 // 4) % 2)
                self.A(e_[:, 0:w], self.pb[sbk][:, 0:w], AF.Exp, [("pb", sbk)], [exk], scale=-0.125)
                self.A(spt[:, g0 * 128:g0 * 128 + w], e_[:, 0:w], AF.Ln, [exk], [spk], bias=1.0)
                self.STT("dve", lkt[:, g0 * 128:g0 * 128 + w], self.pb[sbk][:, 0:w], -0.125, spt[:, g0 * 128:g0 * 128 + w],
                         ALU.mult, ALU.subtract, [("pb", sbk), spk], [lkk])
            self.TT("dve", lkt[:, qb * 128:(qb + 1) * 128], lkt[:, qb * 128:(qb + 1) * 128], self.cmask[:, 1, :],
                    ALU.mult, [lkk, "cmask"], [lkk])

        def rest_(idx):
            nonlocal ai_
            c_ = ctx(idx)
            qb, h, qs, yt, yk, hp_, spt, lkt, tst, spk, lkk, tsk, nk = (c_[k] for k in (
                "qb", "h", "qs", "yt", "yk", "hp_", "spt", "lkt", "tst", "spk", "lkk", "tsk", "nk"))
            bts = self.short()
            for kb in range(nk):
                self.mm(self.pb[bts][0:16, 0:128], indt[:, kb, :], lkt[:, kb * 128:(kb + 1) * 128], kb == 0, kb == nk - 1,
                        ["indt", lkk], [("pb", bts)])
            self.CP("act", tst, self.pb[bts][0:16, 0:128], [("pb", bts)], [tsk])
            ob = self.accb()

            def front3(g0):
                nonlocal ai_
                n = min(4, nk - g0)
                w = n * 128
                lb = self.short()
                for i in range(n):
                    kb = g0 + i
                    self.mm(self.pb[lb][:, i * 128:(i + 1) * 128], self.cmask[:, 2, :], lkt[:, kb * 128:(kb + 1) * 128],
                            True, False, ["cmask", lkk], [("pb", lb)])
                    self.mm(self.pb[lb][:, i * 128:(i + 1) * 128], selgt[:, kb, :], tst, False, True,
                            ["selgt", tsk], [("pb", lb)])
                a_ = ar[(g0 // 4) % 2]
                ark = ("ar", (g0 // 4) % 2)
                self.TT("dve", a_[:, 0:w], self.pb[lb][:, 0:w], spt[:, g0 * 128:g0 * 128 + w], ALU.subtract,
                        [("pb", lb), spk], [ark])
                at = aa[ai_ % 5]
                ak = ("aa", ai_ % 5)
                ai_ += 1
                self.A(at[:, 0:w], a_[:, 0:w], AF.Exp, [ark], [ak])
                if g0 + n == nk:
                    i = n - 1
                    self.TT("dve", at[:, i * 128:(i + 1) * 128], at[:, i * 128:(i + 1) * 128], self.cmask[:, 1, :],
                            ALU.mult, [ak, "cmask"], [ak])
                return g0, n, at, ak

            def back3(st_):
                g0, n, at, ak = st_
                for i in range(n):
                    kb = g0 + i
                    self.mm(self.pb[ob][:, 0:64], at[:, i * 128:(i + 1) * 128], V[:, kb, h * 64:(h + 1) * 64],
                            kb == 0, kb == nk - 1, [ak, "V"], [("pb", ob)])

            prev = None
            for g0 in range(0, nk, 4):
                cur = front3(g0)
                if prev is not None:
                    back3(prev)
                prev = cur
            back3(prev)
            self.CP("act", yt[:, h * 64:(h + 1) * 64], self.pb[ob][:, 0:64], [("pb", ob)], [yk])
            if h == 3:
                self.y_to_yT(yt, yk, 3, qb)

        p1(0)
        for idx in range(len(iters)):
            if idx + 1 < len(iters):
                p1(idx + 1)
            rest_(idx)

    def layer_norm_block(self, h, hk, gb, tb, res_out, li, route):
        st = self.lnst[tb % 2]
        sk = ("lnst", tb % 2)
        junk = self.lnjunk
        self.A(junk, h, AF.Copy, [hk], ["lnjunk", sk], accum=st[:, 0:1])
        self.A(junk, h, AF.Square, [hk], ["lnjunk", sk], accum=st[:, 1:2])
        self.TS("dve", st[:, 2:3], st[:, 0:1], 1.0 / D, None, ALU.mult, None, [sk], [sk])
        self.TT("dve", st[:, 3:4], st[:, 2:3], st[:, 2:3], ALU.mult, [sk], [sk])
        self.STT("dve", st[:, 4:5], st[:, 1:2], 1.0 / D, st[:, 3:4], ALU.mult, ALU.subtract, [sk], [sk])
        self.A(st[:, 4:5], st[:, 4:5], AF.Sqrt, [sk], [sk], bias=LN_EPS)
        self.S.op("dve", lambda e, s_=st: e.reciprocal(out=s_[:, 5:6], in_=s_[:, 4:5]), reads=[sk], writes=[sk])
        self.STT("dve", st[:, 6:7], st[:, 2:3], -1.0, st[:, 5:6], ALU.mult, ALU.mult, [sk], [sk])
        self.A(h, h, AF.Identity, [hk, sk], [hk], scale=st[:, 5:6], bias=st[:, 6:7])
        self.TT("dve", h, h, gb[:, 0, :], ALU.mult, [hk, "lngb"], [hk])
        self.TT("dve", h, h, gb[:, 1, :], ALU.add, [hk, "lngb"], [hk])
        o = self.dma(res_out[tb * 128:(tb + 1) * 128, :], h, [hk], [("res", id(res_out), tb)])
        self.x_to_xT(h, hk, tb, rt=route)
        return o

    def merge_ln1(self, li, res_in, res_out):
        d = self.dram
        win = d["win"]
        HT = 1024
        mp = self.carve([8, HT], BF16)
        accm = self.carve([8, HT], F32)
        sgt = [self.carve([512], BF16) for _ in range(2)]
        prod = [self.carve([512], F32) for _ in range(2)]
        gb = self.carve([2, D], F32)
        hbuf = [self.carve([D], F32) for _ in range(2)]
        xin = [self.carve([D], F32) for _ in range(2)]
        self.lnst = [self.carve([8], F32) for _ in range(2)]
        self.lnjunk = self.carve([D], BF16)
        self.dma(gb[:, 0, :], d["ln1g"][li:li + 1, :].to_broadcast([128, D]), [], ["lngb"])
        self.dma(gb[:, 1, :], d["ln1b"][li:li + 1, :].to_broadcast([128, D]), [], ["lngb"])
        route = None
        if li == 1:
            route = self.make_router(li)
        for th in range(2):
            for n in range(4):
                wvb, wkb = self.wload(d["wbr"][li, n], 2, D)
                for q4 in range(2):
                    c0 = OFF_GATE + n * D + q4 * 512
                    wvg, wkg = self.wload(win[li][:, c0:c0 + 512], 8, 512)
                    for cc in range(4):
                        dc = q4 * 4 + cc
                        for t2 in range(2):
                            tt = th * 2 + t2
                            sl = slice(t2 * 512, (t2 + 1) * 512)
                            bg = self.proj_fm(wvg, wkg, cc * 128, 128, tt)
                            bp = self.proj_fm(wvb, wkb, dc * 128, 128, tt, src=self.yT[n], srckey="yT%d" % n, kc=2)
                            s_ = sgt[t2]
                            sk = ("sgt", t2)
                            self.A(s_, self.pb[bg][:, :], AF.Sigmoid, [("pb", bg)], [sk])
                            ak = ("accm", dc, t2)
                            if n == 0:
                                self.TT("dve", accm[:, dc, sl], self.pb[bp][:, :], s_, ALU.mult, [("pb", bp), sk], [ak])
                            else:
                                p_ = prod[t2]
                                pk = ("prod", t2)
                                self.TT("dve", p_, self.pb[bp][:, :], s_, ALU.mult, [("pb", bp), sk], [pk])
                                if n < 3:
                                    self.TT("dve", accm[:, dc, sl], accm[:, dc, sl], p_, ALU.add, [ak, pk], [ak])
                                else:
                                    self.TT("dve", mp[:, dc, sl], accm[:, dc, sl], p_, ALU.add, [ak, pk], [("mp", dc, t2)])
            wo = [self.wload(d["wout"][li][:, hh * 512:(hh + 1) * 512], 8, 512) for hh in range(2)]
            for j in range(8):
                tb = th * 8 + j
                xi = xin[tb % 2]
                xk = ("xin", tb % 2)
                self.dma(xi, res_in[tb * 128:(tb + 1) * 128, :], [("res", id(res_in), tb)], [xk])
                h = hbuf[tb % 2]
                hk = ("hbuf", tb % 2)
                for hh in range(2):
                    ob = self.accb()
                    for k in range(8):
                        self.mm(self.pb[ob][:, :], mp[:, k, j * 128:(j + 1) * 128], wo[hh][0][:, k, :], k == 0, k == 7,
                                [("mp", k, j // 4), wo[hh][1]], [("pb", ob)])
                    self.STT("dve", h[:, hh * 512:(hh + 1) * 512], xi[:, hh * 512:(hh + 1) * 512], ALPHA, self.pb[ob][:, :],
                             ALU.mult, ALU.add, [xk, ("pb", ob)], [hk])
                rt = (lambda half, b, tb=tb: route(tb, half, b)) if route else None
                o = self.layer_norm_block(h, hk, gb, tb, res_out, li, rt)
                if self.stop_after == ("mix", li):
                    self.finals.append(o)

    def layer_norm_block(self, h, hk, gb, tb, res_out, li, route):
        st = self.lnst[tb % 2]
        sk = ("lnst", tb % 2)
        junk = self.lnjunk
        self.MS("dve", st[:, 0:2], 0.0, [sk])
        self.A(junk, h, AF.Copy, [hk, sk], ["lnjunk", sk], accum=st[:, 0:1])
        self.A(junk, h, AF.Square, [hk, sk], ["lnjunk", sk], accum=st[:, 1:2])
        self.TS("dve", st[:, 2:3], st[:, 0:1], 1.0 / D, None, ALU.mult, None, [sk], [sk])
        self.TT("dve", st[:, 3:4], st[:, 2:3], st[:, 2:3], ALU.mult, [sk], [sk])
        self.STT("dve", st[:, 4:5], st[:, 1:2], 1.0 / D, st[:, 3:4], ALU.mult, ALU.subtract, [sk], [sk])
        self.A(st[:, 4:5], st[:, 4:5], AF.Sqrt, [sk], [sk], bias=LN_EPS)
        self.S.op("dve", lambda e, s_=st: e.reciprocal(out=s_[:, 5:6], in_=s_[:, 4:5]), reads=[sk], writes=[sk])
        self.STT("dve", st[:, 6:7], st[:, 2:3], -1.0, st[:, 5:6], ALU.mult, ALU.mult, [sk], [sk])
        self.A(h, h, AF.Identity, [hk, sk], [hk], scale=st[:, 5:6], bias=st[:, 6:7])
        self.TT("dve", h, h, gb[:, 0, :], ALU.mult, [hk, "lngb"], [hk])
        self.TT("dve", h, h, gb[:, 1, :], ALU.add, [hk, "lngb"], [hk])
        o = self.dma(res_out[tb * 128:(tb + 1) * 128, :], h, [hk], [("res", id(res_out), tb)])
        self.x_to_xT(h, hk, tb, rt=route)
        return o

    def make_router(self, li):
        d = self.dram
        rw = self.carve([8, NE], F32)
        self.dma(rw, d["router"].rearrange("(c p) e -> p c e", p=128), [], ["rw"])
        xf = [self.carve([512], F32) for _ in range(2)]
        lg = [self.carve([32], F32) for _ in range(2)]
        state = {}

        def route(tb, half, b):
            x_ = xf[half]
            xk = ("xf", half)
            self.CP("dve", x_, self.pb[b][:, :], [("pb", b)], [xk])
            if half == 0:
                state["bank"] = self.accb()
            rb = state["bank"]
            for c in range(4):
                k = half * 4 + c
                self.mm(self.pb[rb][:, 0:NE], x_[:, c * 128:(c + 1) * 128], rw[:, k, :], k == 0, k == 7,
                        [xk, "rw"], [("pb", rb)])
            if half == 1:
                l_ = lg[tb % 2]
                lk = ("lg", tb % 2)
                self.CP("dve", l_[:, 0:8], self.pb[rb][:, 0:NE], [("pb", rb)], [lk])
                self.S.op("dve", lambda e, l_=l_: e.max(out=l_[:, 8:16], in_=l_[:, 0:8]), reads=[lk], writes=[lk])
                self.TT("dve", l_[:, 16:17], l_[:, 9:10], l_[:, 8:9], ALU.subtract, [lk], [lk])
                self.A(l_[:, 16:17], l_[:, 16:17], AF.Exp, [lk], [lk])
                self.TS("dve", l_[:, 16:17], l_[:, 16:17], 1.0, None, ALU.add, None, [lk], [lk])
                self.S.op("dve", lambda e, l_=l_: e.reciprocal(out=l_[:, 17:18], in_=l_[:, 16:17]), reads=[lk], writes=[lk])
                self.TS("dve", l_[:, 18:19], l_[:, 8:9], -1.0, None, ALU.mult, None, [lk], [lk])
                self.A(l_[:, 24:32], l_[:, 0:8], AF.Exp, [lk], [lk], bias=l_[:, 18:19])
                self.TS("dve", l_[:, 0:8], l_[:, 0:8], l_[:, 9:10], l_[:, 17:18], ALU.is_ge, ALU.mult, [lk], [lk])
                self.TT("dve", self.gates[:, tb, :], l_[:, 0:8], l_[:, 24:32], ALU.mult, [lk], ["gates"])
        return route

    MOE_CAP = 512

    def ffn_phase(self, li, res_in, res_out):
        self.arena_reset()
        G = 1024
        moe = (li == 1)
        dff = D_FFE if moe else D_FF
        nfc = dff // 128
        hT = self.carve([nfc, self.MOE_CAP if moe else G], BF16)
        facc = self.carve([8, D], F32)
        self.stage = self.stage[0:2] + [self.carve([2048], F32)]
        base = self.aoff
        for g in range(T // G):
            self.aoff = base
            if g > 0:
                self.S.barrier()
            if moe:
                self.moe_group(li, g, res_in, hT, facc, nfc, dff)
            else:
                self.dense_group(li, g, hT, facc, nfc, dff)
            self.S.barrier()
            self.aoff = base
            self.ple_ln2_group(li, g, res_in, res_out, facc)

    def hidden_fm(self, w_in, dff, nfc, hT, src, srckey, tiles, sa):
        for f0 in range(0, nfc, 4):
            nf = min(4, nfc - f0)
            wa, wak = self.wload(w_in[:, f0 * 128:(f0 + nf) * 128], 8, nf * 128)
            wu, wuk = self.wload(w_in[:, dff + f0 * 128:dff + (f0 + nf) * 128], 8, nf * 128)
            for fi in range(nf):
                fc = f0 + fi
                for t2, tt in enumerate(tiles):
                    ba = self.proj_fm(wa, wak, fi * 128, 128, tt, src=src, srckey=srckey)
                    bu = self.proj_fm(wu, wuk, fi * 128, 128, tt, src=src, srckey=srckey)
                    s_ = sa[t2 % 2]
                    sk = ("sa", t2 % 2)
                    self.A(s_, self.pb[ba][:, :], AF.Silu, [("pb", ba)], [sk])
                    self.TT("dve", hT[:, fc, t2 * 512:(t2 + 1) * 512], self.pb[bu][:, :], s_, ALU.mult,
                            [("pb", bu), sk], [("hT", fc)])

    def out_tm(self, w_out, nfc, hT, blocks, evac):
        for f0 in range(0, nfc, 4):
            nf = min(4, nfc - f0)
            wo, wok = self.wload(w_out[f0 * 128:(f0 + nf) * 128, :], nf, D)
            for fi in range(nf):
                fc = f0 + fi
                for i, j in enumerate(blocks):
                    for hh in range(2):
                        b = i * 2 + hh
                        self.mm(self.pb[b][:, :], hT[:, fc, j * 128:(j + 1) * 128], wo[:, fi, hh * 512:(hh + 1) * 512],
                                fc == 0, fc == nfc - 1, [("hT", fc), wok], [("pb", b)])
        for i, j in enumerate(blocks):
            for hh in range(2):
                evac(i, j, hh, i * 2 + hh)

    def dense_group(self, li, g, hT, facc, nfc, dff):
        d = self.dram
        sa = [self.carve([512], BF16) for _ in range(2)]
        self.hidden_fm(d["ffn_in"], dff, nfc, hT, None, None, [g * 2, g * 2 + 1], sa)
        for ps_ in range(2):
            def evac(i, j, hh, b):
                self.CP("act" if hh == 0 else "dve", facc[:, j, hh * 512:(hh + 1) * 512], self.pb[b][:, :],
                        [("pb", b)], [("facc", j, hh)])
            self.out_tm(d["ffn_out"], nfc, hT, [ps_ * 4 + i for i in range(4)], evac)

    def moe_group(self, li, g, res_in, hT, facc, nfc, dff):
        d = self.dram
        C = self.MOE_CAP
        NR = C // 128
        xtok = self.carve([8, D], BF16)
        Pb = self.carve([8, C], BF16)
        PT = self.carve([NR, 8, 128], BF16)
        xsT = self.carve([8, C], BF16)
        ys = self.carve([NR, D], BF16)
        iota = self.carve([C], F32)
        sa = [self.carve([512], BF16) for _ in range(2)]
        mf = self.carve([64], F32)
        mb = self.carve([64], BF16)
        rk = self.carve([64], F32)
        off = self.carve([64], F32)
        gs = self.carve([64, 2], BF16)
        gt = self.carve([64], F32)
        wr = self.carve([NR, 2], F32)
        gsl = self.gates[:, g * 8:(g + 1) * 8, :].rearrange("p j e -> p (j e)")
        self.dma(iota, d["cst"]["iota"], [], ["iota"])
        for j in range(8):
            tb = g * 8 + j
            self.cast_load(xtok[:, j, :], res_in[tb * 128:(tb + 1) * 128, :], 128, [D], ("xtok", j))
        self.TS("dve", mf, gsl, 0.0, None, ALU.is_gt, None, ["gates"], ["mf"])
        self.CP("dve", mb, mf, ["mf"], ["mb"])
        b1 = self.short()
        self.mm(self.pb[b1][:, 0:64], self.cmask[:, 0, :], mb, True, True, ["cmask", "mb"], [("pb", b1)])
        b2 = self.short()
        self.mm(self.pb[b2][:, 0:64], self.cmask[:, 3, :], mb, True, True, ["cmask", "mb"], [("pb", b2)])
        self.MS("dve", off[:, 0:8], 0.0, ["off"])
        for j in range(1, 8):
            self.TT("dve", off[:, j * 8:(j + 1) * 8], off[:, (j - 1) * 8:j * 8], self.pb[b2][:, (j - 1) * 8:j * 8], ALU.add,
                    ["off", ("pb", b2)], ["off"])
        self.TT("dve", rk, self.pb[b1][:, 0:64], off, ALU.add, [("pb", b1), "off"], ["rk"])
        self.TT("dve", rk, rk, mf, ALU.mult, ["rk", "mf"], ["rk"])
        self.TS("dve", rk, rk, -1.0, None, ALU.add, None, ["rk"], ["rk"])
        self.CP("dve", gs[:, :, 0], gsl, ["gates"], ["gs"])
        self.TT("dve", gt, gsl, gs[:, :, 0], ALU.subtract, ["gates", "gs"], ["gt"])
        self.CP("dve", gs[:, :, 1], gt, ["gt"], ["gs"])
        for e in range(NE):
            for j in range(8):
                self.TS("dve", Pb[:, j, :], iota, rk[:, j * 8 + e:j * 8 + e + 1], None, ALU.is_equal, None,
                        ["iota", "rk"], [("Pb", j)])
            for rb in range(NR):
                for j0 in range(0, 8, 4):
                    b = self.short()
                    for jj in range(4):
                        j = j0 + jj
                        self.mm(self.pb[b][:, jj * 128:(jj + 1) * 128], Pb[:, j, rb * 128:(rb + 1) * 128], self.cmask[:, 4, :],
                                True, True, [("Pb", j), "cmask"], [("pb", b)])
                    self.CP("act", PT[:, rb, j0:j0 + 4, :], self.pb[b][:, :].rearrange("p (j t) -> p j t", j=4),
                            [("pb", b)], [("PT", rb)])
            for dc in range(8):
                b = self.short()
                for j in range(8):
                    self.mm(self.pb[b][:, 0:C], xtok[:, j, dc * 128:(dc + 1) * 128], Pb[:, j, :], j == 0, j == 7,
                            [("xtok", j), ("Pb", j)], [("pb", b)])
                self.CP("dve" if dc % 2 else "act", xsT[:, dc, :], self.pb[b][:, 0:C], [("pb", b)], ["xsT"])
            bw = self.short()
            for rb in range(NR):
                for j in range(8):
                    self.mm(self.pb[bw][:, 2 * rb:2 * rb + 2], Pb[:, j, rb * 128:(rb + 1) * 128], gs[:, j * 8 + e, :],
                            j == 0, j == 7, [("Pb", j), "gs"], [("pb", bw)])
            self.CP("dve", wr, self.pb[bw][:, 0:2 * NR].rearrange("p (r c) -> p r c", c=2), [("pb", bw)], ["wr"])
            self.TT("dve", wr[:, :, 0], wr[:, :, 0], wr[:, :, 1], ALU.add, ["wr"], ["wr"])
            self.hidden_fm(d["moe_in"][e], dff, nfc, hT, xsT, "xsT", [0], sa)

            def evac(i, j, hh, b):
                self.A(ys[:, i, hh * 512:(hh + 1) * 512], self.pb[b][:, :], AF.Copy, [("pb", b), "wr"], [("ys", i)],
                       scale=wr[:, i, 0:1])
            self.out_tm(d["moe_out"][e], nfc, hT, list(range(NR)), evac)
            for j in range(8):
                for hh in range(2):
                    b = self.short()
                    for rb in range(NR):
                        self.mm(self.pb[b][:, :], PT[:, rb, j, :], ys[:, rb, hh * 512:(hh + 1) * 512], rb == 0, rb == NR - 1,
                                [("PT", rb), ("ys", rb)], [("pb", b)])
                    dst = facc[:, j, hh * 512:(hh + 1) * 512]
                    fk = ("facc", j, hh)
                    if e == 0:
                        self.CP("dve", dst, self.pb[b][:, :], [("pb", b)], [fk])
                    else:
                        self.TT("dve", dst, dst, self.pb[b][:, :], ALU.add, [fk, ("pb", b)], [fk])

    def ple_ln2_group(self, li, g, res_in, res_out, facc):
        d = self.dram
        gb = self.carve([2, D], F32)
        pblk = [self.carve([256], F32) for _ in range(2)]
        pT = [self.carve([2, 128], BF16) for _ in range(2)]
        ple = self.carve([D], F32)
        hbuf = [self.carve([D], F32) for _ in range(2)]
        xin = self.carve([D], F32)
        self.lnst = [self.carve([8], F32) for _ in range(2)]
        self.lnjunk = self.carve([D], BF16)
        self.dma(gb[:, 0, :], d["ln2g"][li:li + 1, :].to_broadcast([128, D]), [], ["lngb"])
        self.dma(gb[:, 1, :], d["ln2b"][li:li + 1, :].to_broadcast([128, D]), [], ["lngb"])
        wg = [self.wload(d["pleg"][li][:, hh * 512:(hh + 1) * 512], 8, 512) for hh in range(2)]
        wp, wpk = self.wload(d["plep"][li], 2, D)
        for j in range(8):
            tb = g * 8 + j
            pb_ = pblk[j % 2]
            pk = ("pblk", j % 2)
            self.dma(pb_, d["p_in"][li, tb * 128:(tb + 1) * 128, :], [], [pk])
            b = self.short()
            for c in range(2):
                self.tr(self.pb[b][:, c * 128:(c + 1) * 128], pb_[:, c * 128:(c + 1) * 128], [pk], [("pb", b)])
            pt = pT[j % 2]
            ptk = ("pT", j % 2)
            self.CP("act", pt, self.pb[b][:, 0:256].rearrange("p (c t) -> p c t", c=2), [("pb", b)], [ptk])
            for hh in range(2):
                bg_ = self.short()
                for k in range(8):
                    self.mm(self.pb[bg_][:, :], self.xT[:, k, tb * 128:(tb + 1) * 128], wg[hh][0][:, k, :], k == 0, k == 7,
                            [("xT", tb // 4), wg[hh][1]], [("pb", bg_)])
                bp_ = self.short()
                for c in range(2):
                    self.mm(self.pb[bp_][:, :], pt[:, c, :], wp[:, c, hh * 512:(hh + 1) * 512],
                            c == 0, c == 1, [ptk, wpk], [("pb", bp_)])
                self.A(ple[:, hh * 512:(hh + 1) * 512], self.pb[bg_][:, :], AF.Sigmoid, [("pb", bg_)], ["ple"])
                self.TT("dve", ple[:, hh * 512:(hh + 1) * 512], ple[:, hh * 512:(hh + 1) * 512], self.pb[bp_][:, :],
                        ALU.mult, ["ple", ("pb", bp_)], ["ple"])
            self.dma(xin, res_in[tb * 128:(tb + 1) * 128, :], [("res", id(res_in), tb)], ["xin"])
            h = hbuf[j % 2]
            hk = ("hbuf", j % 2)
            self.STT("dve", h, xin, ALPHA, ple, ALU.mult, ALU.add, ["xin", "ple"], [hk])
            self.TT("dve", h, h, facc[:, j, :], ALU.add, [hk], [hk])
            o = self.layer_norm_block(h, hk, gb, tb, res_out, li, None)
            if li == self.layers[-1] or self.stop_after == ("ffn", li):
                self.finals.append(o)


_IDX = _win_index()


def prepare_inputs(inputs):
    f = lambda a: np.ascontiguousarray(np.asarray(a))
    w_in = f(inputs["w_in"])
    win = np.zeros((2, D, NCOLS_R), np.float32)
    valid = _IDX >= 0
    win[:, :, valid] = w_in[:, :, _IDX[valid]]
    conv_w = f(inputs["conv_w"])
    convp = np.zeros((2, 128, 2, 34), np.float32)
    for c in range(2):
        convp[:, :, c, 0:31] = conv_w[:, :, c * 128:(c + 1) * 128].transpose(0, 2, 1)
        convp[:, :, c, 31] = f(inputs["conv_b"])[:, c * 128:(c + 1) * 128]
        convp[:, :, c, 32] = f(inputs["conv_ln_g"])[:, c * 128:(c + 1) * 128]
        convp[:, :, c, 33] = f(inputs["conv_ln_b"])[:, c * 128:(c + 1) * 128]
    pe = f(inputs["nsa_cmp_pe"])
    pe_r = np.ascontiguousarray(pe.transpose(0, 2, 3, 1).reshape(2, 128, 32))
    w2 = f(inputs["nsa_cmp_w2"])
    w2k = np.ascontiguousarray(np.concatenate([w2[:, 0], w2[:, 0]], axis=2))
    w2v = np.ascontiguousarray(w2[:, 1])
    qn = np.ascontiguousarray(f(inputs["mla_q_norm"]).reshape(2, 2, 128).transpose(0, 2, 1))
    kvn = np.ascontiguousarray(f(inputs["mla_kv_norm"]).reshape(2, 128, 1))
    wuq = f(inputs["mla_w_uq"])
    cols = []
    for h in range(4):
        b = h * 96
        cols += list(range(b, b + 96))
        cols += list(range(b, b + 64)) + list(range(b + 80, b + 96)) + list(range(b + 64, b + 80))
    uq = np.ascontiguousarray(wuq[:, :, cols])
    wukv = f(inputs["mla_w_ukv"])
    cols = []
    for h in range(4):
        cols += list(range(h * 128, h * 128 + 64))
    for h in range(4):
        cols += list(range(h * 128 + 64, h * 128 + 128))
    ukv = np.ascontiguousarray(wukv[:, :, cols])
    shared = {
        "win": win, "convp": convp, "pe_r": pe_r, "w1": f(inputs["nsa_cmp_w1"]), "w2k": w2k, "w2v": w2v,
        "qn": qn, "kvn": kvn, "uq": uq, "ukv": ukv, "wbr": f(inputs["w_branch"]), "wout": f(inputs["w_out"]),
        "ln1g": f(inputs["ln1_g"]), "ln1b": f(inputs["ln1_b"]), "ln2g": f(inputs["ln2_g"]), "ln2b": f(inputs["ln2_b"]),
        "ffn_in": f(inputs["ffn_w_in"])[0], "ffn_out": f(inputs["ffn_w_out"])[0], "router": f(inputs["moe_router"])[0],
        "moe_in": f(inputs["moe_w_in"])[0], "moe_out": f(inputs["moe_w_out"])[0],
        "pleg": f(inputs["ple_w_gate"]), "plep": f(inputs["ple_w_proj"]),
    }
    for k, v in _host_consts().items():
        shared["c_" + k] = v
    x = f(inputs["x"])
    p = f(inputs["p"])
    pos = f(inputs["positions"]).astype(np.int32)
    in_maps = []
    for b in range(8):
        m = dict(shared)
        m["x"] = x[b]
        m["p"] = np.ascontiguousarray(p[:, b])
        m["pos"] = pos[b:b + 1]
        in_maps.append(m)
    return in_maps


def kernel(**inputs):
    in_maps = prepare_inputs(inputs)
    nc = MK().build()
    res = run_bass_kernel_spmd(nc, in_maps, core_ids=list(range(8)))
    return np.stack([np.asarray(r["y"], dtype=np.float32) for r in res.results], axis=0)
```
